# Optimizing a Trainium2 kernel written in Bass

```python
import jax
import jax.numpy as jnp
from jax import lax
import numpy as np

D_MODEL = 1024
BATCH = 8
SEQ = 4096
DEPTH = 4

GRID_W = 64
CTX_LEN = 256
N_MIXERS = 4
ROPE_BASE = 10000.0
NORM_EPS = 1e-6
NEG_INF = -1e30
Q_BLOCK = 128

NA_HEADS = 16
NA_HEAD_DIM = D_MODEL // NA_HEADS
NA_WIN_ROWS = 8
NA_WIN_COLS = 16

RW_HEAD = 64
RW_HEADS = D_MODEL // RW_HEAD
RW_DECAY_LORA = 64
RW_ICLR_LORA = 64
RW_GATE_LORA = 128
RW_GN_EPS = 64e-5

MLA_HEADS = 16
MLA_Q_RANK = 384
MLA_KV_RANK = 256
MLA_NOPE = 64
MLA_ROPE = 32
MLA_V = 64

SWA_Q_HEADS = 16
SWA_KV_HEADS = 4
SWA_GROUP = SWA_Q_HEADS // SWA_KV_HEADS
SWA_HEAD_DIM = 64
SWA_WINDOW = 128

N_EXPERTS = 16
EC_CAPACITY = 2
D_FF_EXPERT = 1024

kernel_name = 'hybrid_dit_na_rwkv7_mla_swa_ecmoe'


def rms_norm(x, g):
    xf = x.astype(jnp.float32)
    y = xf * lax.rsqrt(jnp.mean(xf * xf, axis=-1, keepdims=True) + NORM_EPS)
    return (y * g.astype(jnp.float32)).astype(x.dtype)


def modulate(h, shift, scale):
    return h * (1 + scale) + shift


def ada_modulation(cond, w, b):
    m = jax.nn.silu(cond) @ w + b
    return jnp.split(m, 6, axis=-1)


def axial_rope(n, d_rot):
    t = jnp.arange(n)
    row = (t // GRID_W).astype(jnp.float32)
    col = (t % GRID_W).astype(jnp.float32)
    d_axis = d_rot // 2
    inv = ROPE_BASE ** (-jnp.arange(0, d_axis, 2, dtype=jnp.float32) / d_axis)
    ang = jnp.concatenate([row[:, None] * inv, col[:, None] * inv], axis=-1)
    return jnp.cos(ang), jnp.sin(ang)


def apply_rope(x, cos, sin):
    half = x.shape[-1] // 2
    xf = x.astype(jnp.float32)
    x1, x2 = xf[..., :half], xf[..., half:]
    return jnp.concatenate([x1 * cos - x2 * sin, x2 * cos + x1 * sin], axis=-1).astype(x.dtype)


def context_attention(q, k, v, scale, sink=None):
    L = k.shape[1]
    s = jnp.einsum('bqhgd,bkhd->bhgqk', q, k).astype(jnp.float32) * scale
    if sink is not None:
        snk = jnp.broadcast_to(sink.astype(jnp.float32)[None, :, :, None, None], s.shape[:-1] + (1,))
        s = jnp.concatenate([s, snk], axis=-1)
    p = jax.nn.softmax(s, axis=-1)[..., :L]
    return jnp.einsum('bhgqk,bkhd->bqhgd', p.astype(v.dtype), v)


def neighbourhood_attention(hc, hl, w_qkv, q_g, k_g, rpb, w_o, ctx_out):
    B, n, D = hl.shape
    L = hc.shape[1]
    rows = n // GRID_W
    H, dh = NA_HEADS, NA_HEAD_DIM
    scale = dh ** -0.5

    def project(h):
        qkv = (h @ w_qkv).reshape(h.shape[0], h.shape[1], 3, H, dh)
        return rms_norm(qkv[:, :, 0], q_g), rms_norm(qkv[:, :, 1], k_g), qkv[:, :, 2]

    qc, kc, vc = project(hc)
    ql, kl, vl = project(hl)

    kr = min(NA_WIN_ROWS, rows)
    n_cb = GRID_W // NA_WIN_COLS
    strip = 2 * NA_WIN_COLS
    qcol = np.arange(GRID_W).reshape(n_cb, NA_WIN_COLS)
    strip0 = np.clip(np.arange(n_cb) * NA_WIN_COLS - NA_WIN_COLS // 2, 0, GRID_W - strip)
    kcol = strip0[:, None] + np.arange(strip)
    cstart = np.clip(qcol - NA_WIN_COLS // 2, 0, GRID_W - NA_WIN_COLS)
    kc3 = kcol[:, None, :]
    col_ok = (kc3 >= cstart[:, :, None]) & (kc3 < cstart[:, :, None] + NA_WIN_COLS)
    dcol_idx = np.clip(kc3 - qcol[:, :, None] + NA_WIN_COLS - 1, 0, 2 * NA_WIN_COLS - 2)
    rpb_cols = rpb[:, :, dcol_idx]
    col_ok = jnp.asarray(col_ok)[:, :, None, :]

    qg = ql.reshape(B, rows, GRID_W, H, dh)
    kg = kl.reshape(B, rows, GRID_W, H, dh)
    vg = vl.reshape(B, rows, GRID_W, H, dh)

    def row_step(r):
        r0 = jnp.clip(r - kr // 2, 0, rows - kr)
        kw = lax.dynamic_slice_in_dim(kg, r0, kr, axis=1)[:, :, kcol]
        vw = lax.dynamic_slice_in_dim(vg, r0, kr, axis=1)[:, :, kcol]
        q = lax.dynamic_index_in_dim(qg, r, axis=1, keepdims=False)[:, qcol]
        dr_idx = r0 + jnp.arange(kr) - r + (NA_WIN_ROWS - 1)
        bias = jnp.transpose(rpb_cols[:, dr_idx], (0, 2, 3, 1, 4)).astype(jnp.float32)
        s_lat = jnp.einsum('bjqhd,brjkhd->bhjqrk', q, kw).astype(jnp.float32) * scale + bias[None]
        s_lat = jnp.where(col_ok, s_lat, NEG_INF).reshape(B, H, n_cb, NA_WIN_COLS, kr * strip)
        s_ctx = jnp.einsum('bjqhd,bkhd->bhjqk', q, kc).astype(jnp.float32) * scale
        p = jax.nn.softmax(jnp.concatenate([s_lat, s_ctx], axis=-1), axis=-1).astype(vl.dtype)
        p_lat = p[..., :kr * strip].reshape(B, H, n_cb, NA_WIN_COLS, kr, strip)
        o = (jnp.einsum('bhjqrk,brjkhd->bjqhd', p_lat, vw)
             + jnp.einsum('bhjqk,bkhd->bjqhd', p[..., kr * strip:], vc))
        return o.reshape(B, GRID_W, H, dh)

    o = lax.map(row_step, jnp.arange(rows))
    ol = jnp.moveaxis(o, 0, 1).reshape(B, n, H * dh) @ w_o
    oc = None
    if ctx_out:
        oc = context_attention(qc[:, :, :, None], kc, vc, scale).reshape(B, L, H * dh) @ w_o
    return oc, ol


def centred_shift(h):
    hp = jnp.pad(h, ((0, 0), (1, 1), (0, 0)))
    return 0.5 * (hp[:, :-2] + hp[:, 2:])


def _rwkv_step(S, inp):
    r, w, kk, kka, kt, v = inp
    sk = jnp.einsum('bhij,bhj->bhi', S, kk)
    S = S * w[:, :, None, :] - sk[..., None] * kka[:, :, None, :] + v[..., None] * kt[:, :, None, :]
    return S, jnp.einsum('bhij,bhj->bhi', S, r)


def _rwkv_scan(S0, seqs, reverse):
    xs = tuple(jnp.swapaxes(t, 0, 1) for t in seqs)
    S, y = lax.scan(_rwkv_step, S0, xs, reverse=reverse)
    return S, jnp.swapaxes(y, 0, 1)


def rwkv7_bidirectional(hc, hl, mix, w_r, w_k, w_v, w0, w1, w2, a0, a1, a2, g1, g2,
                        k_k, k_a, r_k, ln_g, ln_b, w_o, ctx_out):
    f32 = jnp.float32
    H, N = RW_HEADS, RW_HEAD

    def heads(t):
        return t.reshape(t.shape[:-1] + (H, N))

    def stream(h, S0):
        B = h.shape[0]
        xx = centred_shift(h) - h
        xr, xw, xk, xv, xa, xg = (h + xx * mix[s] for s in range(6))
        r = heads((xr @ w_r).astype(f32))
        k = (xk @ w_k).astype(f32)
        v = heads((xv @ w_v).astype(f32))
        g = jax.nn.sigmoid(xg @ g1) @ g2
        dec = w0[:, None, None, :] + jnp.einsum('zbtr,zrd->zbtd', jnp.tanh(jnp.einsum('btd,zdr->zbtr', xw, w1)), w2)
        logw = -jax.nn.softplus(-dec.astype(f32)) - 0.5
        w = heads(jnp.exp(-jnp.exp(logw)))
        a = heads(jax.nn.sigmoid((a0[:, None, None, :] + jnp.einsum(
            'zbtr,zrd->zbtd', jnp.einsum('btd,zdr->zbtr', xa, a1), a2)).astype(f32)))
        kk = heads(k * k_k.astype(f32))
        kk = kk / jnp.maximum(jnp.linalg.norm(kk, axis=-1, keepdims=True), 1e-12)
        kt = heads(k)[None] * (1 + (a - 1) * heads(k_a.astype(f32)))
        if S0 is None:
            z = jnp.zeros((B, H, N, N), f32)
            S0 = (z, z)
        S_f, y_f = _rwkv_scan(S0[0], (r, w[0], kk, kk * a[0], kt[0], v), False)
        S_b, y_b = _rwkv_scan(S0[1], (r, w[1], kk, kk * a[1], kt[1], v), True)
        return (S_f, S_b), (y_f + y_b, r, kt, v, g)

    def readout(y, r, kt, v, g, dtype):
        B, T = y.shape[:2]
        mu = jnp.mean(y, axis=-1, keepdims=True)
        var = jnp.mean(jnp.square(y - mu), axis=-1, keepdims=True)
        yn = ((y - mu) * lax.rsqrt(var + RW_GN_EPS)).reshape(B, T, -1) * ln_g.astype(f32) + ln_b.astype(f32)
        bonus = jnp.sum(jnp.sum(r[None] * kt * r_k.astype(f32), axis=-1, keepdims=True), axis=0) * v
        return ((yn + bonus.reshape(B, T, -1)).astype(dtype) * g) @ w_o

    ctx_state, ctx_feats = stream(hc, None)
    _, lat_feats = stream(hl, ctx_state)
    ol = readout(*lat_feats, hl.dtype)
    oc = readout(*ctx_feats, hc.dtype) if ctx_out else None
    return oc, ol


def multi_head_latent_attention(hc, hl, w_down, q_norm_g, kv_norm_g, w_uq, w_ukv,
                                qn_g, qr_g, kn_g, kr_g, w_o, cos, sin, ctx_out):
    f32 = jnp.float32
    H = MLA_HEADS
    scale = (MLA_NOPE + MLA_ROPE) ** -0.5

    def project(h, rope):
        B, T, _ = h.shape
        d = h @ w_down
        cq = rms_norm(d[..., :MLA_Q_RANK], q_norm_g)
        ckv = rms_norm(d[..., MLA_Q_RANK:MLA_Q_RANK + MLA_KV_RANK], kv_norm_g)
        k_r = rms_norm(d[..., MLA_Q_RANK + MLA_KV_RANK:], kr_g)
        q = (cq @ w_uq).reshape(B, T, H, MLA_NOPE + MLA_ROPE)
        kv = (ckv @ w_ukv).reshape(B, T, H, MLA_NOPE + MLA_V)
        q_n = rms_norm(q[..., :MLA_NOPE], qn_g)
        q_r = rms_norm(q[..., MLA_NOPE:], qr_g)
        k_n = rms_norm(kv[..., :MLA_NOPE], kn_g)
        v = kv[..., MLA_NOPE:]
        if rope:
            q_r = apply_rope(q_r, cos[:, None], sin[:, None])
            k_r = apply_rope(k_r, cos, sin)
        return q_n, q_r, k_n, k_r, v

    qn_l, qr_l, kn_l, kr_l, v_l = project(hl, True)
    qn_c, qr_c, kn_c, kr_c, v_c = project(hc, False)
    B, n = hl.shape[:2]
    L = hc.shape[1]

    def block(b):
        s0 = b * Q_BLOCK
        qn = lax.dynamic_slice_in_dim(qn_l, s0, Q_BLOCK, axis=1)
        qr = lax.dynamic_slice_in_dim(qr_l, s0, Q_BLOCK, axis=1)
        s_lat = jnp.einsum('bqhd,bkhd->bhqk', qn, kn_l) + jnp.einsum('bqhd,bkd->bhqk', qr, kr_l)
        s_ctx = jnp.einsum('bqhd,bkhd->bhqk', qn, kn_c) + jnp.einsum('bqhd,bkd->bhqk', qr, kr_c)
        s = jnp.concatenate([s_lat, s_ctx], axis=-1).astype(f32) * scale
        p = jax.nn.softmax(s, axis=-1).astype(v_l.dtype)
        return (jnp.einsum('bhqk,bkhd->bqhd', p[..., :n], v_l)
                + jnp.einsum('bhqk,bkhd->bqhd', p[..., n:], v_c))

    o = lax.map(block, jnp.arange(n // Q_BLOCK))
    ol = jnp.moveaxis(o, 0, 1).reshape(B, n, H * MLA_V) @ w_o
    oc = None
    if ctx_out:
        q = jnp.concatenate([qn_c, qr_c], axis=-1)
        k = jnp.concatenate([kn_c, jnp.broadcast_to(kr_c[:, :, None, :], (B, L, H, MLA_ROPE))], axis=-1)
        oc = context_attention(q[:, :, :, None], k, v_c, scale).reshape(B, L, H * MLA_V) @ w_o
    return oc, ol


def window_gqa_sink(hc, hl, w_qkv, q_g, k_g, sink, w_o, cos, sin, ctx_out):
    f32 = jnp.float32
    Hk, G, dh = SWA_KV_HEADS, SWA_GROUP, SWA_HEAD_DIM
    scale = dh ** -0.5
    sink_hg = sink.reshape(Hk, G)
    nq = Hk * G * dh

    def project(h, rope):
        B, T, _ = h.shape
        qkv = h @ w_qkv
        q = rms_norm(qkv[..., :nq].reshape(B, T, Hk, G, dh), q_g)
        k = rms_norm(qkv[..., nq:nq + Hk * dh].reshape(B, T, Hk, dh), k_g)
        v = qkv[..., nq + Hk * dh:].reshape(B, T, Hk, dh)
        if rope:
            q = apply_rope(q, cos[:, None, None], sin[:, None, None])
            k = apply_rope(k, cos[:, None], sin[:, None])
        return q, k, v

    ql, kl, vl = project(hl, True)
    qc, kc, vc = project(hc, False)
    B, n = hl.shape[:2]
    L = hc.shape[1]
    pad = ((0, 0), (Q_BLOCK, Q_BLOCK), (0, 0), (0, 0))
    kp, vp = jnp.pad(kl, pad), jnp.pad(vl, pad)
    q_off = jnp.arange(Q_BLOCK)
    k_off = jnp.arange(3 * Q_BLOCK) - Q_BLOCK

    def block(b):
        s0 = b * Q_BLOCK
        q = lax.dynamic_slice_in_dim(ql, s0, Q_BLOCK, axis=1)
        k = lax.dynamic_slice_in_dim(kp, s0, 3 * Q_BLOCK, axis=1)
        v = lax.dynamic_slice_in_dim(vp, s0, 3 * Q_BLOCK, axis=1)
        qpos, kpos = s0 + q_off, s0 + k_off
        ok = ((kpos >= 0) & (kpos < n))[None, :] & (jnp.abs(qpos[:, None] - kpos[None, :]) <= SWA_WINDOW)
        s_loc = jnp.where(ok, jnp.einsum('bqhgd,bkhd->bhgqk', q, k).astype(f32) * scale, NEG_INF)
        s_ctx = jnp.einsum('bqhgd,bkhd->bhgqk', q, kc).astype(f32) * scale
        s_snk = jnp.broadcast_to(sink_hg.astype(f32)[None, :, :, None, None], s_loc.shape[:-1] + (1,))
        p = jax.nn.softmax(jnp.concatenate([s_loc, s_ctx, s_snk], axis=-1), axis=-1).astype(vl.dtype)
        return (jnp.einsum('bhgqk,bkhd->bqhgd', p[..., :3 * Q_BLOCK], v)
                + jnp.einsum('bhgqk,bkhd->bqhgd', p[..., 3 * Q_BLOCK:-1], vc))

    o = lax.map(block, jnp.arange(n // Q_BLOCK))
    ol = jnp.moveaxis(o, 0, 1).reshape(B, n, nq) @ w_o
    oc = None
    if ctx_out:
        oc = context_attention(qc, kc, vc, scale, sink_hg).reshape(B, L, nq) @ w_o
    return oc, ol


def expert_choice_ffn(h, w_router, w1, w3, w2):
    B, T, D = h.shape
    cap = max(1, EC_CAPACITY * T // N_EXPERTS)
    aff = jax.nn.softmax((h @ w_router).astype(jnp.float32), axis=-1)
    gate, idx = lax.top_k(jnp.swapaxes(aff, 1, 2), cap)
    xin = jax.vmap(lambda hb, ib: hb[ib])(h, idx)
    hid = jax.nn.silu(jnp.einsum('becd,edf->becf', xin, w1)) * jnp.einsum('becd,edf->becf', xin, w3)
    y = jnp.einsum('becf,efd->becd', hid, w2) * gate[..., None].astype(h.dtype)
    return jax.vmap(lambda yb, ib: jax.ops.segment_sum(
        yb.reshape(-1, D), ib.reshape(-1), num_segments=T))(y, idx)


def _layers_of(mixer):
    return len(range(mixer, DEPTH, N_MIXERS))


def setup_inputs(seed: int = 0) -> dict:
    key = jax.random.key(seed)
    keys = iter(jax.random.split(key, 96))
    f32 = jnp.float32
    D = D_MODEL

    def normal(shape, scale):
        return jax.random.normal(next(keys), shape, f32) * scale

    def gain(shape):
        return 1.0 + 0.1 * jax.random.normal(next(keys), shape, f32)

    def uniform(shape, lo, hi):
        return jax.random.uniform(next(keys), shape, f32, lo, hi)

    nA, nB, nC, nD = (_layers_of(m) for m in range(N_MIXERS))
    swa_cols = (SWA_Q_HEADS + 2 * SWA_KV_HEADS) * SWA_HEAD_DIM
    return {
        'x': normal((BATCH, SEQ, D), 1.0),
        'c': normal((BATCH, D), 1.0),
        'ctx': normal((BATCH, CTX_LEN, D), 1.0),
        'c_ctx': normal((D,), 1.0),
        'norm1_g': gain((DEPTH, D)),
        'norm2_g': gain((DEPTH, D)),
        'ada_w': normal((DEPTH, D, 6 * D), 0.5 * D ** -0.5),
        'ada_b': normal((DEPTH, 6 * D), 0.02),
        'na_w_qkv': normal((nA, D, 3 * D), D ** -0.5),
        'na_q_g': gain((nA, NA_HEAD_DIM)),
        'na_k_g': gain((nA, NA_HEAD_DIM)),
        'na_rpb': normal((nA, NA_HEADS, 2 * NA_WIN_ROWS - 1, 2 * NA_WIN_COLS - 1), 0.5),
        'na_w_o': normal((nA, D, D), D ** -0.5),
        'rw_mix': uniform((nB, 6, D), 0.0, 1.0),
        'rw_w_r': normal((nB, D, D), D ** -0.5),
        'rw_w_k': normal((nB, D, D), D ** -0.5),
        'rw_w_v': normal((nB, D, D), D ** -0.5),
        'rw_w0': uniform((nB, 2, D), -6.0, -1.0),
        'rw_w1': normal((nB, 2, D, RW_DECAY_LORA), D ** -0.5),
        'rw_w2': normal((nB, 2, RW_DECAY_LORA, D), 0.5 * RW_DECAY_LORA ** -0.5),
        'rw_a0': normal((nB, 2, D), 0.5),
        'rw_a1': normal((nB, 2, D, RW_ICLR_LORA), D ** -0.5),
        'rw_a2': normal((nB, 2, RW_ICLR_LORA, D), 0.5 * RW_ICLR_LORA ** -0.5),
        'rw_g1': normal((nB, D, RW_GATE_LORA), D ** -0.5),
        'rw_g2': normal((nB, RW_GATE_LORA, D), RW_GATE_LORA ** -0.5),
        'rw_k_k': uniform((nB, D), 0.7, 1.0),
        'rw_k_a': uniform((nB, D), 0.8, 1.2),
        'rw_r_k': normal((nB, RW_HEADS, RW_HEAD), 0.1),
        'rw_ln_g': gain((nB, D)),
        'rw_ln_b': normal((nB, D), 0.02),
        'rw_w_o': normal((nB, D, D), D ** -0.5),
        'mla_w_down': normal((nC, D, MLA_Q_RANK + MLA_KV_RANK + MLA_ROPE), D ** -0.5),
        'mla_q_norm_g': gain((nC, MLA_Q_RANK)),
        'mla_kv_norm_g': gain((nC, MLA_KV_RANK)),
        'mla_w_uq': normal((nC, MLA_Q_RANK, MLA_HEADS * (MLA_NOPE + MLA_ROPE)), MLA_Q_RANK ** -0.5),
        'mla_w_ukv': normal((nC, MLA_KV_RANK, MLA_HEADS * (MLA_NOPE + MLA_V)), MLA_KV_RANK ** -0.5),
        'mla_qn_g': gain((nC, MLA_NOPE)),
        'mla_qr_g': gain((nC, MLA_ROPE)),
        'mla_kn_g': gain((nC, MLA_NOPE)),
        'mla_kr_g': gain((nC, MLA_ROPE)),
        'mla_w_o': normal((nC, MLA_HEADS * MLA_V, D), (MLA_HEADS * MLA_V) ** -0.5),
        'swa_w_qkv': normal((nD, D, swa_cols), D ** -0.5),
        'swa_q_g': gain((nD, SWA_HEAD_DIM)),
        'swa_k_g': gain((nD, SWA_HEAD_DIM)),
        'swa_sink': normal((nD, SWA_Q_HEADS), 0.5),
        'swa_w_o': normal((nD, SWA_Q_HEADS * SWA_HEAD_DIM, D), (SWA_Q_HEADS * SWA_HEAD_DIM) ** -0.5),
        'moe_router': normal((DEPTH, D, N_EXPERTS), D ** -0.5),
        'moe_w1': normal((DEPTH, N_EXPERTS, D, D_FF_EXPERT), D ** -0.5),
        'moe_w3': normal((DEPTH, N_EXPERTS, D, D_FF_EXPERT), D ** -0.5),
        'moe_w2': normal((DEPTH, N_EXPERTS, D_FF_EXPERT, D), D_FF_EXPERT ** -0.5),
    }


def reference(x, c, ctx, c_ctx, norm1_g, norm2_g, ada_w, ada_b,
              na_w_qkv, na_q_g, na_k_g, na_rpb, na_w_o,
              rw_mix, rw_w_r, rw_w_k, rw_w_v, rw_w0, rw_w1, rw_w2, rw_a0, rw_a1, rw_a2,
              rw_g1, rw_g2, rw_k_k, rw_k_a, rw_r_k, rw_ln_g, rw_ln_b, rw_w_o,
              mla_w_down, mla_q_norm_g, mla_kv_norm_g, mla_w_uq, mla_w_ukv,
              mla_qn_g, mla_qr_g, mla_kn_g, mla_kr_g, mla_w_o,
              swa_w_qkv, swa_q_g, swa_k_g, swa_sink, swa_w_o,
              moe_router, moe_w1, moe_w3, moe_w2):
    n = x.shape[1]
    cos_mla, sin_mla = axial_rope(n, MLA_ROPE)
    cos_swa, sin_swa = axial_rope(n, SWA_HEAD_DIM)
    xl, xc = x, ctx
    for i in range(DEPTH):
        mixer, j = i % N_MIXERS, i // N_MIXERS
        ctx_needed = i < DEPTH - 1
        sh1, sc1, gt1, sh2, sc2, gt2 = (t[:, None, :] for t in ada_modulation(c, ada_w[i], ada_b[i]))
        csh1, csc1, cgt1, csh2, csc2, cgt2 = ada_modulation(c_ctx, ada_w[i], ada_b[i])
        hl = modulate(rms_norm(xl, norm1_g[i]), sh1, sc1)
        hc = modulate(rms_norm(xc, norm1_g[i]), csh1, csc1)
        if mixer == 0:
            oc, ol = neighbourhood_attention(hc, hl, na_w_qkv[j], na_q_g[j], na_k_g[j], na_rpb[j],
                                             na_w_o[j], ctx_needed)
        elif mixer == 1:
            oc, ol = rwkv7_bidirectional(hc, hl, rw_mix[j], rw_w_r[j], rw_w_k[j], rw_w_v[j],
                                         rw_w0[j], rw_w1[j], rw_w2[j], rw_a0[j], rw_a1[j], rw_a2[j],
                                         rw_g1[j], rw_g2[j], rw_k_k[j], rw_k_a[j], rw_r_k[j],
                                         rw_ln_g[j], rw_ln_b[j], rw_w_o[j], ctx_needed)
        elif mixer == 2:
            oc, ol = multi_head_latent_attention(hc, hl, mla_w_down[j], mla_q_norm_g[j], mla_kv_norm_g[j],
                                                 mla_w_uq[j], mla_w_ukv[j], mla_qn_g[j], mla_qr_g[j],
                                                 mla_kn_g[j], mla_kr_g[j], mla_w_o[j],
                                                 cos_mla, sin_mla, ctx_needed)
        else:
            oc, ol = window_gqa_sink(hc, hl, swa_w_qkv[j], swa_q_g[j], swa_k_g[j], swa_sink[j],
                                     swa_w_o[j], cos_swa, sin_swa, ctx_needed)
        xl = xl + gt1 * ol
        hl = modulate(rms_norm(xl, norm2_g[i]), sh2, sc2)
        xl = xl + gt2 * expert_choice_ffn(hl, moe_router[i], moe_w1[i], moe_w3[i], moe_w2[i])
        if ctx_needed:
            xc = xc + cgt1 * oc
            hc = modulate(rms_norm(xc, norm2_g[i]), csh2, csc2)
            xc = xc + cgt2 * expert_choice_ffn(hc, moe_router[i], moe_w1[i], moe_w3[i], moe_w2[i])
    return xl
```

```python
from concourse.bass_utils import run_bass_kernel_spmd
import contextlib
import numpy as np
import concourse.bass as bass
import concourse.mybir as mybir

F32 = mybir.dt.float32
BF16 = mybir.dt.bfloat16
I32 = mybir.dt.int32
U32 = mybir.dt.uint32
AF = mybir.ActivationFunctionType
ALU = mybir.AluOpType
AX = mybir.AxisListType

SEM_CAP = 30000
DMA_POOL = 12


class U:
    __slots__ = ("name", "w", "r")

    def __init__(self, name):
        self.name = name
        self.w = None
        self.r = {}


class Prog:
    def __init__(self):
        self.nc = bass.Bass("TRN2", target_bir_lowering=False)
        self.es = contextlib.ExitStack()
        self.ops = {e: [] for e in ("pe", "dve", "act", "pool", "sp")}
        self.cnt = {e: 0 for e in self.ops}
        self.ep = {e: 0 for e in self.ops}
        self.sems = {}
        self.waited = {e: {} for e in self.ops}
        self.dma_n = {e: 0 for e in self.ops}
        self.dma_val = {}
        self.n_inst = 0
        self.out_ticks = []

    def sb(self, name, shape, dt):
        return self.es.enter_context(self.nc.sbuf_tensor(name, list(shape), dt))

    def ps(self, name, shape, dt=F32):
        return self.es.enter_context(self.nc.psum_tensor(name, list(shape), dt))

    def dram(self, name, shape, dt, kind="Internal"):
        return self.nc.dram_tensor(name, list(shape), dt, kind=kind).ap()

    def _sem(self, key):
        if key not in self.sems:
            self.sems[key] = self.es.enter_context(self.nc.semaphore("s_%s_%s" % key))
        return self.sems[key]

    def _wait(self, eng, tick):
        if tick is None:
            return
        key, val = tick
        if self.waited[eng].get(key, 0) >= val:
            return
        self.waited[eng][key] = val
        sem = self._sem(key)
        self.ops[eng].append(lambda e, sem=sem, val=val: e.wait_ge(sem, val))

    def _deps(self, eng, reads, writes, skip_self=False):
        ticks = []
        for u in reads:
            if u.w is not None:
                ticks.append(u.w)
        for u in writes:
            if u.w is not None:
                ticks.append(u.w)
            for k, v in u.r.items():
                ticks.append((k, v))
        mykey = (eng, self.ep[eng])
        for t in ticks:
            if skip_self and t[0] == mykey:
                continue
            self._wait(eng, t)

    def _mark(self, tick, reads, writes):
        k, v = tick
        for u in reads:
            if u.r.get(k, 0) < v:
                u.r[k] = v
        for u in writes:
            u.w = tick
            u.r = {}

    def op(self, eng, fn, reads=(), writes=(), skip_self=False):
        reads = list(reads)
        writes = list(writes)
        self._deps(eng, reads, writes, skip_self=skip_self)
        if self.cnt[eng] >= SEM_CAP:
            self.ep[eng] += 1
            self.cnt[eng] = 0
        self.cnt[eng] += 1
        key = (eng, self.ep[eng])
        sem = self._sem(key)
        val = self.cnt[eng]
        self.ops[eng].append(lambda e, fn=fn, sem=sem: fn(e).then_inc(sem, 1))
        self._mark((key, val), reads, writes)
        self.n_inst += 1
        return (key, val)

    def dma(self, q, fn, reads=(), writes=(), is_out=False):
        reads = list(reads)
        writes = list(writes)
        self._deps(q, reads, writes)
        slot = self.dma_n[q] % DMA_POOL
        self.dma_n[q] += 1
        key = ("d" + q, slot)
        prev = self.dma_val.get(key, 0)
        if prev >= SEM_CAP:
            gen = 1
            while ("d%s_g%d" % (q, gen), slot) in self.dma_val and \
                    self.dma_val[("d%s_g%d" % (q, gen), slot)] >= SEM_CAP:
                gen += 1
            raise RuntimeError("dma semaphore cap reached")
        if prev:
            self._wait(q, (key, prev))
        val = prev + 16
        self.dma_val[key] = val
        sem = self._sem(key)
        self.ops[q].append(lambda e, fn=fn, sem=sem: fn(e).then_inc(sem, 16))
        self._mark((key, val), reads, writes)
        self.n_inst += 1
        if is_out:
            self.out_ticks.append((key, val))
        return (key, val)

    def barrier(self):
        ticks = []
        for e in self.ops:
            if self.cnt[e] > 0:
                ticks.append(((e, self.ep[e]), self.cnt[e]))
        for key, val in self.dma_val.items():
            ticks.append((key, val))
        for e in self.ops:
            for t in ticks:
                if t[0][0] == e:
                    continue
                self._wait(e, t)

    def phase_begin(self):
        self.pes = contextlib.ExitStack()

    def psb(self, name, shape, dt):
        self.uid = getattr(self, "uid", 0) + 1
        return self.pes.enter_context(self.nc.sbuf_tensor("%s_%d" % (name, self.uid), list(shape), dt))

    def phase_end(self):
        self.barrier()
        self.flush()
        self.pes.close()

    def flush(self):
        nc = self.nc
        with nc.Block() as block:
            @block.tensor
            def _(e):
                for f in self.ops["pe"]:
                    f(e)

            @block.vector
            def _(e):
                for f in self.ops["dve"]:
                    f(e)

            @block.scalar
            def _(e):
                for f in self.ops["act"]:
                    f(e)

            @block.gpsimd
            def _(e):
                for f in self.ops["pool"]:
                    f(e)

            @block.sync
            def _(e):
                for f in self.ops["sp"]:
                    f(e)
        for e in self.ops:
            self.ops[e] = []

    def finish(self):
        for t in self.out_ticks:
            self._wait("sp", t)
        self.flush()
        self.es.close()
        return self.nc
D = 1024
NCTX = 256
NLAT = 4096
NT = NCTX + NLAT
NTT = NT // 128
EPS = 1e-6


class K:
    def __init__(self, cfg):
        self.cfg = cfg
        self.P = P = Prog()
        self.inp = {}
        self.uin = {}
        self.build_io()
        self.setup_persistent()

    SHAPES = {
        "x": [NLAT, D], "c": [8, 128], "ctx": [NCTX, D], "c_ctx": [8, 128],
        "norm1_g": [4, 8, 128], "norm2_g": [4, 8, 128], "ada_w": [4, D, 6 * D], "ada_b": [4, 48, 128],
        "moe_router": [4, D, 16], "moe_w1": [4, 16, D, D], "moe_w3": [4, 16, D, D], "moe_w2": [4, 16, D, D],
        "na_w_qkv": [D, 3 * D], "na_q_g": [1, 64], "na_k_g": [1, 64], "na_bias": [16, 128, 14, 64], "na_w_o": [D, D],
        "mla_w_down": [D, 672], "mla_q_norm_g": [1, 384], "mla_kv_norm_g": [1, 256], "mla_w_uq": [384, 1536],
        "mla_w_ukv": [256, 2048], "mla_qn_g": [1, 64], "mla_qr_g": [1, 32], "mla_kn_g": [1, 64], "mla_kr_g": [1, 32],
        "mla_w_o": [D, D], "mla_cos": [NLAT, 16], "mla_sin": [NLAT, 16],
        "swa_w_qkv": [D, 1536], "swa_q_g": [1, 64], "swa_k_g": [1, 64], "swa_sink": [1, 16], "swa_w_o": [D, D],
        "swa_cos": [NLAT, 32], "swa_sin": [NLAT, 32],
        "rw_mix": [48, 128], "rw_w_r": [D, D], "rw_w_k": [D, D], "rw_w_v": [D, D], "rw_w0": [2, D], "rw_w1": [2, D, 64],
        "rw_w2": [2, 64, D], "rw_a0": [2, D], "rw_a1": [2, D, 64], "rw_a2": [2, 64, D], "rw_g1": [D, 128], "rw_g2": [128, D],
        "rw_k_k": [1, D], "rw_k_a": [1, D], "rw_r_k": [1, D], "rw_ln_g": [1, D], "rw_ln_b": [1, D], "rw_w_o": [D, D],
    }

    def build_io(self):
        P = self.P
        self.out = P.dram("out", [NLAT, D], F32, kind="ExternalOutput")
        self.xres = P.dram("xres", [NT, D], F32); self.u_xres = U("xres")
        self.xs2 = P.dram("xs2", [NT, D], BF16); self.u_xs2 = U("xs2")
        self.modd = P.dram("modd", [1, 4 * 96 * 128], F32); self.u_modd = U("modd")

    def I(self, name):
        if name not in self.inp:
            self.inp[name] = self.P.dram(name, self.SHAPES[name], F32, kind="ExternalInput")
        return self.inp[name]

    def setup_persistent(self):
        P = self.P
        self.ident_f = P.sb("ident_f", [128, 128], F32); self.u_const = U("const")
        self.ident_b = P.sb("ident_b", [128, 128], BF16)
        self.ones_f = P.sb("ones_f", [128, 128], F32)
        self.ones_b = P.sb("ones_b", [128, 128], BF16)
        self.scT = P.sb("scT", [128, 8, 2], F32); self.u_scT = U("scT")
        self.AB = P.sb("AB", [128, 16, 8, 2], F32); self.u_AB = U("AB")
        self.hT = P.sb("hT", [128, 8, NT], BF16); self.u_hT = [U("hT%d" % t) for t in range(NTT)]
        self.gateT = P.sb("gateT", [128, 5, 16], F32)
        self.idxT = P.sb("idxT", [128, 5, 16], I32)
        self.psf = [P.ps("psf%d" % i, [128, 512], F32) for i in range(6)]
        self.u_psf = [U("psf%d" % i) for i in range(6)]
        self.psb = [P.ps("psb%d" % i, [128, 1024], BF16) for i in range(2)]
        self.u_psb = [U("psb%d" % i) for i in range(2)]
        self.psf_n = 0
        self.psb_n = 0
        uc = self.u_const
        P.op("pool", lambda e: e.memset(self.ident_f[:], 0.0), writes=[uc])
        P.op("pool", lambda e: e.affine_select(out=self.ident_f[:], in_=self.ident_f[:], pattern=[[-1, 128]],
                                               compare_op=ALU.not_equal, fill=1.0, base=0, channel_multiplier=1),
             reads=[uc], writes=[uc])
        P.op("pool", lambda e: e.tensor_copy(out=self.ident_b[:], in_=self.ident_f[:]), reads=[uc], writes=[uc])
        P.op("pool", lambda e: e.memset(self.ones_f[:], 1.0), writes=[uc])
        P.op("pool", lambda e: e.memset(self.ones_b[:], 1.0), writes=[uc])

    def nps(self):
        i = self.psf_n % 4
        self.psf_n += 1
        return self.psf[i], self.u_psf[i]

    def npsb(self):
        i = self.psb_n % 2
        self.psb_n += 1
        return self.psb[i], self.u_psb[i]

    def mm(self, out, lhsT, rhs, start, stop, reads, writes):
        self.P.op("pe", lambda e: e.matmul(out=out, lhsT=lhsT, rhs=rhs, start=start, stop=stop),
                  reads=reads, writes=writes, skip_self=True)

    def tr(self, out, in_, ident, reads, writes):
        self.P.op("pe", lambda e: e.transpose(out=out, in_=in_, identity=ident),
                  reads=reads, writes=writes, skip_self=True)

    def prologue(self):
        P = self.P
        I = self.I
        for nm in ("x", "c", "ctx", "c_ctx"):
            I(nm)
        P.phase_begin()
        P.dma("sp", lambda e: e.dma_start(out=self.xres[0:NCTX, :], in_=I("ctx")[:, :]), writes=[self.u_xres])
        for j in range(8):
            P.dma("sp", lambda e, j=j: e.dma_start(out=self.xres[NCTX + j * 512:NCTX + (j + 1) * 512, :],
                                                    in_=I("x")[j * 512:(j + 1) * 512, :]), writes=[self.u_xres])
        c16 = P.psb("c16", [8, 256], F32); u_c16 = U("c16")
        P.dma("sp", lambda e: e.dma_start(out=c16[:, 0:128], in_=I("c")[:, :]), writes=[u_c16])
        P.dma("sp", lambda e: e.dma_start(out=c16[:, 128:256], in_=I("c_ctx")[:, :]), writes=[u_c16])
        ps, ups = self.nps()
        for s in range(2):
            self.tr(ps[:, s * 8:(s + 1) * 8], c16[:, s * 128:(s + 1) * 128], self.ident_f[0:8, 0:8],
                    [u_c16, self.u_const], [ups])
        for s in range(2):
            P.op("act", lambda e, s=s: e.activation(out=self.scT[:, :, s], in_=ps[:, s * 8:(s + 1) * 8], func=AF.Silu),
                 reads=[ups], writes=[self.u_scT])
        P.phase_end()

    def ada_phase(self, i):
        P = self.P
        I = self.I
        for nm in ("ada_w", "ada_b", "norm1_g", "norm2_g"):
            I(nm)
        P.phase_begin()
        wbuf = [P.psb("adaw", [128, 8, 512], F32) for _ in range(3)]
        uw = [U("adaw%d" % j) for j in range(3)]
        ab48 = P.psb("ab48", [48, 128], F32); g16 = P.psb("g16", [16, 128], F32); u_ld = U("ld")
        abT = P.psb("abT", [128, 48], F32); gT = P.psb("gT", [128, 16], F32); u_T = U("T")
        modT = P.psb("modT", [128, 48, 2], F32); u_modT = U("modT")
        modTT = P.psb("modTT", [96, 128], F32); u_modTT = U("modTT")
        P.dma("sp", lambda e: e.dma_start(out=ab48[:], in_=I("ada_b")[i]), writes=[u_ld])
        P.dma("sp", lambda e: e.dma_start(out=g16[0:8, :], in_=I("norm1_g")[i]), writes=[u_ld])
        P.dma("sp", lambda e: e.dma_start(out=g16[8:16, :], in_=I("norm2_g")[i]), writes=[u_ld])
        psA, upsA = self.nps()
        self.tr(psA[:, 0:48], ab48[:], self.ident_f[0:48, 0:48], [u_ld, self.u_const], [upsA])
        self.tr(psA[:, 64:80], g16[:], self.ident_f[0:16, 0:16], [u_ld, self.u_const], [upsA])
        P.op("dve", lambda e: e.tensor_copy(out=abT[:], in_=psA[:, 0:48]), reads=[upsA], writes=[u_T])
        P.op("dve", lambda e: e.tensor_copy(out=gT[:], in_=psA[:, 64:80]), reads=[upsA], writes=[u_T])
        psM, upsM = self.nps()
        wsrc = I("ada_w")[i].rearrange("(k p) n -> p k n", p=128)
        for piece in range(12):
            b = piece % 3
            P.dma("sp", lambda e, b=b, piece=piece: e.dma_start(out=wbuf[b][:], in_=wsrc[:, :, piece * 512:(piece + 1) * 512]),
                  writes=[uw[b]])
            for ml in range(4):
                m = piece * 4 + ml
                for k in range(8):
                    self.mm(psM[:, 2 * m:2 * m + 2], wbuf[b][:, k, ml * 128:(ml + 1) * 128], self.scT[:, k, :],
                            k == 0, k == 7, [uw[b], self.u_scT], [upsM])
        P.op("dve", lambda e: e.tensor_tensor(out=modT[:], in0=psM[:, 0:96].rearrange("p (m s) -> p m s", s=2),
                                              in1=abT[:].unsqueeze(2).to_broadcast([128, 48, 2]), op=ALU.add),
             reads=[upsM, u_T], writes=[u_modT])
        AB = self.AB
        for (slot, m0, g0) in ((0, 8, 0), (2, 32, 8)):
            P.op("dve", lambda e, slot=slot, m0=m0, g0=g0: e.scalar_tensor_tensor(
                out=AB[:, i * 4 + slot], in0=modT[:, m0:m0 + 8, :], scalar=1.0,
                in1=gT[:, g0:g0 + 8].unsqueeze(2).to_broadcast([128, 8, 2]), op0=ALU.add, op1=ALU.mult),
                reads=[u_modT, u_T], writes=[self.u_AB])
        for (slot, m0) in ((1, 0), (3, 24)):
            P.op("dve", lambda e, slot=slot, m0=m0: e.tensor_copy(out=AB[:, i * 4 + slot], in_=modT[:, m0:m0 + 8, :]),
                 reads=[u_modT], writes=[self.u_AB])
        psT, upsT = self.nps()
        self.tr(psT[0:96, 0:128], modT[:].rearrange("p m s -> p (m s)"), self.ident_f[:], [u_modT, self.u_const], [upsT])
        P.op("dve", lambda e: e.tensor_copy(out=modTT[:], in_=psT[0:96, 0:128]), reads=[upsT], writes=[u_modTT])
        P.dma("sp", lambda e: e.dma_start(out=self.modd[0, i * 12288:(i + 1) * 12288].rearrange("(r p) -> r p", p=128),
                                          in_=modTT[:]), reads=[u_modTT], writes=[self.u_modd])
        P.phase_end()

    def gate_bcast_src(self, i, which, s):
        m0 = 16 if which == 0 else 40
        base = i * 12288 + (m0 * 2 + s) * 128
        v = self.modd[0:1, base:base + 8 * 256].rearrange("o (j r) -> o j r", r=256)[:, :, 0:128]
        return v.partition_broadcast(128)[:, 0]

    def norm_phase(self, i, which, write_xs2, tiles=None):
        P = self.P
        tiles = list(range(NTT)) if tiles is None else tiles
        P.phase_begin()
        xt = [P.psb("xt", [128, D], F32) for _ in range(2)]; u_xt = [U("xt0"), U("xt1")]
        xs = [P.psb("xs", [128, D], F32) for _ in range(2)]; u_xs = [U("xs0"), U("xs1")]
        xsb = [P.psb("xsb", [128, D], BF16) for _ in range(2)]; u_xsb = [U("xsb0"), U("xsb1")]
        junk = P.psb("junk", [128, D], BF16)
        ss = P.psb("ss", [128, NTT], F32); u_ss = [U("ss%d" % t) for t in range(NTT)]
        rs = P.psb("rs", [128, NTT], F32); u_rs = [U("rs%d" % t) for t in range(NTT)]
        A = self.AB[:, i * 4 + 2 * which]
        B = self.AB[:, i * 4 + 2 * which + 1]
        for n, tt in enumerate(tiles):
            s = 1 if tt < 2 else 0
            b = n % 2
            P.dma("sp", lambda e, b=b, tt=tt: e.dma_start(out=xt[b][:], in_=self.xres[tt * 128:(tt + 1) * 128, :]),
                  reads=[self.u_xres], writes=[u_xt[b]])
            P.op("act", lambda e, b=b, tt=tt: e.activation(out=junk[:], in_=xt[b][:], func=AF.Square,
                                                           accum_out=ss[:, tt:tt + 1]),
                 reads=[u_xt[b]], writes=[u_ss[tt]])
            P.op("dve", lambda e, tt=tt: e.tensor_scalar(out=rs[:, tt:tt + 1], in0=ss[:, tt:tt + 1], scalar1=1.0 / D,
                                                         scalar2=EPS, op0=ALU.mult, op1=ALU.add),
                 reads=[u_ss[tt]], writes=[u_rs[tt]])
            P.op("act", lambda e, tt=tt: e.sqrt(out=rs[:, tt:tt + 1], in_=rs[:, tt:tt + 1]),
                 reads=[u_rs[tt]], writes=[u_rs[tt]])
            P.op("dve", lambda e, tt=tt: e.reciprocal(out=rs[:, tt:tt + 1], in_=rs[:, tt:tt + 1]),
                 reads=[u_rs[tt]], writes=[u_rs[tt]])
            P.op("dve", lambda e, b=b, tt=tt: e.tensor_scalar(out=xs[b][:], in0=xt[b][:], scalar1=rs[:, tt:tt + 1],
                                                              scalar2=None, op0=ALU.mult),
                 reads=[u_xt[b], u_rs[tt]], writes=[u_xs[b]])
            if write_xs2:
                P.op("pool", lambda e, b=b: e.tensor_copy(out=xsb[b][:], in_=xs[b][:]), reads=[u_xs[b]], writes=[u_xsb[b]])
                P.dma("pool", lambda e, b=b, tt=tt: e.dma_start(out=self.xs2[tt * 128:(tt + 1) * 128, :], in_=xsb[b][:]),
                      reads=[u_xsb[b]], writes=[self.u_xs2])
            for half in range(2):
                ps, ups = self.nps()
                for kk in range(4):
                    k = half * 4 + kk
                    self.tr(ps[:, kk * 128:(kk + 1) * 128], xs[b][:, k * 128:(k + 1) * 128], self.ident_f[:],
                            [u_xs[b], self.u_const], [ups])
                for kk in range(4):
                    k = half * 4 + kk
                    P.op("act", lambda e, k=k, kk=kk, tt=tt, s=s, ps=ps: e.activation(
                        out=self.hT[:, k, tt * 128:(tt + 1) * 128], in_=ps[:, kk * 128:(kk + 1) * 128],
                        func=AF.Identity, scale=A[:, k, s:s + 1], bias=B[:, k, s:s + 1]),
                        reads=[ups, self.u_AB], writes=[self.u_hT[tt]])
        P.phase_end()

    def moe_phase(self, i, do_ctx):
        P = self.P
        I = self.I
        for nm in ("moe_router", "moe_w1", "moe_w3", "moe_w2"):
            I(nm)
        NS = 544 if do_ctx else 512
        P.phase_begin()
        wr = P.psb("wr", [128, 8, 16], BF16); u_wr = U("wr")
        E = P.psb("E", [16, NT], F32); u_E = U("E")
        wk = P.psb("wk", [16, NT], F32); u_wkl = U("wkl"); u_wkc = U("wkc")
        mx = P.psb("mx", [16, 544], F32); u_mx = U("mx")
        ix = P.psb("ix", [16, 544], U32); u_ix = U("ix")
        ixf = P.psb("ixf", [16, 544], F32); u_ixf = U("ixf")
        rcp = P.psb("rcp", [16, 512], F32); u_rcp = U("rcp")
        P.dma("pool", lambda e: e.dma_start(out=wr[:], in_=I("moe_router")[i].rearrange("(k p) n -> p k n", p=128)),
              writes=[u_wr])
        chunks = [(c0, min(512, NT - c0)) for c0 in range(0, NT, 512)]
        for (c0, n) in chunks:
            ps, ups = self.nps()
            tts = list(range(c0 // 128, (c0 + n) // 128))
            for k in range(8):
                self.mm(ps[0:16, 0:n], wr[:, k, :], self.hT[:, k, c0:c0 + n], k == 0, k == 7,
                        [u_wr] + [self.u_hT[t] for t in tts], [ups])
            P.op("act", lambda e, ps=ps, c0=c0, n=n: e.activation(out=E[:, c0:c0 + n], in_=ps[0:16, 0:n], func=AF.Exp),
                 reads=[ups], writes=[u_E])
        for (c0, n) in chunks:
            ps, ups = self.nps()
            self.mm(ps[0:16, 0:n], self.ones_f[0:16, 0:16], E[:, c0:c0 + n], True, True, [u_E, self.u_const], [ups])
            P.op("dve", lambda e, ps=ps, n=n: e.reciprocal(out=rcp[:, 0:n], in_=ps[0:16, 0:n]),
                 reads=[ups], writes=[u_rcp])
            P.op("dve", lambda e, c0=c0, n=n: e.tensor_tensor(out=E[:, c0:c0 + n], in0=E[:, c0:c0 + n],
                                                              in1=rcp[:, 0:n], op=ALU.mult),
                 reads=[u_rcp, u_E], writes=[u_E])
        sets = [(NCTX, NLAT, 0, 64, u_wkl)]
        if do_ctx:
            sets.append((0, NCTX, 512, 4, u_wkc))
        for (t0, n, s0, iters, u_wk) in sets:
            src, us = E, u_E
            for it in range(iters):
                sl = slice(s0 + it * 8, s0 + it * 8 + 8)
                P.op("dve", lambda e, src=src, sl=sl, t0=t0, n=n: e.max(out=mx[:, sl], in_=src[:, t0:t0 + n]),
                     reads=[us], writes=[u_mx])
                P.op("dve", lambda e, src=src, sl=sl, t0=t0, n=n: e.max_index(out=ix[:, sl], in_max=mx[:, sl],
                                                                               in_values=src[:, t0:t0 + n]),
                     reads=[us, u_mx], writes=[u_ix])
                if it < iters - 1:
                    P.op("dve", lambda e, src=src, sl=sl, t0=t0, n=n: e.match_replace(
                        out=wk[:, t0:t0 + n], in_to_replace=mx[:, sl], in_values=src[:, t0:t0 + n], imm_value=0.0),
                        reads=[us, u_mx], writes=[u_wk])
                src, us = wk, u_wk
        P.op("dve", lambda e: e.tensor_copy(out=ixf[:, 0:NS], in_=ix[:, 0:NS]), reads=[u_ix], writes=[u_ixf])
        P.op("dve", lambda e: e.tensor_scalar(out=ixf[:, 0:512], in0=ixf[:, 0:512], scalar1=float(NCTX), scalar2=None,
                                              op0=ALU.add), reads=[u_ixf], writes=[u_ixf])
        u_gateT = U("gateT"); u_idxT = U("idxT")
        nch = 5 if do_ctx else 4
        for ch in range(nch):
            rows = 128 if ch < 4 else 32
            ps, ups = self.nps()
            self.tr(ps[0:rows, 0:16], mx[:, ch * 128:ch * 128 + rows], self.ident_f[0:16, 0:16], [u_mx, self.u_const], [ups])
            self.tr(ps[0:rows, 16:32], ixf[:, ch * 128:ch * 128 + rows], self.ident_f[0:16, 0:16], [u_ixf, self.u_const], [ups])
            P.op("dve", lambda e, ps=ps, ch=ch, rows=rows: e.tensor_copy(out=self.gateT[0:rows, ch, :], in_=ps[0:rows, 0:16]),
                 reads=[ups], writes=[u_gateT])
            P.op("dve", lambda e, ps=ps, ch=ch, rows=rows: e.tensor_copy(out=self.idxT[0:rows, ch, :], in_=ps[0:rows, 16:32]),
                 reads=[ups], writes=[u_idxT])
        P.phase_end()
        P.phase_begin()
        NWB = 4
        wb = [P.psb("wb", [128, 8, D], BF16) for _ in range(NWB)]; u_wb = [U("wb%d" % j) for j in range(NWB)]
        xin = [P.psb("xin", [128, D], BF16) for _ in range(3)]; u_xin = [U("xin%d" % j) for j in range(3)]
        xinT = [P.psb("xinT", [128, 8, 640], BF16) for _ in range(2)]; u_xinT = [U("xinT0"), U("xinT1")]
        hidT = P.psb("hidT", [128, 8, 640], BF16); u_hidT = U("hidT")
        sg = [P.psb("sg", [128, 512], F32) for _ in range(2)]; u_sg = [U("sg0"), U("sg1")]
        ysb = [P.psb("ysb", [128, D], F32) for _ in range(2)]; u_ysb = [U("ysb0"), U("ysb1")]
        G = P.psb("G", [128, 2, 8, 128], F32); u_G = U("G")
        for s in range(2 if do_ctx else 1):
            P.dma("sp", lambda e, s=s: e.dma_start(out=G[:, s], in_=self.gate_bcast_src(i, 1, s)),
                  reads=[self.u_modd], writes=[u_G])
        A2 = self.AB[:, i * 4 + 2]
        B2 = self.AB[:, i * 4 + 3]
        wn = 0
        gn = 0
        yn = 0
        segs = [(0, 512, 0)] + ([(512, 32, 1)] if do_ctx else [])
        for ex in range(16):
            wl = {}
            for nm in ("moe_w1", "moe_w3", "moe_w2"):
                b = wn % NWB
                wn += 1
                P.dma("pool", lambda e, nm=nm, b=b, ex=ex: e.dma_start(
                    out=wb[b][:], in_=I(nm)[i, ex].rearrange("(k p) n -> p k n", p=128)), writes=[u_wb[b]])
                wl[nm] = (wb[b], u_wb[b])
            xT, u_xT = xinT[ex % 2], u_xinT[ex % 2]
            for ch in range(nch):
                rows = 128 if ch < 4 else 32
                s = 0 if ch < 4 else 1
                t0, tn = (NCTX, NLAT) if ch < 4 else (0, NCTX)
                b = gn % 3
                gn += 1
                P.dma("pool", lambda e, b=b, ch=ch, rows=rows, t0=t0, tn=tn, ex=ex: e.indirect_dma_start(
                    out=xin[b][0:rows, :], out_offset=None, in_=self.xs2[:, :],
                    in_offset=bass.IndirectOffsetOnAxis(ap=self.idxT[0:rows, ch, ex:ex + 1], axis=0)),
                    reads=[u_idxT, self.u_xs2], writes=[u_xin[b]])
                pb, upb = self.npsb()
                for k in range(8):
                    self.tr(pb[:, k * 128:k * 128 + rows], xin[b][0:rows, k * 128:(k + 1) * 128],
                            self.ident_b[0:rows, 0:rows], [u_xin[b], self.u_const], [upb])
                for k in range(8):
                    P.op("act", lambda e, k=k, ch=ch, rows=rows, s=s, pb=pb, xT=xT: e.activation(
                        out=xT[:, k, ch * 128:ch * 128 + rows], in_=pb[:, k * 128:k * 128 + rows],
                        func=AF.Identity, scale=A2[:, k, s:s + 1], bias=B2[:, k, s:s + 1]),
                        reads=[upb, self.u_AB], writes=[u_xT])
            w1, u_w1 = wl["moe_w1"]
            w3, u_w3 = wl["moe_w3"]
            w2, u_w2 = wl["moe_w2"]
            for f in range(8):
                for (lo, n, s) in segs:
                    p1, up1 = self.nps()
                    p3, up3 = self.nps()
                    for k in range(8):
                        self.mm(p1[:, 0:n], w1[:, k, f * 128:(f + 1) * 128], xT[:, k, lo:lo + n], k == 0, k == 7,
                                [u_w1, u_xT], [up1])
                    for k in range(8):
                        self.mm(p3[:, 0:n], w3[:, k, f * 128:(f + 1) * 128], xT[:, k, lo:lo + n], k == 0, k == 7,
                                [u_w3, u_xT], [up3])
                    sb_ = (f * 2 + s) % 2
                    P.op("act", lambda e, p1=p1, n=n, sb_=sb_: e.activation(out=sg[sb_][:, 0:n], in_=p1[:, 0:n], func=AF.Silu),
                         reads=[up1], writes=[u_sg[sb_]])
                    P.op("dve", lambda e, p3=p3, n=n, sb_=sb_, f=f, lo=lo: e.tensor_tensor(
                        out=hidT[:, f, lo:lo + n], in0=sg[sb_][:, 0:n], in1=p3[:, 0:n], op=ALU.mult),
                        reads=[up3, u_sg[sb_]], writes=[u_hidT])
            for ch in range(nch):
                rows = 128 if ch < 4 else 32
                s = 0 if ch < 4 else 1
                t0, tn = (NCTX, NLAT) if ch < 4 else (0, NCTX)
                yb = yn % 2
                yn += 1
                for half in range(2):
                    py, upy = self.nps()
                    for k in range(8):
                        self.mm(py[0:rows, :], hidT[:, k, ch * 128:ch * 128 + rows], w2[:, k, half * 512:(half + 1) * 512],
                                k == 0, k == 7, [u_w2, u_hidT], [upy])
                    P.op("dve", lambda e, py=py, rows=rows, ch=ch, half=half, s=s, yb=yb, ex=ex: e.scalar_tensor_tensor(
                        out=ysb[yb][0:rows, half * 512:(half + 1) * 512], in0=py[0:rows, :],
                        scalar=self.gateT[0:rows, ch, ex:ex + 1],
                        in1=G[0:rows, s, half * 4:(half + 1) * 4, :].rearrange("p a b -> p (a b)"),
                        op0=ALU.mult, op1=ALU.mult), reads=[upy, u_gateT, u_G], writes=[u_ysb[yb]])
                P.dma("pool", lambda e, rows=rows, ch=ch, yb=yb, t0=t0, tn=tn, ex=ex: e.indirect_dma_start(
                    out=self.xres[:, :],
                    out_offset=bass.IndirectOffsetOnAxis(ap=self.idxT[0:rows, ch, ex:ex + 1], axis=0),
                    in_=ysb[yb][0:rows, :], in_offset=None, compute_op=ALU.add),
                    reads=[u_ysb[yb], u_idxT], writes=[self.u_xres])
        P.phase_end()

    def epilogue(self):
        P = self.P
        for j in range(8):
            P.dma("sp", lambda e, j=j: e.dma_start(out=self.out[j * 512:(j + 1) * 512, :],
                                                    in_=self.xres[NCTX + j * 512:NCTX + (j + 1) * 512, :]),
                  reads=[self.u_xres], is_out=True)
        if self.cfg.get("dump_ctx"):
            oc = P.dram("out_ctx", [NCTX, D], F32, kind="ExternalOutput")
            P.dma("sp", lambda e: e.dma_start(out=oc[:, :], in_=self.xres[0:NCTX, :]), reads=[self.u_xres], is_out=True)
        return P.finish()
NEG = -30000.0


class KMix:
    def mix_scratch(self):
        if hasattr(self, "QTd"):
            return
        P = self.P
        self.QTd = P.dram("QTd", [1536, NT], BF16); self.u_QT = U("QTd")
        self.KTd = P.dram("KTd", [1024, NT], BF16); self.u_KT = U("KTd")
        self.KRd = P.dram("KRd", [32, NT], BF16); self.u_KR = U("KRd")
        self.Vd = P.dram("Vd", [NT, 1024], BF16); self.u_V = U("Vd")
        self.OTd = P.dram("OTd", [1024, NT], BF16); self.u_OT = U("OTd")

    def wload(self, name, src, kch, ncols):
        P = self.P
        t = P.psb(name, [128, kch, ncols], BF16); u = U(name)
        P.dma("pool", lambda e: e.dma_start(out=t[:], in_=src.rearrange("(k p) n -> p k n", p=128)), writes=[u])
        return t, u

    def bload(self, name, src_row, n, scale=None, parts=128):
        P = self.P
        t = P.psb(name, [parts, n], F32); u = U(name)
        P.dma("sp", lambda e: e.dma_start(out=t[:], in_=src_row.partition_broadcast(parts)[:, 0]), writes=[u])
        if scale is not None:
            P.op("dve", lambda e: e.tensor_scalar(out=t[:], in0=t[:], scalar1=float(scale), scalar2=None, op0=ALU.mult),
                 reads=[u], writes=[u])
        return t, u

    def norm_scratch(self):
        P = self.P
        sc = {"sq": P.psb("nsq", [128, 1536], F32), "u_sq": U("nsq"),
              "ss": P.psb("nss", [128, 16], F32), "u_ss": U("nss"),
              "r": [P.psb("rp", [128, 512], F32) for _ in range(4)], "u_r": [U("rp%d" % j) for j in range(4)]}
        return sc

    def headnorm(self, src, us, H, n, gb, ugb, out, uo, sc):
        P = self.P
        sq = sc["sq"][:, 0:H * n].rearrange("p (h n) -> p h n", n=n)
        ss = sc["ss"][:, 0:H]
        u_sq, u_ss = sc["u_sq"], sc["u_ss"]
        P.op("dve", lambda e: e.tensor_tensor(out=sq, in0=src, in1=src, op=ALU.mult), reads=[us], writes=[u_sq])
        P.op("dve", lambda e: e.tensor_reduce(out=ss, in_=sq, axis=AX.X, op=ALU.add), reads=[u_sq], writes=[u_ss])
        P.op("dve", lambda e: e.tensor_scalar(out=ss, in0=ss, scalar1=1.0 / n, scalar2=EPS, op0=ALU.mult, op1=ALU.add),
             reads=[u_ss], writes=[u_ss])
        P.op("act", lambda e: e.sqrt(out=ss, in_=ss), reads=[u_ss], writes=[u_ss])
        P.op("dve", lambda e: e.reciprocal(out=ss, in_=ss), reads=[u_ss], writes=[u_ss])
        P.op("dve", lambda e: e.tensor_tensor(out=sq, in0=src, in1=ss.unsqueeze(2).to_broadcast([128, H, n]), op=ALU.mult),
             reads=[us, u_ss], writes=[u_sq])
        P.op("dve", lambda e: e.tensor_tensor(out=out, in0=sq, in1=gb[:, 0:n].unsqueeze(1).to_broadcast([128, H, n]),
                                              op=ALU.mult), reads=[u_sq, ugb], writes=[uo])

    def rope(self, x, ux, H, half, cs, sn, ucs, sc):
        P = self.P
        x1 = x[:, :, 0:half]
        x2 = x[:, :, half:2 * half]
        cb = cs.unsqueeze(1).to_broadcast([128, H, half])
        sb = sn.unsqueeze(1).to_broadcast([128, H, half])
        t = [sc["r"][j][:, 0:H * half].rearrange("p (h n) -> p h n", n=half) for j in range(4)]
        ut = sc["u_r"]
        for j, (a, b) in enumerate(((x1, cb), (x2, sb), (x2, cb), (x1, sb))):
            P.op("dve", lambda e, j=j, a=a, b=b: e.tensor_tensor(out=t[j], in0=a, in1=b, op=ALU.mult),
                 reads=[ux, ucs], writes=[ut[j]])
        P.op("dve", lambda e: e.tensor_tensor(out=x1, in0=t[0], in1=t[1], op=ALU.subtract), reads=[ut[0], ut[1]], writes=[ux])
        P.op("dve", lambda e: e.tensor_tensor(out=x2, in0=t[2], in1=t[3], op=ALU.add), reads=[ut[2], ut[3]], writes=[ux])

    def tpose_to_dram(self, blocks, ub, rows, dst, udst, stage, ustage):
        P = self.P
        pb, upb = self.npsb()
        nb = len(blocks)
        for j, blk in enumerate(blocks):
            self.tr(pb[0:rows, j * 128:(j + 1) * 128], blk, self.ident_b[:], [ub, self.u_const], [upb])
        P.op("act", lambda e: e.copy(out=stage[0:rows, 0:nb, :], in_=pb[0:rows, 0:nb * 128].rearrange("p (j t) -> p j t", t=128)),
             reads=[upb], writes=[ustage])
        P.dma("sp", lambda e: e.dma_start(out=dst, in_=stage[0:rows, 0:nb, :]), reads=[ustage], writes=[udst])

    def attn_setup(self):
        P = self.P
        a = {"pt": [P.psb("pt", [128, 512], BF16) for _ in range(3)], "u_pt": [U("pt%d" % j) for j in range(3)],
             "tmp": [P.psb("tmpb", [128, 512], F32) for _ in range(2)], "u_tmp": [U("tmp0"), U("tmp1")],
             "rd": P.psb("rd", [64, 512], F32), "u_rd": U("rd"), "n": 0}
        return a

    def attn_block(self, a, rhs_q, uq, nq, dk, keys, out_ap, uout, sink_fn=None, qg=1):
        P = self.P
        pso, upso = self.psf[4], self.u_psf[4]
        psd, upsd = self.psf[5], self.u_psf[5]
        last = len(keys) - 1
        for j, (kt, uk, v, uv, nk, bias, ubias) in enumerate(keys):
            ps, ups = self.nps()
            so = ps[0:nk, 0:nq] if qg == 1 else ps[0:nk, 0:nq].rearrange("p (g t) -> p g t", g=qg)
            self.mm(so, kt, rhs_q, True, True, [uk, uq], [ups])
            n = a["n"]; a["n"] += 1
            pt, upt = a["pt"][n % 3], a["u_pt"][n % 3]
            if bias is not None:
                tmp, utmp = a["tmp"][n % 2], a["u_tmp"][n % 2]
                P.op("dve", lambda e, ps=ps, nk=nk, tmp=tmp, bias=bias: e.tensor_tensor(
                    out=tmp[0:nk, 0:nq], in0=ps[0:nk, 0:nq], in1=bias, op=ALU.add), reads=[ups, ubias], writes=[utmp])
                P.op("act", lambda e, nk=nk, tmp=tmp, pt=pt: e.activation(out=pt[0:nk, 0:nq], in_=tmp[0:nk, 0:nq], func=AF.Exp),
                     reads=[utmp], writes=[upt])
            else:
                P.op("act", lambda e, ps=ps, nk=nk, pt=pt: e.activation(out=pt[0:nk, 0:nq], in_=ps[0:nk, 0:nq], func=AF.Exp),
                     reads=[ups], writes=[upt])
            self.mm(pso[0:64, 0:nq], v, pt[0:nk, 0:nq], j == 0, j == last, [uv, upt], [upso])
            self.mm(psd[0:64, 0:nq], self.ones_b[0:nk, 0:64], pt[0:nk, 0:nq], j == 0, j == last, [upt, self.u_const], [upsd])
        rd, urd = a["rd"], a["u_rd"]
        if sink_fn is not None:
            sink_fn(psd, upsd, rd, urd)
        else:
            P.op("dve", lambda e: e.tensor_copy(out=rd[:, 0:nq], in_=psd[0:64, 0:nq]), reads=[upsd], writes=[urd])
        P.op("dve", lambda e: e.reciprocal(out=rd[:, 0:nq], in_=rd[:, 0:nq]), reads=[urd], writes=[urd])
        if qg == 1:
            o_in, r_in = pso[0:64, 0:nq], rd[:, 0:nq]
        else:
            o_in = pso[0:64, 0:nq].rearrange("p (g t) -> p g t", g=qg)
            r_in = rd[:, 0:nq].rearrange("p (g t) -> p g t", g=qg)
        P.op("dve", lambda e: e.tensor_tensor(out=out_ap, in0=o_in, in1=r_in, op=ALU.mult),
             reads=[upso, urd], writes=[uout])

    def outproj_phase(self, i, wname, do_ctx):
        P = self.P
        wsrc = self.I(wname)
        P.phase_begin()
        wo, uwo = self.wload("wo", wsrc, 8, D)
        for k in range(8):
            P.dma("sp", lambda e, k=k: e.dma_start(out=self.hT[:, k, :], in_=self.OTd[k * 128:(k + 1) * 128, :]),
                  reads=[self.u_OT], writes=self.u_hT)
        G = P.psb("G1", [128, 2, 8, 128], F32); u_G = U("G1")
        for s in range(2):
            P.dma("sp", lambda e, s=s: e.dma_start(out=G[:, s], in_=self.gate_bcast_src(i, 0, s)),
                  reads=[self.u_modd], writes=[u_G])
        xt = [P.psb("xto", [128, D], F32) for _ in range(2)]; u_xt = [U("xto0"), U("xto1")]
        tmp = [P.psb("tmpo", [128, 512], F32) for _ in range(2)]; u_tmp = [U("tmpo0"), U("tmpo1")]
        tiles = list(range(NTT)) if do_ctx else list(range(2, NTT))
        for n, tt in enumerate(tiles):
            s = 1 if tt < 2 else 0
            b = n % 2
            P.dma("sp", lambda e, b=b, tt=tt: e.dma_start(out=xt[b][:], in_=self.xres[tt * 128:(tt + 1) * 128, :]),
                  reads=[self.u_xres], writes=[u_xt[b]])
            for half in range(2):
                ps, ups = self.nps()
                for k in range(8):
                    self.mm(ps[:, :], self.hT[:, k, tt * 128:(tt + 1) * 128], wo[:, k, half * 512:(half + 1) * 512],
                            k == 0, k == 7, [self.u_hT[tt], uwo], [ups])
                P.op("dve", lambda e, ps=ps, half=half, s=s: e.tensor_tensor(
                    out=tmp[half][:], in0=ps[:, :], in1=G[:, s, half * 4:(half + 1) * 4, :].rearrange("p a b -> p (a b)"),
                    op=ALU.mult), reads=[ups, u_G], writes=[u_tmp[half]])
                P.op("dve", lambda e, half=half, b=b: e.tensor_tensor(
                    out=xt[b][:, half * 512:(half + 1) * 512], in0=xt[b][:, half * 512:(half + 1) * 512],
                    in1=tmp[half][:], op=ALU.add), reads=[u_tmp[half], u_xt[b]], writes=[u_xt[b]])
            P.dma("pool", lambda e, b=b, tt=tt: e.dma_start(out=self.xres[tt * 128:(tt + 1) * 128, :], in_=xt[b][:]),
                  reads=[u_xt[b]], writes=[self.u_xres])
        P.phase_end()

    def mla_proj(self, i):
        P = self.P
        I = self.I
        for nm in ("mla_w_down", "mla_q_norm_g", "mla_kv_norm_g", "mla_w_uq", "mla_w_ukv", "mla_qn_g", "mla_qr_g",
                   "mla_kn_g", "mla_kr_g", "mla_cos", "mla_sin"):
            I(nm)
        scale = 96.0 ** -0.5
        P.phase_begin()
        wd, uwd = self.wload("wd", I("mla_w_down"), 8, 672)
        wuq, uwuq = self.wload("wuq", I("mla_w_uq"), 3, 1536)
        wukv, uwukv = self.wload("wukv", I("mla_w_ukv"), 2, 2048)
        gq, ugq = self.bload("gq", I("mla_q_norm_g")[0:1, :], 384)
        gkv, ugkv = self.bload("gkv", I("mla_kv_norm_g")[0:1, :], 256)
        gkr, ugkr = self.bload("gkr", I("mla_kr_g")[0:1, :], 32)
        gqn, ugqn = self.bload("gqn", I("mla_qn_g")[0:1, :], 64, scale=scale)
        gqr, ugqr = self.bload("gqr", I("mla_qr_g")[0:1, :], 32, scale=scale)
        gkn, ugkn = self.bload("gkn", I("mla_kn_g")[0:1, :], 64)
        sc = self.norm_scratch()
        cqT = P.psb("cqT", [128, 3, NT], BF16); u_cqT = [U("cqT%d" % t) for t in range(NTT)]
        ckvT = P.psb("ckvT", [128, 2, NT], BF16); u_ckvT = [U("ckvT%d" % t) for t in range(NTT)]
        df = P.psb("df", [128, 672], F32); u_df = U("df")
        dn = P.psb("dn", [128, 672], F32); u_dn = U("dn")
        db = P.psb("db", [128, 768], BF16); u_db = U("db")
        cs = [P.psb("cs", [128, 16], F32) for _ in range(2)]; sn = [P.psb("sn", [128, 16], F32) for _ in range(2)]
        u_cs = [U("cs0"), U("cs1")]
        krs = P.psb("krs", [32, 1, 128], BF16); u_krs = U("krs")
        P.op("pool", lambda e: e.memset(db[:], 0.0), writes=[u_db])
        for tt in range(NTT):
            lat = tt >= 2
            ps0, up0 = self.nps()
            ps1, up1 = self.nps()
            for k in range(8):
                self.mm(ps0[:, 0:512], self.hT[:, k, tt * 128:(tt + 1) * 128], wd[:, k, 0:512], k == 0, k == 7,
                        [self.u_hT[tt], uwd], [up0])
            for k in range(8):
                self.mm(ps1[:, 0:160], self.hT[:, k, tt * 128:(tt + 1) * 128], wd[:, k, 512:672], k == 0, k == 7,
                        [self.u_hT[tt], uwd], [up1])
            P.op("act", lambda e, ps0=ps0: e.copy(out=df[:, 0:512], in_=ps0[:, 0:512]), reads=[up0], writes=[u_df])
            P.op("act", lambda e, ps1=ps1: e.copy(out=df[:, 512:672], in_=ps1[:, 0:160]), reads=[up1], writes=[u_df])
            for (c0, n, g, ug) in ((0, 384, gq, ugq), (384, 256, gkv, ugkv), (640, 32, gkr, ugkr)):
                self.headnorm(df[:, c0:c0 + n].unsqueeze(1), u_df, 1, n, g, ug, dn[:, c0:c0 + n].unsqueeze(1), u_dn, sc)
            if lat:
                b = tt % 2
                t0 = (tt - 2) * 128
                P.dma("sp", lambda e, b=b, t0=t0: e.dma_start(out=cs[b][:], in_=I("mla_cos")[t0:t0 + 128, :]), writes=[u_cs[b]])
                P.dma("sp", lambda e, b=b, t0=t0: e.dma_start(out=sn[b][:], in_=I("mla_sin")[t0:t0 + 128, :]), writes=[u_cs[b]])
                self.rope(dn[:, 640:672].unsqueeze(1), u_dn, 1, 16, cs[b][:], sn[b][:], u_cs[b], sc)
            P.op("act", lambda e: e.copy(out=db[:, 0:672], in_=dn[:, 0:672]), reads=[u_dn], writes=[u_db])
            pb, upb = self.npsb()
            for j in range(5):
                self.tr(pb[:, j * 128:(j + 1) * 128], db[:, j * 128:(j + 1) * 128], self.ident_b[:], [u_db, self.u_const], [upb])
            self.tr(pb[:, 640:768], db[:, 640:768], self.ident_b[:], [u_db, self.u_const], [upb])
            P.op("act", lambda e, pb=pb, tt=tt: e.copy(out=cqT[:, :, tt * 128:(tt + 1) * 128],
                                                       in_=pb[:, 0:384].rearrange("p (j t) -> p j t", t=128)),
                 reads=[upb], writes=[u_cqT[tt]])
            P.op("act", lambda e, pb=pb, tt=tt: e.copy(out=ckvT[:, :, tt * 128:(tt + 1) * 128],
                                                       in_=pb[:, 384:640].rearrange("p (j t) -> p j t", t=128)),
                 reads=[upb], writes=[u_ckvT[tt]])
            P.op("act", lambda e, pb=pb: e.copy(out=krs[:, 0, :], in_=pb[0:32, 640:768]), reads=[upb], writes=[u_krs])
            P.dma("sp", lambda e, tt=tt: e.dma_start(out=self.KRd[:, tt * 128:(tt + 1) * 128], in_=krs[:, 0, :]),
                  reads=[u_krs], writes=[self.u_KR])
        qf = P.psb("qf", [128, 16, 96], F32); u_qf = U("qf")
        qn = P.psb("qn", [128, 16, 96], F32); u_qn = U("qn")
        qb = P.psb("qb", [128, 16, 96], BF16); u_qb = U("qb")
        kvf = P.psb("kvf", [128, 16, 128], F32); u_kvf = U("kvf")
        knf = P.psb("knf", [128, 16, 64], F32); u_knf = U("knf")
        knb = P.psb("knb", [128, 16, 64], BF16); u_knb = U("knb")
        vb = P.psb("vb", [128, 16, 64], BF16); u_vb = U("vb")
        stq = [P.psb("stq", [96, 8, 128], BF16) for _ in range(2)]; u_stq = [U("stq0"), U("stq1")]
        stk = P.psb("stk", [128, 8, 128], BF16); u_stk = U("stk")
        qflat = qf[:].rearrange("p h n -> p (h n)")
        kvflat = kvf[:].rearrange("p h n -> p (h n)")
        for tt in range(NTT):
            lat = tt >= 2
            for cc in range(3):
                ps, ups = self.nps()
                for k in range(3):
                    self.mm(ps[:, :], cqT[:, k, tt * 128:(tt + 1) * 128], wuq[:, k, cc * 512:(cc + 1) * 512], k == 0, k == 2,
                            [u_cqT[tt], uwuq], [ups])
                P.op("act", lambda e, ps=ps, cc=cc: e.copy(out=qflat[:, cc * 512:(cc + 1) * 512], in_=ps[:, :]),
                     reads=[ups], writes=[u_qf])
            self.headnorm(qf[:, :, 0:64], u_qf, 16, 64, gqn, ugqn, qn[:, :, 0:64], u_qn, sc)
            self.headnorm(qf[:, :, 64:96], u_qf, 16, 32, gqr, ugqr, qn[:, :, 64:96], u_qn, sc)
            if lat:
                b = tt % 2
                t0 = (tt - 2) * 128
                P.dma("sp", lambda e, b=b, t0=t0: e.dma_start(out=cs[b][:], in_=I("mla_cos")[t0:t0 + 128, :]), writes=[u_cs[b]])
                P.dma("sp", lambda e, b=b, t0=t0: e.dma_start(out=sn[b][:], in_=I("mla_sin")[t0:t0 + 128, :]), writes=[u_cs[b]])
                self.rope(qn[:, :, 64:96], u_qn, 16, 16, cs[b][:], sn[b][:], u_cs[b], sc)
            P.op("act", lambda e: e.copy(out=qb[:], in_=qn[:]), reads=[u_qn], writes=[u_qb])
            for hh in range(2):
                self.tpose_to_dram([qb[:, hh * 8 + j, :] for j in range(8)], u_qb, 96,
                                   self.QTd[hh * 768:(hh + 1) * 768, tt * 128:(tt + 1) * 128].rearrange("(j d) t -> d j t", d=96),
                                   self.u_QT, stq[hh], u_stq[hh])
            for cc in range(4):
                ps, ups = self.nps()
                for k in range(2):
                    self.mm(ps[:, :], ckvT[:, k, tt * 128:(tt + 1) * 128], wukv[:, k, cc * 512:(cc + 1) * 512], k == 0, k == 1,
                            [u_ckvT[tt], uwukv], [ups])
                P.op("act", lambda e, ps=ps, cc=cc: e.copy(out=kvflat[:, cc * 512:(cc + 1) * 512], in_=ps[:, :]),
                     reads=[ups], writes=[u_kvf])
            self.headnorm(kvf[:, :, 0:64], u_kvf, 16, 64, gkn, ugkn, knf[:], u_knf, sc)
            P.op("act", lambda e: e.copy(out=knb[:], in_=knf[:]), reads=[u_knf], writes=[u_knb])
            P.op("pool", lambda e: e.tensor_copy(out=vb[:], in_=kvf[:, :, 64:128]), reads=[u_kvf], writes=[u_vb])
            P.dma("sp", lambda e, tt=tt: e.dma_start(out=self.Vd[tt * 128:(tt + 1) * 128, :].rearrange("t (h n) -> t h n", n=64),
                                                     in_=vb[:]), reads=[u_vb], writes=[self.u_V])
            knb2 = knb[:].rearrange("p h n -> p (h n)")
            self.tpose_to_dram([knb2[:, j * 128:(j + 1) * 128] for j in range(8)], u_knb, 128,
                               self.KTd[:, tt * 128:(tt + 1) * 128].rearrange("(j p) t -> p j t", p=128),
                               self.u_KT, stk, u_stk)
        P.phase_end()

    def mla_attn(self, do_ctx):
        P = self.P
        P.phase_begin()
        a = self.attn_setup()
        QT = [P.psb("QTh", [96, NT], BF16) for _ in range(2)]; uQ = [U("QTh0"), U("QTh1")]
        KT = [P.psb("KTh", [96, NT], BF16) for _ in range(2)]; uK = [U("KTh0"), U("KTh1")]
        V = [P.psb("Vh", [128, NTT, 64], BF16) for _ in range(2)]; uV = [U("Vh0"), U("Vh1")]
        OS = [P.psb("OSh", [64, NT], BF16) for _ in range(2)]; uOS = [U("OSh0"), U("OSh1")]
        for h in range(16):
            b = h % 2
            P.dma("sp", lambda e, b=b, h=h: e.dma_start(out=QT[b][:], in_=self.QTd[h * 96:(h + 1) * 96, :]),
                  reads=[self.u_QT], writes=[uQ[b]])
            P.dma("sp", lambda e, b=b, h=h: e.dma_start(out=KT[b][0:64, :], in_=self.KTd[h * 64:(h + 1) * 64, :]),
                  reads=[self.u_KT], writes=[uK[b]])
            P.dma("sp", lambda e, b=b: e.dma_start(out=KT[b][64:96, :], in_=self.KRd[:, :]), reads=[self.u_KR], writes=[uK[b]])
            P.dma("sp", lambda e, b=b, h=h: e.dma_start(
                out=V[b][:], in_=self.Vd[:, h * 64:(h + 1) * 64].rearrange("(t p) c -> p t c", p=128)),
                reads=[self.u_V], writes=[uV[b]])
            blocks = [(NCTX + c * 512, 512, list(range(NTT))) for c in range(8)]
            if do_ctx:
                blocks.append((0, NCTX, [0, 1]))
            for (q0, nq, kts) in blocks:
                keys = [(KT[b][:, kt * 128:(kt + 1) * 128], uK[b], V[b][:, kt, :], uV[b], 128, None, None) for kt in kts]
                self.attn_block(a, QT[b][:, q0:q0 + nq], uQ[b], nq, 96, keys, OS[b][:, q0:q0 + nq], uOS[b])
            c0 = 0 if do_ctx else NCTX
            P.dma("pool", lambda e, b=b, h=h, c0=c0: e.dma_start(out=self.OTd[h * 64:(h + 1) * 64, c0:NT], in_=OS[b][:, c0:NT]),
                  reads=[uOS[b]], writes=[self.u_OT])
        P.phase_end()


    def qkv_proj(self, wname, Hk, gqname, gkname, scale, rope=None):
        P = self.P
        I = self.I
        for nm in (wname, gqname, gkname) + (tuple(rope) if rope else ()):
            I(nm)
        ncol = 1024 + 2 * Hk * 64
        P.phase_begin()
        w, uw = self.wload("wqkv", I(wname), 8, ncol)
        gq, ugq = self.bload("gq", I(gqname)[0:1, :], 64, scale=scale)
        gk, ugk = self.bload("gk", I(gkname)[0:1, :], 64)
        sc = self.norm_scratch()
        qf = P.psb("qf", [128, ncol], F32); u_qf = U("qf")
        qn = P.psb("qn", [128, 16, 64], F32); u_qn = U("qn")
        kn = P.psb("kn", [128, Hk, 64], F32); u_kn = U("kn")
        qb = P.psb("qb", [128, 1024], BF16); u_qb = U("qb")
        kb = P.psb("kb", [128, Hk * 64], BF16); u_kb = U("kb")
        vb = P.psb("vb", [128, Hk * 64], BF16); u_vb = U("vb")
        stq = P.psb("stq", [128, 8, 128], BF16); u_stq = U("stq")
        stk = P.psb("stk", [128, 8, 128], BF16); u_stk = U("stk")
        if rope:
            cs = [P.psb("cs", [128, 32], F32) for _ in range(2)]; sn = [P.psb("sn", [128, 32], F32) for _ in range(2)]
            u_cs = [U("cs0"), U("cs1")]
        nb = ncol // 512
        kc0 = 1024
        vc0 = 1024 + Hk * 64
        for tt in range(NTT):
            lat = tt >= 2
            for cc in range(nb):
                ps, ups = self.nps()
                for k in range(8):
                    self.mm(ps[:, :], self.hT[:, k, tt * 128:(tt + 1) * 128], w[:, k, cc * 512:(cc + 1) * 512], k == 0, k == 7,
                            [self.u_hT[tt], uw], [ups])
                P.op("act", lambda e, ps=ps, cc=cc: e.copy(out=qf[:, cc * 512:(cc + 1) * 512], in_=ps[:, :]),
                     reads=[ups], writes=[u_qf])
            self.headnorm(qf[:, 0:1024].rearrange("p (h n) -> p h n", n=64), u_qf, 16, 64, gq, ugq, qn[:], u_qn, sc)
            self.headnorm(qf[:, kc0:kc0 + Hk * 64].rearrange("p (h n) -> p h n", n=64), u_qf, Hk, 64, gk, ugk, kn[:], u_kn, sc)
            if rope and lat:
                b = tt % 2
                t0 = (tt - 2) * 128
                P.dma("sp", lambda e, b=b, t0=t0: e.dma_start(out=cs[b][:], in_=I(rope[0])[t0:t0 + 128, :]), writes=[u_cs[b]])
                P.dma("sp", lambda e, b=b, t0=t0: e.dma_start(out=sn[b][:], in_=I(rope[1])[t0:t0 + 128, :]), writes=[u_cs[b]])
                self.rope(qn[:], u_qn, 16, 32, cs[b][:], sn[b][:], u_cs[b], sc)
                self.rope(kn[:], u_kn, Hk, 32, cs[b][:], sn[b][:], u_cs[b], sc)
            P.op("act", lambda e: e.copy(out=qb[:], in_=qn[:].rearrange("p h n -> p (h n)")), reads=[u_qn], writes=[u_qb])
            P.op("act", lambda e: e.copy(out=kb[:], in_=kn[:].rearrange("p h n -> p (h n)")), reads=[u_kn], writes=[u_kb])
            P.op("pool", lambda e: e.tensor_copy(out=vb[:], in_=qf[:, vc0:vc0 + Hk * 64]), reads=[u_qf], writes=[u_vb])
            P.dma("sp", lambda e, tt=tt: e.dma_start(out=self.Vd[tt * 128:(tt + 1) * 128, 0:Hk * 64], in_=vb[:]),
                  reads=[u_vb], writes=[self.u_V])
            self.tpose_to_dram([qb[:, j * 128:(j + 1) * 128] for j in range(8)], u_qb, 128,
                               self.QTd[0:1024, tt * 128:(tt + 1) * 128].rearrange("(j p) t -> p j t", p=128),
                               self.u_QT, stq, u_stq)
            nkb = Hk * 64 // 128
            self.tpose_to_dram([kb[:, j * 128:(j + 1) * 128] for j in range(nkb)], u_kb, 128,
                               self.KTd[0:Hk * 64, tt * 128:(tt + 1) * 128].rearrange("(j p) t -> p j t", p=128),
                               self.u_KT, stk, u_stk)
        P.phase_end()

    def na_proj(self, i):
        self.qkv_proj("na_w_qkv", 16, "na_q_g", "na_k_g", 64.0 ** -0.5)

    def swa_proj(self, i):
        self.qkv_proj("swa_w_qkv", 4, "swa_q_g", "swa_k_g", 64.0 ** -0.5, rope=("swa_cos", "swa_sin"))

    def swa_attn(self, do_ctx):
        P = self.P
        I = self.I
        I("swa_sink")
        P.phase_begin()
        a = self.attn_setup()
        Mp = P.psb("Mp", [128, 4, 128], F32); Mn = P.psb("Mn", [128, 4, 128], F32); u_M = U("M")
        P.op("pool", lambda e: e.memset(Mp[:], 0.0), writes=[u_M])
        P.op("pool", lambda e: e.memset(Mn[:], 0.0), writes=[u_M])
        P.op("pool", lambda e: e.affine_select(out=Mp[:], in_=Mp[:], pattern=[[0, 4], [-1, 128]], compare_op=ALU.is_ge,
                                               fill=NEG, base=0, channel_multiplier=1), reads=[u_M], writes=[u_M])
        P.op("pool", lambda e: e.affine_select(out=Mn[:], in_=Mn[:], pattern=[[0, 4], [1, 128]], compare_op=ALU.is_ge,
                                               fill=NEG, base=0, channel_multiplier=-1), reads=[u_M], writes=[u_M])
        Mp2 = Mp[:].rearrange("p g t -> p (g t)")
        Mn2 = Mn[:].rearrange("p g t -> p (g t)")
        esink, u_es = self.bload("esink", I("swa_sink")[0:1, :], 16, parts=64)
        P.op("act", lambda e: e.activation(out=esink[:], in_=esink[:], func=AF.Exp), reads=[u_es], writes=[u_es])
        Q = P.psb("Qall", [64, 4, NT], BF16); uQ = U("Qall")
        KT = P.psb("KTh", [64, NT], BF16); uK = U("KTh")
        V = P.psb("Vh", [128, NTT, 64], BF16); uV = U("Vh")
        OS = P.psb("OSh", [64, 4, NT], BF16); uOS = U("OSh")
        for hk in range(4):
            P.dma("sp", lambda e, hk=hk: e.dma_start(out=Q[:], in_=self.QTd[hk * 256:(hk + 1) * 256, :].rearrange("(g d) t -> d g t", d=64)),
                  reads=[self.u_QT], writes=[uQ])
            P.dma("sp", lambda e, hk=hk: e.dma_start(out=KT[:], in_=self.KTd[hk * 64:(hk + 1) * 64, :]),
                  reads=[self.u_KT], writes=[uK])
            P.dma("sp", lambda e, hk=hk: e.dma_start(out=V[:], in_=self.Vd[:, hk * 64:(hk + 1) * 64].rearrange("(t p) c -> p t c", p=128)),
                  reads=[self.u_V], writes=[uV])

            def sink_fn(psd, upsd, rd, urd, hk=hk):
                for g in range(4):
                    P.op("dve", lambda e, g=g: e.tensor_scalar(out=rd[:, g * 128:(g + 1) * 128], in0=psd[0:64, g * 128:(g + 1) * 128],
                                                                scalar1=esink[:, hk * 4 + g:hk * 4 + g + 1], scalar2=None, op0=ALU.add),
                         reads=[upsd, u_es], writes=[urd])
            tiles = list(range(2, NTT)) + ([0, 1] if do_ctx else [])
            for tile in tiles:
                def key(kt, bias):
                    return (KT[:, kt * 128:(kt + 1) * 128], uK, V[:, kt, :], uV, 128, bias, u_M)
                keys = [key(0, None), key(1, None)]
                if tile >= 2:
                    if tile > 2:
                        keys.append(key(tile - 1, Mp2))
                    keys.append(key(tile, None))
                    if tile < NTT - 1:
                        keys.append(key(tile + 1, Mn2))
                self.attn_block(a, Q[:, :, tile * 128:(tile + 1) * 128], uQ, 512, 64, keys,
                                OS[:, :, tile * 128:(tile + 1) * 128], uOS, sink_fn=sink_fn, qg=4)
            c0 = 0 if do_ctx else NCTX
            P.dma("pool", lambda e, hk=hk, c0=c0: e.dma_start(
                out=self.OTd[hk * 256:(hk + 1) * 256, c0:NT].rearrange("(g d) t -> d g t", d=64), in_=OS[:, :, c0:NT]),
                reads=[uOS], writes=[self.u_OT])
        P.phase_end()

    def na_attn(self, do_ctx):
        P = self.P
        I = self.I
        I("na_bias")
        P.phase_begin()
        a = self.attn_setup()
        QT = [P.psb("QTh", [64, NT], BF16) for _ in range(2)]; uQ = [U("QTh0"), U("QTh1")]
        KT = [P.psb("KTh", [64, NT], BF16) for _ in range(2)]; uK = [U("KTh0"), U("KTh1")]
        V = [P.psb("Vh", [128, NTT, 64], BF16) for _ in range(2)]; uV = [U("Vh0"), U("Vh1")]
        Vs = [P.psb("Vsh", [128, NTT - 1, 64], BF16) for _ in range(2)]
        B = [P.psb("Bh", [128, 14, 64], F32) for _ in range(2)]; uB = [U("Bh0"), U("Bh1")]
        OS = [P.psb("OSh", [64, NT], BF16) for _ in range(2)]; uOS = [U("OSh0"), U("OSh1")]
        for h in range(16):
            b = h % 2
            P.dma("sp", lambda e, b=b, h=h: e.dma_start(out=QT[b][:], in_=self.QTd[h * 64:(h + 1) * 64, :]),
                  reads=[self.u_QT], writes=[uQ[b]])
            P.dma("sp", lambda e, b=b, h=h: e.dma_start(out=KT[b][:], in_=self.KTd[h * 64:(h + 1) * 64, :]),
                  reads=[self.u_KT], writes=[uK[b]])
            P.dma("sp", lambda e, b=b, h=h: e.dma_start(
                out=V[b][:], in_=self.Vd[:, h * 64:(h + 1) * 64].rearrange("(t p) c -> p t c", p=128)),
                reads=[self.u_V], writes=[uV[b]])
            P.dma("sp", lambda e, b=b, h=h: e.dma_start(
                out=Vs[b][:], in_=self.Vd[64:64 + (NTT - 1) * 128, h * 64:(h + 1) * 64].rearrange("(t p) c -> p t c", p=128)),
                reads=[self.u_V], writes=[uV[b]])
            P.dma("sp", lambda e, b=b, h=h: e.dma_start(out=B[b][:], in_=I("na_bias")[h]), writes=[uB[b]])
            for r in range(64):
                q0 = NCTX + r * 64
                r0 = min(max(r - 4, 0), 56)
                keys = [(KT[b][:, 0:128], uK[b], V[b][:, 0, :], uV[b], 128, None, None),
                        (KT[b][:, 128:256], uK[b], V[b][:, 1, :], uV[b], 128, None, None)]
                for j in range(4):
                    krow = r0 + 2 * j
                    tok = NCTX + krow * 64
                    vv = V[b][:, tok // 128, :] if tok % 128 == 0 else Vs[b][:, (tok - 64) // 128, :]
                    keys.append((KT[b][:, tok:tok + 128], uK[b], vv, uV[b], 128, B[b][:, krow - r + 7, :], uB[b]))
                self.attn_block(a, QT[b][:, q0:q0 + 64], uQ[b], 64, 64, keys, OS[b][:, q0:q0 + 64], uOS[b])
            if do_ctx:
                keys = [(KT[b][:, 0:128], uK[b], V[b][:, 0, :], uV[b], 128, None, None),
                        (KT[b][:, 128:256], uK[b], V[b][:, 1, :], uV[b], 128, None, None)]
                self.attn_block(a, QT[b][:, 0:NCTX], uQ[b], NCTX, 64, keys, OS[b][:, 0:NCTX], uOS[b])
            c0 = 0 if do_ctx else NCTX
            P.dma("pool", lambda e, b=b, h=h, c0=c0: e.dma_start(out=self.OTd[h * 64:(h + 1) * 64, c0:NT], in_=OS[b][:, c0:NT]),
                  reads=[uOS[b]], writes=[self.u_OT])
        P.phase_end()

    def mixer(self, i, do_ctx):
        self.mix_scratch()
        m = i % 4
        self.norm_phase(i, 0, False)
        if m == 0:
            self.na_proj(i); self.na_attn(do_ctx); self.outproj_phase(i, "na_w_o", do_ctx)
        elif m == 1:
            self.rwkv(i, do_ctx)
        elif m == 2:
            self.mla_proj(i); self.mla_attn(do_ctx); self.outproj_phase(i, "mla_w_o", do_ctx)
        else:
            self.swa_proj(i); self.swa_attn(do_ctx); self.outproj_phase(i, "swa_w_o", do_ctx)


for _n, _f in list(vars(KMix).items()):
    if callable(_f):
        setattr(K, _n, _f)
C0 = -0.6065306597126334


class KRw:
    def rw_scratch(self):
        if hasattr(self, "rwd"):
            return
        P = self.P
        self.rwd = {}
        self.u_rwd = {}
        for nm in ("R", "K", "KK", "V", "G", "LW0", "LW1", "B0", "B1", "KT0", "KT1", "Y"):
            self.rwd[nm] = P.dram("rw_" + nm, [NT, D], F32)
            self.u_rwd[nm] = U("rw_" + nm)

    def rw_xs(self, tt, S, u_S, xx, u_xx, tmp, u_tmp, xs, u_xs, streams, mixT, u_mixT):
        P = self.P
        h = self.hT
        t0 = tt * 128
        noprev = tt in (0, 2)
        nonext = tt in (1, NTT - 1)
        a = 1 if noprev else 0
        b = 127 if nonext else 128
        uh = [self.u_hT[tt]]
        P.op("dve", lambda e: e.tensor_tensor(out=S[:, :, a:b], in0=h[:, :, t0 - 1 + a:t0 - 1 + b],
                                              in1=h[:, :, t0 + 1 + a:t0 + 1 + b], op=ALU.add), reads=uh, writes=[u_S])
        if noprev:
            P.op("dve", lambda e: e.tensor_copy(out=S[:, :, 0:1], in_=h[:, :, t0 + 1:t0 + 2]), reads=uh, writes=[u_S])
        if nonext:
            P.op("dve", lambda e: e.tensor_copy(out=S[:, :, 127:128], in_=h[:, :, t0 + 126:t0 + 127]), reads=uh, writes=[u_S])
        P.op("dve", lambda e: e.scalar_tensor_tensor(out=xx[:], in0=S[:], scalar=0.5, in1=h[:, :, t0:t0 + 128],
                                                     op0=ALU.mult, op1=ALU.subtract), reads=[u_S] + uh, writes=[u_xx])
        for n, s in enumerate(streams):
            tb = n % 2
            P.op("pool", lambda e, s=s, tb=tb: e.tensor_tensor(
                out=tmp[tb][:], in0=xx[:], in1=mixT[:, s * 8:(s + 1) * 8].unsqueeze(2).to_broadcast([128, 8, 128]),
                op=ALU.mult), reads=[u_xx, u_mixT], writes=[u_tmp[tb]])
            P.op("dve", lambda e, n=n, tb=tb: e.tensor_tensor(out=xs[n][:], in0=tmp[tb][:], in1=h[:, :, t0:t0 + 128], op=ALU.add),
                 reads=[u_tmp[tb]] + uh, writes=[u_xs[n]])

    def rw_common(self):
        P = self.P
        I = self.I
        m48 = P.psb("m48", [48, 128], F32); u_m48 = U("m48")
        mixT = P.psb("mixT", [128, 48], F32); u_mixT = U("mixT")
        P.dma("sp", lambda e: e.dma_start(out=m48[:], in_=I("rw_mix")[:, :]), writes=[u_m48])
        ps, ups = self.nps()
        self.tr(ps[:, 0:48], m48[:], self.ident_f[0:48, 0:48], [u_m48, self.u_const], [ups])
        P.op("dve", lambda e: e.tensor_copy(out=mixT[:], in_=ps[:, 0:48]), reads=[ups], writes=[u_mixT])
        S = P.psb("S", [128, 8, 128], F32); xx = P.psb("xx", [128, 8, 128], F32)
        tmp = [P.psb("xtmp", [128, 8, 128], F32) for _ in range(2)]
        xs = [P.psb("xsT", [128, 8, 128], BF16) for _ in range(3)]
        return dict(mixT=mixT, u_mixT=u_mixT, S=S, u_S=U("S"), xx=xx, u_xx=U("xx"), tmp=tmp, u_tmp=[U("xt0"), U("xt1")],
                    xs=xs, u_xs=[U("xs0"), U("xs1"), U("xs2")])

    def rw_out(self, ob, u_ob, n, name, tt):
        b = n % len(ob)
        self.P.dma("pool", lambda e: e.dma_start(out=self.rwd[name][tt * 128:(tt + 1) * 128, :], in_=ob[b][:]),
                   reads=[u_ob[b]], writes=[self.u_rwd[name]])

    def rw_proj_a(self):
        P = self.P
        I = self.I
        for nm in ("rw_mix", "rw_w_r", "rw_w_k", "rw_w_v", "rw_k_k"):
            I(nm)
        P.phase_begin()
        c = self.rw_common()
        W = {}
        for nm in ("rw_w_r", "rw_w_k", "rw_w_v"):
            W[nm] = self.wload(nm, I(nm), 8, D)
        kkb, u_kkb = self.bload("kkb", I("rw_k_k")[0:1, :], D)
        ob = [P.psb("ob", [128, D], F32) for _ in range(5)]; u_ob = [U("ob%d" % j) for j in range(5)]
        ss = P.psb("ss", [128, 16], F32); u_ss = U("ss")
        sq = P.psb("sq", [128, D], F32); u_sq = U("sq")
        n = 0
        for tt in range(NTT):
            self.rw_xs(tt, c["S"], c["u_S"], c["xx"], c["u_xx"], c["tmp"], c["u_tmp"], c["xs"], c["u_xs"], (0, 2, 3),
                       c["mixT"], c["u_mixT"])
            for si, (nm, dst) in enumerate((("rw_w_r", "R"), ("rw_w_k", "K"), ("rw_w_v", "V"))):
                w, uw = W[nm]
                b = n % 5
                for half in range(2):
                    ps, ups = self.nps()
                    for k in range(8):
                        self.mm(ps[:, :], c["xs"][si][:, k, :], w[:, k, half * 512:(half + 1) * 512], k == 0, k == 7,
                                [c["u_xs"][si], uw], [ups])
                    P.op("act", lambda e, ps=ps, b=b, half=half: e.copy(out=ob[b][:, half * 512:(half + 1) * 512], in_=ps[:, :]),
                         reads=[ups], writes=[u_ob[b]])
                self.rw_out(ob, u_ob, n, dst, tt)
                kb = b
                n += 1
                if dst == "K":
                    b2 = n % 5
                    n += 1
                    kks = ob[b2]
                    P.op("dve", lambda e, kb=kb, kks=kks: e.tensor_tensor(out=kks[:], in0=ob[kb][:], in1=kkb[:], op=ALU.mult),
                         reads=[u_ob[kb], u_kkb], writes=[u_ob[b2]])
                    P.op("dve", lambda e, kks=kks: e.tensor_tensor(out=sq[:], in0=kks[:], in1=kks[:], op=ALU.mult),
                         reads=[u_ob[b2]], writes=[u_sq])
                    P.op("dve", lambda e: e.tensor_reduce(out=ss[:], in_=sq[:].rearrange("p (h n) -> p h n", n=64), axis=AX.X,
                                                          op=ALU.add), reads=[u_sq], writes=[u_ss])
                    P.op("dve", lambda e: e.tensor_scalar(out=ss[:], in0=ss[:], scalar1=1e-24, scalar2=None, op0=ALU.max),
                         reads=[u_ss], writes=[u_ss])
                    P.op("act", lambda e: e.sqrt(out=ss[:], in_=ss[:]), reads=[u_ss], writes=[u_ss])
                    P.op("dve", lambda e: e.reciprocal(out=ss[:], in_=ss[:]), reads=[u_ss], writes=[u_ss])
                    P.op("dve", lambda e, kks=kks: e.tensor_tensor(
                        out=kks[:].rearrange("p (h n) -> p h n", n=64), in0=kks[:].rearrange("p (h n) -> p h n", n=64),
                        in1=ss[:].unsqueeze(2).to_broadcast([128, 16, 64]), op=ALU.mult), reads=[u_ob[b2], u_ss], writes=[u_ob[b2]])
                    self.rw_out(ob, u_ob, b2, "KK", tt)
        P.phase_end()

    def rw_proj_b(self):
        P = self.P
        I = self.I
        for nm in ("rw_mix", "rw_g1", "rw_g2", "rw_w0", "rw_w1", "rw_w2", "rw_a0", "rw_a1", "rw_a2", "rw_k_a"):
            I(nm)
        P.phase_begin()
        c = self.rw_common()
        g1w, u_g1 = self.wload("g1w", I("rw_g1"), 8, 128)
        g2w, u_g2 = self.wload("g2w", I("rw_g2"), 1, D)
        w1 = [self.wload("w1_%d" % z, I("rw_w1")[z], 8, 64) for z in range(2)]
        a1 = [self.wload("a1_%d" % z, I("rw_a1")[z], 8, 64) for z in range(2)]
        w2 = []; a2 = []
        for z in range(2):
            for (lst, nm) in ((w2, "rw_w2"), (a2, "rw_a2")):
                t = P.psb(nm, [64, D], BF16); u = U(nm)
                P.dma("pool", lambda e, t=t, nm=nm, z=z: e.dma_start(out=t[:], in_=I(nm)[z]), writes=[u])
                lst.append((t, u))
        def hilo(nm):
            f = P.psb(nm + "f", [33, 2 * D], F32); uf = U(nm + "f")
            hl = P.psb(nm + "hl", [33, 2 * D], BF16); uhl = U(nm + "hl")
            bk = P.psb(nm + "bk", [33, 2 * D], F32); ubk = U(nm + "bk")
            P.op("pool", lambda e: e.memset(f[:], 0.0), writes=[uf])
            src = I(nm).rearrange("(o z) n -> o (z n)", o=1)
            P.dma("sp", lambda e: e.dma_start(out=f[0:1, :], in_=src), reads=[uf], writes=[uf])
            P.dma("sp", lambda e: e.dma_start(out=f[32:33, :], in_=src), reads=[uf], writes=[uf])
            P.op("dve", lambda e: e.tensor_copy(out=hl[:], in_=f[:]), reads=[uf], writes=[uhl])
            P.op("dve", lambda e: e.tensor_copy(out=bk[:], in_=hl[:]), reads=[uhl], writes=[ubk])
            P.op("dve", lambda e: e.tensor_tensor(out=bk[:], in0=f[:], in1=bk[:], op=ALU.subtract), reads=[uf, ubk], writes=[ubk])
            P.op("dve", lambda e: e.tensor_copy(out=hl[32:33, :], in_=bk[32:33, :]), reads=[ubk, uhl], writes=[uhl])
            return hl, uhl
        w0hl, u_w0 = hilo("rw_w0")
        a0hl, u_a0 = hilo("rw_a0")
        kab, u_kab = self.bload("kab", I("rw_k_a")[0:1, :], D)
        c1b = P.psb("c1b", [128, D], F32); u_c1b = U("c1b")
        P.op("dve", lambda e: e.tensor_scalar(out=c1b[:], in0=kab[:], scalar1=-1.0, scalar2=1.0, op0=ALU.mult, op1=ALU.add),
             reads=[u_kab], writes=[u_c1b])
        ob = [P.psb("ob", [128, D], F32) for _ in range(4)]; u_ob = [U("ob%d" % j) for j in range(4)]
        kin = P.psb("kin", [128, D], F32); kkin = P.psb("kkin", [128, D], F32); u_kin = U("kin"); u_kkin = U("kkin")
        az = P.psb("az", [128, D], F32); u_az = U("az")
        lt = [P.psb("lt", [128, 128], BF16) for _ in range(2)]; u_lt = [U("lt0"), U("lt1")]
        n = 0
        nl = 0
        for tt in range(NTT):
            self.rw_xs(tt, c["S"], c["u_S"], c["xx"], c["u_xx"], c["tmp"], c["u_tmp"], c["xs"], c["u_xs"], (1, 4, 5),
                       c["mixT"], c["u_mixT"])
            xw, u_xw = c["xs"][0], c["u_xs"][0]
            xa, u_xa = c["xs"][1], c["u_xs"][1]
            xg, u_xg = c["xs"][2], c["u_xs"][2]
            P.dma("sp", lambda e, tt=tt: e.dma_start(out=kin[:], in_=self.rwd["K"][tt * 128:(tt + 1) * 128, :]),
                  reads=[self.u_rwd["K"]], writes=[u_kin])
            P.dma("sp", lambda e, tt=tt: e.dma_start(out=kkin[:], in_=self.rwd["KK"][tt * 128:(tt + 1) * 128, :]),
                  reads=[self.u_rwd["KK"]], writes=[u_kkin])

            def lora(x, u_x, w1t, u_w1, rows, func, w2t, u_w2, bias, u_bias, z, out_ap, u_out, fin):
                nonlocal nl
                psI, upsI = self.nps()
                for k in range(8):
                    self.mm(psI[0:rows, 0:128], w1t[:, k, :], x[:, k, :], k == 0, k == 7, [u_w1, u_x], [upsI])
                l = nl % 2
                nl += 1
                P.op("act", lambda e: e.activation(out=lt[l][0:rows, :], in_=psI[0:rows, 0:128], func=func),
                     reads=[upsI], writes=[u_lt[l]])
                for half in range(2):
                    ps, ups = self.nps()
                    self.mm(ps[:, :], lt[l][0:rows, :], w2t[0:rows, half * 512:(half + 1) * 512], True, bias is None,
                            [u_lt[l], u_w2], [ups])
                    if bias is not None:
                        self.mm(ps[:, :], self.ones_b[0:33, 0:128], bias[:, z * D + half * 512:z * D + (half + 1) * 512],
                                False, True, [u_bias, self.u_const], [ups])
                    P.op("act", lambda e, ps=ps, half=half: e.activation(out=out_ap[:, half * 512:(half + 1) * 512], in_=ps[:, :],
                                                                         func=fin), reads=[ups], writes=[u_out])
            b = n % 4; n += 1
            lora(xg, u_xg, g1w, u_g1, 128, AF.Sigmoid, g2w[:, 0, :], u_g2, None, None, 0, ob[b], u_ob[b], AF.Copy)
            self.rw_out(ob, u_ob, b, "G", tt)
            for z in range(2):
                b = n % 4; n += 1
                lora(xw, u_xw, w1[z][0], w1[z][1], 64, AF.Tanh, w2[z][0], w2[z][1], w0hl, u_w0, z, ob[b], u_ob[b], AF.Sigmoid)
                self.rw_out(ob, u_ob, b, "LW%d" % z, tt)
                lora(xa, u_xa, a1[z][0], a1[z][1], 64, AF.Copy, a2[z][0], a2[z][1], a0hl, u_a0, z, az, u_az, AF.Sigmoid)
                b = n % 4; n += 1
                P.op("dve", lambda e, b=b: e.tensor_tensor(out=ob[b][:], in0=az[:], in1=kab[:], op=ALU.mult),
                     reads=[u_az, u_kab], writes=[u_ob[b]])
                P.op("dve", lambda e, b=b: e.tensor_tensor(out=ob[b][:], in0=ob[b][:], in1=c1b[:], op=ALU.add),
                     reads=[u_ob[b], u_c1b], writes=[u_ob[b]])
                P.op("dve", lambda e, b=b: e.tensor_tensor(out=ob[b][:], in0=ob[b][:], in1=kin[:], op=ALU.mult),
                     reads=[u_ob[b], u_kin], writes=[u_ob[b]])
                self.rw_out(ob, u_ob, b, "KT%d" % z, tt)
                b = n % 4; n += 1
                P.op("dve", lambda e, b=b: e.tensor_tensor(out=ob[b][:], in0=az[:], in1=kkin[:], op=ALU.mult),
                     reads=[u_az, u_kkin], writes=[u_ob[b]])
                self.rw_out(ob, u_ob, b, "B%d" % z, tt)
        P.phase_end()

    def rw_scan(self, z, do_ctx):
        P = self.P
        I = self.I
        for nm in ("rw_r_k", "rw_ln_g", "rw_ln_b"):
            I(nm)
        P.phase_begin()
        fwd = z == 0
        tri = P.psb("tri", [128, 128], F32); mA = P.psb("mA", [128, 4, 128], F32); mN = P.psb("mN", [128, 128], F32)
        cvec = P.psb("cvec", [128, 2], F32); u_mk = U("masks")
        cm, pat = (-1, 1) if fwd else (1, -1)
        P.op("pool", lambda e: e.memset(tri[:], C0), writes=[u_mk])
        P.op("pool", lambda e: e.memset(mA[:], 1.0), writes=[u_mk])
        P.op("pool", lambda e: e.memset(mN[:], 1.0), writes=[u_mk])
        P.op("pool", lambda e: e.memset(cvec[:], C0), writes=[u_mk])
        P.op("pool", lambda e: e.affine_select(out=tri[:], in_=tri[:], pattern=[[pat, 128]], compare_op=ALU.is_ge, fill=0.0,
                                               base=0, channel_multiplier=cm), reads=[u_mk], writes=[u_mk])
        for q in range(4):
            P.op("pool", lambda e, q=q: e.affine_select(out=mA[:, q, :], in_=mA[:, q, :], pattern=[[pat, 128]],
                                                        compare_op=ALU.is_ge, fill=0.0, base=-(q % 2), channel_multiplier=cm),
                 reads=[u_mk], writes=[u_mk])
        P.op("pool", lambda e: e.affine_select(out=mN[:], in_=mN[:], pattern=[[-pat, 128]], compare_op=ALU.is_ge, fill=0.0,
                                               base=-1, channel_multiplier=-cm), reads=[u_mk], writes=[u_mk])
        Hst = P.psb("Hst", [64, 16, 64], F32); u_H = U("Hst")
        P.op("pool", lambda e: e.memset(Hst[:], 0.0), writes=[u_H])
        names = ("R", "KK", "V", "LW%d" % z, "B%d" % z, "KT%d" % z)
        tin = {nm: P.psb("in_" + nm, [128, D], F32) for nm in names}
        u_in = {nm: U("in_" + nm) for nm in names}
        r, kk, v, sg, bb, kt = (tin[nm] for nm in names)
        u_r, u_kk, u_v, u_sg, u_bb, u_kt = (u_in[nm] for nm in names)
        E = [P.psb("E", [128, D], F32) for _ in range(3)]; u_E = [U("E0"), U("E1"), U("E2")]
        F4 = [P.psb("F4", [128, D], F32) for _ in range(4)]; u_F4 = [U("F4_%d" % j) for j in range(4)]
        FT = P.psb("FT", [64, 8, 4, 128], F32); u_FT = [U("FT%d" % j) for j in range(8)]
        AA = P.psb("AA", [128, 8, 4, 128], F32); u_AA = [U("AA%d" % j) for j in range(8)]
        Nn = P.psb("Nn", [128, 8, 128], F32); u_Nn = [U("Nn0"), U("Nn1")]
        MB = [P.psb("MB", [128, 4, 128], F32) for _ in range(2)]; u_MB = [U("MB0"), U("MB1")]
        NB = [P.psb("NB", [128, 4, 128], F32) for _ in range(2)]; u_NB = [U("NB0"), U("NB1")]
        Pm = P.psb("Pm", [128, 8, 128], F32); u_Pm = [U("Pm0"), U("Pm1")]
        X = P.psb("X", [128, 512], F32); u_X = U("X")
        nU = P.psb("nU", [128, 512], F32); u_nU = U("nU")
        ysb = P.psb("ysb", [128, D], F32); u_y = U("ysb")
        gl = P.psb("gl", [64, 16], F32); u_gl = U("gl")
        if not fwd:
            rkb, u_rkb = self.bload("rkb", I("rw_r_k")[0:1, :], D)
            lng, u_lng = self.bload("lng", I("rw_ln_g")[0:1, :], D)
            lnb, u_lnb = self.bload("lnb", I("rw_ln_b")[0:1, :], D)
            st = P.psb("st", [128, 48], F32); u_st = U("st")
            obf = P.psb("obf", [128, D], BF16); u_obf = U("obf")
            stg = P.psb("stg", [128, 8, 128], BF16); u_stg = U("stg")
        order = list(range(NTT)) if fwd else [1, 0] + list(range(NTT - 1, 1, -1))
        cut = self.cfg.get("scan_cut", 99)
        if "scan_tiles" in self.cfg:
            order = order[:self.cfg["scan_tiles"]]
        v3 = lambda t: t[:].rearrange("p (h n) -> p h n", n=64)
        for tt in order:
            for nm in names:
                P.dma("sp", lambda e, nm=nm, tt=tt: e.dma_start(out=tin[nm][:], in_=self.rwd[nm][tt * 128:(tt + 1) * 128, :]),
                      reads=[self.u_rwd[nm]], writes=[u_in[nm]])
            if cut < -1:
                continue
            for half in range(2):
                hs = slice(half * 512, (half + 1) * 512)
                ps, ups = self.nps()
                self.mm(ps[:, :], tri[:], sg[:, hs], True, True, [u_mk, u_sg], [ups])
                P.op("dve", lambda e, ps=ps, hs=hs: e.tensor_copy(out=E[0][:, hs], in_=ps[:, :]), reads=[ups], writes=[u_E[0]])
                P.op("dve", lambda e, hs=hs: e.scalar_tensor_tensor(out=E[1][:, hs], in0=sg[:, hs], scalar=-C0, in1=E[0][:, hs],
                                                                    op0=ALU.mult, op1=ALU.add), reads=[u_E[0], u_sg], writes=[u_E[1]])
            P.op("act", lambda e: e.activation(out=E[2][:], in_=E[0][:], func=AF.Exp, scale=-1.0), reads=[u_E[0]], writes=[u_E[2]])
            P.op("act", lambda e: e.activation(out=E[0][:], in_=E[0][:], func=AF.Exp), reads=[u_E[0], u_E[2], u_E[1]], writes=[u_E[0]])
            P.op("act", lambda e: e.activation(out=E[1][:], in_=E[1][:], func=AF.Exp), reads=[u_E[1]], writes=[u_E[1]])
            if cut < 0:
                continue
            for j, (src, us, ex) in enumerate(((r, u_r, 0), (kk, u_kk, 1), (bb, u_bb, 2), (kt, u_kt, 2))):
                eng = "dve" if j % 2 == 0 else "pool"
                P.op(eng, lambda e, j=j, src=src, ex=ex: e.tensor_tensor(out=F4[j][:], in0=src[:], in1=E[ex][:], op=ALU.mult),
                     reads=[us, u_E[ex]], writes=[u_F4[j]])
            if cut < 1:
                continue
            psG, upsG = self.nps()
            for h in range(16):
                self.mm(psG[0:64, 2 * h:2 * h + 2], sg[:, h * 64:(h + 1) * 64], cvec[:, 0:2], True, True, [u_sg, u_mk], [upsG])
            P.op("dve", lambda e, psG=psG: e.tensor_copy(
                out=gl[:], in_=psG[0:64, 0:32].rearrange("p (h two) -> p h two", two=2)[:, :, 0]), reads=[upsG], writes=[u_gl])
            P.op("act", lambda e: e.activation(out=gl[:], in_=gl[:], func=AF.Exp), reads=[u_gl], writes=[u_gl])
            if cut < 2:
                continue
            for half in range(2):
                for hh in range(8):
                    h = half * 8 + hh
                    ps, ups = self.nps()
                    for j in range(4):
                        self.tr(ps[0:64, j * 128:(j + 1) * 128], F4[j][:, h * 64:(h + 1) * 64], self.ident_f[:],
                                [u_F4[j], self.u_const], [ups])
                    eng = "dve"
                    if eng == "act":
                        P.op("act", lambda e, ps=ps, hh=hh: e.copy(out=FT[:, hh].rearrange("p a t -> p (a t)"), in_=ps[0:64, :]),
                             reads=[ups], writes=[u_FT[hh]])
                    else:
                        P.op("dve", lambda e, ps=ps, hh=hh: e.tensor_copy(out=FT[:, hh].rearrange("p a t -> p (a t)"), in_=ps[0:64, :]),
                             reads=[ups], writes=[u_FT[hh]])
                if cut < 3:
                    continue
                for hh in range(8):
                    ps, ups = self.nps()
                    rhs2 = FT[:, hh, 0:2, :]
                    self.mm(ps[:, 0:256].rearrange("p (a t) -> p a t", a=2), FT[:, hh, 2, :], rhs2, True, True, [u_FT[hh]], [ups])
                    self.mm(ps[:, 256:512].rearrange("p (a t) -> p a t", a=2), FT[:, hh, 3, :], rhs2, True, True, [u_FT[hh]], [ups])
                    P.op("dve", lambda e, ps=ps, hh=hh: e.tensor_tensor(out=AA[:, hh], in0=ps[:, :].rearrange("p (a t) -> p a t", a=4),
                                                                        in1=mA[:], op=ALU.mult), reads=[ups, u_mk], writes=[u_AA[hh]])
                for g in range(2):
                    ps, ups = self.nps()
                    for j in range(4):
                        hh = g * 4 + j
                        self.mm(ps[:, j * 128:(j + 1) * 128], FT[:, hh, 1, :], FT[:, hh, 2, :], True, True, [u_FT[hh]], [ups])
                    P.op("dve", lambda e, ps=ps, g=g: e.tensor_tensor(
                        out=Nn[:, g * 4:(g + 1) * 4, :], in0=ps[:, :].rearrange("p (a t) -> p a t", a=4),
                        in1=mN[:].unsqueeze(1).to_broadcast([128, 4, 128]), op=ALU.mult), reads=[ups, u_mk], writes=[u_Nn[g]])
                if cut < 4:
                    continue
                for g in range(2):
                    grp = [g * 4 + j for j in range(4)]
                    P.op("dve", lambda e, g=g: e.tensor_tensor(
                        out=Pm[:, g * 4:(g + 1) * 4, :], in0=self.ident_f[:].unsqueeze(1).to_broadcast([128, 4, 128]),
                        in1=AA[:, g * 4:(g + 1) * 4, 1, :], op=ALU.subtract), reads=[u_AA[hh] for hh in grp] + [self.u_const],
                        writes=[u_Pm[g]])
                    Ms = [AA[:, hh, 1, :] for hh in grp]; uM = [u_AA[hh] for hh in grp]
                    Ns = [Nn[:, hh, :] for hh in grp]; uN = [u_Nn[g]]
                    for li in range(6):
                        lastl = li == 5
                        pp = li % 2
                        if not lastl:
                            bM, ubM = self.nps()
                            for j in range(4):
                                self.mm(bM[:, j * 128:(j + 1) * 128], Ns[j], Ms[j], True, True, uM + uN, [ubM])
                        bN, ubN = self.nps()
                        for j in range(4):
                            self.mm(bN[:, j * 128:(j + 1) * 128], Ms[j], Ns[j], True, True, uM + uN, [ubN])
                        if not lastl:
                            P.op("dve", lambda e, bM=bM, pp=pp: e.tensor_copy(out=MB[pp][:].rearrange("p a t -> p (a t)"), in_=bM[:, :]),
                                 reads=[ubM], writes=[u_MB[pp]])
                        P.op("dve", lambda e, bN=bN, pp=pp: e.tensor_copy(out=NB[pp][:].rearrange("p a t -> p (a t)"), in_=bN[:, :]),
                             reads=[ubN], writes=[u_NB[pp]])
                        Ms = [MB[pp][:, j, :] for j in range(4)]; uM = [u_MB[pp]]
                        Ns = [NB[pp][:, j, :] for j in range(4)]; uN = [u_NB[pp]]
                        bP, ubP = self.nps()
                        for j in range(4):
                            self.mm(bP[:, j * 128:(j + 1) * 128], Ns[j], Pm[:, grp[j], :], True, True, uN + [u_Pm[g]], [ubP])
                        P.op("dve", lambda e, bP=bP, g=g: e.tensor_tensor(
                            out=Pm[:, g * 4:(g + 1) * 4, :], in0=Pm[:, g * 4:(g + 1) * 4, :],
                            in1=bP[:, :].rearrange("p (a t) -> p a t", a=4), op=ALU.add), reads=[ubP, u_Pm[g]], writes=[u_Pm[g]])
                if cut < 5:
                    continue
                ps, ups = self.nps()
                for hh in range(8):
                    h = half * 8 + hh
                    self.mm(ps[:, hh * 64:(hh + 1) * 64], FT[:, hh, 1, :], Hst[:, h, :], True, False, [u_FT[hh], u_H], [ups])
                    self.mm(ps[:, hh * 64:(hh + 1) * 64], AA[:, hh, 3, :], v[:, h * 64:(h + 1) * 64], False, True, [u_AA[hh], u_v], [ups])
                P.op("dve", lambda e, ps=ps: e.tensor_copy(out=X[:], in_=ps[:, :]), reads=[ups], writes=[u_X])
                ps, ups = self.nps()
                for hh in range(8):
                    self.mm(ps[:, hh * 64:(hh + 1) * 64], Pm[:, hh, :], X[:, hh * 64:(hh + 1) * 64], True, True, [u_Pm[hh // 4], u_X], [ups])
                P.op("dve", lambda e, ps=ps: e.tensor_scalar(out=nU[:], in0=ps[:, :], scalar1=-1.0, scalar2=None, op0=ALU.mult),
                     reads=[ups], writes=[u_nU])
                if cut < 6:
                    continue
                ps, ups = self.nps()
                for hh in range(8):
                    h = half * 8 + hh
                    o = ps[:, hh * 64:(hh + 1) * 64]
                    self.mm(o, FT[:, hh, 0, :], Hst[:, h, :], True, False, [u_FT[hh], u_H], [ups])
                    self.mm(o, AA[:, hh, 2, :], v[:, h * 64:(h + 1) * 64], False, False, [u_AA[hh], u_v], [ups])
                    self.mm(o, AA[:, hh, 0, :], nU[:, hh * 64:(hh + 1) * 64], False, True, [u_AA[hh], u_nU], [ups])
                P.op("dve", lambda e, ps=ps, half=half: e.tensor_copy(out=ysb[:, half * 512:(half + 1) * 512], in_=ps[:, :]),
                     reads=[ups], writes=[u_y])
                if cut < 7:
                    continue
                ps, ups = self.nps()
                for hh in range(8):
                    h = half * 8 + hh
                    o = ps[0:64, hh * 64:(hh + 1) * 64]
                    hc = slice(h * 64, (h + 1) * 64)
                    self.mm(o, F4[3][:, hc], v[:, hc], True, False, [u_F4[3], u_v], [ups])
                    self.mm(o, F4[2][:, hc], nU[:, hh * 64:(hh + 1) * 64], False, False, [u_F4[2], u_nU], [ups])
                    self.mm(o, self.ident_f[0:64, 0:64], Hst[:, h, :], False, True, [u_H, self.u_const], [ups])
                P.op("dve", lambda e, ps=ps, half=half: e.tensor_tensor(
                    out=Hst[:, half * 8:(half + 1) * 8, :], in0=ps[0:64, :].rearrange("p (a t) -> p a t", a=8),
                    in1=gl[:, half * 8:(half + 1) * 8].unsqueeze(2).to_broadcast([64, 8, 64]), op=ALU.mult),
                    reads=[ups, u_gl], writes=[u_H])
            if cut < 8:
                continue
            if fwd:
                P.dma("pool", lambda e, tt=tt: e.dma_start(out=self.rwd["Y"][tt * 128:(tt + 1) * 128, :], in_=ysb[:]),
                      reads=[u_y], writes=[self.u_rwd["Y"]])
                continue
            if tt < 2 and not do_ctx:
                continue
            yf, u_yf = E[0], u_E[0]
            P.dma("sp", lambda e, tt=tt: e.dma_start(out=yf[:], in_=self.rwd["Y"][tt * 128:(tt + 1) * 128, :]),
                  reads=[self.u_rwd["Y"]], writes=[u_yf])
            kt0, u_kt0 = E[1], u_E[1]
            P.dma("sp", lambda e, tt=tt: e.dma_start(out=kt0[:], in_=self.rwd["KT0"][tt * 128:(tt + 1) * 128, :]),
                  reads=[self.u_rwd["KT0"]], writes=[u_kt0])
            gg, u_gg = E[2], u_E[2]
            P.dma("sp", lambda e, tt=tt: e.dma_start(out=gg[:], in_=self.rwd["G"][tt * 128:(tt + 1) * 128, :]),
                  reads=[self.u_rwd["G"]], writes=[u_gg])
            T0, T1, T2, T3 = F4
            uT0, uT1, uT2, uT3 = u_F4
            P.op("dve", lambda e: e.tensor_tensor(out=ysb[:], in0=ysb[:], in1=yf[:], op=ALU.add), reads=[u_y, u_yf], writes=[u_y])
            P.op("dve", lambda e: e.tensor_reduce(out=st[:, 0:16], in_=v3(ysb), axis=AX.X, op=ALU.add), reads=[u_y], writes=[u_st])
            P.op("dve", lambda e: e.tensor_scalar(out=st[:, 0:16], in0=st[:, 0:16], scalar1=1.0 / 64, scalar2=None, op0=ALU.mult),
                 reads=[u_st], writes=[u_st])
            P.op("dve", lambda e: e.tensor_tensor(out=v3(T0), in0=v3(ysb), in1=st[:, 0:16].unsqueeze(2).to_broadcast([128, 16, 64]),
                                                  op=ALU.subtract), reads=[u_y, u_st], writes=[uT0])
            P.op("dve", lambda e: e.tensor_tensor(out=T1[:], in0=T0[:], in1=T0[:], op=ALU.mult), reads=[uT0], writes=[uT1])
            P.op("dve", lambda e: e.tensor_reduce(out=st[:, 16:32], in_=v3(T1), axis=AX.X, op=ALU.add), reads=[uT1], writes=[u_st])
            P.op("dve", lambda e: e.tensor_scalar(out=st[:, 16:32], in0=st[:, 16:32], scalar1=1.0 / 64, scalar2=64e-5,
                                                  op0=ALU.mult, op1=ALU.add), reads=[u_st], writes=[u_st])
            P.op("act", lambda e: e.sqrt(out=st[:, 16:32], in_=st[:, 16:32]), reads=[u_st], writes=[u_st])
            P.op("dve", lambda e: e.reciprocal(out=st[:, 16:32], in_=st[:, 16:32]), reads=[u_st], writes=[u_st])
            P.op("dve", lambda e: e.tensor_tensor(out=v3(T0), in0=v3(T0), in1=st[:, 16:32].unsqueeze(2).to_broadcast([128, 16, 64]),
                                                  op=ALU.mult), reads=[uT0, u_st], writes=[uT0])
            P.op("dve", lambda e: e.tensor_tensor(out=T0[:], in0=T0[:], in1=lng[:], op=ALU.mult), reads=[uT0, u_lng], writes=[uT0])
            P.op("dve", lambda e: e.tensor_tensor(out=T0[:], in0=T0[:], in1=lnb[:], op=ALU.add), reads=[uT0, u_lnb], writes=[uT0])
            P.op("pool", lambda e: e.tensor_tensor(out=T1[:], in0=r[:], in1=rkb[:], op=ALU.mult), reads=[u_r, u_rkb], writes=[uT1])
            P.op("pool", lambda e: e.tensor_tensor(out=T2[:], in0=kt[:], in1=kt0[:], op=ALU.add), reads=[u_kt, u_kt0], writes=[uT2])
            P.op("dve", lambda e: e.tensor_tensor(out=T2[:], in0=T2[:], in1=T1[:], op=ALU.mult), reads=[uT1, uT2], writes=[uT2])
            P.op("dve", lambda e: e.tensor_reduce(out=st[:, 32:48], in_=v3(T2), axis=AX.X, op=ALU.add), reads=[uT2], writes=[u_st])
            P.op("dve", lambda e: e.tensor_tensor(out=v3(T3), in0=v3(v), in1=st[:, 32:48].unsqueeze(2).to_broadcast([128, 16, 64]),
                                                  op=ALU.mult), reads=[u_v, u_st], writes=[uT3])
            P.op("dve", lambda e: e.tensor_tensor(out=T0[:], in0=T0[:], in1=T3[:], op=ALU.add), reads=[uT0, uT3], writes=[uT0])
            P.op("dve", lambda e: e.tensor_tensor(out=obf[:], in0=T0[:], in1=gg[:], op=ALU.mult), reads=[uT0, u_gg], writes=[u_obf])
            self.tpose_to_dram([obf[:, j * 128:(j + 1) * 128] for j in range(8)], u_obf, 128,
                               self.OTd[0:1024, tt * 128:(tt + 1) * 128].rearrange("(j p) t -> p j t", p=128),
                               self.u_OT, stg, u_stg)
        P.phase_end()

    def rwkv(self, i, do_ctx):
        stop = self.cfg.get("rw_stop", 9)
        self.rw_scratch()
        self.rw_proj_a()
        if stop >= 2:
            self.rw_proj_b()
        if stop >= 3:
            self.rw_scan(0, do_ctx)
        if stop >= 4:
            self.rw_scan(1, do_ctx)
            self.outproj_phase(i, "rw_w_o", do_ctx)
        if self.cfg.get("rw_dump"):
            P = self.P
            for nm in self.cfg["rw_dump"]:
                o = P.dram("dump_" + nm, [NT, D], F32, kind="ExternalOutput")
                for j in range(2):
                    P.dma("sp", lambda e, o=o, nm=nm, j=j: e.dma_start(out=o[j * 2176:(j + 1) * 2176, :],
                                                                      in_=self.rwd[nm][j * 2176:(j + 1) * 2176, :]),
                          reads=[self.u_rwd[nm]], is_out=True)


for _n, _f in list(vars(KRw).items()):
    if callable(_f):
        setattr(K, _n, _f)
IN_SHAPES = None


def build_program(cfg):
    k = K(cfg)
    k.prologue()
    if cfg.get("only_scan") is not None:
        k.mix_scratch()
        k.rw_scratch()
        k.rw_scan(cfg["only_scan"], True)
        return k, k.epilogue()
    for i in cfg.get("layers", [0, 1, 2, 3]):
        do_ctx = i < 3 or cfg.get("force_ctx", False)
        if not cfg.get("skip_ada"):
            k.ada_phase(i)
        if not cfg.get("skip_mixer"):
            k.mixer(i, do_ctx)
        if not cfg.get("skip_moe"):
            tiles = None if do_ctx else list(range(2, NTT))
            k.norm_phase(i, 1, True, tiles)
            k.moe_phase(i, do_ctx)
    nc = k.epilogue()
    return k, nc


def rope_tables(d_rot):
    t = np.arange(NLAT)
    row = (t // 64).astype(np.float32)
    col = (t % 64).astype(np.float32)
    d_axis = d_rot // 2
    inv = (np.float32(10000.0) ** (-np.arange(0, d_axis, 2, dtype=np.float32) / np.float32(d_axis))).astype(np.float32)
    ang = np.concatenate([row[:, None] * inv, col[:, None] * inv], axis=-1).astype(np.float32)
    return np.cos(ang).astype(np.float32), np.sin(ang).astype(np.float32)


def na_bias_table(rpb):
    rpb = np.asarray(rpb, dtype=np.float32)
    kl = np.arange(128) // 64
    kc = np.arange(128) % 64
    c = np.arange(64)
    cstart = np.clip(c - 8, 0, 48)
    ok = (kc[:, None] >= cstart[None, :]) & (kc[:, None] < cstart[None, :] + 16)
    dcol = np.clip(kc[:, None] - c[None, :] + 15, 0, 30)
    out = np.empty((16, 128, 14, 64), np.float32)
    for di in range(14):
        dr = np.clip(di - 7 + kl + 7, 0, 14)
        g = rpb[:, dr[:, None], dcol]
        out[:, :, di, :] = np.where(ok[None], g, np.float32(-30000.0))
    return out


def make_in_maps(inputs, names):
    f = np.ascontiguousarray
    shared = {}
    for nm in names:
        if nm in ("x", "c", "ctx"):
            continue
        if nm in ("mla_cos", "mla_sin", "swa_cos", "swa_sin"):
            cs, sn = rope_tables(32 if nm.startswith("mla") else 64)
            shared[nm] = f(cs if nm.endswith("cos") else sn)
            continue
        if nm == "na_bias":
            shared[nm] = f(na_bias_table(inputs["na_rpb"][0]))
            continue
        a = np.asarray(inputs[nm], dtype=np.float32)
        if nm == "c_ctx":
            a = a.reshape(8, 128)
        elif nm in ("norm1_g", "norm2_g"):
            a = a.reshape(4, 8, 128)
        elif nm == "ada_b":
            a = a.reshape(4, 48, 128)
        elif nm.startswith(("na_", "rw_", "mla_", "swa_")):
            a = a[0]
            if nm == "rw_mix":
                a = a.reshape(48, 128)
            elif nm == "rw_r_k":
                a = a.reshape(1, -1)
            if a.ndim == 1:
                a = a.reshape(1, -1)
        shared[nm] = f(a)
    maps = []
    for b in range(8):
        m = dict(shared)
        m["x"] = f(np.asarray(inputs["x"][b], dtype=np.float32))
        m["c"] = f(np.asarray(inputs["c"][b], dtype=np.float32).reshape(8, 128))
        m["ctx"] = f(np.asarray(inputs["ctx"][b], dtype=np.float32))
        maps.append(m)
    return maps


def run(inputs, cfg):
    k, nc = build_program(cfg)
    maps = make_in_maps(inputs, list(k.inp.keys()))
    res = run_bass_kernel_spmd(nc, maps, core_ids=list(range(8)))
    return res


def kernel(**inputs):
    res = run(inputs, {})
    return np.stack([np.asarray(r["out"], dtype=np.float32) for r in res.results], axis=0)
```

```python
from concourse.bass_utils import run_bass_kernel_spmd
import contextlib
import numpy as np
import concourse.bass as bass
import concourse.mybir as mybir

F32 = mybir.dt.float32
BF16 = mybir.dt.bfloat16
I32 = mybir.dt.int32
U32 = mybir.dt.uint32
AF = mybir.ActivationFunctionType
ALU = mybir.AluOpType
AX = mybir.AxisListType

SEM_CAP = 30000
DMA_POOL = 12


class U:
    __slots__ = ("name", "w", "r")

    def __init__(self, name):
        self.name = name
        self.w = None
        self.r = {}


class Prog:
    def __init__(self):
        self.nc = bass.Bass("TRN2", target_bir_lowering=False)
        self.es = contextlib.ExitStack()
        self.ops = {e: [] for e in ("pe", "dve", "act", "pool", "sp")}
        self.cnt = {e: 0 for e in self.ops}
        self.ep = {e: 0 for e in self.ops}
        self.sems = {}
        self.waited = {e: {} for e in self.ops}
        self.dma_n = {e: 0 for e in self.ops}
        self.dma_val = {}
        self.n_inst = 0
        self.out_ticks = []

    def sb(self, name, shape, dt):
        return self.es.enter_context(self.nc.sbuf_tensor(name, list(shape), dt))

    def ps(self, name, shape, dt=F32):
        return self.es.enter_context(self.nc.psum_tensor(name, list(shape), dt))

    def dram(self, name, shape, dt, kind="Internal"):
        return self.nc.dram_tensor(name, list(shape), dt, kind=kind).ap()

    def _sem(self, key):
        if key not in self.sems:
            self.sems[key] = self.es.enter_context(self.nc.semaphore("s_%s_%s" % key))
        return self.sems[key]

    def _wait(self, eng, tick):
        if tick is None:
            return
        key, val = tick
        if self.waited[eng].get(key, 0) >= val:
            return
        self.waited[eng][key] = val
        sem = self._sem(key)
        self.ops[eng].append(lambda e, sem=sem, val=val: e.wait_ge(sem, val))

    def _deps(self, eng, reads, writes, skip_self=False):
        ticks = []
        for u in reads:
            if u.w is not None:
                ticks.append(u.w)
        for u in writes:
            if u.w is not None:
                ticks.append(u.w)
            for k, v in u.r.items():
                ticks.append((k, v))
        mykey = (eng, self.ep[eng])
        for t in ticks:
            if skip_self and t[0] == mykey:
                continue
            self._wait(eng, t)

    def _mark(self, tick, reads, writes):
        k, v = tick
        for u in reads:
            if u.r.get(k, 0) < v:
                u.r[k] = v
        for u in writes:
            u.w = tick
            u.r = {}

    def op(self, eng, fn, reads=(), writes=(), skip_self=False):
        reads = list(reads)
        writes = list(writes)
        self._deps(eng, reads, writes, skip_self=skip_self)
        if self.cnt[eng] >= SEM_CAP:
            self.ep[eng] += 1
            self.cnt[eng] = 0
        self.cnt[eng] += 1
        key = (eng, self.ep[eng])
        sem = self._sem(key)
        val = self.cnt[eng]
        self.ops[eng].append(lambda e, fn=fn, sem=sem: fn(e).then_inc(sem, 1))
        self._mark((key, val), reads, writes)
        self.n_inst += 1
        return (key, val)

    def dma(self, q, fn, reads=(), writes=(), is_out=False):
        reads = list(reads)
        writes = list(writes)
        self._deps(q, reads, writes)
        slot = self.dma_n[q] % DMA_POOL
        self.dma_n[q] += 1
        key = ("d" + q, slot)
        prev = self.dma_val.get(key, 0)
        if prev >= SEM_CAP:
            gen = 1
            while ("d%s_g%d" % (q, gen), slot) in self.dma_val and \
                    self.dma_val[("d%s_g%d" % (q, gen), slot)] >= SEM_CAP:
                gen += 1
            raise RuntimeError("dma semaphore cap reached")
        if prev:
            self._wait(q, (key, prev))
        val = prev + 16
        self.dma_val[key] = val
        sem = self._sem(key)
        self.ops[q].append(lambda e, fn=fn, sem=sem: fn(e).then_inc(sem, 16))
        self._mark((key, val), reads, writes)
        self.n_inst += 1
        if is_out:
            self.out_ticks.append((key, val))
        return (key, val)

    def barrier(self):
        ticks = []
        for e in self.ops:
            if self.cnt[e] > 0:
                ticks.append(((e, self.ep[e]), self.cnt[e]))
        for key, val in self.dma_val.items():
            ticks.append((key, val))
        for e in self.ops:
            for t in ticks:
                if t[0][0] == e:
                    continue
                self._wait(e, t)

    def phase_begin(self):
        self.pes = contextlib.ExitStack()

    def psb(self, name, shape, dt):
        self.uid = getattr(self, "uid", 0) + 1
        return self.pes.enter_context(self.nc.sbuf_tensor("%s_%d" % (name, self.uid), list(shape), dt))

    def phase_end(self):
        self.barrier()
        self.flush()
        self.pes.close()

    def flush(self):
        nc = self.nc
        with nc.Block() as block:
            @block.tensor
            def _(e):
                for f in self.ops["pe"]:
                    f(e)

            @block.vector
            def _(e):
                for f in self.ops["dve"]:
                    f(e)

            @block.scalar
            def _(e):
                for f in self.ops["act"]:
                    f(e)

            @block.gpsimd
            def _(e):
                for f in self.ops["pool"]:
                    f(e)

            @block.sync
            def _(e):
                for f in self.ops["sp"]:
                    f(e)
        for e in self.ops:
            self.ops[e] = []

    def finish(self):
        for t in self.out_ticks:
            self._wait("sp", t)
        self.flush()
        self.es.close()
        return self.nc
D = 1024
NCTX = 256
NLAT = 4096
NT = NCTX + NLAT
NTT = NT // 128
EPS = 1e-6


class K:
    def __init__(self, cfg):
        self.cfg = cfg
        self.P = P = Prog()
        self.inp = {}
        self.uin = {}
        self.build_io()
        self.setup_persistent()

    SHAPES = {
        "x": [NLAT, D], "c": [8, 128], "ctx": [NCTX, D], "c_ctx": [8, 128],
        "norm1_g": [4, 8, 128], "norm2_g": [4, 8, 128], "ada_w": [4, D, 6 * D], "ada_b": [4, 48, 128],
        "moe_router": [4, D, 16], "moe_w1": [4, 16, D, D], "moe_w3": [4, 16, D, D], "moe_w2": [4, 16, D, D],
        "na_w_qkv": [D, 3 * D], "na_q_g": [1, 64], "na_k_g": [1, 64], "na_bias": [16, 128, 14, 64], "na_w_o": [D, D],
        "mla_w_down": [D, 672], "mla_q_norm_g": [1, 384], "mla_kv_norm_g": [1, 256], "mla_w_uq": [384, 1536],
        "mla_w_ukv": [256, 2048], "mla_qn_g": [1, 64], "mla_qr_g": [1, 32], "mla_kn_g": [1, 64], "mla_kr_g": [1, 32],
        "mla_w_o": [D, D], "mla_cos": [NLAT, 16], "mla_sin": [NLAT, 16],
        "swa_w_qkv": [D, 1536], "swa_q_g": [1, 64], "swa_k_g": [1, 64], "swa_sink": [1, 16], "swa_w_o": [D, D],
        "swa_cos": [NLAT, 32], "swa_sin": [NLAT, 32],
        "rw_mix": [48, 128], "rw_w_r": [D, D], "rw_w_k": [D, D], "rw_w_v": [D, D], "rw_w0": [2, D], "rw_w1": [2, D, 64],
        "rw_w2": [2, 64, D], "rw_a0": [2, D], "rw_a1": [2, D, 64], "rw_a2": [2, 64, D], "rw_g1": [D, 128], "rw_g2": [128, D],
        "rw_k_k": [1, D], "rw_k_a": [1, D], "rw_r_k": [1, D], "rw_ln_g": [1, D], "rw_ln_b": [1, D], "rw_w_o": [D, D],
    }

    def build_io(self):
        P = self.P
        self.out = P.dram("out", [NLAT, D], F32, kind="ExternalOutput")
        self.xres = P.dram("xres", [NT, D], F32); self.u_xres = U("xres")
        self.xs2 = P.dram("xs2", [NT, D], BF16); self.u_xs2 = U("xs2")
        self.modd = P.dram("modd", [1, 4 * 96 * 128], F32); self.u_modd = U("modd")

    def I(self, name):
        if name not in self.inp:
            self.inp[name] = self.P.dram(name, self.SHAPES[name], F32, kind="ExternalInput")
        return self.inp[name]

    def setup_persistent(self):
        P = self.P
        self.ident_f = P.sb("ident_f", [128, 128], F32); self.u_const = U("const")
        self.ident_b = P.sb("ident_b", [128, 128], BF16)
        self.ones_f = P.sb("ones_f", [128, 128], F32)
        self.ones_b = P.sb("ones_b", [128, 128], BF16)
        self.scT = P.sb("scT", [128, 8, 2], F32); self.u_scT = U("scT")
        self.AB = P.sb("AB", [128, 16, 8, 2], F32); self.u_AB = U("AB")
        self.hT = P.sb("hT", [128, 8, NT], BF16); self.u_hT = [U("hT%d" % t) for t in range(NTT)]
        self.gateT = P.sb("gateT", [128, 5, 16], F32)
        self.idxT = P.sb("idxT", [128, 5, 16], I32)
        self.psf = [P.ps("psf%d" % i, [128, 512], F32) for i in range(6)]
        self.u_psf = [U("psf%d" % i) for i in range(6)]
        self.psb = [P.ps("psb%d" % i, [128, 1024], BF16) for i in range(2)]
        self.u_psb = [U("psb%d" % i) for i in range(2)]
        self.psf_n = 0
        self.psb_n = 0
        uc = self.u_const
        P.op("pool", lambda e: e.memset(self.ident_f[:], 0.0), writes=[uc])
        P.op("pool", lambda e: e.affine_select(out=self.ident_f[:], in_=self.ident_f[:], pattern=[[-1, 128]],
                                               compare_op=ALU.not_equal, fill=1.0, base=0, channel_multiplier=1),
             reads=[uc], writes=[uc])
        P.op("pool", lambda e: e.tensor_copy(out=self.ident_b[:], in_=self.ident_f[:]), reads=[uc], writes=[uc])
        P.op("pool", lambda e: e.memset(self.ones_f[:], 1.0), writes=[uc])
        P.op("pool", lambda e: e.memset(self.ones_b[:], 1.0), writes=[uc])

    def nps(self):
        i = self.psf_n % 4
        self.psf_n += 1
        return self.psf[i], self.u_psf[i]

    def npsb(self):
        i = self.psb_n % 2
        self.psb_n += 1
        return self.psb[i], self.u_psb[i]

    def mm(self, out, lhsT, rhs, start, stop, reads, writes):
        self.P.op("pe", lambda e: e.matmul(out=out, lhsT=lhsT, rhs=rhs, start=start, stop=stop),
                  reads=reads, writes=writes, skip_self=True)

    def tr(self, out, in_, ident, reads, writes):
        self.P.op("pe", lambda e: e.transpose(out=out, in_=in_, identity=ident),
                  reads=reads, writes=writes, skip_self=True)

    def prologue(self):
        P = self.P
        I = self.I
        for nm in ("x", "c", "ctx", "c_ctx"):
            I(nm)
        P.phase_begin()
        P.dma("sp", lambda e: e.dma_start(out=self.xres[0:NCTX, :], in_=I("ctx")[:, :]), writes=[self.u_xres])
        for j in range(8):
            P.dma("sp", lambda e, j=j: e.dma_start(out=self.xres[NCTX + j * 512:NCTX + (j + 1) * 512, :],
                                                    in_=I("x")[j * 512:(j + 1) * 512, :]), writes=[self.u_xres])
        c16 = P.psb("c16", [8, 256], F32); u_c16 = U("c16")
        P.dma("sp", lambda e: e.dma_start(out=c16[:, 0:128], in_=I("c")[:, :]), writes=[u_c16])
        P.dma("sp", lambda e: e.dma_start(out=c16[:, 128:256], in_=I("c_ctx")[:, :]), writes=[u_c16])
        ps, ups = self.nps()
        for s in range(2):
            self.tr(ps[:, s * 8:(s + 1) * 8], c16[:, s * 128:(s + 1) * 128], self.ident_f[0:8, 0:8],
                    [u_c16, self.u_const], [ups])
        for s in range(2):
            P.op("act", lambda e, s=s: e.activation(out=self.scT[:, :, s], in_=ps[:, s * 8:(s + 1) * 8], func=AF.Silu),
                 reads=[ups], writes=[self.u_scT])
        P.phase_end()

    def ada_phase(self, i):
        P = self.P
        I = self.I
        for nm in ("ada_w", "ada_b", "norm1_g", "norm2_g"):
            I(nm)
        P.phase_begin()
        wbuf = [P.psb("adaw", [128, 8, 512], F32) for _ in range(3)]
        uw = [U("adaw%d" % j) for j in range(3)]
        ab48 = P.psb("ab48", [48, 128], F32); g16 = P.psb("g16", [16, 128], F32); u_ld = U("ld")
        abT = P.psb("abT", [128, 48], F32); gT = P.psb("gT", [128, 16], F32); u_T = U("T")
        modT = P.psb("modT", [128, 48, 2], F32); u_modT = U("modT")
        modTT = P.psb("modTT", [96, 128], F32); u_modTT = U("modTT")
        P.dma("sp", lambda e: e.dma_start(out=ab48[:], in_=I("ada_b")[i]), writes=[u_ld])
        P.dma("sp", lambda e: e.dma_start(out=g16[0:8, :], in_=I("norm1_g")[i]), writes=[u_ld])
        P.dma("sp", lambda e: e.dma_start(out=g16[8:16, :], in_=I("norm2_g")[i]), writes=[u_ld])
        psA, upsA = self.nps()
        self.tr(psA[:, 0:48], ab48[:], self.ident_f[0:48, 0:48], [u_ld, self.u_const], [upsA])
        self.tr(psA[:, 64:80], g16[:], self.ident_f[0:16, 0:16], [u_ld, self.u_const], [upsA])
        P.op("dve", lambda e: e.tensor_copy(out=abT[:], in_=psA[:, 0:48]), reads=[upsA], writes=[u_T])
        P.op("dve", lambda e: e.tensor_copy(out=gT[:], in_=psA[:, 64:80]), reads=[upsA], writes=[u_T])
        psM, upsM = self.nps()
        wsrc = I("ada_w")[i].rearrange("(k p) n -> p k n", p=128)
        for piece in range(12):
            b = piece % 3
            P.dma("sp", lambda e, b=b, piece=piece: e.dma_start(out=wbuf[b][:], in_=wsrc[:, :, piece * 512:(piece + 1) * 512]),
                  writes=[uw[b]])
            for ml in range(4):
                m = piece * 4 + ml
                for k in range(8):
                    self.mm(psM[:, 2 * m:2 * m + 2], wbuf[b][:, k, ml * 128:(ml + 1) * 128], self.scT[:, k, :],
                            k == 0, k == 7, [uw[b], self.u_scT], [upsM])
        P.op("dve", lambda e: e.tensor_tensor(out=modT[:], in0=psM[:, 0:96].rearrange("p (m s) -> p m s", s=2),
                                              in1=abT[:].unsqueeze(2).to_broadcast([128, 48, 2]), op=ALU.add),
             reads=[upsM, u_T], writes=[u_modT])
        AB = self.AB
        for (slot, m0, g0) in ((0, 8, 0), (2, 32, 8)):
            P.op("dve", lambda e, slot=slot, m0=m0, g0=g0: e.scalar_tensor_tensor(
                out=AB[:, i * 4 + slot], in0=modT[:, m0:m0 + 8, :], scalar=1.0,
                in1=gT[:, g0:g0 + 8].unsqueeze(2).to_broadcast([128, 8, 2]), op0=ALU.add, op1=ALU.mult),
                reads=[u_modT, u_T], writes=[self.u_AB])
        for (slot, m0) in ((1, 0), (3, 24)):
            P.op("dve", lambda e, slot=slot, m0=m0: e.tensor_copy(out=AB[:, i * 4 + slot], in_=modT[:, m0:m0 + 8, :]),
                 reads=[u_modT], writes=[self.u_AB])
        psT, upsT = self.nps()
        self.tr(psT[0:96, 0:128], modT[:].rearrange("p m s -> p (m s)"), self.ident_f[:], [u_modT, self.u_const], [upsT])
        P.op("dve", lambda e: e.tensor_copy(out=modTT[:], in_=psT[0:96, 0:128]), reads=[upsT], writes=[u_modTT])
        P.dma("sp", lambda e: e.dma_start(out=self.modd[0, i * 12288:(i + 1) * 12288].rearrange("(r p) -> r p", p=128),
                                          in_=modTT[:]), reads=[u_modTT], writes=[self.u_modd])
        P.phase_end()

    def gate_bcast_src(self, i, which, s):
        m0 = 16 if which == 0 else 40
        base = i * 12288 + (m0 * 2 + s) * 128
        v = self.modd[0:1, base:base + 8 * 256].rearrange("o (j r) -> o j r", r=256)[:, :, 0:128]
        return v.partition_broadcast(128)[:, 0]

    def norm_phase(self, i, which, write_xs2, tiles=None):
        P = self.P
        tiles = list(range(NTT)) if tiles is None else tiles
        P.phase_begin()
        xt = [P.psb("xt", [128, D], F32) for _ in range(2)]; u_xt = [U("xt0"), U("xt1")]
        xs = [P.psb("xs", [128, D], F32) for _ in range(2)]; u_xs = [U("xs0"), U("xs1")]
        xsb = [P.psb("xsb", [128, D], BF16) for _ in range(2)]; u_xsb = [U("xsb0"), U("xsb1")]
        junk = P.psb("junk", [128, D], BF16)
        ss = P.psb("ss", [128, NTT], F32); u_ss = [U("ss%d" % t) for t in range(NTT)]
        rs = P.psb("rs", [128, NTT], F32); u_rs = [U("rs%d" % t) for t in range(NTT)]
        A = self.AB[:, i * 4 + 2 * which]
        B = self.AB[:, i * 4 + 2 * which + 1]
        for n, tt in enumerate(tiles):
            s = 1 if tt < 2 else 0
            b = n % 2
            P.dma("sp", lambda e, b=b, tt=tt: e.dma_start(out=xt[b][:], in_=self.xres[tt * 128:(tt + 1) * 128, :]),
                  reads=[self.u_xres], writes=[u_xt[b]])
            P.op("act", lambda e, b=b, tt=tt: e.activation(out=junk[:], in_=xt[b][:], func=AF.Square,
                                                           accum_out=ss[:, tt:tt + 1]),
                 reads=[u_xt[b]], writes=[u_ss[tt]])
            P.op("dve", lambda e, tt=tt: e.tensor_scalar(out=rs[:, tt:tt + 1], in0=ss[:, tt:tt + 1], scalar1=1.0 / D,
                                                         scalar2=EPS, op0=ALU.mult, op1=ALU.add),
                 reads=[u_ss[tt]], writes=[u_rs[tt]])
            P.op("act", lambda e, tt=tt: e.sqrt(out=rs[:, tt:tt + 1], in_=rs[:, tt:tt + 1]),
                 reads=[u_rs[tt]], writes=[u_rs[tt]])
            P.op("dve", lambda e, tt=tt: e.reciprocal(out=rs[:, tt:tt + 1], in_=rs[:, tt:tt + 1]),
                 reads=[u_rs[tt]], writes=[u_rs[tt]])
            P.op("dve", lambda e, b=b, tt=tt: e.tensor_scalar(out=xs[b][:], in0=xt[b][:], scalar1=rs[:, tt:tt + 1],
                                                              scalar2=None, op0=ALU.mult),
                 reads=[u_xt[b], u_rs[tt]], writes=[u_xs[b]])
            if write_xs2:
                P.op("pool", lambda e, b=b: e.tensor_copy(out=xsb[b][:], in_=xs[b][:]), reads=[u_xs[b]], writes=[u_xsb[b]])
                P.dma("pool", lambda e, b=b, tt=tt: e.dma_start(out=self.xs2[tt * 128:(tt + 1) * 128, :], in_=xsb[b][:]),
                      reads=[u_xsb[b]], writes=[self.u_xs2])
            for half in range(2):
                ps, ups = self.nps()
                for kk in range(4):
                    k = half * 4 + kk
                    self.tr(ps[:, kk * 128:(kk + 1) * 128], xs[b][:, k * 128:(k + 1) * 128], self.ident_f[:],
                            [u_xs[b], self.u_const], [ups])
                for kk in range(4):
                    k = half * 4 + kk
                    P.op("act", lambda e, k=k, kk=kk, tt=tt, s=s, ps=ps: e.activation(
                        out=self.hT[:, k, tt * 128:(tt + 1) * 128], in_=ps[:, kk * 128:(kk + 1) * 128],
                        func=AF.Identity, scale=A[:, k, s:s + 1], bias=B[:, k, s:s + 1]),
                        reads=[ups, self.u_AB], writes=[self.u_hT[tt]])
        P.phase_end()

    def moe_phase(self, i, do_ctx):
        P = self.P
        I = self.I
        for nm in ("moe_router", "moe_w1", "moe_w3", "moe_w2"):
            I(nm)
        NS = 544 if do_ctx else 512
        P.phase_begin()
        wr = P.psb("wr", [128, 8, 16], BF16); u_wr = U("wr")
        E = P.psb("E", [16, NT], F32); u_E = U("E")
        wk = P.psb("wk", [16, NT], F32); u_wkl = U("wkl"); u_wkc = U("wkc")
        mx = P.psb("mx", [16, 544], F32); u_mx = U("mx")
        ix = P.psb("ix", [16, 544], U32); u_ix = U("ix")
        ixf = P.psb("ixf", [16, 544], F32); u_ixf = U("ixf")
        rcp = P.psb("rcp", [16, 512], F32); u_rcp = U("rcp")
        P.dma("pool", lambda e: e.dma_start(out=wr[:], in_=I("moe_router")[i].rearrange("(k p) n -> p k n", p=128)),
              writes=[u_wr])
        chunks = [(c0, min(512, NT - c0)) for c0 in range(0, NT, 512)]
        for (c0, n) in chunks:
            ps, ups = self.nps()
            tts = list(range(c0 // 128, (c0 + n) // 128))
            for k in range(8):
                self.mm(ps[0:16, 0:n], wr[:, k, :], self.hT[:, k, c0:c0 + n], k == 0, k == 7,
                        [u_wr] + [self.u_hT[t] for t in tts], [ups])
            P.op("act", lambda e, ps=ps, c0=c0, n=n: e.activation(out=E[:, c0:c0 + n], in_=ps[0:16, 0:n], func=AF.Exp),
                 reads=[ups], writes=[u_E])
        for (c0, n) in chunks:
            ps, ups = self.nps()
            self.mm(ps[0:16, 0:n], self.ones_f[0:16, 0:16], E[:, c0:c0 + n], True, True, [u_E, self.u_const], [ups])
            P.op("dve", lambda e, ps=ps, n=n: e.reciprocal(out=rcp[:, 0:n], in_=ps[0:16, 0:n]),
                 reads=[ups], writes=[u_rcp])
            P.op("dve", lambda e, c0=c0, n=n: e.tensor_tensor(out=E[:, c0:c0 + n], in0=E[:, c0:c0 + n],
                                                              in1=rcp[:, 0:n], op=ALU.mult),
                 reads=[u_rcp, u_E], writes=[u_E])
        sets = [(NCTX, NLAT, 0, 64, u_wkl)]
        if do_ctx:
            sets.append((0, NCTX, 512, 4, u_wkc))
        for (t0, n, s0, iters, u_wk) in sets:
            src, us = E, u_E
            for it in range(iters):
                sl = slice(s0 + it * 8, s0 + it * 8 + 8)
                P.op("dve", lambda e, src=src, sl=sl, t0=t0, n=n: e.max(out=mx[:, sl], in_=src[:, t0:t0 + n]),
                     reads=[us], writes=[u_mx])
                P.op("dve", lambda e, src=src, sl=sl, t0=t0, n=n: e.max_index(out=ix[:, sl], in_max=mx[:, sl],
                                                                               in_values=src[:, t0:t0 + n]),
                     reads=[us, u_mx], writes=[u_ix])
                if it < iters - 1:
                    P.op("dve", lambda e, src=src, sl=sl, t0=t0, n=n: e.match_replace(
                        out=wk[:, t0:t0 + n], in_to_replace=mx[:, sl], in_values=src[:, t0:t0 + n], imm_value=0.0),
                        reads=[us, u_mx], writes=[u_wk])
                src, us = wk, u_wk
        P.op("dve", lambda e: e.tensor_copy(out=ixf[:, 0:NS], in_=ix[:, 0:NS]), reads=[u_ix], writes=[u_ixf])
        P.op("dve", lambda e: e.tensor_scalar(out=ixf[:, 0:512], in0=ixf[:, 0:512], scalar1=float(NCTX), scalar2=None,
                                              op0=ALU.add), reads=[u_ixf], writes=[u_ixf])
        u_gateT = U("gateT"); u_idxT = U("idxT")
        nch = 5 if do_ctx else 4
        for ch in range(nch):
            rows = 128 if ch < 4 else 32
            ps, ups = self.nps()
            self.tr(ps[0:rows, 0:16], mx[:, ch * 128:ch * 128 + rows], self.ident_f[0:16, 0:16], [u_mx, self.u_const], [ups])
            self.tr(ps[0:rows, 16:32], ixf[:, ch * 128:ch * 128 + rows], self.ident_f[0:16, 0:16], [u_ixf, self.u_const], [ups])
            P.op("dve", lambda e, ps=ps, ch=ch, rows=rows: e.tensor_copy(out=self.gateT[0:rows, ch, :], in_=ps[0:rows, 0:16]),
                 reads=[ups], writes=[u_gateT])
            P.op("dve", lambda e, ps=ps, ch=ch, rows=rows: e.tensor_copy(out=self.idxT[0:rows, ch, :], in_=ps[0:rows, 16:32]),
                 reads=[ups], writes=[u_idxT])
        P.phase_end()
        P.phase_begin()
        NWB = 4
        wb = [P.psb("wb", [128, 8, D], BF16) for _ in range(NWB)]; u_wb = [U("wb%d" % j) for j in range(NWB)]
        xin = [P.psb("xin", [128, D], BF16) for _ in range(3)]; u_xin = [U("xin%d" % j) for j in range(3)]
        xinT = [P.psb("xinT", [128, 8, 640], BF16) for _ in range(2)]; u_xinT = [U("xinT0"), U("xinT1")]
        hidT = P.psb("hidT", [128, 8, 640], BF16); u_hidT = U("hidT")
        sg = [P.psb("sg", [128, 512], F32) for _ in range(2)]; u_sg = [U("sg0"), U("sg1")]
        ysb = [P.psb("ysb", [128, D], F32) for _ in range(2)]; u_ysb = [U("ysb0"), U("ysb1")]
        G = P.psb("G", [128, 2, 8, 128], F32); u_G = U("G")
        for s in range(2 if do_ctx else 1):
            P.dma("sp", lambda e, s=s: e.dma_start(out=G[:, s], in_=self.gate_bcast_src(i, 1, s)),
                  reads=[self.u_modd], writes=[u_G])
        A2 = self.AB[:, i * 4 + 2]
        B2 = self.AB[:, i * 4 + 3]
        wn = 0
        gn = 0
        yn = 0
        segs = [(0, 512, 0)] + ([(512, 32, 1)] if do_ctx else [])
        for ex in range(16):
            wl = {}
            for nm in ("moe_w1", "moe_w3", "moe_w2"):
                b = wn % NWB
                wn += 1
                P.dma("pool", lambda e, nm=nm, b=b, ex=ex: e.dma_start(
                    out=wb[b][:], in_=I(nm)[i, ex].rearrange("(k p) n -> p k n", p=128)), writes=[u_wb[b]])
                wl[nm] = (wb[b], u_wb[b])
            xT, u_xT = xinT[ex % 2], u_xinT[ex % 2]
            for ch in range(nch):
                rows = 128 if ch < 4 else 32
                s = 0 if ch < 4 else 1
                t0, tn = (NCTX, NLAT) if ch < 4 else (0, NCTX)
                b = gn % 3
                gn += 1
                P.dma("pool", lambda e, b=b, ch=ch, rows=rows, t0=t0, tn=tn, ex=ex: e.indirect_dma_start(
                    out=xin[b][0:rows, :], out_offset=None, in_=self.xs2[:, :],
                    in_offset=bass.IndirectOffsetOnAxis(ap=self.idxT[0:rows, ch, ex:ex + 1], axis=0)),
                    reads=[u_idxT, self.u_xs2], writes=[u_xin[b]])
                pb, upb = self.npsb()
                for k in range(8):
                    self.tr(pb[:, k * 128:k * 128 + rows], xin[b][0:rows, k * 128:(k + 1) * 128],
                            self.ident_b[0:rows, 0:rows], [u_xin[b], self.u_const], [upb])
                for k in range(8):
                    P.op("act", lambda e, k=k, ch=ch, rows=rows, s=s, pb=pb, xT=xT: e.activation(
                        out=xT[:, k, ch * 128:ch * 128 + rows], in_=pb[:, k * 128:k * 128 + rows],
                        func=AF.Identity, scale=A2[:, k, s:s + 1], bias=B2[:, k, s:s + 1]),
                        reads=[upb, self.u_AB], writes=[u_xT])
            w1, u_w1 = wl["moe_w1"]
            w3, u_w3 = wl["moe_w3"]
            w2, u_w2 = wl["moe_w2"]
            for f in range(8):
                for (lo, n, s) in segs:
                    p1, up1 = self.nps()
                    p3, up3 = self.nps()
                    for k in range(8):
                        self.mm(p1[:, 0:n], w1[:, k, f * 128:(f + 1) * 128], xT[:, k, lo:lo + n], k == 0, k == 7,
                                [u_w1, u_xT], [up1])
                    for k in range(8):
                        self.mm(p3[:, 0:n], w3[:, k, f * 128:(f + 1) * 128], xT[:, k, lo:lo + n], k == 0, k == 7,
                                [u_w3, u_xT], [up3])
                    sb_ = (f * 2 + s) % 2
                    P.op("act", lambda e, p1=p1, n=n, sb_=sb_: e.activation(out=sg[sb_][:, 0:n], in_=p1[:, 0:n], func=AF.Silu),
                         reads=[up1], writes=[u_sg[sb_]])
                    P.op("dve", lambda e, p3=p3, n=n, sb_=sb_, f=f, lo=lo: e.tensor_tensor(
                        out=hidT[:, f, lo:lo + n], in0=sg[sb_][:, 0:n], in1=p3[:, 0:n], op=ALU.mult),
                        reads=[up3, u_sg[sb_]], writes=[u_hidT])
            for ch in range(nch):
                rows = 128 if ch < 4 else 32
                s = 0 if ch < 4 else 1
                t0, tn = (NCTX, NLAT) if ch < 4 else (0, NCTX)
                yb = yn % 2
                yn += 1
                for half in range(2):
                    py, upy = self.nps()
                    for k in range(8):
                        self.mm(py[0:rows, :], hidT[:, k, ch * 128:ch * 128 + rows], w2[:, k, half * 512:(half + 1) * 512],
                                k == 0, k == 7, [u_w2, u_hidT], [upy])
                    P.op("dve", lambda e, py=py, rows=rows, ch=ch, half=half, s=s, yb=yb, ex=ex: e.scalar_tensor_tensor(
                        out=ysb[yb][0:rows, half * 512:(half + 1) * 512], in0=py[0:rows, :],
                        scalar=self.gateT[0:rows, ch, ex:ex + 1],
                        in1=G[0:rows, s, half * 4:(half + 1) * 4, :].rearrange("p a b -> p (a b)"),
                        op0=ALU.mult, op1=ALU.mult), reads=[upy, u_gateT, u_G], writes=[u_ysb[yb]])
                P.dma("pool", lambda e, rows=rows, ch=ch, yb=yb, t0=t0, tn=tn, ex=ex: e.indirect_dma_start(
                    out=self.xres[:, :],
                    out_offset=bass.IndirectOffsetOnAxis(ap=self.idxT[0:rows, ch, ex:ex + 1], axis=0),
                    in_=ysb[yb][0:rows, :], in_offset=None, compute_op=ALU.add),
                    reads=[u_ysb[yb], u_idxT], writes=[self.u_xres])
        P.phase_end()

    def epilogue(self):
        P = self.P
        for j in range(8):
            P.dma("sp", lambda e, j=j: e.dma_start(out=self.out[j * 512:(j + 1) * 512, :],
                                                    in_=self.xres[NCTX + j * 512:NCTX + (j + 1) * 512, :]),
                  reads=[self.u_xres], is_out=True)
        if self.cfg.get("dump_ctx"):
            oc = P.dram("out_ctx", [NCTX, D], F32, kind="ExternalOutput")
            P.dma("sp", lambda e: e.dma_start(out=oc[:, :], in_=self.xres[0:NCTX, :]), reads=[self.u_xres], is_out=True)
        return P.finish()
NEG = -30000.0


class KMix:
    def mix_scratch(self):
        if hasattr(self, "QTd"):
            return
        P = self.P
        self.QTd = P.dram("QTd", [1536, NT], BF16); self.u_QT = U("QTd")
        self.KTd = P.dram("KTd", [1024, NT], BF16); self.u_KT = U("KTd")
        self.KRd = P.dram("KRd", [32, NT], BF16); self.u_KR = U("KRd")
        self.Vd = P.dram("Vd", [NT, 1024], BF16); self.u_V = U("Vd")
        self.OTd = P.dram("OTd", [1024, NT], BF16); self.u_OT = U("OTd")

    def wload(self, name, src, kch, ncols):
        P = self.P
        t = P.psb(name, [128, kch, ncols], BF16); u = U(name)
        P.dma("pool", lambda e: e.dma_start(out=t[:], in_=src.rearrange("(k p) n -> p k n", p=128)), writes=[u])
        return t, u

    def bload(self, name, src_row, n, scale=None, parts=128):
        P = self.P
        t = P.psb(name, [parts, n], F32); u = U(name)
        P.dma("sp", lambda e: e.dma_start(out=t[:], in_=src_row.partition_broadcast(parts)[:, 0]), writes=[u])
        if scale is not None:
            P.op("dve", lambda e: e.tensor_scalar(out=t[:], in0=t[:], scalar1=float(scale), scalar2=None, op0=ALU.mult),
                 reads=[u], writes=[u])
        return t, u

    def norm_scratch(self):
        P = self.P
        sc = {"sq": P.psb("nsq", [128, 1536], F32), "u_sq": U("nsq"),
              "ss": P.psb("nss", [128, 16], F32), "u_ss": U("nss"),
              "r": [P.psb("rp", [128, 512], F32) for _ in range(4)], "u_r": [U("rp%d" % j) for j in range(4)]}
        return sc

    def headnorm(self, src, us, H, n, gb, ugb, out, uo, sc):
        P = self.P
        sq = sc["sq"][:, 0:H * n].rearrange("p (h n) -> p h n", n=n)
        ss = sc["ss"][:, 0:H]
        u_sq, u_ss = sc["u_sq"], sc["u_ss"]
        P.op("dve", lambda e: e.tensor_tensor(out=sq, in0=src, in1=src, op=ALU.mult), reads=[us], writes=[u_sq])
        P.op("dve", lambda e: e.tensor_reduce(out=ss, in_=sq, axis=AX.X, op=ALU.add), reads=[u_sq], writes=[u_ss])
        P.op("dve", lambda e: e.tensor_scalar(out=ss, in0=ss, scalar1=1.0 / n, scalar2=EPS, op0=ALU.mult, op1=ALU.add),
             reads=[u_ss], writes=[u_ss])
        P.op("act", lambda e: e.sqrt(out=ss, in_=ss), reads=[u_ss], writes=[u_ss])
        P.op("dve", lambda e: e.reciprocal(out=ss, in_=ss), reads=[u_ss], writes=[u_ss])
        P.op("dve", lambda e: e.tensor_tensor(out=sq, in0=src, in1=ss.unsqueeze(2).to_broadcast([128, H, n]), op=ALU.mult),
             reads=[us, u_ss], writes=[u_sq])
        P.op("dve", lambda e: e.tensor_tensor(out=out, in0=sq, in1=gb[:, 0:n].unsqueeze(1).to_broadcast([128, H, n]),
                                              op=ALU.mult), reads=[u_sq, ugb], writes=[uo])

    def rope(self, x, ux, H, half, cs, sn, ucs, sc):
        P = self.P
        x1 = x[:, :, 0:half]
        x2 = x[:, :, half:2 * half]
        cb = cs.unsqueeze(1).to_broadcast([128, H, half])
        sb = sn.unsqueeze(1).to_broadcast([128, H, half])
        t = [sc["r"][j][:, 0:H * half].rearrange("p (h n) -> p h n", n=half) for j in range(4)]
        ut = sc["u_r"]
        for j, (a, b) in enumerate(((x1, cb), (x2, sb), (x2, cb), (x1, sb))):
            P.op("dve", lambda e, j=j, a=a, b=b: e.tensor_tensor(out=t[j], in0=a, in1=b, op=ALU.mult),
                 reads=[ux, ucs], writes=[ut[j]])
        P.op("dve", lambda e: e.tensor_tensor(out=x1, in0=t[0], in1=t[1], op=ALU.subtract), reads=[ut[0], ut[1]], writes=[ux])
        P.op("dve", lambda e: e.tensor_tensor(out=x2, in0=t[2], in1=t[3], op=ALU.add), reads=[ut[2], ut[3]], writes=[ux])

    def tpose_to_dram(self, blocks, ub, rows, dst, udst, stage, ustage):
        P = self.P
        pb, upb = self.npsb()
        nb = len(blocks)
        for j, blk in enumerate(blocks):
            self.tr(pb[0:rows, j * 128:(j + 1) * 128], blk, self.ident_b[:], [ub, self.u_const], [upb])
        P.op("act", lambda e: e.copy(out=stage[0:rows, 0:nb, :], in_=pb[0:rows, 0:nb * 128].rearrange("p (j t) -> p j t", t=128)),
             reads=[upb], writes=[ustage])
        P.dma("sp", lambda e: e.dma_start(out=dst, in_=stage[0:rows, 0:nb, :]), reads=[ustage], writes=[udst])

    def attn_setup(self):
        P = self.P
        a = {"pt": [P.psb("pt", [128, 512], BF16) for _ in range(4)], "u_pt": [U("pt%d" % j) for j in range(4)], "nblk": 0,
             "tmp": [P.psb("tmpb", [128, 512], F32) for _ in range(2)], "u_tmp": [U("tmp0"), U("tmp1")],
             "rd": P.psb("rd", [64, 512], F32), "u_rd": U("rd"), "n": 0}
        return a

    def attn_block(self, a, rhs_q, uq, nq, dk, keys, out_ap, uout, sink_fn=None, qg=1):
        P = self.P
        LA = 2
        pso, upso = self.psf[4][0:64, 0:nq], self.u_psf[4]
        psd, upsd = self.psf[5][0:64, 0:nq], self.u_psf[5]
        n_k = len(keys)
        last = n_k - 1
        pend = {}
        for j in range(n_k + LA):
            if j < n_k:
                (kt, uk, v, uv, nk, bias, ubias) = keys[j]
                ps, ups = self.nps()
                so = ps[0:nk, 0:nq] if qg == 1 else ps[0:nk, 0:nq].rearrange("p (g t) -> p g t", g=qg)
                self.mm(so, kt, rhs_q, True, True, [uk, uq], [ups])
                n = a["n"]; a["n"] += 1
                pt, upt = a["pt"][n % 4], a["u_pt"][n % 4]
                if bias is not None:
                    tmp, utmp = a["tmp"][n % 2], a["u_tmp"][n % 2]
                    P.op("dve", lambda e, ps=ps, nk=nk, tmp=tmp, bias=bias: e.tensor_tensor(
                        out=tmp[0:nk, 0:nq], in0=ps[0:nk, 0:nq], in1=bias, op=ALU.add), reads=[ups, ubias], writes=[utmp])
                    P.op("act", lambda e, nk=nk, tmp=tmp, pt=pt: e.activation(out=pt[0:nk, 0:nq], in_=tmp[0:nk, 0:nq], func=AF.Exp),
                         reads=[utmp], writes=[upt])
                else:
                    P.op("act", lambda e, ps=ps, nk=nk, pt=pt: e.activation(out=pt[0:nk, 0:nq], in_=ps[0:nk, 0:nq], func=AF.Exp),
                         reads=[ups], writes=[upt])
                pend[j] = (pt, upt)
            jj = j - LA
            if jj >= 0:
                (kt, uk, v, uv, nk, bias, ubias) = keys[jj]
                pt, upt = pend.pop(jj)
                self.mm(pso, v, pt[0:nk, 0:nq], jj == 0, jj == last, [uv, upt], [upso])
                self.mm(psd, self.ones_b[0:nk, 0:64], pt[0:nk, 0:nq], jj == 0, jj == last, [upt, self.u_const], [upsd])
        rd, urd = a["rd"], a["u_rd"]
        if sink_fn is not None:
            sink_fn(psd, upsd, rd, urd)
        else:
            P.op("dve", lambda e: e.tensor_copy(out=rd[:, 0:nq], in_=psd), reads=[upsd], writes=[urd])
        P.op("dve", lambda e: e.reciprocal(out=rd[:, 0:nq], in_=rd[:, 0:nq]), reads=[urd], writes=[urd])
        if qg == 1:
            o_in, r_in = pso, rd[:, 0:nq]
        else:
            o_in = pso.rearrange("p (g t) -> p g t", g=qg)
            r_in = rd[:, 0:nq].rearrange("p (g t) -> p g t", g=qg)
        P.op("dve", lambda e: e.tensor_tensor(out=out_ap, in0=o_in, in1=r_in, op=ALU.mult),
             reads=[upso, urd], writes=[uout])

    def outproj_phase(self, i, wname, do_ctx):
        P = self.P
        wsrc = self.I(wname)
        P.phase_begin()
        wo, uwo = self.wload("wo", wsrc, 8, D)
        for k in range(8):
            P.dma("sp", lambda e, k=k: e.dma_start(out=self.hT[:, k, :], in_=self.OTd[k * 128:(k + 1) * 128, :]),
                  reads=[self.u_OT], writes=self.u_hT)
        G = P.psb("G1", [128, 2, 8, 128], F32); u_G = U("G1")
        for s in range(2):
            P.dma("sp", lambda e, s=s: e.dma_start(out=G[:, s], in_=self.gate_bcast_src(i, 0, s)),
                  reads=[self.u_modd], writes=[u_G])
        xt = [P.psb("xto", [128, D], F32) for _ in range(2)]; u_xt = [U("xto0"), U("xto1")]
        tmp = [P.psb("tmpo", [128, 512], F32) for _ in range(2)]; u_tmp = [U("tmpo0"), U("tmpo1")]
        tiles = list(range(NTT)) if do_ctx else list(range(2, NTT))
        for n, tt in enumerate(tiles):
            s = 1 if tt < 2 else 0
            b = n % 2
            P.dma("sp", lambda e, b=b, tt=tt: e.dma_start(out=xt[b][:], in_=self.xres[tt * 128:(tt + 1) * 128, :]),
                  reads=[self.u_xres], writes=[u_xt[b]])
            for half in range(2):
                ps, ups = self.nps()
                for k in range(8):
                    self.mm(ps[:, :], self.hT[:, k, tt * 128:(tt + 1) * 128], wo[:, k, half * 512:(half + 1) * 512],
                            k == 0, k == 7, [self.u_hT[tt], uwo], [ups])
                P.op("dve", lambda e, ps=ps, half=half, s=s: e.tensor_tensor(
                    out=tmp[half][:], in0=ps[:, :], in1=G[:, s, half * 4:(half + 1) * 4, :].rearrange("p a b -> p (a b)"),
                    op=ALU.mult), reads=[ups, u_G], writes=[u_tmp[half]])
                P.op("dve", lambda e, half=half, b=b: e.tensor_tensor(
                    out=xt[b][:, half * 512:(half + 1) * 512], in0=xt[b][:, half * 512:(half + 1) * 512],
                    in1=tmp[half][:], op=ALU.add), reads=[u_tmp[half], u_xt[b]], writes=[u_xt[b]])
            P.dma("pool", lambda e, b=b, tt=tt: e.dma_start(out=self.xres[tt * 128:(tt + 1) * 128, :], in_=xt[b][:]),
                  reads=[u_xt[b]], writes=[self.u_xres])
        P.phase_end()

    def mla_proj(self, i):
        P = self.P
        I = self.I
        for nm in ("mla_w_down", "mla_q_norm_g", "mla_kv_norm_g", "mla_w_uq", "mla_w_ukv", "mla_qn_g", "mla_qr_g",
                   "mla_kn_g", "mla_kr_g", "mla_cos", "mla_sin"):
            I(nm)
        scale = 96.0 ** -0.5
        P.phase_begin()
        wd, uwd = self.wload("wd", I("mla_w_down"), 8, 672)
        wuq, uwuq = self.wload("wuq", I("mla_w_uq"), 3, 1536)
        wukv, uwukv = self.wload("wukv", I("mla_w_ukv"), 2, 2048)
        gq, ugq = self.bload("gq", I("mla_q_norm_g")[0:1, :], 384)
        gkv, ugkv = self.bload("gkv", I("mla_kv_norm_g")[0:1, :], 256)
        gkr, ugkr = self.bload("gkr", I("mla_kr_g")[0:1, :], 32)
        gqn, ugqn = self.bload("gqn", I("mla_qn_g")[0:1, :], 64, scale=scale)
        gqr, ugqr = self.bload("gqr", I("mla_qr_g")[0:1, :], 32, scale=scale)
        gkn, ugkn = self.bload("gkn", I("mla_kn_g")[0:1, :], 64)
        sc = self.norm_scratch()
        cqT = P.psb("cqT", [128, 3, NT], BF16); u_cqT = [U("cqT%d" % t) for t in range(NTT)]
        ckvT = P.psb("ckvT", [128, 2, NT], BF16); u_ckvT = [U("ckvT%d" % t) for t in range(NTT)]
        df = P.psb("df", [128, 672], F32); u_df = U("df")
        dn = P.psb("dn", [128, 672], F32); u_dn = U("dn")
        db = P.psb("db", [128, 768], BF16); u_db = U("db")
        cs = [P.psb("cs", [128, 16], F32) for _ in range(2)]; sn = [P.psb("sn", [128, 16], F32) for _ in range(2)]
        u_cs = [U("cs0"), U("cs1")]
        krs = P.psb("krs", [32, 1, 128], BF16); u_krs = U("krs")
        P.op("pool", lambda e: e.memset(db[:], 0.0), writes=[u_db])
        for tt in range(NTT):
            lat = tt >= 2
            ps0, up0 = self.nps()
            ps1, up1 = self.nps()
            for k in range(8):
                self.mm(ps0[:, 0:512], self.hT[:, k, tt * 128:(tt + 1) * 128], wd[:, k, 0:512], k == 0, k == 7,
                        [self.u_hT[tt], uwd], [up0])
            for k in range(8):
                self.mm(ps1[:, 0:160], self.hT[:, k, tt * 128:(tt + 1) * 128], wd[:, k, 512:672], k == 0, k == 7,
                        [self.u_hT[tt], uwd], [up1])
            P.op("act", lambda e, ps0=ps0: e.copy(out=df[:, 0:512], in_=ps0[:, 0:512]), reads=[up0], writes=[u_df])
            P.op("act", lambda e, ps1=ps1: e.copy(out=df[:, 512:672], in_=ps1[:, 0:160]), reads=[up1], writes=[u_df])
            for (c0, n, g, ug) in ((0, 384, gq, ugq), (384, 256, gkv, ugkv), (640, 32, gkr, ugkr)):
                self.headnorm(df[:, c0:c0 + n].unsqueeze(1), u_df, 1, n, g, ug, dn[:, c0:c0 + n].unsqueeze(1), u_dn, sc)
            if lat:
                b = tt % 2
                t0 = (tt - 2) * 128
                P.dma("sp", lambda e, b=b, t0=t0: e.dma_start(out=cs[b][:], in_=I("mla_cos")[t0:t0 + 128, :]), writes=[u_cs[b]])
                P.dma("sp", lambda e, b=b, t0=t0: e.dma_start(out=sn[b][:], in_=I("mla_sin")[t0:t0 + 128, :]), writes=[u_cs[b]])
                self.rope(dn[:, 640:672].unsqueeze(1), u_dn, 1, 16, cs[b][:], sn[b][:], u_cs[b], sc)
            P.op("act", lambda e: e.copy(out=db[:, 0:672], in_=dn[:, 0:672]), reads=[u_dn], writes=[u_db])
            pb, upb = self.npsb()
            for j in range(5):
                self.tr(pb[:, j * 128:(j + 1) * 128], db[:, j * 128:(j + 1) * 128], self.ident_b[:], [u_db, self.u_const], [upb])
            self.tr(pb[:, 640:768], db[:, 640:768], self.ident_b[:], [u_db, self.u_const], [upb])
            P.op("act", lambda e, pb=pb, tt=tt: e.copy(out=cqT[:, :, tt * 128:(tt + 1) * 128],
                                                       in_=pb[:, 0:384].rearrange("p (j t) -> p j t", t=128)),
                 reads=[upb], writes=[u_cqT[tt]])
            P.op("act", lambda e, pb=pb, tt=tt: e.copy(out=ckvT[:, :, tt * 128:(tt + 1) * 128],
                                                       in_=pb[:, 384:640].rearrange("p (j t) -> p j t", t=128)),
                 reads=[upb], writes=[u_ckvT[tt]])
            P.op("act", lambda e, pb=pb: e.copy(out=krs[:, 0, :], in_=pb[0:32, 640:768]), reads=[upb], writes=[u_krs])
            P.dma("sp", lambda e, tt=tt: e.dma_start(out=self.KRd[:, tt * 128:(tt + 1) * 128], in_=krs[:, 0, :]),
                  reads=[u_krs], writes=[self.u_KR])
        qf = P.psb("qf", [128, 16, 96], F32); u_qf = U("qf")
        qn = P.psb("qn", [128, 16, 96], F32); u_qn = U("qn")
        qb = P.psb("qb", [128, 16, 96], BF16); u_qb = U("qb")
        kvf = P.psb("kvf", [128, 16, 128], F32); u_kvf = U("kvf")
        knf = P.psb("knf", [128, 16, 64], F32); u_knf = U("knf")
        knb = P.psb("knb", [128, 16, 64], BF16); u_knb = U("knb")
        vb = P.psb("vb", [128, 16, 64], BF16); u_vb = U("vb")
        stq = [P.psb("stq", [96, 8, 128], BF16) for _ in range(2)]; u_stq = [U("stq0"), U("stq1")]
        stk = P.psb("stk", [128, 8, 128], BF16); u_stk = U("stk")
        qflat = qf[:].rearrange("p h n -> p (h n)")
        kvflat = kvf[:].rearrange("p h n -> p (h n)")
        for tt in range(NTT):
            lat = tt >= 2
            for cc in range(3):
                ps, ups = self.nps()
                for k in range(3):
                    self.mm(ps[:, :], cqT[:, k, tt * 128:(tt + 1) * 128], wuq[:, k, cc * 512:(cc + 1) * 512], k == 0, k == 2,
                            [u_cqT[tt], uwuq], [ups])
                P.op("act", lambda e, ps=ps, cc=cc: e.copy(out=qflat[:, cc * 512:(cc + 1) * 512], in_=ps[:, :]),
                     reads=[ups], writes=[u_qf])
            self.headnorm(qf[:, :, 0:64], u_qf, 16, 64, gqn, ugqn, qn[:, :, 0:64], u_qn, sc)
            self.headnorm(qf[:, :, 64:96], u_qf, 16, 32, gqr, ugqr, qn[:, :, 64:96], u_qn, sc)
            if lat:
                b = tt % 2
                t0 = (tt - 2) * 128
                P.dma("sp", lambda e, b=b, t0=t0: e.dma_start(out=cs[b][:], in_=I("mla_cos")[t0:t0 + 128, :]), writes=[u_cs[b]])
                P.dma("sp", lambda e, b=b, t0=t0: e.dma_start(out=sn[b][:], in_=I("mla_sin")[t0:t0 + 128, :]), writes=[u_cs[b]])
                self.rope(qn[:, :, 64:96], u_qn, 16, 16, cs[b][:], sn[b][:], u_cs[b], sc)
            P.op("act", lambda e: e.copy(out=qb[:], in_=qn[:]), reads=[u_qn], writes=[u_qb])
            for hh in range(2):
                self.tpose_to_dram([qb[:, hh * 8 + j, :] for j in range(8)], u_qb, 96,
                                   self.QTd[hh * 768:(hh + 1) * 768, tt * 128:(tt + 1) * 128].rearrange("(j d) t -> d j t", d=96),
                                   self.u_QT, stq[hh], u_stq[hh])
            for cc in range(4):
                ps, ups = self.nps()
                for k in range(2):
                    self.mm(ps[:, :], ckvT[:, k, tt * 128:(tt + 1) * 128], wukv[:, k, cc * 512:(cc + 1) * 512], k == 0, k == 1,
                            [u_ckvT[tt], uwukv], [ups])
                P.op("act", lambda e, ps=ps, cc=cc: e.copy(out=kvflat[:, cc * 512:(cc + 1) * 512], in_=ps[:, :]),
                     reads=[ups], writes=[u_kvf])
            self.headnorm(kvf[:, :, 0:64], u_kvf, 16, 64, gkn, ugkn, knf[:], u_knf, sc)
            P.op("act", lambda e: e.copy(out=knb[:], in_=knf[:]), reads=[u_knf], writes=[u_knb])
            P.op("pool", lambda e: e.tensor_copy(out=vb[:], in_=kvf[:, :, 64:128]), reads=[u_kvf], writes=[u_vb])
            P.dma("sp", lambda e, tt=tt: e.dma_start(out=self.Vd[tt * 128:(tt + 1) * 128, :].rearrange("t (h n) -> t h n", n=64),
                                                     in_=vb[:]), reads=[u_vb], writes=[self.u_V])
            knb2 = knb[:].rearrange("p h n -> p (h n)")
            self.tpose_to_dram([knb2[:, j * 128:(j + 1) * 128] for j in range(8)], u_knb, 128,
                               self.KTd[:, tt * 128:(tt + 1) * 128].rearrange("(j p) t -> p j t", p=128),
                               self.u_KT, stk, u_stk)
        P.phase_end()

    def mla_attn(self, do_ctx):
        P = self.P
        P.phase_begin()
        a = self.attn_setup()
        QT = [P.psb("QTh", [96, NT], BF16) for _ in range(2)]; uQ = [U("QTh0"), U("QTh1")]
        KT = [P.psb("KTh", [96, NT], BF16) for _ in range(2)]; uK = [U("KTh0"), U("KTh1")]
        V = [P.psb("Vh", [128, NTT, 64], BF16) for _ in range(2)]; uV = [U("Vh0"), U("Vh1")]
        OS = [P.psb("OSh", [64, NT], BF16) for _ in range(2)]; uOS = [U("OSh0"), U("OSh1")]
        for h in range(16):
            b = h % 2
            P.dma("sp", lambda e, b=b, h=h: e.dma_start(out=QT[b][:], in_=self.QTd[h * 96:(h + 1) * 96, :]),
                  reads=[self.u_QT], writes=[uQ[b]])
            P.dma("sp", lambda e, b=b, h=h: e.dma_start(out=KT[b][0:64, :], in_=self.KTd[h * 64:(h + 1) * 64, :]),
                  reads=[self.u_KT], writes=[uK[b]])
            P.dma("sp", lambda e, b=b: e.dma_start(out=KT[b][64:96, :], in_=self.KRd[:, :]), reads=[self.u_KR], writes=[uK[b]])
            P.dma("sp", lambda e, b=b, h=h: e.dma_start(
                out=V[b][:], in_=self.Vd[:, h * 64:(h + 1) * 64].rearrange("(t p) c -> p t c", p=128)),
                reads=[self.u_V], writes=[uV[b]])
            blocks = [(NCTX + c * 512, 512, list(range(NTT))) for c in range(8)]
            if do_ctx:
                blocks.append((0, NCTX, [0, 1]))
            for (q0, nq, kts) in blocks:
                keys = [(KT[b][:, kt * 128:(kt + 1) * 128], uK[b], V[b][:, kt, :], uV[b], 128, None, None) for kt in kts]
                self.attn_block(a, QT[b][:, q0:q0 + nq], uQ[b], nq, 96, keys, OS[b][:, q0:q0 + nq], uOS[b])
            c0 = 0 if do_ctx else NCTX
            P.dma("pool", lambda e, b=b, h=h, c0=c0: e.dma_start(out=self.OTd[h * 64:(h + 1) * 64, c0:NT], in_=OS[b][:, c0:NT]),
                  reads=[uOS[b]], writes=[self.u_OT])
        P.phase_end()


    def qkv_proj(self, wname, Hk, gqname, gkname, scale, rope=None):
        P = self.P
        I = self.I
        for nm in (wname, gqname, gkname) + (tuple(rope) if rope else ()):
            I(nm)
        ncol = 1024 + 2 * Hk * 64
        P.phase_begin()
        w, uw = self.wload("wqkv", I(wname), 8, ncol)
        gq, ugq = self.bload("gq", I(gqname)[0:1, :], 64, scale=scale)
        gk, ugk = self.bload("gk", I(gkname)[0:1, :], 64)
        sc = self.norm_scratch()
        qf = P.psb("qf", [128, ncol], F32); u_qf = U("qf")
        qn = P.psb("qn", [128, 16, 64], F32); u_qn = U("qn")
        kn = P.psb("kn", [128, Hk, 64], F32); u_kn = U("kn")
        qb = P.psb("qb", [128, 1024], BF16); u_qb = U("qb")
        kb = P.psb("kb", [128, Hk * 64], BF16); u_kb = U("kb")
        vb = P.psb("vb", [128, Hk * 64], BF16); u_vb = U("vb")
        stq = P.psb("stq", [128, 8, 128], BF16); u_stq = U("stq")
        stk = P.psb("stk", [128, 8, 128], BF16); u_stk = U("stk")
        if rope:
            cs = [P.psb("cs", [128, 32], F32) for _ in range(2)]; sn = [P.psb("sn", [128, 32], F32) for _ in range(2)]
            u_cs = [U("cs0"), U("cs1")]
        nb = ncol // 512
        kc0 = 1024
        vc0 = 1024 + Hk * 64
        for tt in range(NTT):
            lat = tt >= 2
            for cc in range(nb):
                ps, ups = self.nps()
                for k in range(8):
                    self.mm(ps[:, :], self.hT[:, k, tt * 128:(tt + 1) * 128], w[:, k, cc * 512:(cc + 1) * 512], k == 0, k == 7,
                            [self.u_hT[tt], uw], [ups])
                P.op("act", lambda e, ps=ps, cc=cc: e.copy(out=qf[:, cc * 512:(cc + 1) * 512], in_=ps[:, :]),
                     reads=[ups], writes=[u_qf])
            self.headnorm(qf[:, 0:1024].rearrange("p (h n) -> p h n", n=64), u_qf, 16, 64, gq, ugq, qn[:], u_qn, sc)
            self.headnorm(qf[:, kc0:kc0 + Hk * 64].rearrange("p (h n) -> p h n", n=64), u_qf, Hk, 64, gk, ugk, kn[:], u_kn, sc)
            if rope and lat:
                b = tt % 2
                t0 = (tt - 2) * 128
                P.dma("sp", lambda e, b=b, t0=t0: e.dma_start(out=cs[b][:], in_=I(rope[0])[t0:t0 + 128, :]), writes=[u_cs[b]])
                P.dma("sp", lambda e, b=b, t0=t0: e.dma_start(out=sn[b][:], in_=I(rope[1])[t0:t0 + 128, :]), writes=[u_cs[b]])
                self.rope(qn[:], u_qn, 16, 32, cs[b][:], sn[b][:], u_cs[b], sc)
                self.rope(kn[:], u_kn, Hk, 32, cs[b][:], sn[b][:], u_cs[b], sc)
            P.op("act", lambda e: e.copy(out=qb[:], in_=qn[:].rearrange("p h n -> p (h n)")), reads=[u_qn], writes=[u_qb])
            P.op("act", lambda e: e.copy(out=kb[:], in_=kn[:].rearrange("p h n -> p (h n)")), reads=[u_kn], writes=[u_kb])
            P.op("pool", lambda e: e.tensor_copy(out=vb[:], in_=qf[:, vc0:vc0 + Hk * 64]), reads=[u_qf], writes=[u_vb])
            P.dma("sp", lambda e, tt=tt: e.dma_start(out=self.Vd[tt * 128:(tt + 1) * 128, 0:Hk * 64], in_=vb[:]),
                  reads=[u_vb], writes=[self.u_V])
            self.tpose_to_dram([qb[:, j * 128:(j + 1) * 128] for j in range(8)], u_qb, 128,
                               self.QTd[0:1024, tt * 128:(tt + 1) * 128].rearrange("(j p) t -> p j t", p=128),
                               self.u_QT, stq, u_stq)
            nkb = Hk * 64 // 128
            self.tpose_to_dram([kb[:, j * 128:(j + 1) * 128] for j in range(nkb)], u_kb, 128,
                               self.KTd[0:Hk * 64, tt * 128:(tt + 1) * 128].rearrange("(j p) t -> p j t", p=128),
                               self.u_KT, stk, u_stk)
        P.phase_end()

    def na_proj(self, i):
        self.qkv_proj("na_w_qkv", 16, "na_q_g", "na_k_g", 64.0 ** -0.5)

    def swa_proj(self, i):
        self.qkv_proj("swa_w_qkv", 4, "swa_q_g", "swa_k_g", 64.0 ** -0.5, rope=("swa_cos", "swa_sin"))

    def swa_attn(self, do_ctx):
        P = self.P
        I = self.I
        I("swa_sink")
        P.phase_begin()
        a = self.attn_setup()
        Mp = P.psb("Mp", [128, 4, 128], F32); Mn = P.psb("Mn", [128, 4, 128], F32); u_M = U("M")
        P.op("pool", lambda e: e.memset(Mp[:], 0.0), writes=[u_M])
        P.op("pool", lambda e: e.memset(Mn[:], 0.0), writes=[u_M])
        P.op("pool", lambda e: e.affine_select(out=Mp[:], in_=Mp[:], pattern=[[0, 4], [-1, 128]], compare_op=ALU.is_ge,
                                               fill=NEG, base=0, channel_multiplier=1), reads=[u_M], writes=[u_M])
        P.op("pool", lambda e: e.affine_select(out=Mn[:], in_=Mn[:], pattern=[[0, 4], [1, 128]], compare_op=ALU.is_ge,
                                               fill=NEG, base=0, channel_multiplier=-1), reads=[u_M], writes=[u_M])
        Mp2 = Mp[:].rearrange("p g t -> p (g t)")
        Mn2 = Mn[:].rearrange("p g t -> p (g t)")
        esink, u_es = self.bload("esink", I("swa_sink")[0:1, :], 16, parts=64)
        P.op("act", lambda e: e.activation(out=esink[:], in_=esink[:], func=AF.Exp), reads=[u_es], writes=[u_es])
        Q = P.psb("Qall", [64, 4, NT], BF16); uQ = U("Qall")
        KT = P.psb("KTh", [64, NT], BF16); uK = U("KTh")
        V = P.psb("Vh", [128, NTT, 64], BF16); uV = U("Vh")
        OS = P.psb("OSh", [64, 4, NT], BF16); uOS = U("OSh")
        for hk in range(4):
            P.dma("sp", lambda e, hk=hk: e.dma_start(out=Q[:], in_=self.QTd[hk * 256:(hk + 1) * 256, :].rearrange("(g d) t -> d g t", d=64)),
                  reads=[self.u_QT], writes=[uQ])
            P.dma("sp", lambda e, hk=hk: e.dma_start(out=KT[:], in_=self.KTd[hk * 64:(hk + 1) * 64, :]),
                  reads=[self.u_KT], writes=[uK])
            P.dma("sp", lambda e, hk=hk: e.dma_start(out=V[:], in_=self.Vd[:, hk * 64:(hk + 1) * 64].rearrange("(t p) c -> p t c", p=128)),
                  reads=[self.u_V], writes=[uV])

            def sink_fn(psd, upsd, rd, urd, hk=hk):
                for g in range(4):
                    P.op("dve", lambda e, g=g: e.tensor_scalar(out=rd[:, g * 128:(g + 1) * 128], in0=psd[:, g * 128:(g + 1) * 128],
                                                                scalar1=esink[:, hk * 4 + g:hk * 4 + g + 1], scalar2=None, op0=ALU.add),
                         reads=[upsd, u_es], writes=[urd])
            tiles = list(range(2, NTT)) + ([0, 1] if do_ctx else [])
            for tile in tiles:
                def key(kt, bias):
                    return (KT[:, kt * 128:(kt + 1) * 128], uK, V[:, kt, :], uV, 128, bias, u_M)
                keys = [key(0, None), key(1, None)]
                if tile >= 2:
                    if tile > 2:
                        keys.append(key(tile - 1, Mp2))
                    keys.append(key(tile, None))
                    if tile < NTT - 1:
                        keys.append(key(tile + 1, Mn2))
                self.attn_block(a, Q[:, :, tile * 128:(tile + 1) * 128], uQ, 512, 64, keys,
                                OS[:, :, tile * 128:(tile + 1) * 128], uOS, sink_fn=sink_fn, qg=4)
            c0 = 0 if do_ctx else NCTX
            P.dma("pool", lambda e, hk=hk, c0=c0: e.dma_start(
                out=self.OTd[hk * 256:(hk + 1) * 256, c0:NT].rearrange("(g d) t -> d g t", d=64), in_=OS[:, :, c0:NT]),
                reads=[uOS], writes=[self.u_OT])
        P.phase_end()

    def na_attn(self, do_ctx):
        P = self.P
        I = self.I
        I("na_bias")
        P.phase_begin()
        a = self.attn_setup()
        QT = [P.psb("QTh", [64, NT], BF16) for _ in range(2)]; uQ = [U("QTh0"), U("QTh1")]
        KT = [P.psb("KTh", [64, NT], BF16) for _ in range(2)]; uK = [U("KTh0"), U("KTh1")]
        V = [P.psb("Vh", [128, NTT, 64], BF16) for _ in range(2)]; uV = [U("Vh0"), U("Vh1")]
        Vs = [P.psb("Vsh", [128, NTT - 1, 64], BF16) for _ in range(2)]
        B = [P.psb("Bh", [128, 14, 64], F32) for _ in range(2)]; uB = [U("Bh0"), U("Bh1")]
        OS = [P.psb("OSh", [64, NT], BF16) for _ in range(2)]; uOS = [U("OSh0"), U("OSh1")]
        for h in range(16):
            b = h % 2
            P.dma("sp", lambda e, b=b, h=h: e.dma_start(out=QT[b][:], in_=self.QTd[h * 64:(h + 1) * 64, :]),
                  reads=[self.u_QT], writes=[uQ[b]])
            P.dma("sp", lambda e, b=b, h=h: e.dma_start(out=KT[b][:], in_=self.KTd[h * 64:(h + 1) * 64, :]),
                  reads=[self.u_KT], writes=[uK[b]])
            P.dma("sp", lambda e, b=b, h=h: e.dma_start(
                out=V[b][:], in_=self.Vd[:, h * 64:(h + 1) * 64].rearrange("(t p) c -> p t c", p=128)),
                reads=[self.u_V], writes=[uV[b]])
            P.dma("sp", lambda e, b=b, h=h: e.dma_start(
                out=Vs[b][:], in_=self.Vd[64:64 + (NTT - 1) * 128, h * 64:(h + 1) * 64].rearrange("(t p) c -> p t c", p=128)),
                reads=[self.u_V], writes=[uV[b]])
            P.dma("sp", lambda e, b=b, h=h: e.dma_start(out=B[b][:], in_=I("na_bias")[h]), writes=[uB[b]])
            for r in range(64):
                q0 = NCTX + r * 64
                r0 = min(max(r - 4, 0), 56)
                keys = [(KT[b][:, 0:128], uK[b], V[b][:, 0, :], uV[b], 128, None, None),
                        (KT[b][:, 128:256], uK[b], V[b][:, 1, :], uV[b], 128, None, None)]
                for j in range(4):
                    krow = r0 + 2 * j
                    tok = NCTX + krow * 64
                    vv = V[b][:, tok // 128, :] if tok % 128 == 0 else Vs[b][:, (tok - 64) // 128, :]
                    keys.append((KT[b][:, tok:tok + 128], uK[b], vv, uV[b], 128, B[b][:, krow - r + 7, :], uB[b]))
                self.attn_block(a, QT[b][:, q0:q0 + 64], uQ[b], 64, 64, keys, OS[b][:, q0:q0 + 64], uOS[b])
            if do_ctx:
                keys = [(KT[b][:, 0:128], uK[b], V[b][:, 0, :], uV[b], 128, None, None),
                        (KT[b][:, 128:256], uK[b], V[b][:, 1, :], uV[b], 128, None, None)]
                self.attn_block(a, QT[b][:, 0:NCTX], uQ[b], NCTX, 64, keys, OS[b][:, 0:NCTX], uOS[b])
            c0 = 0 if do_ctx else NCTX
            P.dma("pool", lambda e, b=b, h=h, c0=c0: e.dma_start(out=self.OTd[h * 64:(h + 1) * 64, c0:NT], in_=OS[b][:, c0:NT]),
                  reads=[uOS[b]], writes=[self.u_OT])
        P.phase_end()

    def mixer(self, i, do_ctx):
        self.mix_scratch()
        m = i % 4
        self.norm_phase(i, 0, False)
        if m == 0:
            self.na_proj(i); self.na_attn(do_ctx); self.outproj_phase(i, "na_w_o", do_ctx)
        elif m == 1:
            self.rwkv(i, do_ctx)
        elif m == 2:
            self.mla_proj(i); self.mla_attn(do_ctx); self.outproj_phase(i, "mla_w_o", do_ctx)
        else:
            self.swa_proj(i); self.swa_attn(do_ctx); self.outproj_phase(i, "swa_w_o", do_ctx)


for _n, _f in list(vars(KMix).items()):
    if callable(_f):
        setattr(K, _n, _f)
C0 = -0.6065306597126334


class KRw:
    def rw_scratch(self):
        if hasattr(self, "rwd"):
            return
        P = self.P
        self.rwd = {}
        self.u_rwd = {}
        for nm in ("R", "K", "KK", "V", "G", "LW0", "LW1", "B0", "B1", "KT0", "KT1", "Y"):
            self.rwd[nm] = P.dram("rw_" + nm, [NT, D], F32)
            self.u_rwd[nm] = U("rw_" + nm)

    def rw_xs(self, tt, S, u_S, xx, u_xx, tmp, u_tmp, xs, u_xs, streams, mixT, u_mixT):
        P = self.P
        h = self.hT
        t0 = tt * 128
        noprev = tt in (0, 2)
        nonext = tt in (1, NTT - 1)
        a = 1 if noprev else 0
        b = 127 if nonext else 128
        uh = [self.u_hT[tt]]
        P.op("dve", lambda e: e.tensor_tensor(out=S[:, :, a:b], in0=h[:, :, t0 - 1 + a:t0 - 1 + b],
                                              in1=h[:, :, t0 + 1 + a:t0 + 1 + b], op=ALU.add), reads=uh, writes=[u_S])
        if noprev:
            P.op("dve", lambda e: e.tensor_copy(out=S[:, :, 0:1], in_=h[:, :, t0 + 1:t0 + 2]), reads=uh, writes=[u_S])
        if nonext:
            P.op("dve", lambda e: e.tensor_copy(out=S[:, :, 127:128], in_=h[:, :, t0 + 126:t0 + 127]), reads=uh, writes=[u_S])
        P.op("dve", lambda e: e.scalar_tensor_tensor(out=xx[:], in0=S[:], scalar=0.5, in1=h[:, :, t0:t0 + 128],
                                                     op0=ALU.mult, op1=ALU.subtract), reads=[u_S] + uh, writes=[u_xx])
        for n, s in enumerate(streams):
            tb = n % 2
            P.op("pool", lambda e, s=s, tb=tb: e.tensor_tensor(
                out=tmp[tb][:], in0=xx[:], in1=mixT[:, s * 8:(s + 1) * 8].unsqueeze(2).to_broadcast([128, 8, 128]),
                op=ALU.mult), reads=[u_xx, u_mixT], writes=[u_tmp[tb]])
            P.op("dve", lambda e, n=n, tb=tb: e.tensor_tensor(out=xs[n][:], in0=tmp[tb][:], in1=h[:, :, t0:t0 + 128], op=ALU.add),
                 reads=[u_tmp[tb]] + uh, writes=[u_xs[n]])

    def rw_common(self):
        P = self.P
        I = self.I
        m48 = P.psb("m48", [48, 128], F32); u_m48 = U("m48")
        mixT = P.psb("mixT", [128, 48], F32); u_mixT = U("mixT")
        P.dma("sp", lambda e: e.dma_start(out=m48[:], in_=I("rw_mix")[:, :]), writes=[u_m48])
        ps, ups = self.nps()
        self.tr(ps[:, 0:48], m48[:], self.ident_f[0:48, 0:48], [u_m48, self.u_const], [ups])
        P.op("dve", lambda e: e.tensor_copy(out=mixT[:], in_=ps[:, 0:48]), reads=[ups], writes=[u_mixT])
        S = P.psb("S", [128, 8, 128], F32); xx = P.psb("xx", [128, 8, 128], F32)
        tmp = [P.psb("xtmp", [128, 8, 128], F32) for _ in range(2)]
        xs = [P.psb("xsT", [128, 8, 128], BF16) for _ in range(3)]
        return dict(mixT=mixT, u_mixT=u_mixT, S=S, u_S=U("S"), xx=xx, u_xx=U("xx"), tmp=tmp, u_tmp=[U("xt0"), U("xt1")],
                    xs=xs, u_xs=[U("xs0"), U("xs1"), U("xs2")])

    def rw_out(self, ob, u_ob, n, name, tt):
        b = n % len(ob)
        self.P.dma("pool", lambda e: e.dma_start(out=self.rwd[name][tt * 128:(tt + 1) * 128, :], in_=ob[b][:]),
                   reads=[u_ob[b]], writes=[self.u_rwd[name]])

    def rw_proj_a(self):
        P = self.P
        I = self.I
        for nm in ("rw_mix", "rw_w_r", "rw_w_k", "rw_w_v", "rw_k_k"):
            I(nm)
        P.phase_begin()
        c = self.rw_common()
        W = {}
        for nm in ("rw_w_r", "rw_w_k", "rw_w_v"):
            W[nm] = self.wload(nm, I(nm), 8, D)
        kkb, u_kkb = self.bload("kkb", I("rw_k_k")[0:1, :], D)
        ob = [P.psb("ob", [128, D], F32) for _ in range(5)]; u_ob = [U("ob%d" % j) for j in range(5)]
        ss = P.psb("ss", [128, 16], F32); u_ss = U("ss")
        sq = P.psb("sq", [128, D], F32); u_sq = U("sq")
        n = 0
        for tt in range(NTT):
            self.rw_xs(tt, c["S"], c["u_S"], c["xx"], c["u_xx"], c["tmp"], c["u_tmp"], c["xs"], c["u_xs"], (0, 2, 3),
                       c["mixT"], c["u_mixT"])
            for si, (nm, dst) in enumerate((("rw_w_r", "R"), ("rw_w_k", "K"), ("rw_w_v", "V"))):
                w, uw = W[nm]
                b = n % 5
                for half in range(2):
                    ps, ups = self.nps()
                    for k in range(8):
                        self.mm(ps[:, :], c["xs"][si][:, k, :], w[:, k, half * 512:(half + 1) * 512], k == 0, k == 7,
                                [c["u_xs"][si], uw], [ups])
                    P.op("act", lambda e, ps=ps, b=b, half=half: e.copy(out=ob[b][:, half * 512:(half + 1) * 512], in_=ps[:, :]),
                         reads=[ups], writes=[u_ob[b]])
                self.rw_out(ob, u_ob, n, dst, tt)
                kb = b
                n += 1
                if dst == "K":
                    b2 = n % 5
                    n += 1
                    kks = ob[b2]
                    P.op("dve", lambda e, kb=kb, kks=kks: e.tensor_tensor(out=kks[:], in0=ob[kb][:], in1=kkb[:], op=ALU.mult),
                         reads=[u_ob[kb], u_kkb], writes=[u_ob[b2]])
                    P.op("dve", lambda e, kks=kks: e.tensor_tensor(out=sq[:], in0=kks[:], in1=kks[:], op=ALU.mult),
                         reads=[u_ob[b2]], writes=[u_sq])
                    P.op("dve", lambda e: e.tensor_reduce(out=ss[:], in_=sq[:].rearrange("p (h n) -> p h n", n=64), axis=AX.X,
                                                          op=ALU.add), reads=[u_sq], writes=[u_ss])
                    P.op("dve", lambda e: e.tensor_scalar(out=ss[:], in0=ss[:], scalar1=1e-24, scalar2=None, op0=ALU.max),
                         reads=[u_ss], writes=[u_ss])
                    P.op("act", lambda e: e.sqrt(out=ss[:], in_=ss[:]), reads=[u_ss], writes=[u_ss])
                    P.op("dve", lambda e: e.reciprocal(out=ss[:], in_=ss[:]), reads=[u_ss], writes=[u_ss])
                    P.op("dve", lambda e, kks=kks: e.tensor_tensor(
                        out=kks[:].rearrange("p (h n) -> p h n", n=64), in0=kks[:].rearrange("p (h n) -> p h n", n=64),
                        in1=ss[:].unsqueeze(2).to_broadcast([128, 16, 64]), op=ALU.mult), reads=[u_ob[b2], u_ss], writes=[u_ob[b2]])
                    self.rw_out(ob, u_ob, b2, "KK", tt)
        P.phase_end()

    def rw_proj_b(self):
        P = self.P
        I = self.I
        for nm in ("rw_mix", "rw_g1", "rw_g2", "rw_w0", "rw_w1", "rw_w2", "rw_a0", "rw_a1", "rw_a2", "rw_k_a"):
            I(nm)
        P.phase_begin()
        c = self.rw_common()
        g1w, u_g1 = self.wload("g1w", I("rw_g1"), 8, 128)
        g2w, u_g2 = self.wload("g2w", I("rw_g2"), 1, D)
        w1 = [self.wload("w1_%d" % z, I("rw_w1")[z], 8, 64) for z in range(2)]
        a1 = [self.wload("a1_%d" % z, I("rw_a1")[z], 8, 64) for z in range(2)]
        w2 = []; a2 = []
        for z in range(2):
            for (lst, nm) in ((w2, "rw_w2"), (a2, "rw_a2")):
                t = P.psb(nm, [64, D], BF16); u = U(nm)
                P.dma("pool", lambda e, t=t, nm=nm, z=z: e.dma_start(out=t[:], in_=I(nm)[z]), writes=[u])
                lst.append((t, u))
        def hilo(nm):
            f = P.psb(nm + "f", [33, 2 * D], F32); uf = U(nm + "f")
            hl = P.psb(nm + "hl", [33, 2 * D], BF16); uhl = U(nm + "hl")
            bk = P.psb(nm + "bk", [33, 2 * D], F32); ubk = U(nm + "bk")
            P.op("pool", lambda e: e.memset(f[:], 0.0), writes=[uf])
            src = I(nm).rearrange("(o z) n -> o (z n)", o=1)
            P.dma("sp", lambda e: e.dma_start(out=f[0:1, :], in_=src), reads=[uf], writes=[uf])
            P.dma("sp", lambda e: e.dma_start(out=f[32:33, :], in_=src), reads=[uf], writes=[uf])
            P.op("dve", lambda e: e.tensor_copy(out=hl[:], in_=f[:]), reads=[uf], writes=[uhl])
            P.op("dve", lambda e: e.tensor_copy(out=bk[:], in_=hl[:]), reads=[uhl], writes=[ubk])
            P.op("dve", lambda e: e.tensor_tensor(out=bk[:], in0=f[:], in1=bk[:], op=ALU.subtract), reads=[uf, ubk], writes=[ubk])
            P.op("dve", lambda e: e.tensor_copy(out=hl[32:33, :], in_=bk[32:33, :]), reads=[ubk, uhl], writes=[uhl])
            return hl, uhl
        w0hl, u_w0 = hilo("rw_w0")
        a0hl, u_a0 = hilo("rw_a0")
        kab, u_kab = self.bload("kab", I("rw_k_a")[0:1, :], D)
        c1b = P.psb("c1b", [128, D], F32); u_c1b = U("c1b")
        P.op("dve", lambda e: e.tensor_scalar(out=c1b[:], in0=kab[:], scalar1=-1.0, scalar2=1.0, op0=ALU.mult, op1=ALU.add),
             reads=[u_kab], writes=[u_c1b])
        ob = [P.psb("ob", [128, D], F32) for _ in range(4)]; u_ob = [U("ob%d" % j) for j in range(4)]
        kin = P.psb("kin", [128, D], F32); kkin = P.psb("kkin", [128, D], F32); u_kin = U("kin"); u_kkin = U("kkin")
        az = P.psb("az", [128, D], F32); u_az = U("az")
        lt = [P.psb("lt", [128, 128], BF16) for _ in range(2)]; u_lt = [U("lt0"), U("lt1")]
        n = 0
        nl = 0
        for tt in range(NTT):
            self.rw_xs(tt, c["S"], c["u_S"], c["xx"], c["u_xx"], c["tmp"], c["u_tmp"], c["xs"], c["u_xs"], (1, 4, 5),
                       c["mixT"], c["u_mixT"])
            xw, u_xw = c["xs"][0], c["u_xs"][0]
            xa, u_xa = c["xs"][1], c["u_xs"][1]
            xg, u_xg = c["xs"][2], c["u_xs"][2]
            P.dma("sp", lambda e, tt=tt: e.dma_start(out=kin[:], in_=self.rwd["K"][tt * 128:(tt + 1) * 128, :]),
                  reads=[self.u_rwd["K"]], writes=[u_kin])
            P.dma("sp", lambda e, tt=tt: e.dma_start(out=kkin[:], in_=self.rwd["KK"][tt * 128:(tt + 1) * 128, :]),
                  reads=[self.u_rwd["KK"]], writes=[u_kkin])

            def lora(x, u_x, w1t, u_w1, rows, func, w2t, u_w2, bias, u_bias, z, out_ap, u_out, fin):
                nonlocal nl
                psI, upsI = self.nps()
                for k in range(8):
                    self.mm(psI[0:rows, 0:128], w1t[:, k, :], x[:, k, :], k == 0, k == 7, [u_w1, u_x], [upsI])
                l = nl % 2
                nl += 1
                P.op("act", lambda e: e.activation(out=lt[l][0:rows, :], in_=psI[0:rows, 0:128], func=func),
                     reads=[upsI], writes=[u_lt[l]])
                for half in range(2):
                    ps, ups = self.nps()
                    self.mm(ps[:, :], lt[l][0:rows, :], w2t[0:rows, half * 512:(half + 1) * 512], True, bias is None,
                            [u_lt[l], u_w2], [ups])
                    if bias is not None:
                        self.mm(ps[:, :], self.ones_b[0:33, 0:128], bias[:, z * D + half * 512:z * D + (half + 1) * 512],
                                False, True, [u_bias, self.u_const], [ups])
                    P.op("act", lambda e, ps=ps, half=half: e.activation(out=out_ap[:, half * 512:(half + 1) * 512], in_=ps[:, :],
                                                                         func=fin), reads=[ups], writes=[u_out])
            b = n % 4; n += 1
            lora(xg, u_xg, g1w, u_g1, 128, AF.Sigmoid, g2w[:, 0, :], u_g2, None, None, 0, ob[b], u_ob[b], AF.Copy)
            self.rw_out(ob, u_ob, b, "G", tt)
            for z in range(2):
                b = n % 4; n += 1
                lora(xw, u_xw, w1[z][0], w1[z][1], 64, AF.Tanh, w2[z][0], w2[z][1], w0hl, u_w0, z, ob[b], u_ob[b], AF.Sigmoid)
                self.rw_out(ob, u_ob, b, "LW%d" % z, tt)
                lora(xa, u_xa, a1[z][0], a1[z][1], 64, AF.Copy, a2[z][0], a2[z][1], a0hl, u_a0, z, az, u_az, AF.Sigmoid)
                b = n % 4; n += 1
                P.op("dve", lambda e, b=b: e.tensor_tensor(out=ob[b][:], in0=az[:], in1=kab[:], op=ALU.mult),
                     reads=[u_az, u_kab], writes=[u_ob[b]])
                P.op("dve", lambda e, b=b: e.tensor_tensor(out=ob[b][:], in0=ob[b][:], in1=c1b[:], op=ALU.add),
                     reads=[u_ob[b], u_c1b], writes=[u_ob[b]])
                P.op("dve", lambda e, b=b: e.tensor_tensor(out=ob[b][:], in0=ob[b][:], in1=kin[:], op=ALU.mult),
                     reads=[u_ob[b], u_kin], writes=[u_ob[b]])
                self.rw_out(ob, u_ob, b, "KT%d" % z, tt)
                b = n % 4; n += 1
                P.op("dve", lambda e, b=b: e.tensor_tensor(out=ob[b][:], in0=az[:], in1=kkin[:], op=ALU.mult),
                     reads=[u_az, u_kkin], writes=[u_ob[b]])
                self.rw_out(ob, u_ob, b, "B%d" % z, tt)
        P.phase_end()

    def rw_scan(self, z, do_ctx):
        P = self.P
        I = self.I
        for nm in ("rw_r_k", "rw_ln_g", "rw_ln_b"):
            I(nm)
        P.phase_begin()
        fwd = z == 0
        tri = P.psb("tri", [128, 128], F32); mA = P.psb("mA", [128, 4, 128], F32); mN = P.psb("mN", [128, 128], F32)
        cvec = P.psb("cvec", [128, 2], F32); u_mk = U("masks")
        cm, pat = (-1, 1) if fwd else (1, -1)
        P.op("pool", lambda e: e.memset(tri[:], C0), writes=[u_mk])
        P.op("pool", lambda e: e.memset(mA[:], 1.0), writes=[u_mk])
        P.op("pool", lambda e: e.memset(mN[:], 1.0), writes=[u_mk])
        P.op("pool", lambda e: e.memset(cvec[:], C0), writes=[u_mk])
        P.op("pool", lambda e: e.affine_select(out=tri[:], in_=tri[:], pattern=[[pat, 128]], compare_op=ALU.is_ge, fill=0.0,
                                               base=0, channel_multiplier=cm), reads=[u_mk], writes=[u_mk])
        for q in range(4):
            P.op("pool", lambda e, q=q: e.affine_select(out=mA[:, q, :], in_=mA[:, q, :], pattern=[[pat, 128]],
                                                        compare_op=ALU.is_ge, fill=0.0, base=-(q % 2), channel_multiplier=cm),
                 reads=[u_mk], writes=[u_mk])
        P.op("pool", lambda e: e.affine_select(out=mN[:], in_=mN[:], pattern=[[-pat, 128]], compare_op=ALU.is_ge, fill=0.0,
                                               base=-1, channel_multiplier=-cm), reads=[u_mk], writes=[u_mk])
        Hst = P.psb("Hst", [64, 16, 64], F32); u_H = U("Hst")
        P.op("pool", lambda e: e.memset(Hst[:], 0.0), writes=[u_H])
        names = ("R", "KK", "V", "LW%d" % z, "B%d" % z, "KT%d" % z)
        tin = {nm: P.psb("in_" + nm, [128, D], F32) for nm in names}
        u_in = {nm: U("in_" + nm) for nm in names}
        r, kk, v, sg, bb, kt = (tin[nm] for nm in names)
        u_r, u_kk, u_v, u_sg, u_bb, u_kt = (u_in[nm] for nm in names)
        E = [P.psb("E", [128, D], F32) for _ in range(3)]; u_E = [U("E0"), U("E1"), U("E2")]
        F4 = [P.psb("F4", [128, D], F32) for _ in range(4)]; u_F4 = [U("F4_%d" % j) for j in range(4)]
        FT = P.psb("FT", [64, 8, 4, 128], F32); u_FT = [U("FT%d" % j) for j in range(8)]
        AA = P.psb("AA", [128, 8, 4, 128], F32); u_AA = [U("AA%d" % j) for j in range(8)]
        Nn = P.psb("Nn", [128, 8, 128], F32); u_Nn = [U("Nn0"), U("Nn1")]
        MB = [P.psb("MB", [128, 4, 128], F32) for _ in range(2)]; u_MB = [U("MB0"), U("MB1")]
        NB = [P.psb("NB", [128, 4, 128], F32) for _ in range(2)]; u_NB = [U("NB0"), U("NB1")]
        if fwd:
            MB2 = [P.psb("MB2", [128, 4, 128], F32) for _ in range(2)]; u_MB2 = [U("MB20"), U("MB21")]
            NB2 = [P.psb("NB2", [128, 4, 128], F32) for _ in range(2)]; u_NB2 = [U("NB20"), U("NB21")]
        else:
            MB2, u_MB2, NB2, u_NB2 = MB, u_MB, NB, u_NB
        MBg = [MB, MB2]; u_MBg = [u_MB, u_MB2]; NBg = [NB, NB2]; u_NBg = [u_NB, u_NB2]
        Pm = P.psb("Pm", [128, 8, 128], F32); u_Pm = [U("Pm0"), U("Pm1")]
        X = P.psb("X", [128, 512], F32); u_X = U("X")
        nU = P.psb("nU", [128, 512], F32); u_nU = U("nU")
        ysb = P.psb("ysb", [128, D], F32); u_y = U("ysb")
        gl = P.psb("gl", [64, 16], F32); u_gl = U("gl")
        if not fwd:
            rkb, u_rkb = self.bload("rkb", I("rw_r_k")[0:1, :], D)
            lng, u_lng = self.bload("lng", I("rw_ln_g")[0:1, :], D)
            lnb, u_lnb = self.bload("lnb", I("rw_ln_b")[0:1, :], D)
            st = P.psb("st", [128, 48], F32); u_st = U("st")
            obf = P.psb("obf", [128, D], BF16); u_obf = U("obf")
            stg = P.psb("stg", [128, 8, 128], BF16); u_stg = U("stg")
        order = list(range(NTT)) if fwd else [1, 0] + list(range(NTT - 1, 1, -1))
        cut = self.cfg.get("scan_cut", 99)
        if "scan_tiles" in self.cfg:
            order = order[:self.cfg["scan_tiles"]]
        v3 = lambda t: t[:].rearrange("p (h n) -> p h n", n=64)
        for tt in order:
            for nm in names:
                P.dma("sp", lambda e, nm=nm, tt=tt: e.dma_start(out=tin[nm][:], in_=self.rwd[nm][tt * 128:(tt + 1) * 128, :]),
                      reads=[self.u_rwd[nm]], writes=[u_in[nm]])
            if cut < -1:
                continue
            for half in range(2):
                hs = slice(half * 512, (half + 1) * 512)
                ps, ups = self.nps()
                self.mm(ps[:, :], tri[:], sg[:, hs], True, True, [u_mk, u_sg], [ups])
                P.op("dve", lambda e, ps=ps, hs=hs: e.tensor_copy(out=E[0][:, hs], in_=ps[:, :]), reads=[ups], writes=[u_E[0]])
                P.op("dve", lambda e, hs=hs: e.scalar_tensor_tensor(out=E[1][:, hs], in0=sg[:, hs], scalar=-C0, in1=E[0][:, hs],
                                                                    op0=ALU.mult, op1=ALU.add), reads=[u_E[0], u_sg], writes=[u_E[1]])
            P.op("act", lambda e: e.activation(out=E[2][:], in_=E[0][:], func=AF.Exp, scale=-1.0), reads=[u_E[0]], writes=[u_E[2]])
            P.op("act", lambda e: e.activation(out=E[0][:], in_=E[0][:], func=AF.Exp), reads=[u_E[0], u_E[2], u_E[1]], writes=[u_E[0]])
            P.op("act", lambda e: e.activation(out=E[1][:], in_=E[1][:], func=AF.Exp), reads=[u_E[1]], writes=[u_E[1]])
            if cut < 0:
                continue
            for j, (src, us, ex) in enumerate(((r, u_r, 0), (kk, u_kk, 1), (bb, u_bb, 2), (kt, u_kt, 2))):
                eng = "dve" if j % 2 == 0 else "pool"
                P.op(eng, lambda e, j=j, src=src, ex=ex: e.tensor_tensor(out=F4[j][:], in0=src[:], in1=E[ex][:], op=ALU.mult),
                     reads=[us, u_E[ex]], writes=[u_F4[j]])
            if cut < 1:
                continue
            psG, upsG = self.nps()
            for h in range(16):
                self.mm(psG[0:64, 2 * h:2 * h + 2], sg[:, h * 64:(h + 1) * 64], cvec[:, 0:2], True, True, [u_sg, u_mk], [upsG])
            P.op("dve", lambda e, psG=psG: e.tensor_copy(
                out=gl[:], in_=psG[0:64, 0:32].rearrange("p (h two) -> p h two", two=2)[:, :, 0]), reads=[upsG], writes=[u_gl])
            P.op("act", lambda e: e.activation(out=gl[:], in_=gl[:], func=AF.Exp), reads=[u_gl], writes=[u_gl])
            if cut < 2:
                continue
            for half in range(2):
                for hh in range(8):
                    h = half * 8 + hh
                    ps, ups = self.nps()
                    for j in range(4):
                        self.tr(ps[0:64, j * 128:(j + 1) * 128], F4[j][:, h * 64:(h + 1) * 64], self.ident_f[:],
                                [u_F4[j], self.u_const], [ups])
                    eng = "dve"
                    if eng == "act":
                        P.op("act", lambda e, ps=ps, hh=hh: e.copy(out=FT[:, hh].rearrange("p a t -> p (a t)"), in_=ps[0:64, :]),
                             reads=[ups], writes=[u_FT[hh]])
                    else:
                        P.op("dve", lambda e, ps=ps, hh=hh: e.tensor_copy(out=FT[:, hh].rearrange("p a t -> p (a t)"), in_=ps[0:64, :]),
                             reads=[ups], writes=[u_FT[hh]])
                if cut < 3:
                    continue
                for hh in range(8):
                    ps, ups = self.nps()
                    rhs2 = FT[:, hh, 0:2, :]
                    self.mm(ps[:, 0:256].rearrange("p (a t) -> p a t", a=2), FT[:, hh, 2, :], rhs2, True, True, [u_FT[hh]], [ups])
                    self.mm(ps[:, 256:512].rearrange("p (a t) -> p a t", a=2), FT[:, hh, 3, :], rhs2, True, True, [u_FT[hh]], [ups])
                    P.op("dve", lambda e, ps=ps, hh=hh: e.tensor_tensor(out=AA[:, hh], in0=ps[:, :].rearrange("p (a t) -> p a t", a=4),
                                                                        in1=mA[:], op=ALU.mult), reads=[ups, u_mk], writes=[u_AA[hh]])
                for g in range(2):
                    ps, ups = self.nps()
                    for j in range(4):
                        hh = g * 4 + j
                        self.mm(ps[:, j * 128:(j + 1) * 128], FT[:, hh, 1, :], FT[:, hh, 2, :], True, True, [u_FT[hh]], [ups])
                    P.op("dve", lambda e, ps=ps, g=g: e.tensor_tensor(
                        out=Nn[:, g * 4:(g + 1) * 4, :], in0=ps[:, :].rearrange("p (a t) -> p a t", a=4),
                        in1=mN[:].unsqueeze(1).to_broadcast([128, 4, 128]), op=ALU.mult), reads=[ups, u_mk], writes=[u_Nn[g]])
                if cut < 4:
                    continue
                stt = {}
                for g in range(2):
                    grp = [g * 4 + j for j in range(4)]
                    P.op("dve", lambda e, g=g: e.tensor_tensor(
                        out=Pm[:, g * 4:(g + 1) * 4, :], in0=self.ident_f[:].unsqueeze(1).to_broadcast([128, 4, 128]),
                        in1=AA[:, g * 4:(g + 1) * 4, 1, :], op=ALU.subtract), reads=[u_AA[hh] for hh in grp] + [self.u_const],
                        writes=[u_Pm[g]])
                    stt[g] = ([AA[:, hh, 1, :] for hh in grp], [u_AA[hh] for hh in grp], [Nn[:, hh, :] for hh in grp], [u_Nn[g]])
                for gs in (([0, 1],) if fwd else ([0], [1])):
                    for li in range(6):
                        lastl = li == 5
                        pp = li % 2
                        banks = {}
                        for g in gs:
                            Ms, uM, Ns, uN = stt[g]
                            bM = ubM = None
                            if not lastl:
                                bM, ubM = self.nps()
                                for j in range(4):
                                    self.mm(bM[:, j * 128:(j + 1) * 128], Ns[j], Ms[j], True, True, uM + uN, [ubM])
                            bN, ubN = self.nps()
                            for j in range(4):
                                self.mm(bN[:, j * 128:(j + 1) * 128], Ms[j], Ns[j], True, True, uM + uN, [ubN])
                            banks[g] = (bM, ubM, bN, ubN)
                        for g in gs:
                            bM, ubM, bN, ubN = banks[g]
                            mb, umb = MBg[g][pp], u_MBg[g][pp]
                            nb_, unb = NBg[g][pp], u_NBg[g][pp]
                            if not lastl:
                                P.op("dve", lambda e, bM=bM, mb=mb: e.tensor_copy(out=mb[:].rearrange("p a t -> p (a t)"), in_=bM[:, :]),
                                     reads=[ubM], writes=[umb])
                            P.op("dve", lambda e, bN=bN, nb_=nb_: e.tensor_copy(out=nb_[:].rearrange("p a t -> p (a t)"), in_=bN[:, :]),
                                 reads=[ubN], writes=[unb])
                            stt[g] = ([mb[:, j, :] for j in range(4)], [umb], [nb_[:, j, :] for j in range(4)], [unb])
                        for g in gs:
                            Ms, uM, Ns, uN = stt[g]
                            bP, ubP = self.nps()
                            for j in range(4):
                                self.mm(bP[:, j * 128:(j + 1) * 128], Ns[j], Pm[:, g * 4 + j, :], True, True, uN + [u_Pm[g]], [ubP])
                            P.op("dve", lambda e, bP=bP, g=g: e.tensor_tensor(
                                out=Pm[:, g * 4:(g + 1) * 4, :], in0=Pm[:, g * 4:(g + 1) * 4, :],
                                in1=bP[:, :].rearrange("p (a t) -> p a t", a=4), op=ALU.add), reads=[ubP, u_Pm[g]], writes=[u_Pm[g]])
                if cut < 5:
                    continue
                ps, ups = self.nps()
                for hh in range(8):
                    h = half * 8 + hh
                    self.mm(ps[:, hh * 64:(hh + 1) * 64], FT[:, hh, 1, :], Hst[:, h, :], True, False, [u_FT[hh], u_H], [ups])
                    self.mm(ps[:, hh * 64:(hh + 1) * 64], AA[:, hh, 3, :], v[:, h * 64:(h + 1) * 64], False, True, [u_AA[hh], u_v], [ups])
                P.op("dve", lambda e, ps=ps: e.tensor_copy(out=X[:], in_=ps[:, :]), reads=[ups], writes=[u_X])
                ps, ups = self.nps()
                for hh in range(8):
                    self.mm(ps[:, hh * 64:(hh + 1) * 64], Pm[:, hh, :], X[:, hh * 64:(hh + 1) * 64], True, True, [u_Pm[hh // 4], u_X], [ups])
                P.op("dve", lambda e, ps=ps: e.tensor_scalar(out=nU[:], in0=ps[:, :], scalar1=-1.0, scalar2=None, op0=ALU.mult),
                     reads=[ups], writes=[u_nU])
                if cut < 6:
                    continue
                ps, ups = self.nps()
                for hh in range(8):
                    h = half * 8 + hh
                    o = ps[:, hh * 64:(hh + 1) * 64]
                    self.mm(o, FT[:, hh, 0, :], Hst[:, h, :], True, False, [u_FT[hh], u_H], [ups])
                    self.mm(o, AA[:, hh, 2, :], v[:, h * 64:(h + 1) * 64], False, False, [u_AA[hh], u_v], [ups])
                    self.mm(o, AA[:, hh, 0, :], nU[:, hh * 64:(hh + 1) * 64], False, True, [u_AA[hh], u_nU], [ups])
                P.op("dve", lambda e, ps=ps, half=half: e.tensor_copy(out=ysb[:, half * 512:(half + 1) * 512], in_=ps[:, :]),
                     reads=[ups], writes=[u_y])
                if cut < 7:
                    continue
                ps, ups = self.nps()
                for hh in range(8):
                    h = half * 8 + hh
                    o = ps[0:64, hh * 64:(hh + 1) * 64]
                    hc = slice(h * 64, (h + 1) * 64)
                    self.mm(o, F4[3][:, hc], v[:, hc], True, False, [u_F4[3], u_v], [ups])
                    self.mm(o, F4[2][:, hc], nU[:, hh * 64:(hh + 1) * 64], False, False, [u_F4[2], u_nU], [ups])
                    self.mm(o, self.ident_f[0:64, 0:64], Hst[:, h, :], False, True, [u_H, self.u_const], [ups])
                P.op("dve", lambda e, ps=ps, half=half: e.tensor_tensor(
                    out=Hst[:, half * 8:(half + 1) * 8, :], in0=ps[0:64, :].rearrange("p (a t) -> p a t", a=8),
                    in1=gl[:, half * 8:(half + 1) * 8].unsqueeze(2).to_broadcast([64, 8, 64]), op=ALU.mult),
                    reads=[ups, u_gl], writes=[u_H])
            if cut < 8:
                continue
            if fwd:
                P.dma("pool", lambda e, tt=tt: e.dma_start(out=self.rwd["Y"][tt * 128:(tt + 1) * 128, :], in_=ysb[:]),
                      reads=[u_y], writes=[self.u_rwd["Y"]])
                continue
            if tt < 2 and not do_ctx:
                continue
            yf, u_yf = E[0], u_E[0]
            P.dma("sp", lambda e, tt=tt: e.dma_start(out=yf[:], in_=self.rwd["Y"][tt * 128:(tt + 1) * 128, :]),
                  reads=[self.u_rwd["Y"]], writes=[u_yf])
            kt0, u_kt0 = E[1], u_E[1]
            P.dma("sp", lambda e, tt=tt: e.dma_start(out=kt0[:], in_=self.rwd["KT0"][tt * 128:(tt + 1) * 128, :]),
                  reads=[self.u_rwd["KT0"]], writes=[u_kt0])
            gg, u_gg = E[2], u_E[2]
            P.dma("sp", lambda e, tt=tt: e.dma_start(out=gg[:], in_=self.rwd["G"][tt * 128:(tt + 1) * 128, :]),
                  reads=[self.u_rwd["G"]], writes=[u_gg])
            T0, T1, T2, T3 = F4
            uT0, uT1, uT2, uT3 = u_F4
            P.op("dve", lambda e: e.tensor_tensor(out=ysb[:], in0=ysb[:], in1=yf[:], op=ALU.add), reads=[u_y, u_yf], writes=[u_y])
            P.op("dve", lambda e: e.tensor_reduce(out=st[:, 0:16], in_=v3(ysb), axis=AX.X, op=ALU.add), reads=[u_y], writes=[u_st])
            P.op("dve", lambda e: e.tensor_scalar(out=st[:, 0:16], in0=st[:, 0:16], scalar1=1.0 / 64, scalar2=None, op0=ALU.mult),
                 reads=[u_st], writes=[u_st])
            P.op("dve", lambda e: e.tensor_tensor(out=v3(T0), in0=v3(ysb), in1=st[:, 0:16].unsqueeze(2).to_broadcast([128, 16, 64]),
                                                  op=ALU.subtract), reads=[u_y, u_st], writes=[uT0])
            P.op("dve", lambda e: e.tensor_tensor(out=T1[:], in0=T0[:], in1=T0[:], op=ALU.mult), reads=[uT0], writes=[uT1])
            P.op("dve", lambda e: e.tensor_reduce(out=st[:, 16:32], in_=v3(T1), axis=AX.X, op=ALU.add), reads=[uT1], writes=[u_st])
            P.op("dve", lambda e: e.tensor_scalar(out=st[:, 16:32], in0=st[:, 16:32], scalar1=1.0 / 64, scalar2=64e-5,
                                                  op0=ALU.mult, op1=ALU.add), reads=[u_st], writes=[u_st])
            P.op("act", lambda e: e.sqrt(out=st[:, 16:32], in_=st[:, 16:32]), reads=[u_st], writes=[u_st])
            P.op("dve", lambda e: e.reciprocal(out=st[:, 16:32], in_=st[:, 16:32]), reads=[u_st], writes=[u_st])
            P.op("dve", lambda e: e.tensor_tensor(out=v3(T0), in0=v3(T0), in1=st[:, 16:32].unsqueeze(2).to_broadcast([128, 16, 64]),
                                                  op=ALU.mult), reads=[uT0, u_st], writes=[uT0])
            P.op("dve", lambda e: e.tensor_tensor(out=T0[:], in0=T0[:], in1=lng[:], op=ALU.mult), reads=[uT0, u_lng], writes=[uT0])
            P.op("dve", lambda e: e.tensor_tensor(out=T0[:], in0=T0[:], in1=lnb[:], op=ALU.add), reads=[uT0, u_lnb], writes=[uT0])
            P.op("pool", lambda e: e.tensor_tensor(out=T1[:], in0=r[:], in1=rkb[:], op=ALU.mult), reads=[u_r, u_rkb], writes=[uT1])
            P.op("pool", lambda e: e.tensor_tensor(out=T2[:], in0=kt[:], in1=kt0[:], op=ALU.add), reads=[u_kt, u_kt0], writes=[uT2])
            P.op("dve", lambda e: e.tensor_tensor(out=T2[:], in0=T2[:], in1=T1[:], op=ALU.mult), reads=[uT1, uT2], writes=[uT2])
            P.op("dve", lambda e: e.tensor_reduce(out=st[:, 32:48], in_=v3(T2), axis=AX.X, op=ALU.add), reads=[uT2], writes=[u_st])
            P.op("dve", lambda e: e.tensor_tensor(out=v3(T3), in0=v3(v), in1=st[:, 32:48].unsqueeze(2).to_broadcast([128, 16, 64]),
                                                  op=ALU.mult), reads=[u_v, u_st], writes=[uT3])
            P.op("dve", lambda e: e.tensor_tensor(out=T0[:], in0=T0[:], in1=T3[:], op=ALU.add), reads=[uT0, uT3], writes=[uT0])
            P.op("dve", lambda e: e.tensor_tensor(out=obf[:], in0=T0[:], in1=gg[:], op=ALU.mult), reads=[uT0, u_gg], writes=[u_obf])
            self.tpose_to_dram([obf[:, j * 128:(j + 1) * 128] for j in range(8)], u_obf, 128,
                               self.OTd[0:1024, tt * 128:(tt + 1) * 128].rearrange("(j p) t -> p j t", p=128),
                               self.u_OT, stg, u_stg)
        P.phase_end()

    def rwkv(self, i, do_ctx):
        stop = self.cfg.get("rw_stop", 9)
        self.rw_scratch()
        self.rw_proj_a()
        if stop >= 2:
            self.rw_proj_b()
        if stop >= 3:
            self.rw_scan(0, do_ctx)
        if stop >= 4:
            self.rw_scan(1, do_ctx)
            self.outproj_phase(i, "rw_w_o", do_ctx)
        if self.cfg.get("rw_dump"):
            P = self.P
            for nm in self.cfg["rw_dump"]:
                o = P.dram("dump_" + nm, [NT, D], F32, kind="ExternalOutput")
                for j in range(2):
                    P.dma("sp", lambda e, o=o, nm=nm, j=j: e.dma_start(out=o[j * 2176:(j + 1) * 2176, :],
                                                                      in_=self.rwd[nm][j * 2176:(j + 1) * 2176, :]),
                          reads=[self.u_rwd[nm]], is_out=True)


for _n, _f in list(vars(KRw).items()):
    if callable(_f):
        setattr(K, _n, _f)
IN_SHAPES = None


def build_program(cfg):
    k = K(cfg)
    k.prologue()
    if cfg.get("only_scan") is not None:
        k.mix_scratch()
        k.rw_scratch()
        k.rw_scan(cfg["only_scan"], True)
        return k, k.epilogue()
    for i in cfg.get("layers", [0, 1, 2, 3]):
        do_ctx = i < 3 or cfg.get("force_ctx", False)
        if not cfg.get("skip_ada"):
            k.ada_phase(i)
        if not cfg.get("skip_mixer"):
            k.mixer(i, do_ctx)
        if not cfg.get("skip_moe"):
            tiles = None if do_ctx else list(range(2, NTT))
            k.norm_phase(i, 1, True, tiles)
            k.moe_phase(i, do_ctx)
    nc = k.epilogue()
    return k, nc


def rope_tables(d_rot):
    t = np.arange(NLAT)
    row = (t // 64).astype(np.float32)
    col = (t % 64).astype(np.float32)
    d_axis = d_rot // 2
    inv = (np.float32(10000.0) ** (-np.arange(0, d_axis, 2, dtype=np.float32) / np.float32(d_axis))).astype(np.float32)
    ang = np.concatenate([row[:, None] * inv, col[:, None] * inv], axis=-1).astype(np.float32)
    return np.cos(ang).astype(np.float32), np.sin(ang).astype(np.float32)


def na_bias_table(rpb):
    rpb = np.asarray(rpb, dtype=np.float32)
    kl = np.arange(128) // 64
    kc = np.arange(128) % 64
    c = np.arange(64)
    cstart = np.clip(c - 8, 0, 48)
    ok = (kc[:, None] >= cstart[None, :]) & (kc[:, None] < cstart[None, :] + 16)
    dcol = np.clip(kc[:, None] - c[None, :] + 15, 0, 30)
    out = np.empty((16, 128, 14, 64), np.float32)
    for di in range(14):
        dr = np.clip(di - 7 + kl + 7, 0, 14)
        g = rpb[:, dr[:, None], dcol]
        out[:, :, di, :] = np.where(ok[None], g, np.float32(-30000.0))
    return out


def make_in_maps(inputs, names):
    f = np.ascontiguousarray
    shared = {}
    for nm in names:
        if nm in ("x", "c", "ctx"):
            continue
        if nm in ("mla_cos", "mla_sin", "swa_cos", "swa_sin"):
            cs, sn = rope_tables(32 if nm.startswith("mla") else 64)
            shared[nm] = f(cs if nm.endswith("cos") else sn)
            continue
        if nm == "na_bias":
            shared[nm] = f(na_bias_table(inputs["na_rpb"][0]))
            continue
        a = np.asarray(inputs[nm], dtype=np.float32)
        if nm == "c_ctx":
            a = a.reshape(8, 128)
        elif nm in ("norm1_g", "norm2_g"):
            a = a.reshape(4, 8, 128)
        elif nm == "ada_b":
            a = a.reshape(4, 48, 128)
        elif nm.startswith(("na_", "rw_", "mla_", "swa_")):
            a = a[0]
            if nm == "rw_mix":
                a = a.reshape(48, 128)
            elif nm == "rw_r_k":
                a = a.reshape(1, -1)
            if a.ndim == 1:
                a = a.reshape(1, -1)
        shared[nm] = f(a)
    maps = []
    for b in range(8):
        m = dict(shared)
        m["x"] = f(np.asarray(inputs["x"][b], dtype=np.float32))
        m["c"] = f(np.asarray(inputs["c"][b], dtype=np.float32).reshape(8, 128))
        m["ctx"] = f(np.asarray(inputs["ctx"][b], dtype=np.float32))
        maps.append(m)
    return maps


def run(inputs, cfg):
    k, nc = build_program(cfg)
    maps = make_in_maps(inputs, list(k.inp.keys()))
    res = run_bass_kernel_spmd(nc, maps, core_ids=list(range(8)))
    return res


def kernel(**inputs):
    res = run(inputs, {})
    return np.stack([np.asarray(r["out"], dtype=np.float32) for r in res.results], axis=0)
```

```python
from concourse.bass_utils import run_bass_kernel_spmd
import contextlib
import numpy as np
import concourse.bass as bass
import concourse.mybir as mybir

F32 = mybir.dt.float32
BF16 = mybir.dt.bfloat16
I32 = mybir.dt.int32
U32 = mybir.dt.uint32
AF = mybir.ActivationFunctionType
ALU = mybir.AluOpType
AX = mybir.AxisListType

SEM_CAP = 30000
DMA_POOL = 12


class U:
    __slots__ = ("name", "w", "r")

    def __init__(self, name):
        self.name = name
        self.w = None
        self.r = {}


class Prog:
    def __init__(self):
        self.nc = bass.Bass("TRN2", target_bir_lowering=False)
        self.es = contextlib.ExitStack()
        self.ops = {e: [] for e in ("pe", "dve", "act", "pool", "sp")}
        self.cnt = {e: 0 for e in self.ops}
        self.ep = {e: 0 for e in self.ops}
        self.sems = {}
        self.waited = {e: {} for e in self.ops}
        self.dma_n = {e: 0 for e in self.ops}
        self.dma_val = {}
        self.n_inst = 0
        self.out_ticks = []

    def sb(self, name, shape, dt):
        return self.es.enter_context(self.nc.sbuf_tensor(name, list(shape), dt))

    def ps(self, name, shape, dt=F32):
        return self.es.enter_context(self.nc.psum_tensor(name, list(shape), dt))

    def dram(self, name, shape, dt, kind="Internal"):
        return self.nc.dram_tensor(name, list(shape), dt, kind=kind).ap()

    def _sem(self, key):
        if key not in self.sems:
            self.sems[key] = self.es.enter_context(self.nc.semaphore("s_%s_%s" % key))
        return self.sems[key]

    def _wait(self, eng, tick):
        if tick is None:
            return
        key, val = tick
        if self.waited[eng].get(key, 0) >= val:
            return
        self.waited[eng][key] = val
        sem = self._sem(key)
        self.ops[eng].append(lambda e, sem=sem, val=val: e.wait_ge(sem, val))

    def _deps(self, eng, reads, writes, skip_self=False):
        ticks = []
        for u in reads:
            if u.w is not None:
                ticks.append(u.w)
        for u in writes:
            if u.w is not None:
                ticks.append(u.w)
            for k, v in u.r.items():
                ticks.append((k, v))
        mykey = (eng, self.ep[eng])
        for t in ticks:
            if skip_self and t[0] == mykey:
                continue
            self._wait(eng, t)

    def _mark(self, tick, reads, writes):
        k, v = tick
        for u in reads:
            if u.r.get(k, 0) < v:
                u.r[k] = v
        for u in writes:
            u.w = tick
            u.r = {}

    def op(self, eng, fn, reads=(), writes=(), skip_self=False):
        reads = list(reads)
        writes = list(writes)
        self._deps(eng, reads, writes, skip_self=skip_self)
        if self.cnt[eng] >= SEM_CAP:
            self.ep[eng] += 1
            self.cnt[eng] = 0
        self.cnt[eng] += 1
        key = (eng, self.ep[eng])
        sem = self._sem(key)
        val = self.cnt[eng]
        self.ops[eng].append(lambda e, fn=fn, sem=sem: fn(e).then_inc(sem, 1))
        self._mark((key, val), reads, writes)
        self.n_inst += 1
        return (key, val)

    def dma(self, q, fn, reads=(), writes=(), is_out=False):
        reads = list(reads)
        writes = list(writes)
        self._deps(q, reads, writes)
        slot = self.dma_n[q] % DMA_POOL
        self.dma_n[q] += 1
        key = ("d" + q, slot)
        prev = self.dma_val.get(key, 0)
        if prev >= SEM_CAP:
            gen = 1
            while ("d%s_g%d" % (q, gen), slot) in self.dma_val and \
                    self.dma_val[("d%s_g%d" % (q, gen), slot)] >= SEM_CAP:
                gen += 1
            raise RuntimeError("dma semaphore cap reached")
        if prev:
            self._wait(q, (key, prev))
        val = prev + 16
        self.dma_val[key] = val
        sem = self._sem(key)
        self.ops[q].append(lambda e, fn=fn, sem=sem: fn(e).then_inc(sem, 16))
        self._mark((key, val), reads, writes)
        self.n_inst += 1
        if is_out:
            self.out_ticks.append((key, val))
        return (key, val)

    def barrier(self):
        ticks = []
        for e in self.ops:
            if self.cnt[e] > 0:
                ticks.append(((e, self.ep[e]), self.cnt[e]))
        for key, val in self.dma_val.items():
            ticks.append((key, val))
        for e in self.ops:
            for t in ticks:
                if t[0][0] == e:
                    continue
                self._wait(e, t)

    def phase_begin(self):
        self.pes = contextlib.ExitStack()

    def psb(self, name, shape, dt):
        self.uid = getattr(self, "uid", 0) + 1
        return self.pes.enter_context(self.nc.sbuf_tensor("%s_%d" % (name, self.uid), list(shape), dt))

    def phase_end(self):
        self.barrier()
        self.flush()
        self.pes.close()

    def flush(self):
        nc = self.nc
        with nc.Block() as block:
            @block.tensor
            def _(e):
                for f in self.ops["pe"]:
                    f(e)

            @block.vector
            def _(e):
                for f in self.ops["dve"]:
                    f(e)

            @block.scalar
            def _(e):
                for f in self.ops["act"]:
                    f(e)

            @block.gpsimd
            def _(e):
                for f in self.ops["pool"]:
                    f(e)

            @block.sync
            def _(e):
                for f in self.ops["sp"]:
                    f(e)
        for e in self.ops:
            self.ops[e] = []

    def finish(self):
        for t in self.out_ticks:
            self._wait("sp", t)
        self.flush()
        self.es.close()
        return self.nc
D = 1024
NCTX = 256
NLAT = 4096
NT = NCTX + NLAT
NTT = NT // 128
EPS = 1e-6


class K:
    def __init__(self, cfg):
        self.cfg = cfg
        self.P = P = Prog()
        self.inp = {}
        self.uin = {}
        self.build_io()
        self.setup_persistent()

    SHAPES = {
        "x": [NLAT, D], "c": [8, 128], "ctx": [NCTX, D], "c_ctx": [8, 128],
        "norm1_g": [4, 8, 128], "norm2_g": [4, 8, 128], "ada_w": [4, D, 6 * D], "ada_b": [4, 48, 128],
        "moe_router": [4, D, 16], "moe_w1": [4, 16, D, D], "moe_w3": [4, 16, D, D], "moe_w2": [4, 16, D, D],
        "na_w_qkv": [D, 3 * D], "na_q_g": [1, 64], "na_k_g": [1, 64], "na_bias": [16, 128, 14, 64], "na_w_o": [D, D],
        "mla_w_down": [D, 672], "mla_q_norm_g": [1, 384], "mla_kv_norm_g": [1, 256], "mla_w_uq": [384, 1536],
        "mla_w_ukv": [256, 2048], "mla_qn_g": [1, 64], "mla_qr_g": [1, 32], "mla_kn_g": [1, 64], "mla_kr_g": [1, 32],
        "mla_w_o": [D, D], "mla_cos": [NLAT, 16], "mla_sin": [NLAT, 16],
        "swa_w_qkv": [D, 1536], "swa_q_g": [1, 64], "swa_k_g": [1, 64], "swa_sink": [1, 16], "swa_w_o": [D, D],
        "swa_cos": [NLAT, 32], "swa_sin": [NLAT, 32],
        "rw_mix": [48, 128], "rw_w_r": [D, D], "rw_w_k": [D, D], "rw_w_v": [D, D], "rw_w0": [2, D], "rw_w1": [2, D, 64],
        "rw_w2": [2, 64, D], "rw_a0": [2, D], "rw_a1": [2, D, 64], "rw_a2": [2, 64, D], "rw_g1": [D, 128], "rw_g2": [128, D],
        "rw_k_k": [1, D], "rw_k_a": [1, D], "rw_r_k": [1, D], "rw_ln_g": [1, D], "rw_ln_b": [1, D], "rw_w_o": [D, D],
    }

    def build_io(self):
        P = self.P
        self.out = P.dram("out", [NLAT, D], F32, kind="ExternalOutput")
        self.xres = P.dram("xres", [NT, D], F32); self.u_xres = U("xres")
        self.xs2 = P.dram("xs2", [NT, D], BF16); self.u_xs2 = U("xs2")
        self.modd = P.dram("modd", [1, 4 * 96 * 128], F32); self.u_modd = U("modd")

    def I(self, name):
        if name not in self.inp:
            self.inp[name] = self.P.dram(name, self.SHAPES[name], F32, kind="ExternalInput")
        return self.inp[name]

    def setup_persistent(self):
        P = self.P
        self.ident_f = P.sb("ident_f", [128, 128], F32); self.u_const = U("const")
        self.ident_b = P.sb("ident_b", [128, 128], BF16)
        self.ones_f = P.sb("ones_f", [128, 128], F32)
        self.ones_b = P.sb("ones_b", [128, 128], BF16)
        self.scT = P.sb("scT", [128, 8, 2], F32); self.u_scT = U("scT")
        self.AB = P.sb("AB", [128, 16, 8, 2], F32); self.u_AB = U("AB")
        self.hT = P.sb("hT", [128, 8, NT], BF16); self.u_hT = [U("hT%d" % t) for t in range(NTT)]
        self.gateT = P.sb("gateT", [128, 5, 16], F32)
        self.idxT = P.sb("idxT", [128, 5, 16], I32)
        self.psf = [P.ps("psf%d" % i, [128, 512], F32) for i in range(6)]
        self.u_psf = [U("psf%d" % i) for i in range(6)]
        self.psb = [P.ps("psb%d" % i, [128, 1024], BF16) for i in range(2)]
        self.u_psb = [U("psb%d" % i) for i in range(2)]
        self.psf_n = 0
        self.psb_n = 0
        uc = self.u_const
        P.op("pool", lambda e: e.memset(self.ident_f[:], 0.0), writes=[uc])
        P.op("pool", lambda e: e.affine_select(out=self.ident_f[:], in_=self.ident_f[:], pattern=[[-1, 128]],
                                               compare_op=ALU.not_equal, fill=1.0, base=0, channel_multiplier=1),
             reads=[uc], writes=[uc])
        P.op("pool", lambda e: e.tensor_copy(out=self.ident_b[:], in_=self.ident_f[:]), reads=[uc], writes=[uc])
        P.op("pool", lambda e: e.memset(self.ones_f[:], 1.0), writes=[uc])
        P.op("pool", lambda e: e.memset(self.ones_b[:], 1.0), writes=[uc])

    def nps(self):
        i = self.psf_n % 4
        self.psf_n += 1
        return self.psf[i], self.u_psf[i]

    def npsb(self):
        i = self.psb_n % 2
        self.psb_n += 1
        return self.psb[i], self.u_psb[i]

    def mm(self, out, lhsT, rhs, start, stop, reads, writes):
        self.P.op("pe", lambda e: e.matmul(out=out, lhsT=lhsT, rhs=rhs, start=start, stop=stop),
                  reads=reads, writes=writes, skip_self=True)

    def tr(self, out, in_, ident, reads, writes):
        self.P.op("pe", lambda e: e.transpose(out=out, in_=in_, identity=ident),
                  reads=reads, writes=writes, skip_self=True)

    def prologue(self):
        P = self.P
        I = self.I
        for nm in ("x", "c", "ctx", "c_ctx"):
            I(nm)
        P.phase_begin()
        P.dma("sp", lambda e: e.dma_start(out=self.xres[0:NCTX, :], in_=I("ctx")[:, :]), writes=[self.u_xres])
        for j in range(8):
            P.dma("sp", lambda e, j=j: e.dma_start(out=self.xres[NCTX + j * 512:NCTX + (j + 1) * 512, :],
                                                    in_=I("x")[j * 512:(j + 1) * 512, :]), writes=[self.u_xres])
        c16 = P.psb("c16", [8, 256], F32); u_c16 = U("c16")
        P.dma("sp", lambda e: e.dma_start(out=c16[:, 0:128], in_=I("c")[:, :]), writes=[u_c16])
        P.dma("sp", lambda e: e.dma_start(out=c16[:, 128:256], in_=I("c_ctx")[:, :]), writes=[u_c16])
        ps, ups = self.nps()
        for s in range(2):
            self.tr(ps[:, s * 8:(s + 1) * 8], c16[:, s * 128:(s + 1) * 128], self.ident_f[0:8, 0:8],
                    [u_c16, self.u_const], [ups])
        for s in range(2):
            P.op("act", lambda e, s=s: e.activation(out=self.scT[:, :, s], in_=ps[:, s * 8:(s + 1) * 8], func=AF.Silu),
                 reads=[ups], writes=[self.u_scT])
        P.phase_end()

    def ada_phase(self, i):
        P = self.P
        I = self.I
        for nm in ("ada_w", "ada_b", "norm1_g", "norm2_g"):
            I(nm)
        P.phase_begin()
        wbuf = [P.psb("adaw", [128, 8, 512], F32) for _ in range(3)]
        uw = [U("adaw%d" % j) for j in range(3)]
        ab48 = P.psb("ab48", [48, 128], F32); g16 = P.psb("g16", [16, 128], F32); u_ld = U("ld")
        abT = P.psb("abT", [128, 48], F32); gT = P.psb("gT", [128, 16], F32); u_T = U("T")
        modT = P.psb("modT", [128, 48, 2], F32); u_modT = U("modT")
        modTT = P.psb("modTT", [96, 128], F32); u_modTT = U("modTT")
        P.dma("sp", lambda e: e.dma_start(out=ab48[:], in_=I("ada_b")[i]), writes=[u_ld])
        P.dma("sp", lambda e: e.dma_start(out=g16[0:8, :], in_=I("norm1_g")[i]), writes=[u_ld])
        P.dma("sp", lambda e: e.dma_start(out=g16[8:16, :], in_=I("norm2_g")[i]), writes=[u_ld])
        psA, upsA = self.nps()
        self.tr(psA[:, 0:48], ab48[:], self.ident_f[0:48, 0:48], [u_ld, self.u_const], [upsA])
        self.tr(psA[:, 64:80], g16[:], self.ident_f[0:16, 0:16], [u_ld, self.u_const], [upsA])
        P.op("dve", lambda e: e.tensor_copy(out=abT[:], in_=psA[:, 0:48]), reads=[upsA], writes=[u_T])
        P.op("dve", lambda e: e.tensor_copy(out=gT[:], in_=psA[:, 64:80]), reads=[upsA], writes=[u_T])
        psM, upsM = self.nps()
        wsrc = I("ada_w")[i].rearrange("(k p) n -> p k n", p=128)
        for piece in range(12):
            b = piece % 3
            P.dma("sp", lambda e, b=b, piece=piece: e.dma_start(out=wbuf[b][:], in_=wsrc[:, :, piece * 512:(piece + 1) * 512]),
                  writes=[uw[b]])
            for ml in range(4):
                m = piece * 4 + ml
                for k in range(8):
                    self.mm(psM[:, 2 * m:2 * m + 2], wbuf[b][:, k, ml * 128:(ml + 1) * 128], self.scT[:, k, :],
                            k == 0, k == 7, [uw[b], self.u_scT], [upsM])
        P.op("dve", lambda e: e.tensor_tensor(out=modT[:], in0=psM[:, 0:96].rearrange("p (m s) -> p m s", s=2),
                                              in1=abT[:].unsqueeze(2).to_broadcast([128, 48, 2]), op=ALU.add),
             reads=[upsM, u_T], writes=[u_modT])
        AB = self.AB
        for (slot, m0, g0) in ((0, 8, 0), (2, 32, 8)):
            P.op("dve", lambda e, slot=slot, m0=m0, g0=g0: e.scalar_tensor_tensor(
                out=AB[:, i * 4 + slot], in0=modT[:, m0:m0 + 8, :], scalar=1.0,
                in1=gT[:, g0:g0 + 8].unsqueeze(2).to_broadcast([128, 8, 2]), op0=ALU.add, op1=ALU.mult),
                reads=[u_modT, u_T], writes=[self.u_AB])
        for (slot, m0) in ((1, 0), (3, 24)):
            P.op("dve", lambda e, slot=slot, m0=m0: e.tensor_copy(out=AB[:, i * 4 + slot], in_=modT[:, m0:m0 + 8, :]),
                 reads=[u_modT], writes=[self.u_AB])
        psT, upsT = self.nps()
        self.tr(psT[0:96, 0:128], modT[:].rearrange("p m s -> p (m s)"), self.ident_f[:], [u_modT, self.u_const], [upsT])
        P.op("dve", lambda e: e.tensor_copy(out=modTT[:], in_=psT[0:96, 0:128]), reads=[upsT], writes=[u_modTT])
        P.dma("sp", lambda e: e.dma_start(out=self.modd[0, i * 12288:(i + 1) * 12288].rearrange("(r p) -> r p", p=128),
                                          in_=modTT[:]), reads=[u_modTT], writes=[self.u_modd])
        P.phase_end()

    def gate_bcast_src(self, i, which, s):
        m0 = 16 if which == 0 else 40
        base = i * 12288 + (m0 * 2 + s) * 128
        v = self.modd[0:1, base:base + 8 * 256].rearrange("o (j r) -> o j r", r=256)[:, :, 0:128]
        return v.partition_broadcast(128)[:, 0]

    def norm_phase(self, i, which, write_xs2, tiles=None):
        P = self.P
        tiles = list(range(NTT)) if tiles is None else tiles
        P.phase_begin()
        xt = [P.psb("xt", [128, D], F32) for _ in range(2)]; u_xt = [U("xt0"), U("xt1")]
        xs = [P.psb("xs", [128, D], F32) for _ in range(2)]; u_xs = [U("xs0"), U("xs1")]
        xsb = [P.psb("xsb", [128, D], BF16) for _ in range(2)]; u_xsb = [U("xsb0"), U("xsb1")]
        junk = P.psb("junk", [128, D], BF16)
        ss = P.psb("ss", [128, NTT], F32); u_ss = [U("ss%d" % t) for t in range(NTT)]
        rs = P.psb("rs", [128, NTT], F32); u_rs = [U("rs%d" % t) for t in range(NTT)]
        A = self.AB[:, i * 4 + 2 * which]
        B = self.AB[:, i * 4 + 2 * which + 1]
        for n, tt in enumerate(tiles):
            s = 1 if tt < 2 else 0
            b = n % 2
            P.dma("sp", lambda e, b=b, tt=tt: e.dma_start(out=xt[b][:], in_=self.xres[tt * 128:(tt + 1) * 128, :]),
                  reads=[self.u_xres], writes=[u_xt[b]])
            P.op("act", lambda e, b=b, tt=tt: e.activation(out=junk[:], in_=xt[b][:], func=AF.Square,
                                                           accum_out=ss[:, tt:tt + 1]),
                 reads=[u_xt[b]], writes=[u_ss[tt]])
            P.op("dve", lambda e, tt=tt: e.tensor_scalar(out=rs[:, tt:tt + 1], in0=ss[:, tt:tt + 1], scalar1=1.0 / D,
                                                         scalar2=EPS, op0=ALU.mult, op1=ALU.add),
                 reads=[u_ss[tt]], writes=[u_rs[tt]])
            P.op("act", lambda e, tt=tt: e.sqrt(out=rs[:, tt:tt + 1], in_=rs[:, tt:tt + 1]),
                 reads=[u_rs[tt]], writes=[u_rs[tt]])
            P.op("dve", lambda e, tt=tt: e.reciprocal(out=rs[:, tt:tt + 1], in_=rs[:, tt:tt + 1]),
                 reads=[u_rs[tt]], writes=[u_rs[tt]])
            P.op("dve", lambda e, b=b, tt=tt: e.tensor_scalar(out=xs[b][:], in0=xt[b][:], scalar1=rs[:, tt:tt + 1],
                                                              scalar2=None, op0=ALU.mult),
                 reads=[u_xt[b], u_rs[tt]], writes=[u_xs[b]])
            if write_xs2:
                P.op("pool", lambda e, b=b: e.tensor_copy(out=xsb[b][:], in_=xs[b][:]), reads=[u_xs[b]], writes=[u_xsb[b]])
                P.dma("pool", lambda e, b=b, tt=tt: e.dma_start(out=self.xs2[tt * 128:(tt + 1) * 128, :], in_=xsb[b][:]),
                      reads=[u_xsb[b]], writes=[self.u_xs2])
            for half in range(2):
                ps, ups = self.nps()
                for kk in range(4):
                    k = half * 4 + kk
                    self.tr(ps[:, kk * 128:(kk + 1) * 128], xs[b][:, k * 128:(k + 1) * 128], self.ident_f[:],
                            [u_xs[b], self.u_const], [ups])
                for kk in range(4):
                    k = half * 4 + kk
                    P.op("act", lambda e, k=k, kk=kk, tt=tt, s=s, ps=ps: e.activation(
                        out=self.hT[:, k, tt * 128:(tt + 1) * 128], in_=ps[:, kk * 128:(kk + 1) * 128],
                        func=AF.Identity, scale=A[:, k, s:s + 1], bias=B[:, k, s:s + 1]),
                        reads=[ups, self.u_AB], writes=[self.u_hT[tt]])
        P.phase_end()

    def moe_phase(self, i, do_ctx):
        P = self.P
        I = self.I
        for nm in ("moe_router", "moe_w1", "moe_w3", "moe_w2"):
            I(nm)
        NS = 544 if do_ctx else 512
        P.phase_begin()
        wr = P.psb("wr", [128, 8, 16], BF16); u_wr = U("wr")
        E = P.psb("E", [16, NT], F32); u_E = U("E")
        wk = P.psb("wk", [16, NT], F32); u_wkl = U("wkl"); u_wkc = U("wkc")
        mx = P.psb("mx", [16, 544], F32); u_mx = U("mx")
        ix = P.psb("ix", [16, 544], U32); u_ix = U("ix")
        ixf = P.psb("ixf", [16, 544], F32); u_ixf = U("ixf")
        rcp = P.psb("rcp", [16, 512], F32); u_rcp = U("rcp")
        P.dma("pool", lambda e: e.dma_start(out=wr[:], in_=I("moe_router")[i].rearrange("(k p) n -> p k n", p=128)),
              writes=[u_wr])
        chunks = [(c0, min(512, NT - c0)) for c0 in range(0, NT, 512)]
        for (c0, n) in chunks:
            ps, ups = self.nps()
            tts = list(range(c0 // 128, (c0 + n) // 128))
            for k in range(8):
                self.mm(ps[0:16, 0:n], wr[:, k, :], self.hT[:, k, c0:c0 + n], k == 0, k == 7,
                        [u_wr] + [self.u_hT[t] for t in tts], [ups])
            P.op("act", lambda e, ps=ps, c0=c0, n=n: e.activation(out=E[:, c0:c0 + n], in_=ps[0:16, 0:n], func=AF.Exp),
                 reads=[ups], writes=[u_E])
        for (c0, n) in chunks:
            ps, ups = self.nps()
            self.mm(ps[0:16, 0:n], self.ones_f[0:16, 0:16], E[:, c0:c0 + n], True, True, [u_E, self.u_const], [ups])
            P.op("dve", lambda e, ps=ps, n=n: e.reciprocal(out=rcp[:, 0:n], in_=ps[0:16, 0:n]),
                 reads=[ups], writes=[u_rcp])
            P.op("dve", lambda e, c0=c0, n=n: e.tensor_tensor(out=E[:, c0:c0 + n], in0=E[:, c0:c0 + n],
                                                              in1=rcp[:, 0:n], op=ALU.mult),
                 reads=[u_rcp, u_E], writes=[u_E])
        sets = [(NCTX, NLAT, 0, 64, u_wkl)]
        if do_ctx:
            sets.append((0, NCTX, 512, 4, u_wkc))
        for (t0, n, s0, iters, u_wk) in sets:
            src, us = E, u_E
            for it in range(iters):
                sl = slice(s0 + it * 8, s0 + it * 8 + 8)
                P.op("dve", lambda e, src=src, sl=sl, t0=t0, n=n: e.max(out=mx[:, sl], in_=src[:, t0:t0 + n]),
                     reads=[us], writes=[u_mx])
                P.op("dve", lambda e, src=src, sl=sl, t0=t0, n=n: e.max_index(out=ix[:, sl], in_max=mx[:, sl],
                                                                               in_values=src[:, t0:t0 + n]),
                     reads=[us, u_mx], writes=[u_ix])
                if it < iters - 1:
                    P.op("dve", lambda e, src=src, sl=sl, t0=t0, n=n: e.match_replace(
                        out=wk[:, t0:t0 + n], in_to_replace=mx[:, sl], in_values=src[:, t0:t0 + n], imm_value=0.0),
                        reads=[us, u_mx], writes=[u_wk])
                src, us = wk, u_wk
        P.op("dve", lambda e: e.tensor_copy(out=ixf[:, 0:NS], in_=ix[:, 0:NS]), reads=[u_ix], writes=[u_ixf])
        P.op("dve", lambda e: e.tensor_scalar(out=ixf[:, 0:512], in0=ixf[:, 0:512], scalar1=float(NCTX), scalar2=None,
                                              op0=ALU.add), reads=[u_ixf], writes=[u_ixf])
        u_gateT = U("gateT"); u_idxT = U("idxT")
        nch = 5 if do_ctx else 4
        for ch in range(nch):
            rows = 128 if ch < 4 else 32
            ps, ups = self.nps()
            self.tr(ps[0:rows, 0:16], mx[:, ch * 128:ch * 128 + rows], self.ident_f[0:16, 0:16], [u_mx, self.u_const], [ups])
            self.tr(ps[0:rows, 16:32], ixf[:, ch * 128:ch * 128 + rows], self.ident_f[0:16, 0:16], [u_ixf, self.u_const], [ups])
            P.op("dve", lambda e, ps=ps, ch=ch, rows=rows: e.tensor_copy(out=self.gateT[0:rows, ch, :], in_=ps[0:rows, 0:16]),
                 reads=[ups], writes=[u_gateT])
            P.op("dve", lambda e, ps=ps, ch=ch, rows=rows: e.tensor_copy(out=self.idxT[0:rows, ch, :], in_=ps[0:rows, 16:32]),
                 reads=[ups], writes=[u_idxT])
        P.phase_end()
        P.phase_begin()
        NWB = 6
        wb = [self.hT[:, :, j * D:(j + 1) * D] for j in range(4)] + [P.psb("wb", [128, 8, D], BF16)[:] for _ in range(2)]
        u_wb = [U("wb%d" % j) for j in range(NWB)]
        NXI = 5
        xin = [P.psb("xin", [128, D], BF16) for _ in range(NXI)]; u_xin = [U("xin%d" % j) for j in range(NXI)]
        xinT = [P.psb("xinT", [128, 8, 640], BF16) for _ in range(2)]; u_xinT = [U("xinT0"), U("xinT1")]
        hidT = P.psb("hidT", [128, 8, 640], BF16); u_hidT = U("hidT")
        sg = [P.psb("sg", [128, 512], F32) for _ in range(2)]; u_sg = [U("sg0"), U("sg1")]
        ysb = [P.psb("ysb", [128, D], F32) for _ in range(2)]; u_ysb = [U("ysb0"), U("ysb1")]
        G = P.psb("G", [128, 2, 8, 128], F32); u_G = U("G")
        for s in range(2 if do_ctx else 1):
            P.dma("sp", lambda e, s=s: e.dma_start(out=G[:, s], in_=self.gate_bcast_src(i, 1, s)),
                  reads=[self.u_modd], writes=[u_G])
        A2 = self.AB[:, i * 4 + 2]
        B2 = self.AB[:, i * 4 + 3]
        wn = 0
        gn = 0
        yn = 0
        segs = [(0, 512, 0)] + ([(512, 32, 1)] if do_ctx else [])
        def emit_weights(ex):
            nonlocal wn
            wl = {}
            for nm in ("moe_w1", "moe_w3", "moe_w2"):
                b = wn % NWB
                wn += 1
                P.dma("pool", lambda e, nm=nm, b=b, ex=ex: e.dma_start(
                    out=wb[b], in_=I(nm)[i, ex].rearrange("(k p) n -> p k n", p=128)), writes=[u_wb[b]])
                wl[nm] = (wb[b], u_wb[b])
            return wl

        def emit_gathers(ex):
            nonlocal gn
            gb = []
            for ch in range(nch):
                rows = 128 if ch < 4 else 32
                b = gn % NXI
                gn += 1
                P.dma("pool", lambda e, b=b, ch=ch, rows=rows, ex=ex: e.indirect_dma_start(
                    out=xin[b][0:rows, :], out_offset=None, in_=self.xs2[:, :],
                    in_offset=bass.IndirectOffsetOnAxis(ap=self.idxT[0:rows, ch, ex:ex + 1], axis=0)),
                    reads=[u_idxT, self.u_xs2], writes=[u_xin[b]])
                gb.append(b)
            return gb

        pre = {0: (emit_weights(0), emit_gathers(0))}
        for ex in range(16):
            wl, gbufs = pre.pop(ex)
            xT, u_xT = xinT[ex % 2], u_xinT[ex % 2]
            for ch in range(nch):
                rows = 128 if ch < 4 else 32
                s = 0 if ch < 4 else 1
                b = gbufs[ch]
                pb, upb = self.npsb()
                for k in range(8):
                    self.tr(pb[:, k * 128:k * 128 + rows], xin[b][0:rows, k * 128:(k + 1) * 128],
                            self.ident_b[0:rows, 0:rows], [u_xin[b], self.u_const], [upb])
                for k in range(8):
                    P.op("act", lambda e, k=k, ch=ch, rows=rows, s=s, pb=pb, xT=xT: e.activation(
                        out=xT[:, k, ch * 128:ch * 128 + rows], in_=pb[:, k * 128:k * 128 + rows],
                        func=AF.Identity, scale=A2[:, k, s:s + 1], bias=B2[:, k, s:s + 1]),
                        reads=[upb, self.u_AB], writes=[u_xT])
            if ex + 1 < 16:
                pre[ex + 1] = (emit_weights(ex + 1), emit_gathers(ex + 1))
            w1, u_w1 = wl["moe_w1"]
            w3, u_w3 = wl["moe_w3"]
            w2, u_w2 = wl["moe_w2"]
            for f in range(8):
                for (lo, n, s) in segs:
                    p1, up1 = self.nps()
                    p3, up3 = self.nps()
                    for k in range(8):
                        self.mm(p1[:, 0:n], w1[:, k, f * 128:(f + 1) * 128], xT[:, k, lo:lo + n], k == 0, k == 7,
                                [u_w1, u_xT], [up1])
                    for k in range(8):
                        self.mm(p3[:, 0:n], w3[:, k, f * 128:(f + 1) * 128], xT[:, k, lo:lo + n], k == 0, k == 7,
                                [u_w3, u_xT], [up3])
                    sb_ = (f * 2 + s) % 2
                    P.op("act", lambda e, p1=p1, n=n, sb_=sb_: e.activation(out=sg[sb_][:, 0:n], in_=p1[:, 0:n], func=AF.Silu),
                         reads=[up1], writes=[u_sg[sb_]])
                    P.op("dve", lambda e, p3=p3, n=n, sb_=sb_, f=f, lo=lo: e.tensor_tensor(
                        out=hidT[:, f, lo:lo + n], in0=sg[sb_][:, 0:n], in1=p3[:, 0:n], op=ALU.mult),
                        reads=[up3, u_sg[sb_]], writes=[u_hidT])
            for ch in range(nch):
                rows = 128 if ch < 4 else 32
                s = 0 if ch < 4 else 1
                t0, tn = (NCTX, NLAT) if ch < 4 else (0, NCTX)
                yb = yn % 2
                yn += 1
                for half in range(2):
                    py, upy = self.nps()
                    for k in range(8):
                        self.mm(py[0:rows, :], hidT[:, k, ch * 128:ch * 128 + rows], w2[:, k, half * 512:(half + 1) * 512],
                                k == 0, k == 7, [u_w2, u_hidT], [upy])
                    P.op("dve", lambda e, py=py, rows=rows, ch=ch, half=half, s=s, yb=yb, ex=ex: e.scalar_tensor_tensor(
                        out=ysb[yb][0:rows, half * 512:(half + 1) * 512], in0=py[0:rows, :],
                        scalar=self.gateT[0:rows, ch, ex:ex + 1],
                        in1=G[0:rows, s, half * 4:(half + 1) * 4, :].rearrange("p a b -> p (a b)"),
                        op0=ALU.mult, op1=ALU.mult), reads=[upy, u_gateT, u_G], writes=[u_ysb[yb]])
                P.dma("pool", lambda e, rows=rows, ch=ch, yb=yb, t0=t0, tn=tn, ex=ex: e.indirect_dma_start(
                    out=self.xres[:, :],
                    out_offset=bass.IndirectOffsetOnAxis(ap=self.idxT[0:rows, ch, ex:ex + 1], axis=0),
                    in_=ysb[yb][0:rows, :], in_offset=None, compute_op=ALU.add),
                    reads=[u_ysb[yb], u_idxT], writes=[self.u_xres])
        P.phase_end()

    def epilogue(self):
        P = self.P
        for j in range(8):
            P.dma("sp", lambda e, j=j: e.dma_start(out=self.out[j * 512:(j + 1) * 512, :],
                                                    in_=self.xres[NCTX + j * 512:NCTX + (j + 1) * 512, :]),
                  reads=[self.u_xres], is_out=True)
        if self.cfg.get("dump_ctx"):
            oc = P.dram("out_ctx", [NCTX, D], F32, kind="ExternalOutput")
            P.dma("sp", lambda e: e.dma_start(out=oc[:, :], in_=self.xres[0:NCTX, :]), reads=[self.u_xres], is_out=True)
        return P.finish()
NEG = -30000.0


class KMix:
    def mix_scratch(self):
        if hasattr(self, "QTd"):
            return
        P = self.P
        self.QTd = P.dram("QTd", [1536, NT], BF16); self.u_QT = U("QTd")
        self.KTd = P.dram("KTd", [1024, NT], BF16); self.u_KT = U("KTd")
        self.KRd = P.dram("KRd", [32, NT], BF16); self.u_KR = U("KRd")
        self.Vd = P.dram("Vd", [NT, 1024], BF16); self.u_V = U("Vd")
        self.OTd = P.dram("OTd", [1024, NT], BF16); self.u_OT = U("OTd")

    def wload(self, name, src, kch, ncols):
        P = self.P
        t = P.psb(name, [128, kch, ncols], BF16); u = U(name)
        P.dma("pool", lambda e: e.dma_start(out=t[:], in_=src.rearrange("(k p) n -> p k n", p=128)), writes=[u])
        return t, u

    def bload(self, name, src_row, n, scale=None, parts=128):
        P = self.P
        t = P.psb(name, [parts, n], F32); u = U(name)
        P.dma("sp", lambda e: e.dma_start(out=t[:], in_=src_row.partition_broadcast(parts)[:, 0]), writes=[u])
        if scale is not None:
            P.op("dve", lambda e: e.tensor_scalar(out=t[:], in0=t[:], scalar1=float(scale), scalar2=None, op0=ALU.mult),
                 reads=[u], writes=[u])
        return t, u

    def norm_scratch(self):
        P = self.P
        sc = {"sq": P.psb("nsq", [128, 1536], F32), "u_sq": U("nsq"),
              "ss": P.psb("nss", [128, 16], F32), "u_ss": U("nss"),
              "r": [P.psb("rp", [128, 512], F32) for _ in range(4)], "u_r": [U("rp%d" % j) for j in range(4)]}
        return sc

    def headnorm(self, src, us, H, n, gb, ugb, out, uo, sc):
        P = self.P
        sq = sc["sq"][:, 0:H * n].rearrange("p (h n) -> p h n", n=n)
        ss = sc["ss"][:, 0:H]
        u_sq, u_ss = sc["u_sq"], sc["u_ss"]
        P.op("dve", lambda e: e.tensor_tensor(out=sq, in0=src, in1=src, op=ALU.mult), reads=[us], writes=[u_sq])
        P.op("dve", lambda e: e.tensor_reduce(out=ss, in_=sq, axis=AX.X, op=ALU.add), reads=[u_sq], writes=[u_ss])
        P.op("dve", lambda e: e.tensor_scalar(out=ss, in0=ss, scalar1=1.0 / n, scalar2=EPS, op0=ALU.mult, op1=ALU.add),
             reads=[u_ss], writes=[u_ss])
        P.op("act", lambda e: e.sqrt(out=ss, in_=ss), reads=[u_ss], writes=[u_ss])
        P.op("dve", lambda e: e.reciprocal(out=ss, in_=ss), reads=[u_ss], writes=[u_ss])
        P.op("dve", lambda e: e.tensor_tensor(out=sq, in0=src, in1=ss.unsqueeze(2).to_broadcast([128, H, n]), op=ALU.mult),
             reads=[us, u_ss], writes=[u_sq])
        P.op("dve", lambda e: e.tensor_tensor(out=out, in0=sq, in1=gb[:, 0:n].unsqueeze(1).to_broadcast([128, H, n]),
                                              op=ALU.mult), reads=[u_sq, ugb], writes=[uo])

    def rope(self, x, ux, H, half, cs, sn, ucs, sc):
        P = self.P
        x1 = x[:, :, 0:half]
        x2 = x[:, :, half:2 * half]
        cb = cs.unsqueeze(1).to_broadcast([128, H, half])
        sb = sn.unsqueeze(1).to_broadcast([128, H, half])
        t = [sc["r"][j][:, 0:H * half].rearrange("p (h n) -> p h n", n=half) for j in range(4)]
        ut = sc["u_r"]
        for j, (a, b) in enumerate(((x1, cb), (x2, sb), (x2, cb), (x1, sb))):
            P.op("dve", lambda e, j=j, a=a, b=b: e.tensor_tensor(out=t[j], in0=a, in1=b, op=ALU.mult),
                 reads=[ux, ucs], writes=[ut[j]])
        P.op("dve", lambda e: e.tensor_tensor(out=x1, in0=t[0], in1=t[1], op=ALU.subtract), reads=[ut[0], ut[1]], writes=[ux])
        P.op("dve", lambda e: e.tensor_tensor(out=x2, in0=t[2], in1=t[3], op=ALU.add), reads=[ut[2], ut[3]], writes=[ux])

    def tpose_to_dram(self, blocks, ub, rows, dst, udst, stage, ustage):
        P = self.P
        pb, upb = self.npsb()
        nb = len(blocks)
        for j, blk in enumerate(blocks):
            self.tr(pb[0:rows, j * 128:(j + 1) * 128], blk, self.ident_b[:], [ub, self.u_const], [upb])
        P.op("act", lambda e: e.copy(out=stage[0:rows, 0:nb, :], in_=pb[0:rows, 0:nb * 128].rearrange("p (j t) -> p j t", t=128)),
             reads=[upb], writes=[ustage])
        P.dma("sp", lambda e: e.dma_start(out=dst, in_=stage[0:rows, 0:nb, :]), reads=[ustage], writes=[udst])

    def attn_setup(self):
        P = self.P
        a = {"pt": [P.psb("pt", [128, 512], BF16) for _ in range(4)], "u_pt": [U("pt%d" % j) for j in range(4)], "nblk": 0,
             "tmp": [P.psb("tmpb", [128, 512], F32) for _ in range(2)], "u_tmp": [U("tmp0"), U("tmp1")],
             "rd": P.psb("rd", [64, 512], F32), "u_rd": U("rd"), "n": 0}
        return a

    def attn_block(self, a, rhs_q, uq, nq, dk, keys, out_ap, uout, sink_fn=None, qg=1):
        P = self.P
        LA = 2
        pso, upso = self.psf[4][0:64, 0:nq], self.u_psf[4]
        psd, upsd = self.psf[5][0:64, 0:nq], self.u_psf[5]
        n_k = len(keys)
        last = n_k - 1
        pend = {}
        for j in range(n_k + LA):
            if j < n_k:
                (kt, uk, v, uv, nk, bias, ubias) = keys[j]
                ps, ups = self.nps()
                so = ps[0:nk, 0:nq] if qg == 1 else ps[0:nk, 0:nq].rearrange("p (g t) -> p g t", g=qg)
                self.mm(so, kt, rhs_q, True, True, [uk, uq], [ups])
                n = a["n"]; a["n"] += 1
                pt, upt = a["pt"][n % 4], a["u_pt"][n % 4]
                if bias is not None:
                    tmp, utmp = a["tmp"][n % 2], a["u_tmp"][n % 2]
                    P.op("dve", lambda e, ps=ps, nk=nk, tmp=tmp, bias=bias: e.tensor_tensor(
                        out=tmp[0:nk, 0:nq], in0=ps[0:nk, 0:nq], in1=bias, op=ALU.add), reads=[ups, ubias], writes=[utmp])
                    P.op("act", lambda e, nk=nk, tmp=tmp, pt=pt: e.activation(out=pt[0:nk, 0:nq], in_=tmp[0:nk, 0:nq], func=AF.Exp),
                         reads=[utmp], writes=[upt])
                else:
                    P.op("act", lambda e, ps=ps, nk=nk, pt=pt: e.activation(out=pt[0:nk, 0:nq], in_=ps[0:nk, 0:nq], func=AF.Exp),
                         reads=[ups], writes=[upt])
                pend[j] = (pt, upt)
            jj = j - LA
            if jj >= 0:
                (kt, uk, v, uv, nk, bias, ubias) = keys[jj]
                pt, upt = pend.pop(jj)
                self.mm(pso, v, pt[0:nk, 0:nq], jj == 0, jj == last, [uv, upt], [upso])
                self.mm(psd, self.ones_b[0:nk, 0:64], pt[0:nk, 0:nq], jj == 0, jj == last, [upt, self.u_const], [upsd])
        rd, urd = a["rd"], a["u_rd"]
        if sink_fn is not None:
            sink_fn(psd, upsd, rd, urd)
        else:
            P.op("dve", lambda e: e.tensor_copy(out=rd[:, 0:nq], in_=psd), reads=[upsd], writes=[urd])
        P.op("dve", lambda e: e.reciprocal(out=rd[:, 0:nq], in_=rd[:, 0:nq]), reads=[urd], writes=[urd])
        if qg == 1:
            o_in, r_in = pso, rd[:, 0:nq]
        else:
            o_in = pso.rearrange("p (g t) -> p g t", g=qg)
            r_in = rd[:, 0:nq].rearrange("p (g t) -> p g t", g=qg)
        P.op("dve", lambda e: e.tensor_tensor(out=out_ap, in0=o_in, in1=r_in, op=ALU.mult),
             reads=[upso, urd], writes=[uout])

    def outproj_phase(self, i, wname, do_ctx):
        P = self.P
        wsrc = self.I(wname)
        P.phase_begin()
        wo, uwo = self.wload("wo", wsrc, 8, D)
        for k in range(8):
            P.dma("sp", lambda e, k=k: e.dma_start(out=self.hT[:, k, :], in_=self.OTd[k * 128:(k + 1) * 128, :]),
                  reads=[self.u_OT], writes=self.u_hT)
        G = P.psb("G1", [128, 2, 8, 128], F32); u_G = U("G1")
        for s in range(2):
            P.dma("sp", lambda e, s=s: e.dma_start(out=G[:, s], in_=self.gate_bcast_src(i, 0, s)),
                  reads=[self.u_modd], writes=[u_G])
        xt = [P.psb("xto", [128, D], F32) for _ in range(2)]; u_xt = [U("xto0"), U("xto1")]
        tmp = [P.psb("tmpo", [128, 512], F32) for _ in range(2)]; u_tmp = [U("tmpo0"), U("tmpo1")]
        tiles = list(range(NTT)) if do_ctx else list(range(2, NTT))
        for n, tt in enumerate(tiles):
            s = 1 if tt < 2 else 0
            b = n % 2
            P.dma("sp", lambda e, b=b, tt=tt: e.dma_start(out=xt[b][:], in_=self.xres[tt * 128:(tt + 1) * 128, :]),
                  reads=[self.u_xres], writes=[u_xt[b]])
            for half in range(2):
                ps, ups = self.nps()
                for k in range(8):
                    self.mm(ps[:, :], self.hT[:, k, tt * 128:(tt + 1) * 128], wo[:, k, half * 512:(half + 1) * 512],
                            k == 0, k == 7, [self.u_hT[tt], uwo], [ups])
                P.op("dve", lambda e, ps=ps, half=half, s=s: e.tensor_tensor(
                    out=tmp[half][:], in0=ps[:, :], in1=G[:, s, half * 4:(half + 1) * 4, :].rearrange("p a b -> p (a b)"),
                    op=ALU.mult), reads=[ups, u_G], writes=[u_tmp[half]])
                P.op("dve", lambda e, half=half, b=b: e.tensor_tensor(
                    out=xt[b][:, half * 512:(half + 1) * 512], in0=xt[b][:, half * 512:(half + 1) * 512],
                    in1=tmp[half][:], op=ALU.add), reads=[u_tmp[half], u_xt[b]], writes=[u_xt[b]])
            P.dma("pool", lambda e, b=b, tt=tt: e.dma_start(out=self.xres[tt * 128:(tt + 1) * 128, :], in_=xt[b][:]),
                  reads=[u_xt[b]], writes=[self.u_xres])
        P.phase_end()

    def mla_proj(self, i):
        P = self.P
        I = self.I
        for nm in ("mla_w_down", "mla_q_norm_g", "mla_kv_norm_g", "mla_w_uq", "mla_w_ukv", "mla_qn_g", "mla_qr_g",
                   "mla_kn_g", "mla_kr_g", "mla_cos", "mla_sin"):
            I(nm)
        scale = 96.0 ** -0.5
        P.phase_begin()
        wd, uwd = self.wload("wd", I("mla_w_down"), 8, 672)
        wuq, uwuq = self.wload("wuq", I("mla_w_uq"), 3, 1536)
        wukv, uwukv = self.wload("wukv", I("mla_w_ukv"), 2, 2048)
        gq, ugq = self.bload("gq", I("mla_q_norm_g")[0:1, :], 384)
        gkv, ugkv = self.bload("gkv", I("mla_kv_norm_g")[0:1, :], 256)
        gkr, ugkr = self.bload("gkr", I("mla_kr_g")[0:1, :], 32)
        gqn, ugqn = self.bload("gqn", I("mla_qn_g")[0:1, :], 64, scale=scale)
        gqr, ugqr = self.bload("gqr", I("mla_qr_g")[0:1, :], 32, scale=scale)
        gkn, ugkn = self.bload("gkn", I("mla_kn_g")[0:1, :], 64)
        sc = self.norm_scratch()
        cqT = P.psb("cqT", [128, 3, NT], BF16); u_cqT = [U("cqT%d" % t) for t in range(NTT)]
        ckvT = P.psb("ckvT", [128, 2, NT], BF16); u_ckvT = [U("ckvT%d" % t) for t in range(NTT)]
        df = P.psb("df", [128, 672], F32); u_df = U("df")
        dn = P.psb("dn", [128, 672], F32); u_dn = U("dn")
        db = P.psb("db", [128, 768], BF16); u_db = U("db")
        cs = [P.psb("cs", [128, 16], F32) for _ in range(2)]; sn = [P.psb("sn", [128, 16], F32) for _ in range(2)]
        u_cs = [U("cs0"), U("cs1")]
        krs = P.psb("krs", [32, 1, 128], BF16); u_krs = U("krs")
        P.op("pool", lambda e: e.memset(db[:], 0.0), writes=[u_db])
        for tt in range(NTT):
            lat = tt >= 2
            ps0, up0 = self.nps()
            ps1, up1 = self.nps()
            for k in range(8):
                self.mm(ps0[:, 0:512], self.hT[:, k, tt * 128:(tt + 1) * 128], wd[:, k, 0:512], k == 0, k == 7,
                        [self.u_hT[tt], uwd], [up0])
            for k in range(8):
                self.mm(ps1[:, 0:160], self.hT[:, k, tt * 128:(tt + 1) * 128], wd[:, k, 512:672], k == 0, k == 7,
                        [self.u_hT[tt], uwd], [up1])
            P.op("act", lambda e, ps0=ps0: e.copy(out=df[:, 0:512], in_=ps0[:, 0:512]), reads=[up0], writes=[u_df])
            P.op("act", lambda e, ps1=ps1: e.copy(out=df[:, 512:672], in_=ps1[:, 0:160]), reads=[up1], writes=[u_df])
            for (c0, n, g, ug) in ((0, 384, gq, ugq), (384, 256, gkv, ugkv), (640, 32, gkr, ugkr)):
                self.headnorm(df[:, c0:c0 + n].unsqueeze(1), u_df, 1, n, g, ug, dn[:, c0:c0 + n].unsqueeze(1), u_dn, sc)
            if lat:
                b = tt % 2
                t0 = (tt - 2) * 128
                P.dma("sp", lambda e, b=b, t0=t0: e.dma_start(out=cs[b][:], in_=I("mla_cos")[t0:t0 + 128, :]), writes=[u_cs[b]])
                P.dma("sp", lambda e, b=b, t0=t0: e.dma_start(out=sn[b][:], in_=I("mla_sin")[t0:t0 + 128, :]), writes=[u_cs[b]])
                self.rope(dn[:, 640:672].unsqueeze(1), u_dn, 1, 16, cs[b][:], sn[b][:], u_cs[b], sc)
            P.op("act", lambda e: e.copy(out=db[:, 0:672], in_=dn[:, 0:672]), reads=[u_dn], writes=[u_db])
            pb, upb = self.npsb()
            for j in range(5):
                self.tr(pb[:, j * 128:(j + 1) * 128], db[:, j * 128:(j + 1) * 128], self.ident_b[:], [u_db, self.u_const], [upb])
            self.tr(pb[:, 640:768], db[:, 640:768], self.ident_b[:], [u_db, self.u_const], [upb])
            P.op("act", lambda e, pb=pb, tt=tt: e.copy(out=cqT[:, :, tt * 128:(tt + 1) * 128],
                                                       in_=pb[:, 0:384].rearrange("p (j t) -> p j t", t=128)),
                 reads=[upb], writes=[u_cqT[tt]])
            P.op("act", lambda e, pb=pb, tt=tt: e.copy(out=ckvT[:, :, tt * 128:(tt + 1) * 128],
                                                       in_=pb[:, 384:640].rearrange("p (j t) -> p j t", t=128)),
                 reads=[upb], writes=[u_ckvT[tt]])
            P.op("act", lambda e, pb=pb: e.copy(out=krs[:, 0, :], in_=pb[0:32, 640:768]), reads=[upb], writes=[u_krs])
            P.dma("sp", lambda e, tt=tt: e.dma_start(out=self.KRd[:, tt * 128:(tt + 1) * 128], in_=krs[:, 0, :]),
                  reads=[u_krs], writes=[self.u_KR])
        qf = P.psb("qf", [128, 16, 96], F32); u_qf = U("qf")
        qn = P.psb("qn", [128, 16, 96], F32); u_qn = U("qn")
        qb = P.psb("qb", [128, 16, 96], BF16); u_qb = U("qb")
        kvf = P.psb("kvf", [128, 16, 128], F32); u_kvf = U("kvf")
        knf = P.psb("knf", [128, 16, 64], F32); u_knf = U("knf")
        knb = P.psb("knb", [128, 16, 64], BF16); u_knb = U("knb")
        vb = P.psb("vb", [128, 16, 64], BF16); u_vb = U("vb")
        stq = [P.psb("stq", [96, 8, 128], BF16) for _ in range(2)]; u_stq = [U("stq0"), U("stq1")]
        stk = P.psb("stk", [128, 8, 128], BF16); u_stk = U("stk")
        qflat = qf[:].rearrange("p h n -> p (h n)")
        kvflat = kvf[:].rearrange("p h n -> p (h n)")
        for tt in range(NTT):
            lat = tt >= 2
            for cc in range(3):
                ps, ups = self.nps()
                for k in range(3):
                    self.mm(ps[:, :], cqT[:, k, tt * 128:(tt + 1) * 128], wuq[:, k, cc * 512:(cc + 1) * 512], k == 0, k == 2,
                            [u_cqT[tt], uwuq], [ups])
                P.op("act", lambda e, ps=ps, cc=cc: e.copy(out=qflat[:, cc * 512:(cc + 1) * 512], in_=ps[:, :]),
                     reads=[ups], writes=[u_qf])
            self.headnorm(qf[:, :, 0:64], u_qf, 16, 64, gqn, ugqn, qn[:, :, 0:64], u_qn, sc)
            self.headnorm(qf[:, :, 64:96], u_qf, 16, 32, gqr, ugqr, qn[:, :, 64:96], u_qn, sc)
            if lat:
                b = tt % 2
                t0 = (tt - 2) * 128
                P.dma("sp", lambda e, b=b, t0=t0: e.dma_start(out=cs[b][:], in_=I("mla_cos")[t0:t0 + 128, :]), writes=[u_cs[b]])
                P.dma("sp", lambda e, b=b, t0=t0: e.dma_start(out=sn[b][:], in_=I("mla_sin")[t0:t0 + 128, :]), writes=[u_cs[b]])
                self.rope(qn[:, :, 64:96], u_qn, 16, 16, cs[b][:], sn[b][:], u_cs[b], sc)
            P.op("act", lambda e: e.copy(out=qb[:], in_=qn[:]), reads=[u_qn], writes=[u_qb])
            for hh in range(2):
                self.tpose_to_dram([qb[:, hh * 8 + j, :] for j in range(8)], u_qb, 96,
                                   self.QTd[hh * 768:(hh + 1) * 768, tt * 128:(tt + 1) * 128].rearrange("(j d) t -> d j t", d=96),
                                   self.u_QT, stq[hh], u_stq[hh])
            for cc in range(4):
                ps, ups = self.nps()
                for k in range(2):
                    self.mm(ps[:, :], ckvT[:, k, tt * 128:(tt + 1) * 128], wukv[:, k, cc * 512:(cc + 1) * 512], k == 0, k == 1,
                            [u_ckvT[tt], uwukv], [ups])
                P.op("act", lambda e, ps=ps, cc=cc: e.copy(out=kvflat[:, cc * 512:(cc + 1) * 512], in_=ps[:, :]),
                     reads=[ups], writes=[u_kvf])
            self.headnorm(kvf[:, :, 0:64], u_kvf, 16, 64, gkn, ugkn, knf[:], u_knf, sc)
            P.op("act", lambda e: e.copy(out=knb[:], in_=knf[:]), reads=[u_knf], writes=[u_knb])
            P.op("pool", lambda e: e.tensor_copy(out=vb[:], in_=kvf[:, :, 64:128]), reads=[u_kvf], writes=[u_vb])
            P.dma("sp", lambda e, tt=tt: e.dma_start(out=self.Vd[tt * 128:(tt + 1) * 128, :].rearrange("t (h n) -> t h n", n=64),
                                                     in_=vb[:]), reads=[u_vb], writes=[self.u_V])
            knb2 = knb[:].rearrange("p h n -> p (h n)")
            self.tpose_to_dram([knb2[:, j * 128:(j + 1) * 128] for j in range(8)], u_knb, 128,
                               self.KTd[:, tt * 128:(tt + 1) * 128].rearrange("(j p) t -> p j t", p=128),
                               self.u_KT, stk, u_stk)
        P.phase_end()

    def mla_attn(self, do_ctx):
        P = self.P
        P.phase_begin()
        a = self.attn_setup()
        QT = [P.psb("QTh", [96, NT], BF16) for _ in range(2)]; uQ = [U("QTh0"), U("QTh1")]
        KT = [P.psb("KTh", [96, NT], BF16) for _ in range(2)]; uK = [U("KTh0"), U("KTh1")]
        V = [P.psb("Vh", [128, NTT, 64], BF16) for _ in range(2)]; uV = [U("Vh0"), U("Vh1")]
        OS = [P.psb("OSh", [64, NT], BF16) for _ in range(2)]; uOS = [U("OSh0"), U("OSh1")]
        for h in range(16):
            b = h % 2
            P.dma("sp", lambda e, b=b, h=h: e.dma_start(out=QT[b][:], in_=self.QTd[h * 96:(h + 1) * 96, :]),
                  reads=[self.u_QT], writes=[uQ[b]])
            P.dma("sp", lambda e, b=b, h=h: e.dma_start(out=KT[b][0:64, :], in_=self.KTd[h * 64:(h + 1) * 64, :]),
                  reads=[self.u_KT], writes=[uK[b]])
            P.dma("sp", lambda e, b=b: e.dma_start(out=KT[b][64:96, :], in_=self.KRd[:, :]), reads=[self.u_KR], writes=[uK[b]])
            P.dma("sp", lambda e, b=b, h=h: e.dma_start(
                out=V[b][:], in_=self.Vd[:, h * 64:(h + 1) * 64].rearrange("(t p) c -> p t c", p=128)),
                reads=[self.u_V], writes=[uV[b]])
            blocks = [(NCTX + c * 512, 512, list(range(NTT))) for c in range(8)]
            if do_ctx:
                blocks.append((0, NCTX, [0, 1]))
            for (q0, nq, kts) in blocks:
                keys = [(KT[b][:, kt * 128:(kt + 1) * 128], uK[b], V[b][:, kt, :], uV[b], 128, None, None) for kt in kts]
                self.attn_block(a, QT[b][:, q0:q0 + nq], uQ[b], nq, 96, keys, OS[b][:, q0:q0 + nq], uOS[b])
            c0 = 0 if do_ctx else NCTX
            P.dma("pool", lambda e, b=b, h=h, c0=c0: e.dma_start(out=self.OTd[h * 64:(h + 1) * 64, c0:NT], in_=OS[b][:, c0:NT]),
                  reads=[uOS[b]], writes=[self.u_OT])
        P.phase_end()


    def qkv_proj(self, wname, Hk, gqname, gkname, scale, rope=None):
        P = self.P
        I = self.I
        for nm in (wname, gqname, gkname) + (tuple(rope) if rope else ()):
            I(nm)
        ncol = 1024 + 2 * Hk * 64
        P.phase_begin()
        w, uw = self.wload("wqkv", I(wname), 8, ncol)
        gq, ugq = self.bload("gq", I(gqname)[0:1, :], 64, scale=scale)
        gk, ugk = self.bload("gk", I(gkname)[0:1, :], 64)
        sc = self.norm_scratch()
        qf = P.psb("qf", [128, ncol], F32); u_qf = U("qf")
        qn = P.psb("qn", [128, 16, 64], F32); u_qn = U("qn")
        kn = P.psb("kn", [128, Hk, 64], F32); u_kn = U("kn")
        qb = P.psb("qb", [128, 1024], BF16); u_qb = U("qb")
        kb = P.psb("kb", [128, Hk * 64], BF16); u_kb = U("kb")
        vb = P.psb("vb", [128, Hk * 64], BF16); u_vb = U("vb")
        stq = P.psb("stq", [128, 8, 128], BF16); u_stq = U("stq")
        stk = P.psb("stk", [128, 8, 128], BF16); u_stk = U("stk")
        if rope:
            cs = [P.psb("cs", [128, 32], F32) for _ in range(2)]; sn = [P.psb("sn", [128, 32], F32) for _ in range(2)]
            u_cs = [U("cs0"), U("cs1")]
        nb = ncol // 512
        kc0 = 1024
        vc0 = 1024 + Hk * 64
        for tt in range(NTT):
            lat = tt >= 2
            for cc in range(nb):
                ps, ups = self.nps()
                for k in range(8):
                    self.mm(ps[:, :], self.hT[:, k, tt * 128:(tt + 1) * 128], w[:, k, cc * 512:(cc + 1) * 512], k == 0, k == 7,
                            [self.u_hT[tt], uw], [ups])
                P.op("act", lambda e, ps=ps, cc=cc: e.copy(out=qf[:, cc * 512:(cc + 1) * 512], in_=ps[:, :]),
                     reads=[ups], writes=[u_qf])
            self.headnorm(qf[:, 0:1024].rearrange("p (h n) -> p h n", n=64), u_qf, 16, 64, gq, ugq, qn[:], u_qn, sc)
            self.headnorm(qf[:, kc0:kc0 + Hk * 64].rearrange("p (h n) -> p h n", n=64), u_qf, Hk, 64, gk, ugk, kn[:], u_kn, sc)
            if rope and lat:
                b = tt % 2
                t0 = (tt - 2) * 128
                P.dma("sp", lambda e, b=b, t0=t0: e.dma_start(out=cs[b][:], in_=I(rope[0])[t0:t0 + 128, :]), writes=[u_cs[b]])
                P.dma("sp", lambda e, b=b, t0=t0: e.dma_start(out=sn[b][:], in_=I(rope[1])[t0:t0 + 128, :]), writes=[u_cs[b]])
                self.rope(qn[:], u_qn, 16, 32, cs[b][:], sn[b][:], u_cs[b], sc)
                self.rope(kn[:], u_kn, Hk, 32, cs[b][:], sn[b][:], u_cs[b], sc)
            P.op("act", lambda e: e.copy(out=qb[:], in_=qn[:].rearrange("p h n -> p (h n)")), reads=[u_qn], writes=[u_qb])
            P.op("act", lambda e: e.copy(out=kb[:], in_=kn[:].rearrange("p h n -> p (h n)")), reads=[u_kn], writes=[u_kb])
            P.op("pool", lambda e: e.tensor_copy(out=vb[:], in_=qf[:, vc0:vc0 + Hk * 64]), reads=[u_qf], writes=[u_vb])
            P.dma("sp", lambda e, tt=tt: e.dma_start(out=self.Vd[tt * 128:(tt + 1) * 128, 0:Hk * 64], in_=vb[:]),
                  reads=[u_vb], writes=[self.u_V])
            self.tpose_to_dram([qb[:, j * 128:(j + 1) * 128] for j in range(8)], u_qb, 128,
                               self.QTd[0:1024, tt * 128:(tt + 1) * 128].rearrange("(j p) t -> p j t", p=128),
                               self.u_QT, stq, u_stq)
            nkb = Hk * 64 // 128
            self.tpose_to_dram([kb[:, j * 128:(j + 1) * 128] for j in range(nkb)], u_kb, 128,
                               self.KTd[0:Hk * 64, tt * 128:(tt + 1) * 128].rearrange("(j p) t -> p j t", p=128),
                               self.u_KT, stk, u_stk)
        P.phase_end()

    def na_proj(self, i):
        self.qkv_proj("na_w_qkv", 16, "na_q_g", "na_k_g", 64.0 ** -0.5)

    def swa_proj(self, i):
        self.qkv_proj("swa_w_qkv", 4, "swa_q_g", "swa_k_g", 64.0 ** -0.5, rope=("swa_cos", "swa_sin"))

    def swa_attn(self, do_ctx):
        P = self.P
        I = self.I
        I("swa_sink")
        P.phase_begin()
        a = self.attn_setup()
        Mp = P.psb("Mp", [128, 4, 128], F32); Mn = P.psb("Mn", [128, 4, 128], F32); u_M = U("M")
        P.op("pool", lambda e: e.memset(Mp[:], 0.0), writes=[u_M])
        P.op("pool", lambda e: e.memset(Mn[:], 0.0), writes=[u_M])
        P.op("pool", lambda e: e.affine_select(out=Mp[:], in_=Mp[:], pattern=[[0, 4], [-1, 128]], compare_op=ALU.is_ge,
                                               fill=NEG, base=0, channel_multiplier=1), reads=[u_M], writes=[u_M])
        P.op("pool", lambda e: e.affine_select(out=Mn[:], in_=Mn[:], pattern=[[0, 4], [1, 128]], compare_op=ALU.is_ge,
                                               fill=NEG, base=0, channel_multiplier=-1), reads=[u_M], writes=[u_M])
        Mp2 = Mp[:].rearrange("p g t -> p (g t)")
        Mn2 = Mn[:].rearrange("p g t -> p (g t)")
        esink, u_es = self.bload("esink", I("swa_sink")[0:1, :], 16, parts=64)
        P.op("act", lambda e: e.activation(out=esink[:], in_=esink[:], func=AF.Exp), reads=[u_es], writes=[u_es])
        Q = P.psb("Qall", [64, 4, NT], BF16); uQ = U("Qall")
        KT = P.psb("KTh", [64, NT], BF16); uK = U("KTh")
        V = P.psb("Vh", [128, NTT, 64], BF16); uV = U("Vh")
        OS = P.psb("OSh", [64, 4, NT], BF16); uOS = U("OSh")
        for hk in range(4):
            P.dma("sp", lambda e, hk=hk: e.dma_start(out=Q[:], in_=self.QTd[hk * 256:(hk + 1) * 256, :].rearrange("(g d) t -> d g t", d=64)),
                  reads=[self.u_QT], writes=[uQ])
            P.dma("sp", lambda e, hk=hk: e.dma_start(out=KT[:], in_=self.KTd[hk * 64:(hk + 1) * 64, :]),
                  reads=[self.u_KT], writes=[uK])
            P.dma("sp", lambda e, hk=hk: e.dma_start(out=V[:], in_=self.Vd[:, hk * 64:(hk + 1) * 64].rearrange("(t p) c -> p t c", p=128)),
                  reads=[self.u_V], writes=[uV])

            def sink_fn(psd, upsd, rd, urd, hk=hk):
                for g in range(4):
                    P.op("dve", lambda e, g=g: e.tensor_scalar(out=rd[:, g * 128:(g + 1) * 128], in0=psd[:, g * 128:(g + 1) * 128],
                                                                scalar1=esink[:, hk * 4 + g:hk * 4 + g + 1], scalar2=None, op0=ALU.add),
                         reads=[upsd, u_es], writes=[urd])
            tiles = list(range(2, NTT)) + ([0, 1] if do_ctx else [])
            for tile in tiles:
                def key(kt, bias):
                    return (KT[:, kt * 128:(kt + 1) * 128], uK, V[:, kt, :], uV, 128, bias, u_M)
                keys = [key(0, None), key(1, None)]
                if tile >= 2:
                    if tile > 2:
                        keys.append(key(tile - 1, Mp2))
                    keys.append(key(tile, None))
                    if tile < NTT - 1:
                        keys.append(key(tile + 1, Mn2))
                self.attn_block(a, Q[:, :, tile * 128:(tile + 1) * 128], uQ, 512, 64, keys,
                                OS[:, :, tile * 128:(tile + 1) * 128], uOS, sink_fn=sink_fn, qg=4)
            c0 = 0 if do_ctx else NCTX
            P.dma("pool", lambda e, hk=hk, c0=c0: e.dma_start(
                out=self.OTd[hk * 256:(hk + 1) * 256, c0:NT].rearrange("(g d) t -> d g t", d=64), in_=OS[:, :, c0:NT]),
                reads=[uOS], writes=[self.u_OT])
        P.phase_end()

    def na_attn(self, do_ctx):
        P = self.P
        I = self.I
        I("na_bias")
        P.phase_begin()
        a = self.attn_setup()
        QT = [P.psb("QTh", [64, NT], BF16) for _ in range(2)]; uQ = [U("QTh0"), U("QTh1")]
        KT = [P.psb("KTh", [64, NT], BF16) for _ in range(2)]; uK = [U("KTh0"), U("KTh1")]
        V = [P.psb("Vh", [128, NTT, 64], BF16) for _ in range(2)]; uV = [U("Vh0"), U("Vh1")]
        Vs = [P.psb("Vsh", [128, NTT - 1, 64], BF16) for _ in range(2)]
        B = [P.psb("Bh", [128, 14, 64], F32) for _ in range(2)]; uB = [U("Bh0"), U("Bh1")]
        OS = [P.psb("OSh", [64, NT], BF16) for _ in range(2)]; uOS = [U("OSh0"), U("OSh1")]
        for h in range(16):
            b = h % 2
            P.dma("sp", lambda e, b=b, h=h: e.dma_start(out=QT[b][:], in_=self.QTd[h * 64:(h + 1) * 64, :]),
                  reads=[self.u_QT], writes=[uQ[b]])
            P.dma("sp", lambda e, b=b, h=h: e.dma_start(out=KT[b][:], in_=self.KTd[h * 64:(h + 1) * 64, :]),
                  reads=[self.u_KT], writes=[uK[b]])
            P.dma("sp", lambda e, b=b, h=h: e.dma_start(
                out=V[b][:], in_=self.Vd[:, h * 64:(h + 1) * 64].rearrange("(t p) c -> p t c", p=128)),
                reads=[self.u_V], writes=[uV[b]])
            P.dma("sp", lambda e, b=b, h=h: e.dma_start(
                out=Vs[b][:], in_=self.Vd[64:64 + (NTT - 1) * 128, h * 64:(h + 1) * 64].rearrange("(t p) c -> p t c", p=128)),
                reads=[self.u_V], writes=[uV[b]])
            P.dma("sp", lambda e, b=b, h=h: e.dma_start(out=B[b][:], in_=I("na_bias")[h]), writes=[uB[b]])
            for r in range(64):
                q0 = NCTX + r * 64
                r0 = min(max(r - 4, 0), 56)
                keys = [(KT[b][:, 0:128], uK[b], V[b][:, 0, :], uV[b], 128, None, None),
                        (KT[b][:, 128:256], uK[b], V[b][:, 1, :], uV[b], 128, None, None)]
                for j in range(4):
                    krow = r0 + 2 * j
                    tok = NCTX + krow * 64
                    vv = V[b][:, tok // 128, :] if tok % 128 == 0 else Vs[b][:, (tok - 64) // 128, :]
                    keys.append((KT[b][:, tok:tok + 128], uK[b], vv, uV[b], 128, B[b][:, krow - r + 7, :], uB[b]))
                self.attn_block(a, QT[b][:, q0:q0 + 64], uQ[b], 64, 64, keys, OS[b][:, q0:q0 + 64], uOS[b])
            if do_ctx:
                keys = [(KT[b][:, 0:128], uK[b], V[b][:, 0, :], uV[b], 128, None, None),
                        (KT[b][:, 128:256], uK[b], V[b][:, 1, :], uV[b], 128, None, None)]
                self.attn_block(a, QT[b][:, 0:NCTX], uQ[b], NCTX, 64, keys, OS[b][:, 0:NCTX], uOS[b])
            c0 = 0 if do_ctx else NCTX
            P.dma("pool", lambda e, b=b, h=h, c0=c0: e.dma_start(out=self.OTd[h * 64:(h + 1) * 64, c0:NT], in_=OS[b][:, c0:NT]),
                  reads=[uOS[b]], writes=[self.u_OT])
        P.phase_end()

    def mixer(self, i, do_ctx):
        self.mix_scratch()
        m = i % 4
        self.norm_phase(i, 0, False)
        if m == 0:
            self.na_proj(i); self.na_attn(do_ctx); self.outproj_phase(i, "na_w_o", do_ctx)
        elif m == 1:
            self.rwkv(i, do_ctx)
        elif m == 2:
            self.mla_proj(i); self.mla_attn(do_ctx); self.outproj_phase(i, "mla_w_o", do_ctx)
        else:
            self.swa_proj(i); self.swa_attn(do_ctx); self.outproj_phase(i, "swa_w_o", do_ctx)


for _n, _f in list(vars(KMix).items()):
    if callable(_f):
        setattr(K, _n, _f)
C0 = -0.6065306597126334


class KRw:
    def rw_scratch(self):
        if hasattr(self, "rwd"):
            return
        P = self.P
        self.rwd = {}
        self.u_rwd = {}
        for nm in ("R", "K", "KK", "V", "G", "LW0", "LW1", "B0", "B1", "KT0", "KT1", "Y"):
            self.rwd[nm] = P.dram("rw_" + nm, [NT, D], F32)
            self.u_rwd[nm] = U("rw_" + nm)

    def rw_xs(self, tt, S, u_S, xx, u_xx, tmp, u_tmp, xs, u_xs, streams, mixT, u_mixT):
        P = self.P
        h = self.hT
        t0 = tt * 128
        noprev = tt in (0, 2)
        nonext = tt in (1, NTT - 1)
        a = 1 if noprev else 0
        b = 127 if nonext else 128
        uh = [self.u_hT[tt]]
        P.op("dve", lambda e: e.tensor_tensor(out=S[:, :, a:b], in0=h[:, :, t0 - 1 + a:t0 - 1 + b],
                                              in1=h[:, :, t0 + 1 + a:t0 + 1 + b], op=ALU.add), reads=uh, writes=[u_S])
        if noprev:
            P.op("dve", lambda e: e.tensor_copy(out=S[:, :, 0:1], in_=h[:, :, t0 + 1:t0 + 2]), reads=uh, writes=[u_S])
        if nonext:
            P.op("dve", lambda e: e.tensor_copy(out=S[:, :, 127:128], in_=h[:, :, t0 + 126:t0 + 127]), reads=uh, writes=[u_S])
        P.op("dve", lambda e: e.scalar_tensor_tensor(out=xx[:], in0=S[:], scalar=0.5, in1=h[:, :, t0:t0 + 128],
                                                     op0=ALU.mult, op1=ALU.subtract), reads=[u_S] + uh, writes=[u_xx])
        for n, s in enumerate(streams):
            tb = n % 2
            P.op("pool", lambda e, s=s, tb=tb: e.tensor_tensor(
                out=tmp[tb][:], in0=xx[:], in1=mixT[:, s * 8:(s + 1) * 8].unsqueeze(2).to_broadcast([128, 8, 128]),
                op=ALU.mult), reads=[u_xx, u_mixT], writes=[u_tmp[tb]])
            P.op("dve", lambda e, n=n, tb=tb: e.tensor_tensor(out=xs[n][:], in0=tmp[tb][:], in1=h[:, :, t0:t0 + 128], op=ALU.add),
                 reads=[u_tmp[tb]] + uh, writes=[u_xs[n]])

    def rw_common(self):
        P = self.P
        I = self.I
        m48 = P.psb("m48", [48, 128], F32); u_m48 = U("m48")
        mixT = P.psb("mixT", [128, 48], F32); u_mixT = U("mixT")
        P.dma("sp", lambda e: e.dma_start(out=m48[:], in_=I("rw_mix")[:, :]), writes=[u_m48])
        ps, ups = self.nps()
        self.tr(ps[:, 0:48], m48[:], self.ident_f[0:48, 0:48], [u_m48, self.u_const], [ups])
        P.op("dve", lambda e: e.tensor_copy(out=mixT[:], in_=ps[:, 0:48]), reads=[ups], writes=[u_mixT])
        S = P.psb("S", [128, 8, 128], F32); xx = P.psb("xx", [128, 8, 128], F32)
        tmp = [P.psb("xtmp", [128, 8, 128], F32) for _ in range(2)]
        xs = [P.psb("xsT", [128, 8, 128], BF16) for _ in range(3)]
        return dict(mixT=mixT, u_mixT=u_mixT, S=S, u_S=U("S"), xx=xx, u_xx=U("xx"), tmp=tmp, u_tmp=[U("xt0"), U("xt1")],
                    xs=xs, u_xs=[U("xs0"), U("xs1"), U("xs2")])

    def rw_out(self, ob, u_ob, n, name, tt):
        b = n % len(ob)
        self.P.dma("pool", lambda e: e.dma_start(out=self.rwd[name][tt * 128:(tt + 1) * 128, :], in_=ob[b][:]),
                   reads=[u_ob[b]], writes=[self.u_rwd[name]])

    def rw_proj_a(self):
        P = self.P
        I = self.I
        for nm in ("rw_mix", "rw_w_r", "rw_w_k", "rw_w_v", "rw_k_k"):
            I(nm)
        P.phase_begin()
        c = self.rw_common()
        W = {}
        for nm in ("rw_w_r", "rw_w_k", "rw_w_v"):
            W[nm] = self.wload(nm, I(nm), 8, D)
        kkb, u_kkb = self.bload("kkb", I("rw_k_k")[0:1, :], D)
        ob = [P.psb("ob", [128, D], F32) for _ in range(5)]; u_ob = [U("ob%d" % j) for j in range(5)]
        ss = P.psb("ss", [128, 16], F32); u_ss = U("ss")
        sq = P.psb("sq", [128, D], F32); u_sq = U("sq")
        n = 0
        for tt in range(NTT):
            self.rw_xs(tt, c["S"], c["u_S"], c["xx"], c["u_xx"], c["tmp"], c["u_tmp"], c["xs"], c["u_xs"], (0, 2, 3),
                       c["mixT"], c["u_mixT"])
            for si, (nm, dst) in enumerate((("rw_w_r", "R"), ("rw_w_k", "K"), ("rw_w_v", "V"))):
                w, uw = W[nm]
                b = n % 5
                for half in range(2):
                    ps, ups = self.nps()
                    for k in range(8):
                        self.mm(ps[:, :], c["xs"][si][:, k, :], w[:, k, half * 512:(half + 1) * 512], k == 0, k == 7,
                                [c["u_xs"][si], uw], [ups])
                    P.op("act", lambda e, ps=ps, b=b, half=half: e.copy(out=ob[b][:, half * 512:(half + 1) * 512], in_=ps[:, :]),
                         reads=[ups], writes=[u_ob[b]])
                self.rw_out(ob, u_ob, n, dst, tt)
                kb = b
                n += 1
                if dst == "K":
                    b2 = n % 5
                    n += 1
                    kks = ob[b2]
                    P.op("dve", lambda e, kb=kb, kks=kks: e.tensor_tensor(out=kks[:], in0=ob[kb][:], in1=kkb[:], op=ALU.mult),
                         reads=[u_ob[kb], u_kkb], writes=[u_ob[b2]])
                    P.op("dve", lambda e, kks=kks: e.tensor_tensor(out=sq[:], in0=kks[:], in1=kks[:], op=ALU.mult),
                         reads=[u_ob[b2]], writes=[u_sq])
                    P.op("dve", lambda e: e.tensor_reduce(out=ss[:], in_=sq[:].rearrange("p (h n) -> p h n", n=64), axis=AX.X,
                                                          op=ALU.add), reads=[u_sq], writes=[u_ss])
                    P.op("dve", lambda e: e.tensor_scalar(out=ss[:], in0=ss[:], scalar1=1e-24, scalar2=None, op0=ALU.max),
                         reads=[u_ss], writes=[u_ss])
                    P.op("act", lambda e: e.sqrt(out=ss[:], in_=ss[:]), reads=[u_ss], writes=[u_ss])
                    P.op("dve", lambda e: e.reciprocal(out=ss[:], in_=ss[:]), reads=[u_ss], writes=[u_ss])
                    P.op("dve", lambda e, kks=kks: e.tensor_tensor(
                        out=kks[:].rearrange("p (h n) -> p h n", n=64), in0=kks[:].rearrange("p (h n) -> p h n", n=64),
                        in1=ss[:].unsqueeze(2).to_broadcast([128, 16, 64]), op=ALU.mult), reads=[u_ob[b2], u_ss], writes=[u_ob[b2]])
                    self.rw_out(ob, u_ob, b2, "KK", tt)
        P.phase_end()

    def rw_proj_b(self):
        P = self.P
        I = self.I
        for nm in ("rw_mix", "rw_g1", "rw_g2", "rw_w0", "rw_w1", "rw_w2", "rw_a0", "rw_a1", "rw_a2", "rw_k_a"):
            I(nm)
        P.phase_begin()
        c = self.rw_common()
        g1w, u_g1 = self.wload("g1w", I("rw_g1"), 8, 128)
        g2w, u_g2 = self.wload("g2w", I("rw_g2"), 1, D)
        w1 = [self.wload("w1_%d" % z, I("rw_w1")[z], 8, 64) for z in range(2)]
        a1 = [self.wload("a1_%d" % z, I("rw_a1")[z], 8, 64) for z in range(2)]
        w2 = []; a2 = []
        for z in range(2):
            for (lst, nm) in ((w2, "rw_w2"), (a2, "rw_a2")):
                t = P.psb(nm, [64, D], BF16); u = U(nm)
                P.dma("pool", lambda e, t=t, nm=nm, z=z: e.dma_start(out=t[:], in_=I(nm)[z]), writes=[u])
                lst.append((t, u))
        def hilo(nm):
            f = P.psb(nm + "f", [33, 2 * D], F32); uf = U(nm + "f")
            hl = P.psb(nm + "hl", [33, 2 * D], BF16); uhl = U(nm + "hl")
            bk = P.psb(nm + "bk", [33, 2 * D], F32); ubk = U(nm + "bk")
            P.op("pool", lambda e: e.memset(f[:], 0.0), writes=[uf])
            src = I(nm).rearrange("(o z) n -> o (z n)", o=1)
            P.dma("sp", lambda e: e.dma_start(out=f[0:1, :], in_=src), reads=[uf], writes=[uf])
            P.dma("sp", lambda e: e.dma_start(out=f[32:33, :], in_=src), reads=[uf], writes=[uf])
            P.op("dve", lambda e: e.tensor_copy(out=hl[:], in_=f[:]), reads=[uf], writes=[uhl])
            P.op("dve", lambda e: e.tensor_copy(out=bk[:], in_=hl[:]), reads=[uhl], writes=[ubk])
            P.op("dve", lambda e: e.tensor_tensor(out=bk[:], in0=f[:], in1=bk[:], op=ALU.subtract), reads=[uf, ubk], writes=[ubk])
            P.op("dve", lambda e: e.tensor_copy(out=hl[32:33, :], in_=bk[32:33, :]), reads=[ubk, uhl], writes=[uhl])
            return hl, uhl
        w0hl, u_w0 = hilo("rw_w0")
        a0hl, u_a0 = hilo("rw_a0")
        kab, u_kab = self.bload("kab", I("rw_k_a")[0:1, :], D)
        c1b = P.psb("c1b", [128, D], F32); u_c1b = U("c1b")
        P.op("dve", lambda e: e.tensor_scalar(out=c1b[:], in0=kab[:], scalar1=-1.0, scalar2=1.0, op0=ALU.mult, op1=ALU.add),
             reads=[u_kab], writes=[u_c1b])
        ob = [P.psb("ob", [128, D], F32) for _ in range(4)]; u_ob = [U("ob%d" % j) for j in range(4)]
        kin = P.psb("kin", [128, D], F32); kkin = P.psb("kkin", [128, D], F32); u_kin = U("kin"); u_kkin = U("kkin")
        az = P.psb("az", [128, D], F32); u_az = U("az")
        lt = [P.psb("lt", [128, 128], BF16) for _ in range(2)]; u_lt = [U("lt0"), U("lt1")]
        n = 0
        nl = 0
        for tt in range(NTT):
            self.rw_xs(tt, c["S"], c["u_S"], c["xx"], c["u_xx"], c["tmp"], c["u_tmp"], c["xs"], c["u_xs"], (1, 4, 5),
                       c["mixT"], c["u_mixT"])
            xw, u_xw = c["xs"][0], c["u_xs"][0]
            xa, u_xa = c["xs"][1], c["u_xs"][1]
            xg, u_xg = c["xs"][2], c["u_xs"][2]
            P.dma("sp", lambda e, tt=tt: e.dma_start(out=kin[:], in_=self.rwd["K"][tt * 128:(tt + 1) * 128, :]),
                  reads=[self.u_rwd["K"]], writes=[u_kin])
            P.dma("sp", lambda e, tt=tt: e.dma_start(out=kkin[:], in_=self.rwd["KK"][tt * 128:(tt + 1) * 128, :]),
                  reads=[self.u_rwd["KK"]], writes=[u_kkin])

            def lora(x, u_x, w1t, u_w1, rows, func, w2t, u_w2, bias, u_bias, z, out_ap, u_out, fin):
                nonlocal nl
                psI, upsI = self.nps()
                for k in range(8):
                    self.mm(psI[0:rows, 0:128], w1t[:, k, :], x[:, k, :], k == 0, k == 7, [u_w1, u_x], [upsI])
                l = nl % 2
                nl += 1
                P.op("act", lambda e: e.activation(out=lt[l][0:rows, :], in_=psI[0:rows, 0:128], func=func),
                     reads=[upsI], writes=[u_lt[l]])
                for half in range(2):
                    ps, ups = self.nps()
                    self.mm(ps[:, :], lt[l][0:rows, :], w2t[0:rows, half * 512:(half + 1) * 512], True, bias is None,
                            [u_lt[l], u_w2], [ups])
                    if bias is not None:
                        self.mm(ps[:, :], self.ones_b[0:33, 0:128], bias[:, z * D + half * 512:z * D + (half + 1) * 512],
                                False, True, [u_bias, self.u_const], [ups])
                    P.op("act", lambda e, ps=ps, half=half: e.activation(out=out_ap[:, half * 512:(half + 1) * 512], in_=ps[:, :],
                                                                         func=fin), reads=[ups], writes=[u_out])
            b = n % 4; n += 1
            lora(xg, u_xg, g1w, u_g1, 128, AF.Sigmoid, g2w[:, 0, :], u_g2, None, None, 0, ob[b], u_ob[b], AF.Copy)
            self.rw_out(ob, u_ob, b, "G", tt)
            for z in range(2):
                b = n % 4; n += 1
                lora(xw, u_xw, w1[z][0], w1[z][1], 64, AF.Tanh, w2[z][0], w2[z][1], w0hl, u_w0, z, ob[b], u_ob[b], AF.Sigmoid)
                self.rw_out(ob, u_ob, b, "LW%d" % z, tt)
                lora(xa, u_xa, a1[z][0], a1[z][1], 64, AF.Copy, a2[z][0], a2[z][1], a0hl, u_a0, z, az, u_az, AF.Sigmoid)
                b = n % 4; n += 1
                P.op("dve", lambda e, b=b: e.tensor_tensor(out=ob[b][:], in0=az[:], in1=kab[:], op=ALU.mult),
                     reads=[u_az, u_kab], writes=[u_ob[b]])
                P.op("dve", lambda e, b=b: e.tensor_tensor(out=ob[b][:], in0=ob[b][:], in1=c1b[:], op=ALU.add),
                     reads=[u_ob[b], u_c1b], writes=[u_ob[b]])
                P.op("dve", lambda e, b=b: e.tensor_tensor(out=ob[b][:], in0=ob[b][:], in1=kin[:], op=ALU.mult),
                     reads=[u_ob[b], u_kin], writes=[u_ob[b]])
                self.rw_out(ob, u_ob, b, "KT%d" % z, tt)
                b = n % 4; n += 1
                P.op("dve", lambda e, b=b: e.tensor_tensor(out=ob[b][:], in0=az[:], in1=kkin[:], op=ALU.mult),
                     reads=[u_az, u_kkin], writes=[u_ob[b]])
                self.rw_out(ob, u_ob, b, "B%d" % z, tt)
        P.phase_end()

    def rw_scan(self, z, do_ctx):
        P = self.P
        I = self.I
        for nm in ("rw_r_k", "rw_ln_g", "rw_ln_b"):
            I(nm)
        P.phase_begin()
        fwd = z == 0
        tri = P.psb("tri", [128, 128], F32); mA = P.psb("mA", [128, 4, 128], F32); mN = P.psb("mN", [128, 128], F32)
        cvec = P.psb("cvec", [128, 2], F32); u_mk = U("masks")
        cm, pat = (-1, 1) if fwd else (1, -1)
        P.op("pool", lambda e: e.memset(tri[:], C0), writes=[u_mk])
        P.op("pool", lambda e: e.memset(mA[:], 1.0), writes=[u_mk])
        P.op("pool", lambda e: e.memset(mN[:], 1.0), writes=[u_mk])
        P.op("pool", lambda e: e.memset(cvec[:], C0), writes=[u_mk])
        P.op("pool", lambda e: e.affine_select(out=tri[:], in_=tri[:], pattern=[[pat, 128]], compare_op=ALU.is_ge, fill=0.0,
                                               base=0, channel_multiplier=cm), reads=[u_mk], writes=[u_mk])
        for q in range(4):
            P.op("pool", lambda e, q=q: e.affine_select(out=mA[:, q, :], in_=mA[:, q, :], pattern=[[pat, 128]],
                                                        compare_op=ALU.is_ge, fill=0.0, base=-(q % 2), channel_multiplier=cm),
                 reads=[u_mk], writes=[u_mk])
        P.op("pool", lambda e: e.affine_select(out=mN[:], in_=mN[:], pattern=[[-pat, 128]], compare_op=ALU.is_ge, fill=0.0,
                                               base=-1, channel_multiplier=-cm), reads=[u_mk], writes=[u_mk])
        Hst = P.psb("Hst", [64, 16, 64], F32); u_H = U("Hst")
        P.op("pool", lambda e: e.memset(Hst[:], 0.0), writes=[u_H])
        names = ("R", "KK", "V", "LW%d" % z, "B%d" % z, "KT%d" % z)
        tin = {nm: P.psb("in_" + nm, [128, D], F32) for nm in names}
        u_in = {nm: U("in_" + nm) for nm in names}
        r, kk, v, sg, bb, kt = (tin[nm] for nm in names)
        u_r, u_kk, u_v, u_sg, u_bb, u_kt = (u_in[nm] for nm in names)
        E = [P.psb("E", [128, D], F32) for _ in range(3)]; u_E = [U("E0"), U("E1"), U("E2")]
        D3 = P.psb("D3", [128, D], F32); u_D3 = U("D3")
        F4 = [E[0], E[1], E[2], D3]; u_F4 = [u_E[0], u_E[1], u_E[2], u_D3]
        FT = P.psb("FT", [64, 8, 4, 128], F32); u_FT = [U("FT%d" % j) for j in range(8)]
        AA = P.psb("AA", [128, 8, 4, 128], F32); u_AA = [U("AA%d" % j) for j in range(8)]
        Nn = P.psb("Nn", [128, 8, 128], F32); u_Nn = [U("Nn0"), U("Nn1")]
        MB = [P.psb("MB", [128, 4, 128], F32) for _ in range(2)]; u_MB = [U("MB0"), U("MB1")]
        NB = [P.psb("NB", [128, 4, 128], F32) for _ in range(2)]; u_NB = [U("NB0"), U("NB1")]
        if True:
            MB2 = [P.psb("MB2", [128, 4, 128], F32) for _ in range(2)]; u_MB2 = [U("MB20"), U("MB21")]
            NB2 = [P.psb("NB2", [128, 4, 128], F32) for _ in range(2)]; u_NB2 = [U("NB20"), U("NB21")]
        else:
            MB2, u_MB2, NB2, u_NB2 = MB, u_MB, NB, u_NB
        MBg = [MB, MB2]; u_MBg = [u_MB, u_MB2]; NBg = [NB, NB2]; u_NBg = [u_NB, u_NB2]
        Pm = P.psb("Pm", [128, 8, 128], F32); u_Pm = [U("Pm0"), U("Pm1")]
        X = P.psb("X", [128, 512], F32); u_X = U("X")
        nU = P.psb("nU", [128, 512], F32); u_nU = U("nU")
        ysb = P.psb("ysb", [128, D], F32); u_y = U("ysb")
        gl = P.psb("gl", [64, 16], F32); u_gl = U("gl")
        if not fwd:
            rkb, u_rkb = self.bload("rkb", I("rw_r_k")[0:1, :], D)
            lng, u_lng = self.bload("lng", I("rw_ln_g")[0:1, :], D)
            lnb, u_lnb = self.bload("lnb", I("rw_ln_b")[0:1, :], D)
            st = P.psb("st", [128, 48], F32); u_st = U("st")
            obf = P.psb("obf", [128, D], BF16); u_obf = U("obf")
            stg = P.psb("stg", [128, 8, 128], BF16); u_stg = U("stg")
        order = list(range(NTT)) if fwd else [1, 0] + list(range(NTT - 1, 1, -1))
        cut = self.cfg.get("scan_cut", 99)
        if "scan_tiles" in self.cfg:
            order = order[:self.cfg["scan_tiles"]]
        v3 = lambda t: t[:].rearrange("p (h n) -> p h n", n=64)
        for tt in order:
            for nm in names:
                P.dma("sp", lambda e, nm=nm, tt=tt: e.dma_start(out=tin[nm][:], in_=self.rwd[nm][tt * 128:(tt + 1) * 128, :]),
                      reads=[self.u_rwd[nm]], writes=[u_in[nm]])
            if cut < -1:
                continue
            for half in range(2):
                hs = slice(half * 512, (half + 1) * 512)
                ps, ups = self.nps()
                self.mm(ps[:, :], tri[:], sg[:, hs], True, True, [u_mk, u_sg], [ups])
                P.op("dve", lambda e, ps=ps, hs=hs: e.tensor_copy(out=E[0][:, hs], in_=ps[:, :]), reads=[ups], writes=[u_E[0]])
                P.op("dve", lambda e, hs=hs: e.scalar_tensor_tensor(out=E[1][:, hs], in0=sg[:, hs], scalar=-C0, in1=E[0][:, hs],
                                                                    op0=ALU.mult, op1=ALU.add), reads=[u_E[0], u_sg], writes=[u_E[1]])
            P.op("act", lambda e: e.activation(out=E[2][:], in_=E[0][:], func=AF.Exp, scale=-1.0), reads=[u_E[0]], writes=[u_E[2]])
            P.op("act", lambda e: e.activation(out=E[0][:], in_=E[0][:], func=AF.Exp), reads=[u_E[0], u_E[2], u_E[1]], writes=[u_E[0]])
            P.op("act", lambda e: e.activation(out=E[1][:], in_=E[1][:], func=AF.Exp), reads=[u_E[1]], writes=[u_E[1]])
            if cut < 0:
                continue
            for j, (src, us, ex) in ((3, (kt, u_kt, 2)), (0, (r, u_r, 0)), (1, (kk, u_kk, 1)), (2, (bb, u_bb, 2))):
                eng = "dve" if j % 2 == 0 else "pool"
                P.op(eng, lambda e, j=j, src=src, ex=ex: e.tensor_tensor(out=F4[j][:], in0=src[:], in1=E[ex][:], op=ALU.mult),
                     reads=[us, u_E[ex]], writes=[u_F4[j]])
            if cut < 1:
                continue
            psG, upsG = self.nps()
            for h in range(16):
                self.mm(psG[0:64, 2 * h:2 * h + 2], sg[:, h * 64:(h + 1) * 64], cvec[:, 0:2], True, True, [u_sg, u_mk], [upsG])
            P.op("dve", lambda e, psG=psG: e.tensor_copy(
                out=gl[:], in_=psG[0:64, 0:32].rearrange("p (h two) -> p h two", two=2)[:, :, 0]), reads=[upsG], writes=[u_gl])
            P.op("act", lambda e: e.activation(out=gl[:], in_=gl[:], func=AF.Exp), reads=[u_gl], writes=[u_gl])
            if cut < 2:
                continue
            for half in range(2):
                for hh in range(8):
                    h = half * 8 + hh
                    ps, ups = self.nps()
                    for j in range(4):
                        self.tr(ps[0:64, j * 128:(j + 1) * 128], F4[j][:, h * 64:(h + 1) * 64], self.ident_f[:],
                                [u_F4[j], self.u_const], [ups])
                    eng = "dve"
                    if eng == "act":
                        P.op("act", lambda e, ps=ps, hh=hh: e.copy(out=FT[:, hh].rearrange("p a t -> p (a t)"), in_=ps[0:64, :]),
                             reads=[ups], writes=[u_FT[hh]])
                    else:
                        P.op("dve", lambda e, ps=ps, hh=hh: e.tensor_copy(out=FT[:, hh].rearrange("p a t -> p (a t)"), in_=ps[0:64, :]),
                             reads=[ups], writes=[u_FT[hh]])
                if cut < 3:
                    continue
                for hh in range(8):
                    ps, ups = self.nps()
                    rhs2 = FT[:, hh, 0:2, :]
                    self.mm(ps[:, 0:256].rearrange("p (a t) -> p a t", a=2), FT[:, hh, 2, :], rhs2, True, True, [u_FT[hh]], [ups])
                    self.mm(ps[:, 256:512].rearrange("p (a t) -> p a t", a=2), FT[:, hh, 3, :], rhs2, True, True, [u_FT[hh]], [ups])
                    P.op("dve", lambda e, ps=ps, hh=hh: e.tensor_tensor(out=AA[:, hh], in0=ps[:, :].rearrange("p (a t) -> p a t", a=4),
                                                                        in1=mA[:], op=ALU.mult), reads=[ups, u_mk], writes=[u_AA[hh]])
                for g in range(2):
                    ps, ups = self.nps()
                    for j in range(4):
                        hh = g * 4 + j
                        self.mm(ps[:, j * 128:(j + 1) * 128], FT[:, hh, 1, :], FT[:, hh, 2, :], True, True, [u_FT[hh]], [ups])
                    P.op("dve", lambda e, ps=ps, g=g: e.tensor_tensor(
                        out=Nn[:, g * 4:(g + 1) * 4, :], in0=ps[:, :].rearrange("p (a t) -> p a t", a=4),
                        in1=mN[:].unsqueeze(1).to_broadcast([128, 4, 128]), op=ALU.mult), reads=[ups, u_mk], writes=[u_Nn[g]])
                if cut < 4:
                    continue
                stt = {}
                for g in range(2):
                    grp = [g * 4 + j for j in range(4)]
                    P.op("dve", lambda e, g=g: e.tensor_tensor(
                        out=Pm[:, g * 4:(g + 1) * 4, :], in0=self.ident_f[:].unsqueeze(1).to_broadcast([128, 4, 128]),
                        in1=AA[:, g * 4:(g + 1) * 4, 1, :], op=ALU.subtract), reads=[u_AA[hh] for hh in grp] + [self.u_const],
                        writes=[u_Pm[g]])
                    stt[g] = ([AA[:, hh, 1, :] for hh in grp], [u_AA[hh] for hh in grp], [Nn[:, hh, :] for hh in grp], [u_Nn[g]])
                for gs in ([0, 1],):
                    for li in range(6):
                        lastl = li == 5
                        pp = li % 2
                        banks = {}
                        for g in gs:
                            Ms, uM, Ns, uN = stt[g]
                            bM = ubM = None
                            if not lastl:
                                bM, ubM = self.nps()
                                for j in range(4):
                                    self.mm(bM[:, j * 128:(j + 1) * 128], Ns[j], Ms[j], True, True, uM + uN, [ubM])
                            bN, ubN = self.nps()
                            for j in range(4):
                                self.mm(bN[:, j * 128:(j + 1) * 128], Ms[j], Ns[j], True, True, uM + uN, [ubN])
                            banks[g] = (bM, ubM, bN, ubN)
                        for g in gs:
                            bM, ubM, bN, ubN = banks[g]
                            mb, umb = MBg[g][pp], u_MBg[g][pp]
                            nb_, unb = NBg[g][pp], u_NBg[g][pp]
                            if not lastl:
                                P.op("dve", lambda e, bM=bM, mb=mb: e.tensor_copy(out=mb[:].rearrange("p a t -> p (a t)"), in_=bM[:, :]),
                                     reads=[ubM], writes=[umb])
                            P.op("dve", lambda e, bN=bN, nb_=nb_: e.tensor_copy(out=nb_[:].rearrange("p a t -> p (a t)"), in_=bN[:, :]),
                                 reads=[ubN], writes=[unb])
                            stt[g] = ([mb[:, j, :] for j in range(4)], [umb], [nb_[:, j, :] for j in range(4)], [unb])
                        for g in gs:
                            Ms, uM, Ns, uN = stt[g]
                            bP, ubP = self.nps()
                            for j in range(4):
                                self.mm(bP[:, j * 128:(j + 1) * 128], Ns[j], Pm[:, g * 4 + j, :], True, True, uN + [u_Pm[g]], [ubP])
                            P.op("dve", lambda e, bP=bP, g=g: e.tensor_tensor(
                                out=Pm[:, g * 4:(g + 1) * 4, :], in0=Pm[:, g * 4:(g + 1) * 4, :],
                                in1=bP[:, :].rearrange("p (a t) -> p a t", a=4), op=ALU.add), reads=[ubP, u_Pm[g]], writes=[u_Pm[g]])
                if cut < 5:
                    continue
                ps, ups = self.nps()
                for hh in range(8):
                    h = half * 8 + hh
                    self.mm(ps[:, hh * 64:(hh + 1) * 64], FT[:, hh, 1, :], Hst[:, h, :], True, False, [u_FT[hh], u_H], [ups])
                    self.mm(ps[:, hh * 64:(hh + 1) * 64], AA[:, hh, 3, :], v[:, h * 64:(h + 1) * 64], False, True, [u_AA[hh], u_v], [ups])
                P.op("dve", lambda e, ps=ps: e.tensor_copy(out=X[:], in_=ps[:, :]), reads=[ups], writes=[u_X])
                ps, ups = self.nps()
                for hh in range(8):
                    self.mm(ps[:, hh * 64:(hh + 1) * 64], Pm[:, hh, :], X[:, hh * 64:(hh + 1) * 64], True, True, [u_Pm[hh // 4], u_X], [ups])
                P.op("dve", lambda e, ps=ps: e.tensor_scalar(out=nU[:], in0=ps[:, :], scalar1=-1.0, scalar2=None, op0=ALU.mult),
                     reads=[ups], writes=[u_nU])
                if cut < 6:
                    continue
                ps, ups = self.nps()
                for hh in range(8):
                    h = half * 8 + hh
                    o = ps[:, hh * 64:(hh + 1) * 64]
                    self.mm(o, FT[:, hh, 0, :], Hst[:, h, :], True, False, [u_FT[hh], u_H], [ups])
                    self.mm(o, AA[:, hh, 2, :], v[:, h * 64:(h + 1) * 64], False, False, [u_AA[hh], u_v], [ups])
                    self.mm(o, AA[:, hh, 0, :], nU[:, hh * 64:(hh + 1) * 64], False, True, [u_AA[hh], u_nU], [ups])
                P.op("dve", lambda e, ps=ps, half=half: e.tensor_copy(out=ysb[:, half * 512:(half + 1) * 512], in_=ps[:, :]),
                     reads=[ups], writes=[u_y])
                if cut < 7:
                    continue
                ps, ups = self.nps()
                for hh in range(8):
                    h = half * 8 + hh
                    o = ps[0:64, hh * 64:(hh + 1) * 64]
                    hc = slice(h * 64, (h + 1) * 64)
                    self.mm(o, F4[3][:, hc], v[:, hc], True, False, [u_F4[3], u_v], [ups])
                    self.mm(o, F4[2][:, hc], nU[:, hh * 64:(hh + 1) * 64], False, False, [u_F4[2], u_nU], [ups])
                    self.mm(o, self.ident_f[0:64, 0:64], Hst[:, h, :], False, True, [u_H, self.u_const], [ups])
                P.op("dve", lambda e, ps=ps, half=half: e.tensor_tensor(
                    out=Hst[:, half * 8:(half + 1) * 8, :], in0=ps[0:64, :].rearrange("p (a t) -> p a t", a=8),
                    in1=gl[:, half * 8:(half + 1) * 8].unsqueeze(2).to_broadcast([64, 8, 64]), op=ALU.mult),
                    reads=[ups, u_gl], writes=[u_H])
            if cut < 8:
                continue
            if fwd:
                P.dma("pool", lambda e, tt=tt: e.dma_start(out=self.rwd["Y"][tt * 128:(tt + 1) * 128, :], in_=ysb[:]),
                      reads=[u_y], writes=[self.u_rwd["Y"]])
                continue
            if tt < 2 and not do_ctx:
                continue
            yf, u_yf = kk, u_kk
            P.dma("sp", lambda e, tt=tt: e.dma_start(out=yf[:], in_=self.rwd["Y"][tt * 128:(tt + 1) * 128, :]),
                  reads=[self.u_rwd["Y"]], writes=[u_yf])
            kt0, u_kt0 = sg, u_sg
            P.dma("sp", lambda e, tt=tt: e.dma_start(out=kt0[:], in_=self.rwd["KT0"][tt * 128:(tt + 1) * 128, :]),
                  reads=[self.u_rwd["KT0"]], writes=[u_kt0])
            gg, u_gg = bb, u_bb
            P.dma("sp", lambda e, tt=tt: e.dma_start(out=gg[:], in_=self.rwd["G"][tt * 128:(tt + 1) * 128, :]),
                  reads=[self.u_rwd["G"]], writes=[u_gg])
            T0, T1, T2, T3 = F4
            uT0, uT1, uT2, uT3 = u_F4
            P.op("dve", lambda e: e.tensor_tensor(out=ysb[:], in0=ysb[:], in1=yf[:], op=ALU.add), reads=[u_y, u_yf], writes=[u_y])
            P.op("dve", lambda e: e.tensor_reduce(out=st[:, 0:16], in_=v3(ysb), axis=AX.X, op=ALU.add), reads=[u_y], writes=[u_st])
            P.op("dve", lambda e: e.tensor_scalar(out=st[:, 0:16], in0=st[:, 0:16], scalar1=1.0 / 64, scalar2=None, op0=ALU.mult),
                 reads=[u_st], writes=[u_st])
            P.op("dve", lambda e: e.tensor_tensor(out=v3(T0), in0=v3(ysb), in1=st[:, 0:16].unsqueeze(2).to_broadcast([128, 16, 64]),
                                                  op=ALU.subtract), reads=[u_y, u_st], writes=[uT0])
            P.op("dve", lambda e: e.tensor_tensor(out=T1[:], in0=T0[:], in1=T0[:], op=ALU.mult), reads=[uT0], writes=[uT1])
            P.op("dve", lambda e: e.tensor_reduce(out=st[:, 16:32], in_=v3(T1), axis=AX.X, op=ALU.add), reads=[uT1], writes=[u_st])
            P.op("dve", lambda e: e.tensor_scalar(out=st[:, 16:32], in0=st[:, 16:32], scalar1=1.0 / 64, scalar2=64e-5,
                                                  op0=ALU.mult, op1=ALU.add), reads=[u_st], writes=[u_st])
            P.op("act", lambda e: e.sqrt(out=st[:, 16:32], in_=st[:, 16:32]), reads=[u_st], writes=[u_st])
            P.op("dve", lambda e: e.reciprocal(out=st[:, 16:32], in_=st[:, 16:32]), reads=[u_st], writes=[u_st])
            P.op("dve", lambda e: e.tensor_tensor(out=v3(T0), in0=v3(T0), in1=st[:, 16:32].unsqueeze(2).to_broadcast([128, 16, 64]),
                                                  op=ALU.mult), reads=[uT0, u_st], writes=[uT0])
            P.op("dve", lambda e: e.tensor_tensor(out=T0[:], in0=T0[:], in1=lng[:], op=ALU.mult), reads=[uT0, u_lng], writes=[uT0])
            P.op("dve", lambda e: e.tensor_tensor(out=T0[:], in0=T0[:], in1=lnb[:], op=ALU.add), reads=[uT0, u_lnb], writes=[uT0])
            P.op("pool", lambda e: e.tensor_tensor(out=T1[:], in0=r[:], in1=rkb[:], op=ALU.mult), reads=[u_r, u_rkb], writes=[uT1])
            P.op("pool", lambda e: e.tensor_tensor(out=T2[:], in0=kt[:], in1=kt0[:], op=ALU.add), reads=[u_kt, u_kt0], writes=[uT2])
            P.op("dve", lambda e: e.tensor_tensor(out=T2[:], in0=T2[:], in1=T1[:], op=ALU.mult), reads=[uT1, uT2], writes=[uT2])
            P.op("dve", lambda e: e.tensor_reduce(out=st[:, 32:48], in_=v3(T2), axis=AX.X, op=ALU.add), reads=[uT2], writes=[u_st])
            P.op("dve", lambda e: e.tensor_tensor(out=v3(T3), in0=v3(v), in1=st[:, 32:48].unsqueeze(2).to_broadcast([128, 16, 64]),
                                                  op=ALU.mult), reads=[u_v, u_st], writes=[uT3])
            P.op("dve", lambda e: e.tensor_tensor(out=T0[:], in0=T0[:], in1=T3[:], op=ALU.add), reads=[uT0, uT3], writes=[uT0])
            P.op("dve", lambda e: e.tensor_tensor(out=obf[:], in0=T0[:], in1=gg[:], op=ALU.mult), reads=[uT0, u_gg], writes=[u_obf])
            self.tpose_to_dram([obf[:, j * 128:(j + 1) * 128] for j in range(8)], u_obf, 128,
                               self.OTd[0:1024, tt * 128:(tt + 1) * 128].rearrange("(j p) t -> p j t", p=128),
                               self.u_OT, stg, u_stg)
        P.phase_end()

    def rwkv(self, i, do_ctx):
        stop = self.cfg.get("rw_stop", 9)
        self.rw_scratch()
        self.rw_proj_a()
        if stop >= 2:
            self.rw_proj_b()
        if stop >= 3:
            self.rw_scan(0, do_ctx)
        if stop >= 4:
            self.rw_scan(1, do_ctx)
            self.outproj_phase(i, "rw_w_o", do_ctx)
        if self.cfg.get("rw_dump"):
            P = self.P
            for nm in self.cfg["rw_dump"]:
                o = P.dram("dump_" + nm, [NT, D], F32, kind="ExternalOutput")
                for j in range(2):
                    P.dma("sp", lambda e, o=o, nm=nm, j=j: e.dma_start(out=o[j * 2176:(j + 1) * 2176, :],
                                                                      in_=self.rwd[nm][j * 2176:(j + 1) * 2176, :]),
                          reads=[self.u_rwd[nm]], is_out=True)


for _n, _f in list(vars(KRw).items()):
    if callable(_f):
        setattr(K, _n, _f)
IN_SHAPES = None


def build_program(cfg):
    k = K(cfg)
    k.prologue()
    if cfg.get("only_scan") is not None:
        k.mix_scratch()
        k.rw_scratch()
        k.rw_scan(cfg["only_scan"], True)
        return k, k.epilogue()
    for i in cfg.get("layers", [0, 1, 2, 3]):
        do_ctx = i < 3 or cfg.get("force_ctx", False)
        if not cfg.get("skip_ada"):
            k.ada_phase(i)
        if not cfg.get("skip_mixer"):
            k.mixer(i, do_ctx)
        if not cfg.get("skip_moe"):
            tiles = None if do_ctx else list(range(2, NTT))
            k.norm_phase(i, 1, True, tiles)
            k.moe_phase(i, do_ctx)
    nc = k.epilogue()
    return k, nc


def rope_tables(d_rot):
    t = np.arange(NLAT)
    row = (t // 64).astype(np.float32)
    col = (t % 64).astype(np.float32)
    d_axis = d_rot // 2
    inv = (np.float32(10000.0) ** (-np.arange(0, d_axis, 2, dtype=np.float32) / np.float32(d_axis))).astype(np.float32)
    ang = np.concatenate([row[:, None] * inv, col[:, None] * inv], axis=-1).astype(np.float32)
    return np.cos(ang).astype(np.float32), np.sin(ang).astype(np.float32)


def na_bias_table(rpb):
    rpb = np.asarray(rpb, dtype=np.float32)
    kl = np.arange(128) // 64
    kc = np.arange(128) % 64
    c = np.arange(64)
    cstart = np.clip(c - 8, 0, 48)
    ok = (kc[:, None] >= cstart[None, :]) & (kc[:, None] < cstart[None, :] + 16)
    dcol = np.clip(kc[:, None] - c[None, :] + 15, 0, 30)
    out = np.empty((16, 128, 14, 64), np.float32)
    for di in range(14):
        dr = np.clip(di - 7 + kl + 7, 0, 14)
        g = rpb[:, dr[:, None], dcol]
        out[:, :, di, :] = np.where(ok[None], g, np.float32(-30000.0))
    return out


def make_in_maps(inputs, names):
    f = np.ascontiguousarray
    shared = {}
    for nm in names:
        if nm in ("x", "c", "ctx"):
            continue
        if nm in ("mla_cos", "mla_sin", "swa_cos", "swa_sin"):
            cs, sn = rope_tables(32 if nm.startswith("mla") else 64)
            shared[nm] = f(cs if nm.endswith("cos") else sn)
            continue
        if nm == "na_bias":
            shared[nm] = f(na_bias_table(inputs["na_rpb"][0]))
            continue
        a = np.asarray(inputs[nm], dtype=np.float32)
        if nm == "c_ctx":
            a = a.reshape(8, 128)
        elif nm in ("norm1_g", "norm2_g"):
            a = a.reshape(4, 8, 128)
        elif nm == "ada_b":
            a = a.reshape(4, 48, 128)
        elif nm.startswith(("na_", "rw_", "mla_", "swa_")):
            a = a[0]
            if nm == "rw_mix":
                a = a.reshape(48, 128)
            elif nm == "rw_r_k":
                a = a.reshape(1, -1)
            if a.ndim == 1:
                a = a.reshape(1, -1)
        shared[nm] = f(a)
    maps = []
    for b in range(8):
        m = dict(shared)
        m["x"] = f(np.asarray(inputs["x"][b], dtype=np.float32))
        m["c"] = f(np.asarray(inputs["c"][b], dtype=np.float32).reshape(8, 128))
        m["ctx"] = f(np.asarray(inputs["ctx"][b], dtype=np.float32))
        maps.append(m)
    return maps


def run(inputs, cfg):
    k, nc = build_program(cfg)
    maps = make_in_maps(inputs, list(k.inp.keys()))
    res = run_bass_kernel_spmd(nc, maps, core_ids=list(range(8)))
    return res


def kernel(**inputs):
    res = run(inputs, {})
    return np.stack([np.asarray(r["out"], dtype=np.float32) for r in res.results], axis=0)
```

```python
from concourse.bass_utils import run_bass_kernel_spmd
import contextlib
import numpy as np
import concourse.bass as bass
import concourse.mybir as mybir

F32 = mybir.dt.float32
BF16 = mybir.dt.bfloat16
I32 = mybir.dt.int32
U32 = mybir.dt.uint32
AF = mybir.ActivationFunctionType
ALU = mybir.AluOpType
AX = mybir.AxisListType

SEM_CAP = 30000
DMA_POOL = 12


class U:
    __slots__ = ("name", "w", "r")

    def __init__(self, name):
        self.name = name
        self.w = None
        self.r = {}


class Prog:
    def __init__(self):
        self.nc = bass.Bass("TRN2", target_bir_lowering=False)
        self.es = contextlib.ExitStack()
        self.ops = {e: [] for e in ("pe", "dve", "act", "pool", "sp")}
        self.cnt = {e: 0 for e in self.ops}
        self.ep = {e: 0 for e in self.ops}
        self.sems = {}
        self.waited = {e: {} for e in self.ops}
        self.dma_n = {e: 0 for e in self.ops}
        self.dma_val = {}
        self.n_inst = 0
        self.out_ticks = []

    def sb(self, name, shape, dt):
        return self.es.enter_context(self.nc.sbuf_tensor(name, list(shape), dt))

    def ps(self, name, shape, dt=F32):
        return self.es.enter_context(self.nc.psum_tensor(name, list(shape), dt))

    def dram(self, name, shape, dt, kind="Internal"):
        return self.nc.dram_tensor(name, list(shape), dt, kind=kind).ap()

    def _sem(self, key):
        if key not in self.sems:
            self.sems[key] = self.es.enter_context(self.nc.semaphore("s_%s_%s" % key))
        return self.sems[key]

    def _wait(self, eng, tick):
        if tick is None:
            return
        key, val = tick
        if self.waited[eng].get(key, 0) >= val:
            return
        self.waited[eng][key] = val
        sem = self._sem(key)
        self.ops[eng].append(lambda e, sem=sem, val=val: e.wait_ge(sem, val))

    def _deps(self, eng, reads, writes, skip_self=False):
        ticks = []
        for u in reads:
            if u.w is not None:
                ticks.append(u.w)
        for u in writes:
            if u.w is not None:
                ticks.append(u.w)
            for k, v in u.r.items():
                ticks.append((k, v))
        mykey = (eng, self.ep[eng])
        for t in ticks:
            if skip_self and t[0] == mykey:
                continue
            self._wait(eng, t)

    def _mark(self, tick, reads, writes):
        k, v = tick
        for u in reads:
            if u.r.get(k, 0) < v:
                u.r[k] = v
        for u in writes:
            u.w = tick
            u.r = {}

    def op(self, eng, fn, reads=(), writes=(), skip_self=False):
        reads = list(reads)
        writes = list(writes)
        self._deps(eng, reads, writes, skip_self=skip_self)
        if self.cnt[eng] >= SEM_CAP:
            self.ep[eng] += 1
            self.cnt[eng] = 0
        self.cnt[eng] += 1
        key = (eng, self.ep[eng])
        sem = self._sem(key)
        val = self.cnt[eng]
        self.ops[eng].append(lambda e, fn=fn, sem=sem: fn(e).then_inc(sem, 1))
        self._mark((key, val), reads, writes)
        self.n_inst += 1
        return (key, val)

    def dma(self, q, fn, reads=(), writes=(), is_out=False):
        reads = list(reads)
        writes = list(writes)
        self._deps(q, reads, writes)
        slot = self.dma_n[q] % DMA_POOL
        self.dma_n[q] += 1
        key = ("d" + q, slot)
        prev = self.dma_val.get(key, 0)
        if prev >= SEM_CAP:
            gen = 1
            while ("d%s_g%d" % (q, gen), slot) in self.dma_val and \
                    self.dma_val[("d%s_g%d" % (q, gen), slot)] >= SEM_CAP:
                gen += 1
            raise RuntimeError("dma semaphore cap reached")
        if prev:
            self._wait(q, (key, prev))
        val = prev + 16
        self.dma_val[key] = val
        sem = self._sem(key)
        self.ops[q].append(lambda e, fn=fn, sem=sem: fn(e).then_inc(sem, 16))
        self._mark((key, val), reads, writes)
        self.n_inst += 1
        if is_out:
            self.out_ticks.append((key, val))
        return (key, val)

    def barrier(self):
        ticks = []
        for e in self.ops:
            if self.cnt[e] > 0:
                ticks.append(((e, self.ep[e]), self.cnt[e]))
        for key, val in self.dma_val.items():
            ticks.append((key, val))
        for e in self.ops:
            for t in ticks:
                if t[0][0] == e:
                    continue
                self._wait(e, t)

    def phase_begin(self):
        self.pes = contextlib.ExitStack()

    def psb(self, name, shape, dt):
        self.uid = getattr(self, "uid", 0) + 1
        return self.pes.enter_context(self.nc.sbuf_tensor("%s_%d" % (name, self.uid), list(shape), dt))

    def phase_end(self):
        self.barrier()
        self.flush()
        self.pes.close()

    def flush(self):
        nc = self.nc
        with nc.Block() as block:
            @block.tensor
            def _(e):
                for f in self.ops["pe"]:
                    f(e)

            @block.vector
            def _(e):
                for f in self.ops["dve"]:
                    f(e)

            @block.scalar
            def _(e):
                for f in self.ops["act"]:
                    f(e)

            @block.gpsimd
            def _(e):
                for f in self.ops["pool"]:
                    f(e)

            @block.sync
            def _(e):
                for f in self.ops["sp"]:
                    f(e)
        for e in self.ops:
            self.ops[e] = []

    def finish(self):
        for t in self.out_ticks:
            self._wait("sp", t)
        self.flush()
        self.es.close()
        return self.nc
D = 1024
NCTX = 256
NLAT = 4096
NT = NCTX + NLAT
NTT = NT // 128
EPS = 1e-6


class K:
    def __init__(self, cfg):
        self.cfg = cfg
        self.P = P = Prog()
        self.inp = {}
        self.uin = {}
        self.build_io()
        self.setup_persistent()

    SHAPES = {
        "x": [NLAT, D], "c": [8, 128], "ctx": [NCTX, D], "c_ctx": [8, 128],
        "norm1_g": [4, 8, 128], "norm2_g": [4, 8, 128], "ada_w": [4, D, 6 * D], "ada_b": [4, 48, 128],
        "moe_router": [4, D, 16], "moe_w1": [4, 16, D, D], "moe_w3": [4, 16, D, D], "moe_w2": [4, 16, D, D],
        "na_w_qkv": [D, 3 * D], "na_q_g": [1, 64], "na_k_g": [1, 64], "na_bias": [16, 128, 14, 64], "na_w_o": [D, D],
        "mla_w_down": [D, 672], "mla_q_norm_g": [1, 384], "mla_kv_norm_g": [1, 256], "mla_w_uq": [384, 1536],
        "mla_w_ukv": [256, 2048], "mla_qn_g": [1, 64], "mla_qr_g": [1, 32], "mla_kn_g": [1, 64], "mla_kr_g": [1, 32],
        "mla_w_o": [D, D], "mla_cos": [NLAT, 16], "mla_sin": [NLAT, 16],
        "swa_w_qkv": [D, 1536], "swa_q_g": [1, 64], "swa_k_g": [1, 64], "swa_sink": [1, 16], "swa_w_o": [D, D],
        "swa_cos": [NLAT, 32], "swa_sin": [NLAT, 32],
        "rw_mix": [48, 128], "rw_w_r": [D, D], "rw_w_k": [D, D], "rw_w_v": [D, D], "rw_w0": [2, D], "rw_w1": [2, D, 64],
        "rw_w2": [2, 64, D], "rw_a0": [2, D], "rw_a1": [2, D, 64], "rw_a2": [2, 64, D], "rw_g1": [D, 128], "rw_g2": [128, D],
        "rw_k_k": [1, D], "rw_k_a": [1, D], "rw_r_k": [1, D], "rw_ln_g": [1, D], "rw_ln_b": [1, D], "rw_w_o": [D, D],
    }

    def build_io(self):
        P = self.P
        self.out = P.dram("out", [NLAT, D], F32, kind="ExternalOutput")
        self.xres = P.dram("xres", [NT, D], F32); self.u_xres = U("xres")
        self.xs2 = P.dram("xs2", [NT, D], BF16); self.u_xs2 = U("xs2")
        self.modd = P.dram("modd", [1, 4 * 96 * 128], F32); self.u_modd = U("modd")

    def I(self, name):
        if name not in self.inp:
            self.inp[name] = self.P.dram(name, self.SHAPES[name], F32, kind="ExternalInput")
        return self.inp[name]

    def setup_persistent(self):
        P = self.P
        self.ident_f = P.sb("ident_f", [128, 128], F32); self.u_const = U("const")
        self.ident_b = P.sb("ident_b", [128, 128], BF16)
        self.ones_f = P.sb("ones_f", [128, 128], F32)
        self.ones_b = P.sb("ones_b", [128, 128], BF16)
        self.scT = P.sb("scT", [128, 8, 2], F32); self.u_scT = U("scT")
        self.AB = P.sb("AB", [128, 16, 8, 2], F32); self.u_AB = U("AB")
        self.hT = P.sb("hT", [128, 8, NT], BF16); self.u_hT = [U("hT%d" % t) for t in range(NTT)]
        self.gateT = P.sb("gateT", [128, 5, 16], F32)
        self.idxT = P.sb("idxT", [128, 5, 16], I32)
        self.psf = [P.ps("psf%d" % i, [128, 512], F32) for i in range(6)]
        self.u_psf = [U("psf%d" % i) for i in range(6)]
        self.psb = [P.ps("psb%d" % i, [128, 1024], BF16) for i in range(2)]
        self.u_psb = [U("psb%d" % i) for i in range(2)]
        self.psf_n = 0
        self.psb_n = 0
        uc = self.u_const
        P.op("pool", lambda e: e.memset(self.ident_f[:], 0.0), writes=[uc])
        P.op("pool", lambda e: e.affine_select(out=self.ident_f[:], in_=self.ident_f[:], pattern=[[-1, 128]],
                                               compare_op=ALU.not_equal, fill=1.0, base=0, channel_multiplier=1),
             reads=[uc], writes=[uc])
        P.op("pool", lambda e: e.tensor_copy(out=self.ident_b[:], in_=self.ident_f[:]), reads=[uc], writes=[uc])
        P.op("pool", lambda e: e.memset(self.ones_f[:], 1.0), writes=[uc])
        P.op("pool", lambda e: e.memset(self.ones_b[:], 1.0), writes=[uc])

    def nps(self):
        i = self.psf_n % 4
        self.psf_n += 1
        return self.psf[i], self.u_psf[i]

    def npsb(self):
        i = self.psb_n % 2
        self.psb_n += 1
        return self.psb[i], self.u_psb[i]

    def mm(self, out, lhsT, rhs, start, stop, reads, writes):
        self.P.op("pe", lambda e: e.matmul(out=out, lhsT=lhsT, rhs=rhs, start=start, stop=stop),
                  reads=reads, writes=writes, skip_self=True)

    def tr(self, out, in_, ident, reads, writes):
        self.P.op("pe", lambda e: e.transpose(out=out, in_=in_, identity=ident),
                  reads=reads, writes=writes, skip_self=True)

    def prologue(self):
        P = self.P
        I = self.I
        for nm in ("x", "c", "ctx", "c_ctx"):
            I(nm)
        P.phase_begin()
        P.dma("sp", lambda e: e.dma_start(out=self.xres[0:NCTX, :], in_=I("ctx")[:, :]), writes=[self.u_xres])
        for j in range(8):
            P.dma("sp", lambda e, j=j: e.dma_start(out=self.xres[NCTX + j * 512:NCTX + (j + 1) * 512, :],
                                                    in_=I("x")[j * 512:(j + 1) * 512, :]), writes=[self.u_xres])
        c16 = P.psb("c16", [8, 256], F32); u_c16 = U("c16")
        P.dma("sp", lambda e: e.dma_start(out=c16[:, 0:128], in_=I("c")[:, :]), writes=[u_c16])
        P.dma("sp", lambda e: e.dma_start(out=c16[:, 128:256], in_=I("c_ctx")[:, :]), writes=[u_c16])
        ps, ups = self.nps()
        for s in range(2):
            self.tr(ps[:, s * 8:(s + 1) * 8], c16[:, s * 128:(s + 1) * 128], self.ident_f[0:8, 0:8],
                    [u_c16, self.u_const], [ups])
        for s in range(2):
            P.op("act", lambda e, s=s: e.activation(out=self.scT[:, :, s], in_=ps[:, s * 8:(s + 1) * 8], func=AF.Silu),
                 reads=[ups], writes=[self.u_scT])
        P.phase_end()

    def ada_phase(self, i):
        P = self.P
        I = self.I
        for nm in ("ada_w", "ada_b", "norm1_g", "norm2_g"):
            I(nm)
        P.phase_begin()
        wbuf = [P.psb("adaw", [128, 8, 512], F32) for _ in range(3)]
        uw = [U("adaw%d" % j) for j in range(3)]
        ab48 = P.psb("ab48", [48, 128], F32); g16 = P.psb("g16", [16, 128], F32); u_ld = U("ld")
        abT = P.psb("abT", [128, 48], F32); gT = P.psb("gT", [128, 16], F32); u_T = U("T")
        modT = P.psb("modT", [128, 48, 2], F32); u_modT = U("modT")
        modTT = P.psb("modTT", [96, 128], F32); u_modTT = U("modTT")
        P.dma("sp", lambda e: e.dma_start(out=ab48[:], in_=I("ada_b")[i]), writes=[u_ld])
        P.dma("sp", lambda e: e.dma_start(out=g16[0:8, :], in_=I("norm1_g")[i]), writes=[u_ld])
        P.dma("sp", lambda e: e.dma_start(out=g16[8:16, :], in_=I("norm2_g")[i]), writes=[u_ld])
        psA, upsA = self.nps()
        self.tr(psA[:, 0:48], ab48[:], self.ident_f[0:48, 0:48], [u_ld, self.u_const], [upsA])
        self.tr(psA[:, 64:80], g16[:], self.ident_f[0:16, 0:16], [u_ld, self.u_const], [upsA])
        P.op("dve", lambda e: e.tensor_copy(out=abT[:], in_=psA[:, 0:48]), reads=[upsA], writes=[u_T])
        P.op("dve", lambda e: e.tensor_copy(out=gT[:], in_=psA[:, 64:80]), reads=[upsA], writes=[u_T])
        psM, upsM = self.nps()
        wsrc = I("ada_w")[i].rearrange("(k p) n -> p k n", p=128)
        for piece in range(12):
            b = piece % 3
            P.dma("sp", lambda e, b=b, piece=piece: e.dma_start(out=wbuf[b][:], in_=wsrc[:, :, piece * 512:(piece + 1) * 512]),
                  writes=[uw[b]])
            for ml in range(4):
                m = piece * 4 + ml
                for k in range(8):
                    self.mm(psM[:, 2 * m:2 * m + 2], wbuf[b][:, k, ml * 128:(ml + 1) * 128], self.scT[:, k, :],
                            k == 0, k == 7, [uw[b], self.u_scT], [upsM])
        P.op("dve", lambda e: e.tensor_tensor(out=modT[:], in0=psM[:, 0:96].rearrange("p (m s) -> p m s", s=2),
                                              in1=abT[:].unsqueeze(2).to_broadcast([128, 48, 2]), op=ALU.add),
             reads=[upsM, u_T], writes=[u_modT])
        AB = self.AB
        for (slot, m0, g0) in ((0, 8, 0), (2, 32, 8)):
            P.op("dve", lambda e, slot=slot, m0=m0, g0=g0: e.scalar_tensor_tensor(
                out=AB[:, i * 4 + slot], in0=modT[:, m0:m0 + 8, :], scalar=1.0,
                in1=gT[:, g0:g0 + 8].unsqueeze(2).to_broadcast([128, 8, 2]), op0=ALU.add, op1=ALU.mult),
                reads=[u_modT, u_T], writes=[self.u_AB])
        for (slot, m0) in ((1, 0), (3, 24)):
            P.op("dve", lambda e, slot=slot, m0=m0: e.tensor_copy(out=AB[:, i * 4 + slot], in_=modT[:, m0:m0 + 8, :]),
                 reads=[u_modT], writes=[self.u_AB])
        psT, upsT = self.nps()
        self.tr(psT[0:96, 0:128], modT[:].rearrange("p m s -> p (m s)"), self.ident_f[:], [u_modT, self.u_const], [upsT])
        P.op("dve", lambda e: e.tensor_copy(out=modTT[:], in_=psT[0:96, 0:128]), reads=[upsT], writes=[u_modTT])
        P.dma("sp", lambda e: e.dma_start(out=self.modd[0, i * 12288:(i + 1) * 12288].rearrange("(r p) -> r p", p=128),
                                          in_=modTT[:]), reads=[u_modTT], writes=[self.u_modd])
        P.phase_end()

    def gate_bcast_src(self, i, which, s):
        m0 = 16 if which == 0 else 40
        base = i * 12288 + (m0 * 2 + s) * 128
        v = self.modd[0:1, base:base + 8 * 256].rearrange("o (j r) -> o j r", r=256)[:, :, 0:128]
        return v.partition_broadcast(128)[:, 0]

    def norm_phase(self, i, which, write_xs2, tiles=None):
        P = self.P
        tiles = list(range(NTT)) if tiles is None else tiles
        P.phase_begin()
        xt = [P.psb("xt", [128, D], F32) for _ in range(2)]; u_xt = [U("xt0"), U("xt1")]
        xs = [P.psb("xs", [128, D], F32) for _ in range(2)]; u_xs = [U("xs0"), U("xs1")]
        xsb = [P.psb("xsb", [128, D], BF16) for _ in range(2)]; u_xsb = [U("xsb0"), U("xsb1")]
        junk = P.psb("junk", [128, D], BF16)
        ss = P.psb("ss", [128, NTT], F32); u_ss = [U("ss%d" % t) for t in range(NTT)]
        rs = P.psb("rs", [128, NTT], F32); u_rs = [U("rs%d" % t) for t in range(NTT)]
        A = self.AB[:, i * 4 + 2 * which]
        B = self.AB[:, i * 4 + 2 * which + 1]
        for n, tt in enumerate(tiles):
            s = 1 if tt < 2 else 0
            b = n % 2
            P.dma("sp", lambda e, b=b, tt=tt: e.dma_start(out=xt[b][:], in_=self.xres[tt * 128:(tt + 1) * 128, :]),
                  reads=[self.u_xres], writes=[u_xt[b]])
            P.op("act", lambda e, b=b, tt=tt: e.activation(out=junk[:], in_=xt[b][:], func=AF.Square,
                                                           accum_out=ss[:, tt:tt + 1]),
                 reads=[u_xt[b]], writes=[u_ss[tt]])
            P.op("dve", lambda e, tt=tt: e.tensor_scalar(out=rs[:, tt:tt + 1], in0=ss[:, tt:tt + 1], scalar1=1.0 / D,
                                                         scalar2=EPS, op0=ALU.mult, op1=ALU.add),
                 reads=[u_ss[tt]], writes=[u_rs[tt]])
            P.op("act", lambda e, tt=tt: e.sqrt(out=rs[:, tt:tt + 1], in_=rs[:, tt:tt + 1]),
                 reads=[u_rs[tt]], writes=[u_rs[tt]])
            P.op("dve", lambda e, tt=tt: e.reciprocal(out=rs[:, tt:tt + 1], in_=rs[:, tt:tt + 1]),
                 reads=[u_rs[tt]], writes=[u_rs[tt]])
            P.op("dve", lambda e, b=b, tt=tt: e.tensor_scalar(out=xs[b][:], in0=xt[b][:], scalar1=rs[:, tt:tt + 1],
                                                              scalar2=None, op0=ALU.mult),
                 reads=[u_xt[b], u_rs[tt]], writes=[u_xs[b]])
            if write_xs2:
                P.op("pool", lambda e, b=b: e.tensor_copy(out=xsb[b][:], in_=xs[b][:]), reads=[u_xs[b]], writes=[u_xsb[b]])
                P.dma("pool", lambda e, b=b, tt=tt: e.dma_start(out=self.xs2[tt * 128:(tt + 1) * 128, :], in_=xsb[b][:]),
                      reads=[u_xsb[b]], writes=[self.u_xs2])
            for half in range(2):
                ps, ups = self.nps()
                for kk in range(4):
                    k = half * 4 + kk
                    self.tr(ps[:, kk * 128:(kk + 1) * 128], xs[b][:, k * 128:(k + 1) * 128], self.ident_f[:],
                            [u_xs[b], self.u_const], [ups])
                for kk in range(4):
                    k = half * 4 + kk
                    P.op("act", lambda e, k=k, kk=kk, tt=tt, s=s, ps=ps: e.activation(
                        out=self.hT[:, k, tt * 128:(tt + 1) * 128], in_=ps[:, kk * 128:(kk + 1) * 128],
                        func=AF.Identity, scale=A[:, k, s:s + 1], bias=B[:, k, s:s + 1]),
                        reads=[ups, self.u_AB], writes=[self.u_hT[tt]])
        P.phase_end()

    def moe_phase(self, i, do_ctx):
        P = self.P
        I = self.I
        for nm in ("moe_router", "moe_w1", "moe_w3", "moe_w2"):
            I(nm)
        NS = 544 if do_ctx else 512
        P.phase_begin()
        wr = P.psb("wr", [128, 8, 16], BF16); u_wr = U("wr")
        E = P.psb("E", [16, NT], F32); u_E = U("E")
        wk = P.psb("wk", [16, NT], F32); u_wkl = U("wkl"); u_wkc = U("wkc")
        mx = P.psb("mx", [16, 544], F32); u_mx = U("mx")
        ix = P.psb("ix", [16, 544], U32); u_ix = U("ix")
        ixf = P.psb("ixf", [16, 544], F32); u_ixf = U("ixf")
        rcp = P.psb("rcp", [16, 512], F32); u_rcp = U("rcp")
        P.dma("pool", lambda e: e.dma_start(out=wr[:], in_=I("moe_router")[i].rearrange("(k p) n -> p k n", p=128)),
              writes=[u_wr])
        chunks = [(c0, min(512, NT - c0)) for c0 in range(0, NT, 512)]
        for (c0, n) in chunks:
            ps, ups = self.nps()
            tts = list(range(c0 // 128, (c0 + n) // 128))
            for k in range(8):
                self.mm(ps[0:16, 0:n], wr[:, k, :], self.hT[:, k, c0:c0 + n], k == 0, k == 7,
                        [u_wr] + [self.u_hT[t] for t in tts], [ups])
            P.op("act", lambda e, ps=ps, c0=c0, n=n: e.activation(out=E[:, c0:c0 + n], in_=ps[0:16, 0:n], func=AF.Exp),
                 reads=[ups], writes=[u_E])
        for (c0, n) in chunks:
            ps, ups = self.nps()
            self.mm(ps[0:16, 0:n], self.ones_f[0:16, 0:16], E[:, c0:c0 + n], True, True, [u_E, self.u_const], [ups])
            P.op("dve", lambda e, ps=ps, n=n: e.reciprocal(out=rcp[:, 0:n], in_=ps[0:16, 0:n]),
                 reads=[ups], writes=[u_rcp])
            P.op("dve", lambda e, c0=c0, n=n: e.tensor_tensor(out=E[:, c0:c0 + n], in0=E[:, c0:c0 + n],
                                                              in1=rcp[:, 0:n], op=ALU.mult),
                 reads=[u_rcp, u_E], writes=[u_E])
        sets = [(NCTX, NLAT, 0, 64, u_wkl)]
        if do_ctx:
            sets.append((0, NCTX, 512, 4, u_wkc))
        for (t0, n, s0, iters, u_wk) in sets:
            src, us = E, u_E
            for it in range(iters):
                sl = slice(s0 + it * 8, s0 + it * 8 + 8)
                P.op("dve", lambda e, src=src, sl=sl, t0=t0, n=n: e.max(out=mx[:, sl], in_=src[:, t0:t0 + n]),
                     reads=[us], writes=[u_mx])
                P.op("dve", lambda e, src=src, sl=sl, t0=t0, n=n: e.max_index(out=ix[:, sl], in_max=mx[:, sl],
                                                                               in_values=src[:, t0:t0 + n]),
                     reads=[us, u_mx], writes=[u_ix])
                if it < iters - 1:
                    P.op("dve", lambda e, src=src, sl=sl, t0=t0, n=n: e.match_replace(
                        out=wk[:, t0:t0 + n], in_to_replace=mx[:, sl], in_values=src[:, t0:t0 + n], imm_value=0.0),
                        reads=[us, u_mx], writes=[u_wk])
                src, us = wk, u_wk
        P.op("dve", lambda e: e.tensor_copy(out=ixf[:, 0:NS], in_=ix[:, 0:NS]), reads=[u_ix], writes=[u_ixf])
        P.op("dve", lambda e: e.tensor_scalar(out=ixf[:, 0:512], in0=ixf[:, 0:512], scalar1=float(NCTX), scalar2=None,
                                              op0=ALU.add), reads=[u_ixf], writes=[u_ixf])
        u_gateT = U("gateT"); u_idxT = U("idxT")
        nch = 5 if do_ctx else 4
        for ch in range(nch):
            rows = 128 if ch < 4 else 32
            ps, ups = self.nps()
            self.tr(ps[0:rows, 0:16], mx[:, ch * 128:ch * 128 + rows], self.ident_f[0:16, 0:16], [u_mx, self.u_const], [ups])
            self.tr(ps[0:rows, 16:32], ixf[:, ch * 128:ch * 128 + rows], self.ident_f[0:16, 0:16], [u_ixf, self.u_const], [ups])
            P.op("dve", lambda e, ps=ps, ch=ch, rows=rows: e.tensor_copy(out=self.gateT[0:rows, ch, :], in_=ps[0:rows, 0:16]),
                 reads=[ups], writes=[u_gateT])
            P.op("dve", lambda e, ps=ps, ch=ch, rows=rows: e.tensor_copy(out=self.idxT[0:rows, ch, :], in_=ps[0:rows, 16:32]),
                 reads=[ups], writes=[u_idxT])
        P.phase_end()
        P.phase_begin()
        NWB = 6
        wb = [self.hT[:, :, j * D:(j + 1) * D] for j in range(4)] + [P.psb("wb", [128, 8, D], BF16)[:] for _ in range(2)]
        u_wb = [U("wb%d" % j) for j in range(NWB)]
        NXI = 5
        xin = [P.psb("xin", [128, D], BF16) for _ in range(NXI)]; u_xin = [U("xin%d" % j) for j in range(NXI)]
        xinT = [P.psb("xinT", [128, 8, 640], BF16) for _ in range(2)]; u_xinT = [U("xinT0"), U("xinT1")]
        hidT = P.psb("hidT", [128, 8, 640], BF16); u_hidT = U("hidT")
        sg = [P.psb("sg", [128, 512], F32) for _ in range(2)]; u_sg = [U("sg0"), U("sg1")]
        ysb = [P.psb("ysb", [128, D], F32) for _ in range(2)]; u_ysb = [U("ysb0"), U("ysb1")]
        G = P.psb("G", [128, 2, 8, 128], F32); u_G = U("G")
        for s in range(2 if do_ctx else 1):
            P.dma("sp", lambda e, s=s: e.dma_start(out=G[:, s], in_=self.gate_bcast_src(i, 1, s)),
                  reads=[self.u_modd], writes=[u_G])
        A2 = self.AB[:, i * 4 + 2]
        B2 = self.AB[:, i * 4 + 3]
        wn = 0
        gn = 0
        yn = 0
        segs = [(0, 512, 0)] + ([(512, 32, 1)] if do_ctx else [])
        def emit_weights(ex):
            nonlocal wn
            wl = {}
            for nm in ("moe_w1", "moe_w3", "moe_w2"):
                b = wn % NWB
                wn += 1
                P.dma("pool", lambda e, nm=nm, b=b, ex=ex: e.dma_start(
                    out=wb[b], in_=I(nm)[i, ex].rearrange("(k p) n -> p k n", p=128)), writes=[u_wb[b]])
                wl[nm] = (wb[b], u_wb[b])
            return wl

        def emit_gathers(ex):
            nonlocal gn
            gb = []
            for ch in range(nch):
                rows = 128 if ch < 4 else 32
                b = gn % NXI
                gn += 1
                P.dma("pool", lambda e, b=b, ch=ch, rows=rows, ex=ex: e.indirect_dma_start(
                    out=xin[b][0:rows, :], out_offset=None, in_=self.xs2[:, :],
                    in_offset=bass.IndirectOffsetOnAxis(ap=self.idxT[0:rows, ch, ex:ex + 1], axis=0)),
                    reads=[u_idxT, self.u_xs2], writes=[u_xin[b]])
                gb.append(b)
            return gb

        pre = {0: (emit_weights(0), emit_gathers(0))}
        for ex in range(16):
            wl, gbufs = pre.pop(ex)
            xT, u_xT = xinT[ex % 2], u_xinT[ex % 2]
            for ch in range(nch):
                rows = 128 if ch < 4 else 32
                s = 0 if ch < 4 else 1
                b = gbufs[ch]
                pb, upb = self.npsb()
                for k in range(8):
                    self.tr(pb[:, k * 128:k * 128 + rows], xin[b][0:rows, k * 128:(k + 1) * 128],
                            self.ident_b[0:rows, 0:rows], [u_xin[b], self.u_const], [upb])
                for k in range(8):
                    P.op("act", lambda e, k=k, ch=ch, rows=rows, s=s, pb=pb, xT=xT: e.activation(
                        out=xT[:, k, ch * 128:ch * 128 + rows], in_=pb[:, k * 128:k * 128 + rows],
                        func=AF.Identity, scale=A2[:, k, s:s + 1], bias=B2[:, k, s:s + 1]),
                        reads=[upb, self.u_AB], writes=[u_xT])
            if ex + 1 < 16:
                pre[ex + 1] = (emit_weights(ex + 1), emit_gathers(ex + 1))
            w1, u_w1 = wl["moe_w1"]
            w3, u_w3 = wl["moe_w3"]
            w2, u_w2 = wl["moe_w2"]
            for f in range(8):
                for (lo, n, s) in segs:
                    p1, up1 = self.nps()
                    p3, up3 = self.nps()
                    for k in range(8):
                        self.mm(p1[:, 0:n], w1[:, k, f * 128:(f + 1) * 128], xT[:, k, lo:lo + n], k == 0, k == 7,
                                [u_w1, u_xT], [up1])
                    for k in range(8):
                        self.mm(p3[:, 0:n], w3[:, k, f * 128:(f + 1) * 128], xT[:, k, lo:lo + n], k == 0, k == 7,
                                [u_w3, u_xT], [up3])
                    sb_ = (f * 2 + s) % 2
                    P.op("act", lambda e, p1=p1, n=n, sb_=sb_: e.activation(out=sg[sb_][:, 0:n], in_=p1[:, 0:n], func=AF.Silu),
                         reads=[up1], writes=[u_sg[sb_]])
                    P.op("dve", lambda e, p3=p3, n=n, sb_=sb_, f=f, lo=lo: e.tensor_tensor(
                        out=hidT[:, f, lo:lo + n], in0=sg[sb_][:, 0:n], in1=p3[:, 0:n], op=ALU.mult),
                        reads=[up3, u_sg[sb_]], writes=[u_hidT])
            for ch in range(nch):
                rows = 128 if ch < 4 else 32
                s = 0 if ch < 4 else 1
                t0, tn = (NCTX, NLAT) if ch < 4 else (0, NCTX)
                yb = yn % 2
                yn += 1
                for half in range(2):
                    py, upy = self.nps()
                    for k in range(8):
                        self.mm(py[0:rows, :], hidT[:, k, ch * 128:ch * 128 + rows], w2[:, k, half * 512:(half + 1) * 512],
                                k == 0, k == 7, [u_w2, u_hidT], [upy])
                    P.op("dve", lambda e, py=py, rows=rows, ch=ch, half=half, s=s, yb=yb, ex=ex: e.scalar_tensor_tensor(
                        out=ysb[yb][0:rows, half * 512:(half + 1) * 512], in0=py[0:rows, :],
                        scalar=self.gateT[0:rows, ch, ex:ex + 1],
                        in1=G[0:rows, s, half * 4:(half + 1) * 4, :].rearrange("p a b -> p (a b)"),
                        op0=ALU.mult, op1=ALU.mult), reads=[upy, u_gateT, u_G], writes=[u_ysb[yb]])
                P.dma("pool", lambda e, rows=rows, ch=ch, yb=yb, t0=t0, tn=tn, ex=ex: e.indirect_dma_start(
                    out=self.xres[:, :],
                    out_offset=bass.IndirectOffsetOnAxis(ap=self.idxT[0:rows, ch, ex:ex + 1], axis=0),
                    in_=ysb[yb][0:rows, :], in_offset=None, compute_op=ALU.add),
                    reads=[u_ysb[yb], u_idxT], writes=[self.u_xres])
        P.phase_end()

    def epilogue(self):
        P = self.P
        for j in range(8):
            P.dma("sp", lambda e, j=j: e.dma_start(out=self.out[j * 512:(j + 1) * 512, :],
                                                    in_=self.xres[NCTX + j * 512:NCTX + (j + 1) * 512, :]),
                  reads=[self.u_xres], is_out=True)
        if self.cfg.get("dump_ctx"):
            oc = P.dram("out_ctx", [NCTX, D], F32, kind="ExternalOutput")
            P.dma("sp", lambda e: e.dma_start(out=oc[:, :], in_=self.xres[0:NCTX, :]), reads=[self.u_xres], is_out=True)
        return P.finish()
NEG = -30000.0


class KMix:
    def mix_scratch(self):
        if hasattr(self, "QTd"):
            return
        P = self.P
        self.QTd = P.dram("QTd", [1536, NT], BF16); self.u_QT = U("QTd")
        self.KTd = P.dram("KTd", [1024, NT], BF16); self.u_KT = U("KTd")
        self.KRd = P.dram("KRd", [32, NT], BF16); self.u_KR = U("KRd")
        self.Vd = P.dram("Vd", [NT, 1024], BF16); self.u_V = U("Vd")
        self.OTd = P.dram("OTd", [1024, NT], BF16); self.u_OT = U("OTd")

    def wload(self, name, src, kch, ncols):
        P = self.P
        t = P.psb(name, [128, kch, ncols], BF16); u = U(name)
        P.dma("pool", lambda e: e.dma_start(out=t[:], in_=src.rearrange("(k p) n -> p k n", p=128)), writes=[u])
        return t, u

    def bload(self, name, src_row, n, scale=None, parts=128):
        P = self.P
        t = P.psb(name, [parts, n], F32); u = U(name)
        P.dma("sp", lambda e: e.dma_start(out=t[:], in_=src_row.partition_broadcast(parts)[:, 0]), writes=[u])
        if scale is not None:
            P.op("dve", lambda e: e.tensor_scalar(out=t[:], in0=t[:], scalar1=float(scale), scalar2=None, op0=ALU.mult),
                 reads=[u], writes=[u])
        return t, u

    def norm_scratch(self):
        P = self.P
        sc = {"sq": P.psb("nsq", [128, 1536], F32), "u_sq": U("nsq"),
              "ss": P.psb("nss", [128, 16], F32), "u_ss": U("nss"),
              "r": [P.psb("rp", [128, 512], F32) for _ in range(4)], "u_r": [U("rp%d" % j) for j in range(4)]}
        return sc

    def headnorm(self, src, us, H, n, gb, ugb, out, uo, sc):
        P = self.P
        sq = sc["sq"][:, 0:H * n].rearrange("p (h n) -> p h n", n=n)
        ss = sc["ss"][:, 0:H]
        u_sq, u_ss = sc["u_sq"], sc["u_ss"]
        P.op("dve", lambda e: e.tensor_tensor(out=sq, in0=src, in1=src, op=ALU.mult), reads=[us], writes=[u_sq])
        P.op("dve", lambda e: e.tensor_reduce(out=ss, in_=sq, axis=AX.X, op=ALU.add), reads=[u_sq], writes=[u_ss])
        P.op("dve", lambda e: e.tensor_scalar(out=ss, in0=ss, scalar1=1.0 / n, scalar2=EPS, op0=ALU.mult, op1=ALU.add),
             reads=[u_ss], writes=[u_ss])
        P.op("act", lambda e: e.sqrt(out=ss, in_=ss), reads=[u_ss], writes=[u_ss])
        P.op("dve", lambda e: e.reciprocal(out=ss, in_=ss), reads=[u_ss], writes=[u_ss])
        P.op("dve", lambda e: e.tensor_tensor(out=sq, in0=src, in1=ss.unsqueeze(2).to_broadcast([128, H, n]), op=ALU.mult),
             reads=[us, u_ss], writes=[u_sq])
        P.op("dve", lambda e: e.tensor_tensor(out=out, in0=sq, in1=gb[:, 0:n].unsqueeze(1).to_broadcast([128, H, n]),
                                              op=ALU.mult), reads=[u_sq, ugb], writes=[uo])

    def rope(self, x, ux, H, half, cs, sn, ucs, sc):
        P = self.P
        x1 = x[:, :, 0:half]
        x2 = x[:, :, half:2 * half]
        cb = cs.unsqueeze(1).to_broadcast([128, H, half])
        sb = sn.unsqueeze(1).to_broadcast([128, H, half])
        t = [sc["r"][j][:, 0:H * half].rearrange("p (h n) -> p h n", n=half) for j in range(4)]
        ut = sc["u_r"]
        for j, (a, b) in enumerate(((x1, cb), (x2, sb), (x2, cb), (x1, sb))):
            P.op("dve", lambda e, j=j, a=a, b=b: e.tensor_tensor(out=t[j], in0=a, in1=b, op=ALU.mult),
                 reads=[ux, ucs], writes=[ut[j]])
        P.op("dve", lambda e: e.tensor_tensor(out=x1, in0=t[0], in1=t[1], op=ALU.subtract), reads=[ut[0], ut[1]], writes=[ux])
        P.op("dve", lambda e: e.tensor_tensor(out=x2, in0=t[2], in1=t[3], op=ALU.add), reads=[ut[2], ut[3]], writes=[ux])

    def tpose_to_dram(self, blocks, ub, rows, dst, udst, stage, ustage):
        P = self.P
        pb, upb = self.npsb()
        nb = len(blocks)
        for j, blk in enumerate(blocks):
            self.tr(pb[0:rows, j * 128:(j + 1) * 128], blk, self.ident_b[:], [ub, self.u_const], [upb])
        P.op("act", lambda e: e.copy(out=stage[0:rows, 0:nb, :], in_=pb[0:rows, 0:nb * 128].rearrange("p (j t) -> p j t", t=128)),
             reads=[upb], writes=[ustage])
        P.dma("sp", lambda e: e.dma_start(out=dst, in_=stage[0:rows, 0:nb, :]), reads=[ustage], writes=[udst])

    def attn_setup(self):
        P = self.P
        a = {"pt": [P.psb("pt", [128, 512], BF16) for _ in range(4)], "u_pt": [U("pt%d" % j) for j in range(4)], "nblk": 0, "nsc": 0,
             "tmp": [P.psb("tmpb", [128, 512], F32) for _ in range(2)], "u_tmp": [U("tmp0"), U("tmp1")],
             "rd": P.psb("rd", [64, 512], F32), "u_rd": U("rd"), "n": 0}
        return a

    def attn_block(self, a, rhs_q, uq, nq, dk, keys, out_ap, uout, sink_fn=None, qg=1):
        P = self.P
        LA = 2
        bi = 2 + 2 * (a["nblk"] % 2)
        a["nblk"] += 1
        pso, upso = self.psf[bi][0:64, 0:nq], self.u_psf[bi]
        psd, upsd = self.psf[bi + 1][0:64, 0:nq], self.u_psf[bi + 1]
        n_k = len(keys)
        last = n_k - 1
        pend = {}
        for j in range(n_k + LA):
            if j < n_k:
                (kt, uk, v, uv, nk, bias, ubias) = keys[j]
                si = a["nsc"] % 2
                a["nsc"] += 1
                ps, ups = self.psf[si], self.u_psf[si]
                so = ps[0:nk, 0:nq] if qg == 1 else ps[0:nk, 0:nq].rearrange("p (g t) -> p g t", g=qg)
                self.mm(so, kt, rhs_q, True, True, [uk, uq], [ups])
                n = a["n"]; a["n"] += 1
                pt, upt = a["pt"][n % 4], a["u_pt"][n % 4]
                if bias is not None:
                    tmp, utmp = a["tmp"][n % 2], a["u_tmp"][n % 2]
                    P.op("dve", lambda e, ps=ps, nk=nk, tmp=tmp, bias=bias: e.tensor_tensor(
                        out=tmp[0:nk, 0:nq], in0=ps[0:nk, 0:nq], in1=bias, op=ALU.add), reads=[ups, ubias], writes=[utmp])
                    P.op("act", lambda e, nk=nk, tmp=tmp, pt=pt: e.activation(out=pt[0:nk, 0:nq], in_=tmp[0:nk, 0:nq], func=AF.Exp),
                         reads=[utmp], writes=[upt])
                else:
                    P.op("act", lambda e, ps=ps, nk=nk, pt=pt: e.activation(out=pt[0:nk, 0:nq], in_=ps[0:nk, 0:nq], func=AF.Exp),
                         reads=[ups], writes=[upt])
                pend[j] = (pt, upt)
            jj = j - LA
            if jj >= 0:
                (kt, uk, v, uv, nk, bias, ubias) = keys[jj]
                pt, upt = pend.pop(jj)
                self.mm(pso, v, pt[0:nk, 0:nq], jj == 0, jj == last, [uv, upt], [upso])
                self.mm(psd, self.ones_b[0:nk, 0:64], pt[0:nk, 0:nq], jj == 0, jj == last, [upt, self.u_const], [upsd])
        rd, urd = a["rd"], a["u_rd"]
        if sink_fn is not None:
            sink_fn(psd, upsd, rd, urd)
        else:
            P.op("dve", lambda e: e.tensor_copy(out=rd[:, 0:nq], in_=psd), reads=[upsd], writes=[urd])
        P.op("dve", lambda e: e.reciprocal(out=rd[:, 0:nq], in_=rd[:, 0:nq]), reads=[urd], writes=[urd])
        if qg == 1:
            o_in, r_in = pso, rd[:, 0:nq]
        else:
            o_in = pso.rearrange("p (g t) -> p g t", g=qg)
            r_in = rd[:, 0:nq].rearrange("p (g t) -> p g t", g=qg)
        P.op("dve", lambda e: e.tensor_tensor(out=out_ap, in0=o_in, in1=r_in, op=ALU.mult),
             reads=[upso, urd], writes=[uout])

    def outproj_phase(self, i, wname, do_ctx):
        P = self.P
        wsrc = self.I(wname)
        P.phase_begin()
        wo, uwo = self.wload("wo", wsrc, 8, D)
        for k in range(8):
            P.dma("sp", lambda e, k=k: e.dma_start(out=self.hT[:, k, :], in_=self.OTd[k * 128:(k + 1) * 128, :]),
                  reads=[self.u_OT], writes=self.u_hT)
        G = P.psb("G1", [128, 2, 8, 128], F32); u_G = U("G1")
        for s in range(2):
            P.dma("sp", lambda e, s=s: e.dma_start(out=G[:, s], in_=self.gate_bcast_src(i, 0, s)),
                  reads=[self.u_modd], writes=[u_G])
        xt = [P.psb("xto", [128, D], F32) for _ in range(2)]; u_xt = [U("xto0"), U("xto1")]
        tmp = [P.psb("tmpo", [128, 512], F32) for _ in range(2)]; u_tmp = [U("tmpo0"), U("tmpo1")]
        tiles = list(range(NTT)) if do_ctx else list(range(2, NTT))
        for n, tt in enumerate(tiles):
            s = 1 if tt < 2 else 0
            b = n % 2
            P.dma("sp", lambda e, b=b, tt=tt: e.dma_start(out=xt[b][:], in_=self.xres[tt * 128:(tt + 1) * 128, :]),
                  reads=[self.u_xres], writes=[u_xt[b]])
            for half in range(2):
                ps, ups = self.nps()
                for k in range(8):
                    self.mm(ps[:, :], self.hT[:, k, tt * 128:(tt + 1) * 128], wo[:, k, half * 512:(half + 1) * 512],
                            k == 0, k == 7, [self.u_hT[tt], uwo], [ups])
                P.op("dve", lambda e, ps=ps, half=half, s=s: e.tensor_tensor(
                    out=tmp[half][:], in0=ps[:, :], in1=G[:, s, half * 4:(half + 1) * 4, :].rearrange("p a b -> p (a b)"),
                    op=ALU.mult), reads=[ups, u_G], writes=[u_tmp[half]])
                P.op("dve", lambda e, half=half, b=b: e.tensor_tensor(
                    out=xt[b][:, half * 512:(half + 1) * 512], in0=xt[b][:, half * 512:(half + 1) * 512],
                    in1=tmp[half][:], op=ALU.add), reads=[u_tmp[half], u_xt[b]], writes=[u_xt[b]])
            P.dma("pool", lambda e, b=b, tt=tt: e.dma_start(out=self.xres[tt * 128:(tt + 1) * 128, :], in_=xt[b][:]),
                  reads=[u_xt[b]], writes=[self.u_xres])
        P.phase_end()

    def mla_proj(self, i):
        P = self.P
        I = self.I
        for nm in ("mla_w_down", "mla_q_norm_g", "mla_kv_norm_g", "mla_w_uq", "mla_w_ukv", "mla_qn_g", "mla_qr_g",
                   "mla_kn_g", "mla_kr_g", "mla_cos", "mla_sin"):
            I(nm)
        scale = 96.0 ** -0.5
        P.phase_begin()
        wd, uwd = self.wload("wd", I("mla_w_down"), 8, 672)
        wuq, uwuq = self.wload("wuq", I("mla_w_uq"), 3, 1536)
        wukv, uwukv = self.wload("wukv", I("mla_w_ukv"), 2, 2048)
        gq, ugq = self.bload("gq", I("mla_q_norm_g")[0:1, :], 384)
        gkv, ugkv = self.bload("gkv", I("mla_kv_norm_g")[0:1, :], 256)
        gkr, ugkr = self.bload("gkr", I("mla_kr_g")[0:1, :], 32)
        gqn, ugqn = self.bload("gqn", I("mla_qn_g")[0:1, :], 64, scale=scale)
        gqr, ugqr = self.bload("gqr", I("mla_qr_g")[0:1, :], 32, scale=scale)
        gkn, ugkn = self.bload("gkn", I("mla_kn_g")[0:1, :], 64)
        sc = self.norm_scratch()
        cqT = P.psb("cqT", [128, 3, NT], BF16); u_cqT = [U("cqT%d" % t) for t in range(NTT)]
        ckvT = P.psb("ckvT", [128, 2, NT], BF16); u_ckvT = [U("ckvT%d" % t) for t in range(NTT)]
        df = P.psb("df", [128, 672], F32); u_df = U("df")
        dn = P.psb("dn", [128, 672], F32); u_dn = U("dn")
        db = P.psb("db", [128, 768], BF16); u_db = U("db")
        cs = [P.psb("cs", [128, 16], F32) for _ in range(2)]; sn = [P.psb("sn", [128, 16], F32) for _ in range(2)]
        u_cs = [U("cs0"), U("cs1")]
        krs = P.psb("krs", [32, 1, 128], BF16); u_krs = U("krs")
        P.op("pool", lambda e: e.memset(db[:], 0.0), writes=[u_db])
        for tt in range(NTT):
            lat = tt >= 2
            ps0, up0 = self.nps()
            ps1, up1 = self.nps()
            for k in range(8):
                self.mm(ps0[:, 0:512], self.hT[:, k, tt * 128:(tt + 1) * 128], wd[:, k, 0:512], k == 0, k == 7,
                        [self.u_hT[tt], uwd], [up0])
            for k in range(8):
                self.mm(ps1[:, 0:160], self.hT[:, k, tt * 128:(tt + 1) * 128], wd[:, k, 512:672], k == 0, k == 7,
                        [self.u_hT[tt], uwd], [up1])
            P.op("act", lambda e, ps0=ps0: e.copy(out=df[:, 0:512], in_=ps0[:, 0:512]), reads=[up0], writes=[u_df])
            P.op("act", lambda e, ps1=ps1: e.copy(out=df[:, 512:672], in_=ps1[:, 0:160]), reads=[up1], writes=[u_df])
            for (c0, n, g, ug) in ((0, 384, gq, ugq), (384, 256, gkv, ugkv), (640, 32, gkr, ugkr)):
                self.headnorm(df[:, c0:c0 + n].unsqueeze(1), u_df, 1, n, g, ug, dn[:, c0:c0 + n].unsqueeze(1), u_dn, sc)
            if lat:
                b = tt % 2
                t0 = (tt - 2) * 128
                P.dma("sp", lambda e, b=b, t0=t0: e.dma_start(out=cs[b][:], in_=I("mla_cos")[t0:t0 + 128, :]), writes=[u_cs[b]])
                P.dma("sp", lambda e, b=b, t0=t0: e.dma_start(out=sn[b][:], in_=I("mla_sin")[t0:t0 + 128, :]), writes=[u_cs[b]])
                self.rope(dn[:, 640:672].unsqueeze(1), u_dn, 1, 16, cs[b][:], sn[b][:], u_cs[b], sc)
            P.op("act", lambda e: e.copy(out=db[:, 0:672], in_=dn[:, 0:672]), reads=[u_dn], writes=[u_db])
            pb, upb = self.npsb()
            for j in range(5):
                self.tr(pb[:, j * 128:(j + 1) * 128], db[:, j * 128:(j + 1) * 128], self.ident_b[:], [u_db, self.u_const], [upb])
            self.tr(pb[:, 640:768], db[:, 640:768], self.ident_b[:], [u_db, self.u_const], [upb])
            P.op("act", lambda e, pb=pb, tt=tt: e.copy(out=cqT[:, :, tt * 128:(tt + 1) * 128],
                                                       in_=pb[:, 0:384].rearrange("p (j t) -> p j t", t=128)),
                 reads=[upb], writes=[u_cqT[tt]])
            P.op("act", lambda e, pb=pb, tt=tt: e.copy(out=ckvT[:, :, tt * 128:(tt + 1) * 128],
                                                       in_=pb[:, 384:640].rearrange("p (j t) -> p j t", t=128)),
                 reads=[upb], writes=[u_ckvT[tt]])
            P.op("act", lambda e, pb=pb: e.copy(out=krs[:, 0, :], in_=pb[0:32, 640:768]), reads=[upb], writes=[u_krs])
            P.dma("sp", lambda e, tt=tt: e.dma_start(out=self.KRd[:, tt * 128:(tt + 1) * 128], in_=krs[:, 0, :]),
                  reads=[u_krs], writes=[self.u_KR])
        qf = P.psb("qf", [128, 16, 96], F32); u_qf = U("qf")
        qn = P.psb("qn", [128, 16, 96], F32); u_qn = U("qn")
        qb = P.psb("qb", [128, 16, 96], BF16); u_qb = U("qb")
        kvf = P.psb("kvf", [128, 16, 128], F32); u_kvf = U("kvf")
        knf = P.psb("knf", [128, 16, 64], F32); u_knf = U("knf")
        knb = P.psb("knb", [128, 16, 64], BF16); u_knb = U("knb")
        vb = P.psb("vb", [128, 16, 64], BF16); u_vb = U("vb")
        stq = [P.psb("stq", [96, 8, 128], BF16) for _ in range(2)]; u_stq = [U("stq0"), U("stq1")]
        stk = P.psb("stk", [128, 8, 128], BF16); u_stk = U("stk")
        qflat = qf[:].rearrange("p h n -> p (h n)")
        kvflat = kvf[:].rearrange("p h n -> p (h n)")
        for tt in range(NTT):
            lat = tt >= 2
            for cc in range(3):
                ps, ups = self.nps()
                for k in range(3):
                    self.mm(ps[:, :], cqT[:, k, tt * 128:(tt + 1) * 128], wuq[:, k, cc * 512:(cc + 1) * 512], k == 0, k == 2,
                            [u_cqT[tt], uwuq], [ups])
                P.op("act", lambda e, ps=ps, cc=cc: e.copy(out=qflat[:, cc * 512:(cc + 1) * 512], in_=ps[:, :]),
                     reads=[ups], writes=[u_qf])
            self.headnorm(qf[:, :, 0:64], u_qf, 16, 64, gqn, ugqn, qn[:, :, 0:64], u_qn, sc)
            self.headnorm(qf[:, :, 64:96], u_qf, 16, 32, gqr, ugqr, qn[:, :, 64:96], u_qn, sc)
            if lat:
                b = tt % 2
                t0 = (tt - 2) * 128
                P.dma("sp", lambda e, b=b, t0=t0: e.dma_start(out=cs[b][:], in_=I("mla_cos")[t0:t0 + 128, :]), writes=[u_cs[b]])
                P.dma("sp", lambda e, b=b, t0=t0: e.dma_start(out=sn[b][:], in_=I("mla_sin")[t0:t0 + 128, :]), writes=[u_cs[b]])
                self.rope(qn[:, :, 64:96], u_qn, 16, 16, cs[b][:], sn[b][:], u_cs[b], sc)
            P.op("act", lambda e: e.copy(out=qb[:], in_=qn[:]), reads=[u_qn], writes=[u_qb])
            for hh in range(2):
                self.tpose_to_dram([qb[:, hh * 8 + j, :] for j in range(8)], u_qb, 96,
                                   self.QTd[hh * 768:(hh + 1) * 768, tt * 128:(tt + 1) * 128].rearrange("(j d) t -> d j t", d=96),
                                   self.u_QT, stq[hh], u_stq[hh])
            for cc in range(4):
                ps, ups = self.nps()
                for k in range(2):
                    self.mm(ps[:, :], ckvT[:, k, tt * 128:(tt + 1) * 128], wukv[:, k, cc * 512:(cc + 1) * 512], k == 0, k == 1,
                            [u_ckvT[tt], uwukv], [ups])
                P.op("act", lambda e, ps=ps, cc=cc: e.copy(out=kvflat[:, cc * 512:(cc + 1) * 512], in_=ps[:, :]),
                     reads=[ups], writes=[u_kvf])
            self.headnorm(kvf[:, :, 0:64], u_kvf, 16, 64, gkn, ugkn, knf[:], u_knf, sc)
            P.op("act", lambda e: e.copy(out=knb[:], in_=knf[:]), reads=[u_knf], writes=[u_knb])
            P.op("pool", lambda e: e.tensor_copy(out=vb[:], in_=kvf[:, :, 64:128]), reads=[u_kvf], writes=[u_vb])
            P.dma("sp", lambda e, tt=tt: e.dma_start(out=self.Vd[tt * 128:(tt + 1) * 128, :].rearrange("t (h n) -> t h n", n=64),
                                                     in_=vb[:]), reads=[u_vb], writes=[self.u_V])
            knb2 = knb[:].rearrange("p h n -> p (h n)")
            self.tpose_to_dram([knb2[:, j * 128:(j + 1) * 128] for j in range(8)], u_knb, 128,
                               self.KTd[:, tt * 128:(tt + 1) * 128].rearrange("(j p) t -> p j t", p=128),
                               self.u_KT, stk, u_stk)
        P.phase_end()

    def mla_attn(self, do_ctx):
        P = self.P
        P.phase_begin()
        a = self.attn_setup()
        QT = [P.psb("QTh", [96, NT], BF16) for _ in range(2)]; uQ = [U("QTh0"), U("QTh1")]
        KT = [P.psb("KTh", [96, NT], BF16) for _ in range(2)]; uK = [U("KTh0"), U("KTh1")]
        V = [P.psb("Vh", [128, NTT, 64], BF16) for _ in range(2)]; uV = [U("Vh0"), U("Vh1")]
        OS = [P.psb("OSh", [64, NT], BF16) for _ in range(2)]; uOS = [U("OSh0"), U("OSh1")]
        for h in range(16):
            b = h % 2
            P.dma("sp", lambda e, b=b, h=h: e.dma_start(out=QT[b][:], in_=self.QTd[h * 96:(h + 1) * 96, :]),
                  reads=[self.u_QT], writes=[uQ[b]])
            P.dma("sp", lambda e, b=b, h=h: e.dma_start(out=KT[b][0:64, :], in_=self.KTd[h * 64:(h + 1) * 64, :]),
                  reads=[self.u_KT], writes=[uK[b]])
            P.dma("sp", lambda e, b=b: e.dma_start(out=KT[b][64:96, :], in_=self.KRd[:, :]), reads=[self.u_KR], writes=[uK[b]])
            P.dma("sp", lambda e, b=b, h=h: e.dma_start(
                out=V[b][:], in_=self.Vd[:, h * 64:(h + 1) * 64].rearrange("(t p) c -> p t c", p=128)),
                reads=[self.u_V], writes=[uV[b]])
            blocks = [(NCTX + c * 512, 512, list(range(NTT))) for c in range(8)]
            if do_ctx:
                blocks.append((0, NCTX, [0, 1]))
            for (q0, nq, kts) in blocks:
                keys = [(KT[b][:, kt * 128:(kt + 1) * 128], uK[b], V[b][:, kt, :], uV[b], 128, None, None) for kt in kts]
                self.attn_block(a, QT[b][:, q0:q0 + nq], uQ[b], nq, 96, keys, OS[b][:, q0:q0 + nq], uOS[b])
            c0 = 0 if do_ctx else NCTX
            P.dma("pool", lambda e, b=b, h=h, c0=c0: e.dma_start(out=self.OTd[h * 64:(h + 1) * 64, c0:NT], in_=OS[b][:, c0:NT]),
                  reads=[uOS[b]], writes=[self.u_OT])
        P.phase_end()


    def qkv_proj(self, wname, Hk, gqname, gkname, scale, rope=None):
        P = self.P
        I = self.I
        for nm in (wname, gqname, gkname) + (tuple(rope) if rope else ()):
            I(nm)
        ncol = 1024 + 2 * Hk * 64
        P.phase_begin()
        w, uw = self.wload("wqkv", I(wname), 8, ncol)
        gq, ugq = self.bload("gq", I(gqname)[0:1, :], 64, scale=scale)
        gk, ugk = self.bload("gk", I(gkname)[0:1, :], 64)
        sc = self.norm_scratch()
        qf = P.psb("qf", [128, ncol], F32); u_qf = U("qf")
        qn = P.psb("qn", [128, 16, 64], F32); u_qn = U("qn")
        kn = P.psb("kn", [128, Hk, 64], F32); u_kn = U("kn")
        qb = P.psb("qb", [128, 1024], BF16); u_qb = U("qb")
        kb = P.psb("kb", [128, Hk * 64], BF16); u_kb = U("kb")
        vb = P.psb("vb", [128, Hk * 64], BF16); u_vb = U("vb")
        stq = P.psb("stq", [128, 8, 128], BF16); u_stq = U("stq")
        stk = P.psb("stk", [128, 8, 128], BF16); u_stk = U("stk")
        if rope:
            cs = [P.psb("cs", [128, 32], F32) for _ in range(2)]; sn = [P.psb("sn", [128, 32], F32) for _ in range(2)]
            u_cs = [U("cs0"), U("cs1")]
        nb = ncol // 512
        kc0 = 1024
        vc0 = 1024 + Hk * 64
        for tt in range(NTT):
            lat = tt >= 2
            for cc in range(nb):
                ps, ups = self.nps()
                for k in range(8):
                    self.mm(ps[:, :], self.hT[:, k, tt * 128:(tt + 1) * 128], w[:, k, cc * 512:(cc + 1) * 512], k == 0, k == 7,
                            [self.u_hT[tt], uw], [ups])
                P.op("act", lambda e, ps=ps, cc=cc: e.copy(out=qf[:, cc * 512:(cc + 1) * 512], in_=ps[:, :]),
                     reads=[ups], writes=[u_qf])
            self.headnorm(qf[:, 0:1024].rearrange("p (h n) -> p h n", n=64), u_qf, 16, 64, gq, ugq, qn[:], u_qn, sc)
            self.headnorm(qf[:, kc0:kc0 + Hk * 64].rearrange("p (h n) -> p h n", n=64), u_qf, Hk, 64, gk, ugk, kn[:], u_kn, sc)
            if rope and lat:
                b = tt % 2
                t0 = (tt - 2) * 128
                P.dma("sp", lambda e, b=b, t0=t0: e.dma_start(out=cs[b][:], in_=I(rope[0])[t0:t0 + 128, :]), writes=[u_cs[b]])
                P.dma("sp", lambda e, b=b, t0=t0: e.dma_start(out=sn[b][:], in_=I(rope[1])[t0:t0 + 128, :]), writes=[u_cs[b]])
                self.rope(qn[:], u_qn, 16, 32, cs[b][:], sn[b][:], u_cs[b], sc)
                self.rope(kn[:], u_kn, Hk, 32, cs[b][:], sn[b][:], u_cs[b], sc)
            P.op("act", lambda e: e.copy(out=qb[:], in_=qn[:].rearrange("p h n -> p (h n)")), reads=[u_qn], writes=[u_qb])
            P.op("act", lambda e: e.copy(out=kb[:], in_=kn[:].rearrange("p h n -> p (h n)")), reads=[u_kn], writes=[u_kb])
            P.op("pool", lambda e: e.tensor_copy(out=vb[:], in_=qf[:, vc0:vc0 + Hk * 64]), reads=[u_qf], writes=[u_vb])
            P.dma("sp", lambda e, tt=tt: e.dma_start(out=self.Vd[tt * 128:(tt + 1) * 128, 0:Hk * 64], in_=vb[:]),
                  reads=[u_vb], writes=[self.u_V])
            self.tpose_to_dram([qb[:, j * 128:(j + 1) * 128] for j in range(8)], u_qb, 128,
                               self.QTd[0:1024, tt * 128:(tt + 1) * 128].rearrange("(j p) t -> p j t", p=128),
                               self.u_QT, stq, u_stq)
            nkb = Hk * 64 // 128
            self.tpose_to_dram([kb[:, j * 128:(j + 1) * 128] for j in range(nkb)], u_kb, 128,
                               self.KTd[0:Hk * 64, tt * 128:(tt + 1) * 128].rearrange("(j p) t -> p j t", p=128),
                               self.u_KT, stk, u_stk)
        P.phase_end()

    def na_proj(self, i):
        self.qkv_proj("na_w_qkv", 16, "na_q_g", "na_k_g", 64.0 ** -0.5)

    def swa_proj(self, i):
        self.qkv_proj("swa_w_qkv", 4, "swa_q_g", "swa_k_g", 64.0 ** -0.5, rope=("swa_cos", "swa_sin"))

    def swa_attn(self, do_ctx):
        P = self.P
        I = self.I
        I("swa_sink")
        P.phase_begin()
        a = self.attn_setup()
        Mp = P.psb("Mp", [128, 4, 128], F32); Mn = P.psb("Mn", [128, 4, 128], F32); u_M = U("M")
        P.op("pool", lambda e: e.memset(Mp[:], 0.0), writes=[u_M])
        P.op("pool", lambda e: e.memset(Mn[:], 0.0), writes=[u_M])
        P.op("pool", lambda e: e.affine_select(out=Mp[:], in_=Mp[:], pattern=[[0, 4], [-1, 128]], compare_op=ALU.is_ge,
                                               fill=NEG, base=0, channel_multiplier=1), reads=[u_M], writes=[u_M])
        P.op("pool", lambda e: e.affine_select(out=Mn[:], in_=Mn[:], pattern=[[0, 4], [1, 128]], compare_op=ALU.is_ge,
                                               fill=NEG, base=0, channel_multiplier=-1), reads=[u_M], writes=[u_M])
        Mp2 = Mp[:].rearrange("p g t -> p (g t)")
        Mn2 = Mn[:].rearrange("p g t -> p (g t)")
        esink, u_es = self.bload("esink", I("swa_sink")[0:1, :], 16, parts=64)
        P.op("act", lambda e: e.activation(out=esink[:], in_=esink[:], func=AF.Exp), reads=[u_es], writes=[u_es])
        Q = P.psb("Qall", [64, 4, NT], BF16); uQ = U("Qall")
        KT = P.psb("KTh", [64, NT], BF16); uK = U("KTh")
        V = P.psb("Vh", [128, NTT, 64], BF16); uV = U("Vh")
        OS = P.psb("OSh", [64, 4, NT], BF16); uOS = U("OSh")
        for hk in range(4):
            P.dma("sp", lambda e, hk=hk: e.dma_start(out=Q[:], in_=self.QTd[hk * 256:(hk + 1) * 256, :].rearrange("(g d) t -> d g t", d=64)),
                  reads=[self.u_QT], writes=[uQ])
            P.dma("sp", lambda e, hk=hk: e.dma_start(out=KT[:], in_=self.KTd[hk * 64:(hk + 1) * 64, :]),
                  reads=[self.u_KT], writes=[uK])
            P.dma("sp", lambda e, hk=hk: e.dma_start(out=V[:], in_=self.Vd[:, hk * 64:(hk + 1) * 64].rearrange("(t p) c -> p t c", p=128)),
                  reads=[self.u_V], writes=[uV])

            def sink_fn(psd, upsd, rd, urd, hk=hk):
                for g in range(4):
                    P.op("dve", lambda e, g=g: e.tensor_scalar(out=rd[:, g * 128:(g + 1) * 128], in0=psd[:, g * 128:(g + 1) * 128],
                                                                scalar1=esink[:, hk * 4 + g:hk * 4 + g + 1], scalar2=None, op0=ALU.add),
                         reads=[upsd, u_es], writes=[urd])
            tiles = list(range(2, NTT)) + ([0, 1] if do_ctx else [])
            for tile in tiles:
                def key(kt, bias):
                    return (KT[:, kt * 128:(kt + 1) * 128], uK, V[:, kt, :], uV, 128, bias, u_M)
                keys = [key(0, None), key(1, None)]
                if tile >= 2:
                    if tile > 2:
                        keys.append(key(tile - 1, Mp2))
                    keys.append(key(tile, None))
                    if tile < NTT - 1:
                        keys.append(key(tile + 1, Mn2))
                self.attn_block(a, Q[:, :, tile * 128:(tile + 1) * 128], uQ, 512, 64, keys,
                                OS[:, :, tile * 128:(tile + 1) * 128], uOS, sink_fn=sink_fn, qg=4)
            c0 = 0 if do_ctx else NCTX
            P.dma("pool", lambda e, hk=hk, c0=c0: e.dma_start(
                out=self.OTd[hk * 256:(hk + 1) * 256, c0:NT].rearrange("(g d) t -> d g t", d=64), in_=OS[:, :, c0:NT]),
                reads=[uOS], writes=[self.u_OT])
        P.phase_end()

    def na_attn(self, do_ctx):
        P = self.P
        I = self.I
        I("na_bias")
        P.phase_begin()
        a = self.attn_setup()
        QT = [P.psb("QTh", [64, NT], BF16) for _ in range(2)]; uQ = [U("QTh0"), U("QTh1")]
        KT = [P.psb("KTh", [64, NT], BF16) for _ in range(2)]; uK = [U("KTh0"), U("KTh1")]
        V = [P.psb("Vh", [128, NTT, 64], BF16) for _ in range(2)]; uV = [U("Vh0"), U("Vh1")]
        Vs = [P.psb("Vsh", [128, NTT - 1, 64], BF16) for _ in range(2)]
        B = [P.psb("Bh", [128, 14, 64], F32) for _ in range(2)]; uB = [U("Bh0"), U("Bh1")]
        OS = [P.psb("OSh", [64, NT], BF16) for _ in range(2)]; uOS = [U("OSh0"), U("OSh1")]
        for h in range(16):
            b = h % 2
            P.dma("sp", lambda e, b=b, h=h: e.dma_start(out=QT[b][:], in_=self.QTd[h * 64:(h + 1) * 64, :]),
                  reads=[self.u_QT], writes=[uQ[b]])
            P.dma("sp", lambda e, b=b, h=h: e.dma_start(out=KT[b][:], in_=self.KTd[h * 64:(h + 1) * 64, :]),
                  reads=[self.u_KT], writes=[uK[b]])
            P.dma("sp", lambda e, b=b, h=h: e.dma_start(
                out=V[b][:], in_=self.Vd[:, h * 64:(h + 1) * 64].rearrange("(t p) c -> p t c", p=128)),
                reads=[self.u_V], writes=[uV[b]])
            P.dma("sp", lambda e, b=b, h=h: e.dma_start(
                out=Vs[b][:], in_=self.Vd[64:64 + (NTT - 1) * 128, h * 64:(h + 1) * 64].rearrange("(t p) c -> p t c", p=128)),
                reads=[self.u_V], writes=[uV[b]])
            P.dma("sp", lambda e, b=b, h=h: e.dma_start(out=B[b][:], in_=I("na_bias")[h]), writes=[uB[b]])
            for r in range(64):
                q0 = NCTX + r * 64
                r0 = min(max(r - 4, 0), 56)
                keys = [(KT[b][:, 0:128], uK[b], V[b][:, 0, :], uV[b], 128, None, None),
                        (KT[b][:, 128:256], uK[b], V[b][:, 1, :], uV[b], 128, None, None)]
                for j in range(4):
                    krow = r0 + 2 * j
                    tok = NCTX + krow * 64
                    vv = V[b][:, tok // 128, :] if tok % 128 == 0 else Vs[b][:, (tok - 64) // 128, :]
                    keys.append((KT[b][:, tok:tok + 128], uK[b], vv, uV[b], 128, B[b][:, krow - r + 7, :], uB[b]))
                self.attn_block(a, QT[b][:, q0:q0 + 64], uQ[b], 64, 64, keys, OS[b][:, q0:q0 + 64], uOS[b])
            if do_ctx:
                keys = [(KT[b][:, 0:128], uK[b], V[b][:, 0, :], uV[b], 128, None, None),
                        (KT[b][:, 128:256], uK[b], V[b][:, 1, :], uV[b], 128, None, None)]
                self.attn_block(a, QT[b][:, 0:NCTX], uQ[b], NCTX, 64, keys, OS[b][:, 0:NCTX], uOS[b])
            c0 = 0 if do_ctx else NCTX
            P.dma("pool", lambda e, b=b, h=h, c0=c0: e.dma_start(out=self.OTd[h * 64:(h + 1) * 64, c0:NT], in_=OS[b][:, c0:NT]),
                  reads=[uOS[b]], writes=[self.u_OT])
        P.phase_end()

    def mixer(self, i, do_ctx):
        self.mix_scratch()
        m = i % 4
        self.norm_phase(i, 0, False)
        if m == 0:
            self.na_proj(i); self.na_attn(do_ctx); self.outproj_phase(i, "na_w_o", do_ctx)
        elif m == 1:
            self.rwkv(i, do_ctx)
        elif m == 2:
            self.mla_proj(i); self.mla_attn(do_ctx); self.outproj_phase(i, "mla_w_o", do_ctx)
        else:
            self.swa_proj(i); self.swa_attn(do_ctx); self.outproj_phase(i, "swa_w_o", do_ctx)


for _n, _f in list(vars(KMix).items()):
    if callable(_f):
        setattr(K, _n, _f)
C0 = -0.6065306597126334


class KRw:
    def rw_scratch(self):
        if hasattr(self, "rwd"):
            return
        P = self.P
        self.rwd = {}
        self.u_rwd = {}
        for nm in ("R", "K", "KK", "V", "G", "LW0", "LW1", "B0", "B1", "KT0", "KT1", "Y"):
            self.rwd[nm] = P.dram("rw_" + nm, [NT, D], F32)
            self.u_rwd[nm] = U("rw_" + nm)

    def rw_xs(self, tt, S, u_S, xx, u_xx, tmp, u_tmp, xs, u_xs, streams, mixT, u_mixT):
        P = self.P
        h = self.hT
        t0 = tt * 128
        noprev = tt in (0, 2)
        nonext = tt in (1, NTT - 1)
        a = 1 if noprev else 0
        b = 127 if nonext else 128
        uh = [self.u_hT[tt]]
        P.op("dve", lambda e: e.tensor_tensor(out=S[:, :, a:b], in0=h[:, :, t0 - 1 + a:t0 - 1 + b],
                                              in1=h[:, :, t0 + 1 + a:t0 + 1 + b], op=ALU.add), reads=uh, writes=[u_S])
        if noprev:
            P.op("dve", lambda e: e.tensor_copy(out=S[:, :, 0:1], in_=h[:, :, t0 + 1:t0 + 2]), reads=uh, writes=[u_S])
        if nonext:
            P.op("dve", lambda e: e.tensor_copy(out=S[:, :, 127:128], in_=h[:, :, t0 + 126:t0 + 127]), reads=uh, writes=[u_S])
        P.op("dve", lambda e: e.scalar_tensor_tensor(out=xx[:], in0=S[:], scalar=0.5, in1=h[:, :, t0:t0 + 128],
                                                     op0=ALU.mult, op1=ALU.subtract), reads=[u_S] + uh, writes=[u_xx])
        for n, s in enumerate(streams):
            tb = n % 2
            P.op("pool", lambda e, s=s, tb=tb: e.tensor_tensor(
                out=tmp[tb][:], in0=xx[:], in1=mixT[:, s * 8:(s + 1) * 8].unsqueeze(2).to_broadcast([128, 8, 128]),
                op=ALU.mult), reads=[u_xx, u_mixT], writes=[u_tmp[tb]])
            P.op("dve", lambda e, n=n, tb=tb: e.tensor_tensor(out=xs[n][:], in0=tmp[tb][:], in1=h[:, :, t0:t0 + 128], op=ALU.add),
                 reads=[u_tmp[tb]] + uh, writes=[u_xs[n]])

    def rw_common(self):
        P = self.P
        I = self.I
        m48 = P.psb("m48", [48, 128], F32); u_m48 = U("m48")
        mixT = P.psb("mixT", [128, 48], F32); u_mixT = U("mixT")
        P.dma("sp", lambda e: e.dma_start(out=m48[:], in_=I("rw_mix")[:, :]), writes=[u_m48])
        ps, ups = self.nps()
        self.tr(ps[:, 0:48], m48[:], self.ident_f[0:48, 0:48], [u_m48, self.u_const], [ups])
        P.op("dve", lambda e: e.tensor_copy(out=mixT[:], in_=ps[:, 0:48]), reads=[ups], writes=[u_mixT])
        S = P.psb("S", [128, 8, 128], F32); xx = P.psb("xx", [128, 8, 128], F32)
        tmp = [P.psb("xtmp", [128, 8, 128], F32) for _ in range(2)]
        xs = [P.psb("xsT", [128, 8, 128], BF16) for _ in range(3)]
        return dict(mixT=mixT, u_mixT=u_mixT, S=S, u_S=U("S"), xx=xx, u_xx=U("xx"), tmp=tmp, u_tmp=[U("xt0"), U("xt1")],
                    xs=xs, u_xs=[U("xs0"), U("xs1"), U("xs2")])

    def rw_out(self, ob, u_ob, n, name, tt):
        b = n % len(ob)
        self.P.dma("pool", lambda e: e.dma_start(out=self.rwd[name][tt * 128:(tt + 1) * 128, :], in_=ob[b][:]),
                   reads=[u_ob[b]], writes=[self.u_rwd[name]])

    def rw_proj_a(self):
        P = self.P
        I = self.I
        for nm in ("rw_mix", "rw_w_r", "rw_w_k", "rw_w_v", "rw_k_k"):
            I(nm)
        P.phase_begin()
        c = self.rw_common()
        W = {}
        for nm in ("rw_w_r", "rw_w_k", "rw_w_v"):
            W[nm] = self.wload(nm, I(nm), 8, D)
        kkb, u_kkb = self.bload("kkb", I("rw_k_k")[0:1, :], D)
        ob = [P.psb("ob", [128, D], F32) for _ in range(5)]; u_ob = [U("ob%d" % j) for j in range(5)]
        ss = P.psb("ss", [128, 16], F32); u_ss = U("ss")
        sq = P.psb("sq", [128, D], F32); u_sq = U("sq")
        n = 0
        for tt in range(NTT):
            self.rw_xs(tt, c["S"], c["u_S"], c["xx"], c["u_xx"], c["tmp"], c["u_tmp"], c["xs"], c["u_xs"], (0, 2, 3),
                       c["mixT"], c["u_mixT"])
            for si, (nm, dst) in enumerate((("rw_w_r", "R"), ("rw_w_k", "K"), ("rw_w_v", "V"))):
                w, uw = W[nm]
                b = n % 5
                for half in range(2):
                    ps, ups = self.nps()
                    for k in range(8):
                        self.mm(ps[:, :], c["xs"][si][:, k, :], w[:, k, half * 512:(half + 1) * 512], k == 0, k == 7,
                                [c["u_xs"][si], uw], [ups])
                    P.op("act", lambda e, ps=ps, b=b, half=half: e.copy(out=ob[b][:, half * 512:(half + 1) * 512], in_=ps[:, :]),
                         reads=[ups], writes=[u_ob[b]])
                self.rw_out(ob, u_ob, n, dst, tt)
                kb = b
                n += 1
                if dst == "K":
                    b2 = n % 5
                    n += 1
                    kks = ob[b2]
                    P.op("dve", lambda e, kb=kb, kks=kks: e.tensor_tensor(out=kks[:], in0=ob[kb][:], in1=kkb[:], op=ALU.mult),
                         reads=[u_ob[kb], u_kkb], writes=[u_ob[b2]])
                    P.op("dve", lambda e, kks=kks: e.tensor_tensor(out=sq[:], in0=kks[:], in1=kks[:], op=ALU.mult),
                         reads=[u_ob[b2]], writes=[u_sq])
                    P.op("dve", lambda e: e.tensor_reduce(out=ss[:], in_=sq[:].rearrange("p (h n) -> p h n", n=64), axis=AX.X,
                                                          op=ALU.add), reads=[u_sq], writes=[u_ss])
                    P.op("dve", lambda e: e.tensor_scalar(out=ss[:], in0=ss[:], scalar1=1e-24, scalar2=None, op0=ALU.max),
                         reads=[u_ss], writes=[u_ss])
                    P.op("act", lambda e: e.sqrt(out=ss[:], in_=ss[:]), reads=[u_ss], writes=[u_ss])
                    P.op("dve", lambda e: e.reciprocal(out=ss[:], in_=ss[:]), reads=[u_ss], writes=[u_ss])
                    P.op("dve", lambda e, kks=kks: e.tensor_tensor(
                        out=kks[:].rearrange("p (h n) -> p h n", n=64), in0=kks[:].rearrange("p (h n) -> p h n", n=64),
                        in1=ss[:].unsqueeze(2).to_broadcast([128, 16, 64]), op=ALU.mult), reads=[u_ob[b2], u_ss], writes=[u_ob[b2]])
                    self.rw_out(ob, u_ob, b2, "KK", tt)
        P.phase_end()

    def rw_proj_b(self):
        P = self.P
        I = self.I
        for nm in ("rw_mix", "rw_g1", "rw_g2", "rw_w0", "rw_w1", "rw_w2", "rw_a0", "rw_a1", "rw_a2", "rw_k_a"):
            I(nm)
        P.phase_begin()
        c = self.rw_common()
        g1w, u_g1 = self.wload("g1w", I("rw_g1"), 8, 128)
        g2w, u_g2 = self.wload("g2w", I("rw_g2"), 1, D)
        w1 = [self.wload("w1_%d" % z, I("rw_w1")[z], 8, 64) for z in range(2)]
        a1 = [self.wload("a1_%d" % z, I("rw_a1")[z], 8, 64) for z in range(2)]
        w2 = []; a2 = []
        for z in range(2):
            for (lst, nm) in ((w2, "rw_w2"), (a2, "rw_a2")):
                t = P.psb(nm, [64, D], BF16); u = U(nm)
                P.dma("pool", lambda e, t=t, nm=nm, z=z: e.dma_start(out=t[:], in_=I(nm)[z]), writes=[u])
                lst.append((t, u))
        def hilo(nm):
            f = P.psb(nm + "f", [33, 2 * D], F32); uf = U(nm + "f")
            hl = P.psb(nm + "hl", [33, 2 * D], BF16); uhl = U(nm + "hl")
            bk = P.psb(nm + "bk", [33, 2 * D], F32); ubk = U(nm + "bk")
            P.op("pool", lambda e: e.memset(f[:], 0.0), writes=[uf])
            src = I(nm).rearrange("(o z) n -> o (z n)", o=1)
            P.dma("sp", lambda e: e.dma_start(out=f[0:1, :], in_=src), reads=[uf], writes=[uf])
            P.dma("sp", lambda e: e.dma_start(out=f[32:33, :], in_=src), reads=[uf], writes=[uf])
            P.op("dve", lambda e: e.tensor_copy(out=hl[:], in_=f[:]), reads=[uf], writes=[uhl])
            P.op("dve", lambda e: e.tensor_copy(out=bk[:], in_=hl[:]), reads=[uhl], writes=[ubk])
            P.op("dve", lambda e: e.tensor_tensor(out=bk[:], in0=f[:], in1=bk[:], op=ALU.subtract), reads=[uf, ubk], writes=[ubk])
            P.op("dve", lambda e: e.tensor_copy(out=hl[32:33, :], in_=bk[32:33, :]), reads=[ubk, uhl], writes=[uhl])
            return hl, uhl
        w0hl, u_w0 = hilo("rw_w0")
        a0hl, u_a0 = hilo("rw_a0")
        kab, u_kab = self.bload("kab", I("rw_k_a")[0:1, :], D)
        c1b = P.psb("c1b", [128, D], F32); u_c1b = U("c1b")
        P.op("dve", lambda e: e.tensor_scalar(out=c1b[:], in0=kab[:], scalar1=-1.0, scalar2=1.0, op0=ALU.mult, op1=ALU.add),
             reads=[u_kab], writes=[u_c1b])
        ob = [P.psb("ob", [128, D], F32) for _ in range(4)]; u_ob = [U("ob%d" % j) for j in range(4)]
        kin = P.psb("kin", [128, D], F32); kkin = P.psb("kkin", [128, D], F32); u_kin = U("kin"); u_kkin = U("kkin")
        az = P.psb("az", [128, D], F32); u_az = U("az")
        lt = [P.psb("lt", [128, 128], BF16) for _ in range(2)]; u_lt = [U("lt0"), U("lt1")]
        n = 0
        nl = 0
        for tt in range(NTT):
            self.rw_xs(tt, c["S"], c["u_S"], c["xx"], c["u_xx"], c["tmp"], c["u_tmp"], c["xs"], c["u_xs"], (1, 4, 5),
                       c["mixT"], c["u_mixT"])
            xw, u_xw = c["xs"][0], c["u_xs"][0]
            xa, u_xa = c["xs"][1], c["u_xs"][1]
            xg, u_xg = c["xs"][2], c["u_xs"][2]
            P.dma("sp", lambda e, tt=tt: e.dma_start(out=kin[:], in_=self.rwd["K"][tt * 128:(tt + 1) * 128, :]),
                  reads=[self.u_rwd["K"]], writes=[u_kin])
            P.dma("sp", lambda e, tt=tt: e.dma_start(out=kkin[:], in_=self.rwd["KK"][tt * 128:(tt + 1) * 128, :]),
                  reads=[self.u_rwd["KK"]], writes=[u_kkin])

            def lora(x, u_x, w1t, u_w1, rows, func, w2t, u_w2, bias, u_bias, z, out_ap, u_out, fin):
                nonlocal nl
                psI, upsI = self.nps()
                for k in range(8):
                    self.mm(psI[0:rows, 0:128], w1t[:, k, :], x[:, k, :], k == 0, k == 7, [u_w1, u_x], [upsI])
                l = nl % 2
                nl += 1
                P.op("act", lambda e: e.activation(out=lt[l][0:rows, :], in_=psI[0:rows, 0:128], func=func),
                     reads=[upsI], writes=[u_lt[l]])
                for half in range(2):
                    ps, ups = self.nps()
                    self.mm(ps[:, :], lt[l][0:rows, :], w2t[0:rows, half * 512:(half + 1) * 512], True, bias is None,
                            [u_lt[l], u_w2], [ups])
                    if bias is not None:
                        self.mm(ps[:, :], self.ones_b[0:33, 0:128], bias[:, z * D + half * 512:z * D + (half + 1) * 512],
                                False, True, [u_bias, self.u_const], [ups])
                    P.op("act", lambda e, ps=ps, half=half: e.activation(out=out_ap[:, half * 512:(half + 1) * 512], in_=ps[:, :],
                                                                         func=fin), reads=[ups], writes=[u_out])
            b = n % 4; n += 1
            lora(xg, u_xg, g1w, u_g1, 128, AF.Sigmoid, g2w[:, 0, :], u_g2, None, None, 0, ob[b], u_ob[b], AF.Copy)
            self.rw_out(ob, u_ob, b, "G", tt)
            for z in range(2):
                b = n % 4; n += 1
                lora(xw, u_xw, w1[z][0], w1[z][1], 64, AF.Tanh, w2[z][0], w2[z][1], w0hl, u_w0, z, ob[b], u_ob[b], AF.Sigmoid)
                self.rw_out(ob, u_ob, b, "LW%d" % z, tt)
                lora(xa, u_xa, a1[z][0], a1[z][1], 64, AF.Copy, a2[z][0], a2[z][1], a0hl, u_a0, z, az, u_az, AF.Sigmoid)
                b = n % 4; n += 1
                P.op("dve", lambda e, b=b: e.tensor_tensor(out=ob[b][:], in0=az[:], in1=kab[:], op=ALU.mult),
                     reads=[u_az, u_kab], writes=[u_ob[b]])
                P.op("dve", lambda e, b=b: e.tensor_tensor(out=ob[b][:], in0=ob[b][:], in1=c1b[:], op=ALU.add),
                     reads=[u_ob[b], u_c1b], writes=[u_ob[b]])
                P.op("dve", lambda e, b=b: e.tensor_tensor(out=ob[b][:], in0=ob[b][:], in1=kin[:], op=ALU.mult),
                     reads=[u_ob[b], u_kin], writes=[u_ob[b]])
                self.rw_out(ob, u_ob, b, "KT%d" % z, tt)
                b = n % 4; n += 1
                P.op("dve", lambda e, b=b: e.tensor_tensor(out=ob[b][:], in0=az[:], in1=kkin[:], op=ALU.mult),
                     reads=[u_az, u_kkin], writes=[u_ob[b]])
                self.rw_out(ob, u_ob, b, "B%d" % z, tt)
        P.phase_end()

    def rw_scan(self, z, do_ctx):
        P = self.P
        I = self.I
        for nm in ("rw_r_k", "rw_ln_g", "rw_ln_b"):
            I(nm)
        P.phase_begin()
        fwd = z == 0
        tri = P.psb("tri", [128, 128], F32); mA = P.psb("mA", [128, 4, 128], F32); mN = P.psb("mN", [128, 128], F32)
        cvec = P.psb("cvec", [128, 2], F32); u_mk = U("masks")
        cm, pat = (-1, 1) if fwd else (1, -1)
        P.op("pool", lambda e: e.memset(tri[:], C0), writes=[u_mk])
        P.op("pool", lambda e: e.memset(mA[:], 1.0), writes=[u_mk])
        P.op("pool", lambda e: e.memset(mN[:], 1.0), writes=[u_mk])
        P.op("pool", lambda e: e.memset(cvec[:], C0), writes=[u_mk])
        P.op("pool", lambda e: e.affine_select(out=tri[:], in_=tri[:], pattern=[[pat, 128]], compare_op=ALU.is_ge, fill=0.0,
                                               base=0, channel_multiplier=cm), reads=[u_mk], writes=[u_mk])
        for q in range(4):
            P.op("pool", lambda e, q=q: e.affine_select(out=mA[:, q, :], in_=mA[:, q, :], pattern=[[pat, 128]],
                                                        compare_op=ALU.is_ge, fill=0.0, base=-(q % 2), channel_multiplier=cm),
                 reads=[u_mk], writes=[u_mk])
        P.op("pool", lambda e: e.affine_select(out=mN[:], in_=mN[:], pattern=[[-pat, 128]], compare_op=ALU.is_ge, fill=0.0,
                                               base=-1, channel_multiplier=-cm), reads=[u_mk], writes=[u_mk])
        Hst = P.psb("Hst", [64, 16, 64], F32); u_H = U("Hst")
        P.op("pool", lambda e: e.memset(Hst[:], 0.0), writes=[u_H])
        names = ("R", "KK", "V", "LW%d" % z, "B%d" % z, "KT%d" % z)
        tin = {nm: P.psb("in_" + nm, [128, D], F32) for nm in names}
        u_in = {nm: U("in_" + nm) for nm in names}
        r, kk, v, sg, bb, kt = (tin[nm] for nm in names)
        u_r, u_kk, u_v, u_sg, u_bb, u_kt = (u_in[nm] for nm in names)
        E = [P.psb("E", [128, D], F32) for _ in range(3)]; u_E = [U("E0"), U("E1"), U("E2")]
        D3 = P.psb("D3", [128, D], F32); u_D3 = U("D3")
        F4 = [E[0], E[1], E[2], D3]; u_F4 = [u_E[0], u_E[1], u_E[2], u_D3]
        FT = P.psb("FT", [64, 8, 4, 128], F32); u_FT = [U("FT%d" % j) for j in range(8)]
        AA = P.psb("AA", [128, 8, 4, 128], F32); u_AA = [U("AA%d" % j) for j in range(8)]
        Nn = P.psb("Nn", [128, 8, 128], F32); u_Nn = [U("Nn0"), U("Nn1")]
        MB = [P.psb("MB", [128, 4, 128], F32) for _ in range(2)]; u_MB = [U("MB0"), U("MB1")]
        NB = [P.psb("NB", [128, 4, 128], F32) for _ in range(2)]; u_NB = [U("NB0"), U("NB1")]
        if True:
            MB2 = [P.psb("MB2", [128, 4, 128], F32) for _ in range(2)]; u_MB2 = [U("MB20"), U("MB21")]
            NB2 = [P.psb("NB2", [128, 4, 128], F32) for _ in range(2)]; u_NB2 = [U("NB20"), U("NB21")]
        else:
            MB2, u_MB2, NB2, u_NB2 = MB, u_MB, NB, u_NB
        MBg = [MB, MB2]; u_MBg = [u_MB, u_MB2]; NBg = [NB, NB2]; u_NBg = [u_NB, u_NB2]
        Pm = P.psb("Pm", [128, 8, 128], F32); u_Pm = [U("Pm0"), U("Pm1")]
        X = P.psb("X", [128, 512], F32); u_X = U("X")
        nU = P.psb("nU", [128, 512], F32); u_nU = U("nU")
        ysb = P.psb("ysb", [128, D], F32); u_y = U("ysb")
        gl = P.psb("gl", [64, 16], F32); u_gl = U("gl")
        if not fwd:
            rkb, u_rkb = self.bload("rkb", I("rw_r_k")[0:1, :], D)
            lng, u_lng = self.bload("lng", I("rw_ln_g")[0:1, :], D)
            lnb, u_lnb = self.bload("lnb", I("rw_ln_b")[0:1, :], D)
            st = P.psb("st", [128, 48], F32); u_st = U("st")
            obf = P.psb("obf", [128, D], BF16); u_obf = U("obf")
            stg = P.psb("stg", [128, 8, 128], BF16); u_stg = U("stg")
        order = list(range(NTT)) if fwd else [1, 0] + list(range(NTT - 1, 1, -1))
        cut = self.cfg.get("scan_cut", 99)
        if "scan_tiles" in self.cfg:
            order = order[:self.cfg["scan_tiles"]]
        v3 = lambda t: t[:].rearrange("p (h n) -> p h n", n=64)
        for tt in order:
            for nm in names:
                P.dma("sp", lambda e, nm=nm, tt=tt: e.dma_start(out=tin[nm][:], in_=self.rwd[nm][tt * 128:(tt + 1) * 128, :]),
                      reads=[self.u_rwd[nm]], writes=[u_in[nm]])
            if cut < -1:
                continue
            for half in range(2):
                hs = slice(half * 512, (half + 1) * 512)
                ps, ups = self.nps()
                self.mm(ps[:, :], tri[:], sg[:, hs], True, True, [u_mk, u_sg], [ups])
                P.op("dve", lambda e, ps=ps, hs=hs: e.tensor_copy(out=E[0][:, hs], in_=ps[:, :]), reads=[ups], writes=[u_E[0]])
                P.op("dve", lambda e, hs=hs: e.scalar_tensor_tensor(out=E[1][:, hs], in0=sg[:, hs], scalar=-C0, in1=E[0][:, hs],
                                                                    op0=ALU.mult, op1=ALU.add), reads=[u_E[0], u_sg], writes=[u_E[1]])
            P.op("act", lambda e: e.activation(out=E[2][:], in_=E[0][:], func=AF.Exp, scale=-1.0), reads=[u_E[0]], writes=[u_E[2]])
            P.op("act", lambda e: e.activation(out=E[0][:], in_=E[0][:], func=AF.Exp), reads=[u_E[0], u_E[2], u_E[1]], writes=[u_E[0]])
            P.op("act", lambda e: e.activation(out=E[1][:], in_=E[1][:], func=AF.Exp), reads=[u_E[1]], writes=[u_E[1]])
            if cut < 0:
                continue
            for j, (src, us, ex) in ((3, (kt, u_kt, 2)), (0, (r, u_r, 0)), (1, (kk, u_kk, 1)), (2, (bb, u_bb, 2))):
                eng = "dve" if j % 2 == 0 else "pool"
                P.op(eng, lambda e, j=j, src=src, ex=ex: e.tensor_tensor(out=F4[j][:], in0=src[:], in1=E[ex][:], op=ALU.mult),
                     reads=[us, u_E[ex]], writes=[u_F4[j]])
            if cut < 1:
                continue
            psG, upsG = self.nps()
            for h in range(16):
                self.mm(psG[0:64, 2 * h:2 * h + 2], sg[:, h * 64:(h + 1) * 64], cvec[:, 0:2], True, True, [u_sg, u_mk], [upsG])
            P.op("dve", lambda e, psG=psG: e.tensor_copy(
                out=gl[:], in_=psG[0:64, 0:32].rearrange("p (h two) -> p h two", two=2)[:, :, 0]), reads=[upsG], writes=[u_gl])
            P.op("act", lambda e: e.activation(out=gl[:], in_=gl[:], func=AF.Exp), reads=[u_gl], writes=[u_gl])
            if cut < 2:
                continue
            for half in range(2):
                for hh in range(8):
                    h = half * 8 + hh
                    ps, ups = self.nps()
                    for j in range(4):
                        self.tr(ps[0:64, j * 128:(j + 1) * 128], F4[j][:, h * 64:(h + 1) * 64], self.ident_f[:],
                                [u_F4[j], self.u_const], [ups])
                    eng = "dve"
                    if eng == "act":
                        P.op("act", lambda e, ps=ps, hh=hh: e.copy(out=FT[:, hh].rearrange("p a t -> p (a t)"), in_=ps[0:64, :]),
                             reads=[ups], writes=[u_FT[hh]])
                    else:
                        P.op("dve", lambda e, ps=ps, hh=hh: e.tensor_copy(out=FT[:, hh].rearrange("p a t -> p (a t)"), in_=ps[0:64, :]),
                             reads=[ups], writes=[u_FT[hh]])
                if cut < 3:
                    continue
                for hh in range(8):
                    ps, ups = self.nps()
                    rhs2 = FT[:, hh, 0:2, :]
                    self.mm(ps[:, 0:256].rearrange("p (a t) -> p a t", a=2), FT[:, hh, 2, :], rhs2, True, True, [u_FT[hh]], [ups])
                    self.mm(ps[:, 256:512].rearrange("p (a t) -> p a t", a=2), FT[:, hh, 3, :], rhs2, True, True, [u_FT[hh]], [ups])
                    P.op("dve", lambda e, ps=ps, hh=hh: e.tensor_tensor(out=AA[:, hh], in0=ps[:, :].rearrange("p (a t) -> p a t", a=4),
                                                                        in1=mA[:], op=ALU.mult), reads=[ups, u_mk], writes=[u_AA[hh]])
                for g in range(2):
                    ps, ups = self.nps()
                    for j in range(4):
                        hh = g * 4 + j
                        self.mm(ps[:, j * 128:(j + 1) * 128], FT[:, hh, 1, :], FT[:, hh, 2, :], True, True, [u_FT[hh]], [ups])
                    P.op("dve", lambda e, ps=ps, g=g: e.tensor_tensor(
                        out=Nn[:, g * 4:(g + 1) * 4, :], in0=ps[:, :].rearrange("p (a t) -> p a t", a=4),
                        in1=mN[:].unsqueeze(1).to_broadcast([128, 4, 128]), op=ALU.mult), reads=[ups, u_mk], writes=[u_Nn[g]])
                if cut < 4:
                    continue
                stt = {}
                for g in range(2):
                    grp = [g * 4 + j for j in range(4)]
                    P.op("dve", lambda e, g=g: e.tensor_tensor(
                        out=Pm[:, g * 4:(g + 1) * 4, :], in0=self.ident_f[:].unsqueeze(1).to_broadcast([128, 4, 128]),
                        in1=AA[:, g * 4:(g + 1) * 4, 1, :], op=ALU.subtract), reads=[u_AA[hh] for hh in grp] + [self.u_const],
                        writes=[u_Pm[g]])
                    stt[g] = ([AA[:, hh, 1, :] for hh in grp], [u_AA[hh] for hh in grp], [Nn[:, hh, :] for hh in grp], [u_Nn[g]])
                for gs in ([0, 1],):
                    for li in range(6):
                        lastl = li == 5
                        pp = li % 2
                        banks = {}
                        for g in gs:
                            Ms, uM, Ns, uN = stt[g]
                            bM = ubM = None
                            if not lastl:
                                bM, ubM = self.nps()
                                for j in range(4):
                                    self.mm(bM[:, j * 128:(j + 1) * 128], Ns[j], Ms[j], True, True, uM + uN, [ubM])
                            bN, ubN = self.nps()
                            for j in range(4):
                                self.mm(bN[:, j * 128:(j + 1) * 128], Ms[j], Ns[j], True, True, uM + uN, [ubN])
                            banks[g] = (bM, ubM, bN, ubN)
                        for g in gs:
                            bM, ubM, bN, ubN = banks[g]
                            mb, umb = MBg[g][pp], u_MBg[g][pp]
                            nb_, unb = NBg[g][pp], u_NBg[g][pp]
                            if not lastl:
                                P.op("dve", lambda e, bM=bM, mb=mb: e.tensor_copy(out=mb[:].rearrange("p a t -> p (a t)"), in_=bM[:, :]),
                                     reads=[ubM], writes=[umb])
                            P.op("dve", lambda e, bN=bN, nb_=nb_: e.tensor_copy(out=nb_[:].rearrange("p a t -> p (a t)"), in_=bN[:, :]),
                                 reads=[ubN], writes=[unb])
                            stt[g] = ([mb[:, j, :] for j in range(4)], [umb], [nb_[:, j, :] for j in range(4)], [unb])
                        for g in gs:
                            Ms, uM, Ns, uN = stt[g]
                            bP, ubP = self.nps()
                            for j in range(4):
                                self.mm(bP[:, j * 128:(j + 1) * 128], Ns[j], Pm[:, g * 4 + j, :], True, True, uN + [u_Pm[g]], [ubP])
                            P.op("dve", lambda e, bP=bP, g=g: e.tensor_tensor(
                                out=Pm[:, g * 4:(g + 1) * 4, :], in0=Pm[:, g * 4:(g + 1) * 4, :],
                                in1=bP[:, :].rearrange("p (a t) -> p a t", a=4), op=ALU.add), reads=[ubP, u_Pm[g]], writes=[u_Pm[g]])
                if cut < 5:
                    continue
                ps, ups = self.nps()
                for hh in range(8):
                    h = half * 8 + hh
                    self.mm(ps[:, hh * 64:(hh + 1) * 64], FT[:, hh, 1, :], Hst[:, h, :], True, False, [u_FT[hh], u_H], [ups])
                    self.mm(ps[:, hh * 64:(hh + 1) * 64], AA[:, hh, 3, :], v[:, h * 64:(h + 1) * 64], False, True, [u_AA[hh], u_v], [ups])
                P.op("dve", lambda e, ps=ps: e.tensor_copy(out=X[:], in_=ps[:, :]), reads=[ups], writes=[u_X])
                ps, ups = self.nps()
                for hh in range(8):
                    self.mm(ps[:, hh * 64:(hh + 1) * 64], Pm[:, hh, :], X[:, hh * 64:(hh + 1) * 64], True, True, [u_Pm[hh // 4], u_X], [ups])
                P.op("dve", lambda e, ps=ps: e.tensor_scalar(out=nU[:], in0=ps[:, :], scalar1=-1.0, scalar2=None, op0=ALU.mult),
                     reads=[ups], writes=[u_nU])
                if cut < 6:
                    continue
                ps, ups = self.nps()
                for hh in range(8):
                    h = half * 8 + hh
                    o = ps[:, hh * 64:(hh + 1) * 64]
                    self.mm(o, FT[:, hh, 0, :], Hst[:, h, :], True, False, [u_FT[hh], u_H], [ups])
                    self.mm(o, AA[:, hh, 2, :], v[:, h * 64:(h + 1) * 64], False, False, [u_AA[hh], u_v], [ups])
                    self.mm(o, AA[:, hh, 0, :], nU[:, hh * 64:(hh + 1) * 64], False, True, [u_AA[hh], u_nU], [ups])
                P.op("dve", lambda e, ps=ps, half=half: e.tensor_copy(out=ysb[:, half * 512:(half + 1) * 512], in_=ps[:, :]),
                     reads=[ups], writes=[u_y])
                if cut < 7:
                    continue
                ps, ups = self.nps()
                for hh in range(8):
                    h = half * 8 + hh
                    o = ps[0:64, hh * 64:(hh + 1) * 64]
                    hc = slice(h * 64, (h + 1) * 64)
                    self.mm(o, F4[3][:, hc], v[:, hc], True, False, [u_F4[3], u_v], [ups])
                    self.mm(o, F4[2][:, hc], nU[:, hh * 64:(hh + 1) * 64], False, False, [u_F4[2], u_nU], [ups])
                    self.mm(o, self.ident_f[0:64, 0:64], Hst[:, h, :], False, True, [u_H, self.u_const], [ups])
                P.op("dve", lambda e, ps=ps, half=half: e.tensor_tensor(
                    out=Hst[:, half * 8:(half + 1) * 8, :], in0=ps[0:64, :].rearrange("p (a t) -> p a t", a=8),
                    in1=gl[:, half * 8:(half + 1) * 8].unsqueeze(2).to_broadcast([64, 8, 64]), op=ALU.mult),
                    reads=[ups, u_gl], writes=[u_H])
            if cut < 8:
                continue
            if fwd:
                P.dma("pool", lambda e, tt=tt: e.dma_start(out=self.rwd["Y"][tt * 128:(tt + 1) * 128, :], in_=ysb[:]),
                      reads=[u_y], writes=[self.u_rwd["Y"]])
                continue
            if tt < 2 and not do_ctx:
                continue
            yf, u_yf = kk, u_kk
            P.dma("sp", lambda e, tt=tt: e.dma_start(out=yf[:], in_=self.rwd["Y"][tt * 128:(tt + 1) * 128, :]),
                  reads=[self.u_rwd["Y"]], writes=[u_yf])
            kt0, u_kt0 = sg, u_sg
            P.dma("sp", lambda e, tt=tt: e.dma_start(out=kt0[:], in_=self.rwd["KT0"][tt * 128:(tt + 1) * 128, :]),
                  reads=[self.u_rwd["KT0"]], writes=[u_kt0])
            gg, u_gg = bb, u_bb
            P.dma("sp", lambda e, tt=tt: e.dma_start(out=gg[:], in_=self.rwd["G"][tt * 128:(tt + 1) * 128, :]),
                  reads=[self.u_rwd["G"]], writes=[u_gg])
            T0, T1, T2, T3 = F4
            uT0, uT1, uT2, uT3 = u_F4
            P.op("dve", lambda e: e.tensor_tensor(out=ysb[:], in0=ysb[:], in1=yf[:], op=ALU.add), reads=[u_y, u_yf], writes=[u_y])
            P.op("dve", lambda e: e.tensor_reduce(out=st[:, 0:16], in_=v3(ysb), axis=AX.X, op=ALU.add), reads=[u_y], writes=[u_st])
            P.op("dve", lambda e: e.tensor_scalar(out=st[:, 0:16], in0=st[:, 0:16], scalar1=1.0 / 64, scalar2=None, op0=ALU.mult),
                 reads=[u_st], writes=[u_st])
            P.op("dve", lambda e: e.tensor_tensor(out=v3(T0), in0=v3(ysb), in1=st[:, 0:16].unsqueeze(2).to_broadcast([128, 16, 64]),
                                                  op=ALU.subtract), reads=[u_y, u_st], writes=[uT0])
            P.op("dve", lambda e: e.tensor_tensor(out=T1[:], in0=T0[:], in1=T0[:], op=ALU.mult), reads=[uT0], writes=[uT1])
            P.op("dve", lambda e: e.tensor_reduce(out=st[:, 16:32], in_=v3(T1), axis=AX.X, op=ALU.add), reads=[uT1], writes=[u_st])
            P.op("dve", lambda e: e.tensor_scalar(out=st[:, 16:32], in0=st[:, 16:32], scalar1=1.0 / 64, scalar2=64e-5,
                                                  op0=ALU.mult, op1=ALU.add), reads=[u_st], writes=[u_st])
            P.op("act", lambda e: e.sqrt(out=st[:, 16:32], in_=st[:, 16:32]), reads=[u_st], writes=[u_st])
            P.op("dve", lambda e: e.reciprocal(out=st[:, 16:32], in_=st[:, 16:32]), reads=[u_st], writes=[u_st])
            P.op("dve", lambda e: e.tensor_tensor(out=v3(T0), in0=v3(T0), in1=st[:, 16:32].unsqueeze(2).to_broadcast([128, 16, 64]),
                                                  op=ALU.mult), reads=[uT0, u_st], writes=[uT0])
            P.op("dve", lambda e: e.tensor_tensor(out=T0[:], in0=T0[:], in1=lng[:], op=ALU.mult), reads=[uT0, u_lng], writes=[uT0])
            P.op("dve", lambda e: e.tensor_tensor(out=T0[:], in0=T0[:], in1=lnb[:], op=ALU.add), reads=[uT0, u_lnb], writes=[uT0])
            P.op("pool", lambda e: e.tensor_tensor(out=T1[:], in0=r[:], in1=rkb[:], op=ALU.mult), reads=[u_r, u_rkb], writes=[uT1])
            P.op("pool", lambda e: e.tensor_tensor(out=T2[:], in0=kt[:], in1=kt0[:], op=ALU.add), reads=[u_kt, u_kt0], writes=[uT2])
            P.op("dve", lambda e: e.tensor_tensor(out=T2[:], in0=T2[:], in1=T1[:], op=ALU.mult), reads=[uT1, uT2], writes=[uT2])
            P.op("dve", lambda e: e.tensor_reduce(out=st[:, 32:48], in_=v3(T2), axis=AX.X, op=ALU.add), reads=[uT2], writes=[u_st])
            P.op("dve", lambda e: e.tensor_tensor(out=v3(T3), in0=v3(v), in1=st[:, 32:48].unsqueeze(2).to_broadcast([128, 16, 64]),
                                                  op=ALU.mult), reads=[u_v, u_st], writes=[uT3])
            P.op("dve", lambda e: e.tensor_tensor(out=T0[:], in0=T0[:], in1=T3[:], op=ALU.add), reads=[uT0, uT3], writes=[uT0])
            P.op("dve", lambda e: e.tensor_tensor(out=obf[:], in0=T0[:], in1=gg[:], op=ALU.mult), reads=[uT0, u_gg], writes=[u_obf])
            self.tpose_to_dram([obf[:, j * 128:(j + 1) * 128] for j in range(8)], u_obf, 128,
                               self.OTd[0:1024, tt * 128:(tt + 1) * 128].rearrange("(j p) t -> p j t", p=128),
                               self.u_OT, stg, u_stg)
        P.phase_end()

    def rwkv(self, i, do_ctx):
        stop = self.cfg.get("rw_stop", 9)
        self.rw_scratch()
        self.rw_proj_a()
        if stop >= 2:
            self.rw_proj_b()
        if stop >= 3:
            self.rw_scan(0, do_ctx)
        if stop >= 4:
            self.rw_scan(1, do_ctx)
            self.outproj_phase(i, "rw_w_o", do_ctx)
        if self.cfg.get("rw_dump"):
            P = self.P
            for nm in self.cfg["rw_dump"]:
                o = P.dram("dump_" + nm, [NT, D], F32, kind="ExternalOutput")
                for j in range(2):
                    P.dma("sp", lambda e, o=o, nm=nm, j=j: e.dma_start(out=o[j * 2176:(j + 1) * 2176, :],
                                                                      in_=self.rwd[nm][j * 2176:(j + 1) * 2176, :]),
                          reads=[self.u_rwd[nm]], is_out=True)


for _n, _f in list(vars(KRw).items()):
    if callable(_f):
        setattr(K, _n, _f)
IN_SHAPES = None


def build_program(cfg):
    k = K(cfg)
    k.prologue()
    if cfg.get("only_scan") is not None:
        k.mix_scratch()
        k.rw_scratch()
        k.rw_scan(cfg["only_scan"], True)
        return k, k.epilogue()
    for i in cfg.get("layers", [0, 1, 2, 3]):
        do_ctx = i < 3 or cfg.get("force_ctx", False)
        if not cfg.get("skip_ada"):
            k.ada_phase(i)
        if not cfg.get("skip_mixer"):
            k.mixer(i, do_ctx)
        if not cfg.get("skip_moe"):
            tiles = None if do_ctx else list(range(2, NTT))
            k.norm_phase(i, 1, True, tiles)
            k.moe_phase(i, do_ctx)
    nc = k.epilogue()
    return k, nc


def rope_tables(d_rot):
    t = np.arange(NLAT)
    row = (t // 64).astype(np.float32)
    col = (t % 64).astype(np.float32)
    d_axis = d_rot // 2
    inv = (np.float32(10000.0) ** (-np.arange(0, d_axis, 2, dtype=np.float32) / np.float32(d_axis))).astype(np.float32)
    ang = np.concatenate([row[:, None] * inv, col[:, None] * inv], axis=-1).astype(np.float32)
    return np.cos(ang).astype(np.float32), np.sin(ang).astype(np.float32)


def na_bias_table(rpb):
    rpb = np.asarray(rpb, dtype=np.float32)
    kl = np.arange(128) // 64
    kc = np.arange(128) % 64
    c = np.arange(64)
    cstart = np.clip(c - 8, 0, 48)
    ok = (kc[:, None] >= cstart[None, :]) & (kc[:, None] < cstart[None, :] + 16)
    dcol = np.clip(kc[:, None] - c[None, :] + 15, 0, 30)
    out = np.empty((16, 128, 14, 64), np.float32)
    for di in range(14):
        dr = np.clip(di - 7 + kl + 7, 0, 14)
        g = rpb[:, dr[:, None], dcol]
        out[:, :, di, :] = np.where(ok[None], g, np.float32(-30000.0))
    return out


def make_in_maps(inputs, names):
    f = np.ascontiguousarray
    shared = {}
    for nm in names:
        if nm in ("x", "c", "ctx"):
            continue
        if nm in ("mla_cos", "mla_sin", "swa_cos", "swa_sin"):
            cs, sn = rope_tables(32 if nm.startswith("mla") else 64)
            shared[nm] = f(cs if nm.endswith("cos") else sn)
            continue
        if nm == "na_bias":
            shared[nm] = f(na_bias_table(inputs["na_rpb"][0]))
            continue
        a = np.asarray(inputs[nm], dtype=np.float32)
        if nm == "c_ctx":
            a = a.reshape(8, 128)
        elif nm in ("norm1_g", "norm2_g"):
            a = a.reshape(4, 8, 128)
        elif nm == "ada_b":
            a = a.reshape(4, 48, 128)
        elif nm.startswith(("na_", "rw_", "mla_", "swa_")):
            a = a[0]
            if nm == "rw_mix":
                a = a.reshape(48, 128)
            elif nm == "rw_r_k":
                a = a.reshape(1, -1)
            if a.ndim == 1:
                a = a.reshape(1, -1)
        shared[nm] = f(a)
    maps = []
    for b in range(8):
        m = dict(shared)
        m["x"] = f(np.asarray(inputs["x"][b], dtype=np.float32))
        m["c"] = f(np.asarray(inputs["c"][b], dtype=np.float32).reshape(8, 128))
        m["ctx"] = f(np.asarray(inputs["ctx"][b], dtype=np.float32))
        maps.append(m)
    return maps


def run(inputs, cfg):
    k, nc = build_program(cfg)
    maps = make_in_maps(inputs, list(k.inp.keys()))
    res = run_bass_kernel_spmd(nc, maps, core_ids=list(range(8)))
    return res


def kernel(**inputs):
    res = run(inputs, {})
    return np.stack([np.asarray(r["out"], dtype=np.float32) for r in res.results], axis=0)
```

```python
from concourse.bass_utils import run_bass_kernel_spmd
import contextlib
import numpy as np
import concourse.bass as bass
import concourse.mybir as mybir

F32 = mybir.dt.float32
BF16 = mybir.dt.bfloat16
I32 = mybir.dt.int32
U32 = mybir.dt.uint32
AF = mybir.ActivationFunctionType
ALU = mybir.AluOpType
AX = mybir.AxisListType

SEM_CAP = 30000
DMA_POOL = 12


class U:
    __slots__ = ("name", "w", "r")

    def __init__(self, name):
        self.name = name
        self.w = None
        self.r = {}


class Prog:
    def __init__(self):
        self.nc = bass.Bass("TRN2", target_bir_lowering=False)
        self.es = contextlib.ExitStack()
        self.ops = {e: [] for e in ("pe", "dve", "act", "pool", "sp")}
        self.cnt = {e: 0 for e in self.ops}
        self.ep = {e: 0 for e in self.ops}
        self.sems = {}
        self.waited = {e: {} for e in self.ops}
        self.dma_n = {e: 0 for e in self.ops}
        self.dma_val = {}
        self.n_inst = 0
        self.out_ticks = []

    def sb(self, name, shape, dt):
        return self.es.enter_context(self.nc.sbuf_tensor(name, list(shape), dt))

    def ps(self, name, shape, dt=F32):
        return self.es.enter_context(self.nc.psum_tensor(name, list(shape), dt))

    def dram(self, name, shape, dt, kind="Internal"):
        return self.nc.dram_tensor(name, list(shape), dt, kind=kind).ap()

    def _sem(self, key):
        if key not in self.sems:
            self.sems[key] = self.es.enter_context(self.nc.semaphore("s_%s_%s" % key))
        return self.sems[key]

    def _wait(self, eng, tick):
        if tick is None:
            return
        key, val = tick
        if self.waited[eng].get(key, 0) >= val:
            return
        self.waited[eng][key] = val
        sem = self._sem(key)
        self.ops[eng].append(lambda e, sem=sem, val=val: e.wait_ge(sem, val))

    def _deps(self, eng, reads, writes, skip_self=False):
        ticks = []
        for u in reads:
            if u.w is not None:
                ticks.append(u.w)
        for u in writes:
            if u.w is not None:
                ticks.append(u.w)
            for k, v in u.r.items():
                ticks.append((k, v))
        mykey = (eng, self.ep[eng])
        for t in ticks:
            if skip_self and t[0] == mykey:
                continue
            self._wait(eng, t)

    def _mark(self, tick, reads, writes):
        k, v = tick
        for u in reads:
            if u.r.get(k, 0) < v:
                u.r[k] = v
        for u in writes:
            u.w = tick
            u.r = {}

    def op(self, eng, fn, reads=(), writes=(), skip_self=False):
        reads = list(reads)
        writes = list(writes)
        self._deps(eng, reads, writes, skip_self=skip_self)
        if self.cnt[eng] >= SEM_CAP:
            self.ep[eng] += 1
            self.cnt[eng] = 0
        self.cnt[eng] += 1
        key = (eng, self.ep[eng])
        sem = self._sem(key)
        val = self.cnt[eng]
        self.ops[eng].append(lambda e, fn=fn, sem=sem: fn(e).then_inc(sem, 1))
        self._mark((key, val), reads, writes)
        self.n_inst += 1
        return (key, val)

    def dma(self, q, fn, reads=(), writes=(), is_out=False):
        reads = list(reads)
        writes = list(writes)
        self._deps(q, reads, writes)
        slot = self.dma_n[q] % DMA_POOL
        self.dma_n[q] += 1
        key = ("d" + q, slot)
        prev = self.dma_val.get(key, 0)
        if prev >= SEM_CAP:
            gen = 1
            while ("d%s_g%d" % (q, gen), slot) in self.dma_val and \
                    self.dma_val[("d%s_g%d" % (q, gen), slot)] >= SEM_CAP:
                gen += 1
            raise RuntimeError("dma semaphore cap reached")
        if prev:
            self._wait(q, (key, prev))
        val = prev + 16
        self.dma_val[key] = val
        sem = self._sem(key)
        self.ops[q].append(lambda e, fn=fn, sem=sem: fn(e).then_inc(sem, 16))
        self._mark((key, val), reads, writes)
        self.n_inst += 1
        if is_out:
            self.out_ticks.append((key, val))
        return (key, val)

    def barrier(self):
        ticks = []
        for e in self.ops:
            if self.cnt[e] > 0:
                ticks.append(((e, self.ep[e]), self.cnt[e]))
        for key, val in self.dma_val.items():
            ticks.append((key, val))
        for e in self.ops:
            for t in ticks:
                if t[0][0] == e:
                    continue
                self._wait(e, t)

    def phase_begin(self):
        self.pes = contextlib.ExitStack()

    def psb(self, name, shape, dt):
        self.uid = getattr(self, "uid", 0) + 1
        return self.pes.enter_context(self.nc.sbuf_tensor("%s_%d" % (name, self.uid), list(shape), dt))

    def phase_end(self):
        self.barrier()
        self.flush()
        self.pes.close()

    def flush(self):
        nc = self.nc
        with nc.Block() as block:
            @block.tensor
            def _(e):
                for f in self.ops["pe"]:
                    f(e)

            @block.vector
            def _(e):
                for f in self.ops["dve"]:
                    f(e)

            @block.scalar
            def _(e):
                for f in self.ops["act"]:
                    f(e)

            @block.gpsimd
            def _(e):
                for f in self.ops["pool"]:
                    f(e)

            @block.sync
            def _(e):
                for f in self.ops["sp"]:
                    f(e)
        for e in self.ops:
            self.ops[e] = []

    def finish(self):
        for t in self.out_ticks:
            self._wait("sp", t)
        self.flush()
        self.es.close()
        return self.nc
D = 1024
NCTX = 256
NLAT = 4096
NT = NCTX + NLAT
NTT = NT // 128
EPS = 1e-6


class K:
    def __init__(self, cfg):
        self.cfg = cfg
        self.P = P = Prog()
        self.inp = {}
        self.uin = {}
        self.build_io()
        self.setup_persistent()

    SHAPES = {
        "x": [NLAT, D], "c": [8, 128], "ctx": [NCTX, D], "c_ctx": [8, 128],
        "norm1_g": [4, 8, 128], "norm2_g": [4, 8, 128], "ada_w": [4, D, 6 * D], "ada_b": [4, 48, 128],
        "moe_router": [4, D, 16], "moe_w1": [4, 16, D, D], "moe_w3": [4, 16, D, D], "moe_w2": [4, 16, D, D],
        "na_w_qkv": [D, 3 * D], "na_q_g": [1, 64], "na_k_g": [1, 64], "na_bias": [16, 128, 14, 64], "na_w_o": [D, D],
        "mla_w_down": [D, 672], "mla_q_norm_g": [1, 384], "mla_kv_norm_g": [1, 256], "mla_w_uq": [384, 1536],
        "mla_w_ukv": [256, 2048], "mla_qn_g": [1, 64], "mla_qr_g": [1, 32], "mla_kn_g": [1, 64], "mla_kr_g": [1, 32],
        "mla_w_o": [D, D], "mla_cos": [NLAT, 16], "mla_sin": [NLAT, 16],
        "swa_w_qkv": [D, 1536], "swa_q_g": [1, 64], "swa_k_g": [1, 64], "swa_sink": [1, 16], "swa_w_o": [D, D],
        "swa_cos": [NLAT, 32], "swa_sin": [NLAT, 32],
        "rw_mix": [48, 128], "rw_w_r": [D, D], "rw_w_k": [D, D], "rw_w_v": [D, D], "rw_w0": [2, D], "rw_w1": [2, D, 64],
        "rw_w2": [2, 64, D], "rw_a0": [2, D], "rw_a1": [2, D, 64], "rw_a2": [2, 64, D], "rw_g1": [D, 128], "rw_g2": [128, D],
        "rw_k_k": [1, D], "rw_k_a": [1, D], "rw_r_k": [1, D], "rw_ln_g": [1, D], "rw_ln_b": [1, D], "rw_w_o": [D, D],
    }

    def build_io(self):
        P = self.P
        self.out = P.dram("out", [NLAT, D], F32, kind="ExternalOutput")
        self.xres = P.dram("xres", [NT, D], F32); self.u_xres = U("xres")
        self.xs2 = P.dram("xs2", [NT, D], BF16); self.u_xs2 = U("xs2")
        self.modd = P.dram("modd", [1, 4 * 96 * 128], F32); self.u_modd = U("modd")

    def I(self, name):
        if name not in self.inp:
            self.inp[name] = self.P.dram(name, self.SHAPES[name], F32, kind="ExternalInput")
        return self.inp[name]

    def setup_persistent(self):
        P = self.P
        self.ident_f = P.sb("ident_f", [128, 128], F32); self.u_const = U("const")
        self.ident_b = P.sb("ident_b", [128, 128], BF16)
        self.ones_f = P.sb("ones_f", [128, 128], F32)
        self.ones_b = P.sb("ones_b", [128, 128], BF16)
        self.scT = P.sb("scT", [128, 8, 2], F32); self.u_scT = U("scT")
        self.AB = P.sb("AB", [128, 16, 8, 2], F32); self.u_AB = U("AB")
        self.hT = P.sb("hT", [128, 8, NT], BF16); self.u_hT = [U("hT%d" % t) for t in range(NTT)]
        self.gateT = P.sb("gateT", [128, 5, 16], F32)
        self.idxT = P.sb("idxT", [128, 5, 16], I32)
        self.psf = [P.ps("psf%d" % i, [128, 512], F32) for i in range(6)]
        self.u_psf = [U("psf%d" % i) for i in range(6)]
        self.psb = [P.ps("psb%d" % i, [128, 1024], BF16) for i in range(2)]
        self.u_psb = [U("psb%d" % i) for i in range(2)]
        self.psf_n = 0
        self.psb_n = 0
        uc = self.u_const
        P.op("pool", lambda e: e.memset(self.ident_f[:], 0.0), writes=[uc])
        P.op("pool", lambda e: e.affine_select(out=self.ident_f[:], in_=self.ident_f[:], pattern=[[-1, 128]],
                                               compare_op=ALU.not_equal, fill=1.0, base=0, channel_multiplier=1),
             reads=[uc], writes=[uc])
        P.op("pool", lambda e: e.tensor_copy(out=self.ident_b[:], in_=self.ident_f[:]), reads=[uc], writes=[uc])
        P.op("pool", lambda e: e.memset(self.ones_f[:], 1.0), writes=[uc])
        P.op("pool", lambda e: e.memset(self.ones_b[:], 1.0), writes=[uc])

    def nps(self):
        i = self.psf_n % 4
        self.psf_n += 1
        return self.psf[i], self.u_psf[i]

    def npsb(self):
        i = self.psb_n % 2
        self.psb_n += 1
        return self.psb[i], self.u_psb[i]

    def mm(self, out, lhsT, rhs, start, stop, reads, writes):
        self.P.op("pe", lambda e: e.matmul(out=out, lhsT=lhsT, rhs=rhs, start=start, stop=stop),
                  reads=reads, writes=writes, skip_self=True)

    def tr(self, out, in_, ident, reads, writes):
        self.P.op("pe", lambda e: e.transpose(out=out, in_=in_, identity=ident),
                  reads=reads, writes=writes, skip_self=True)

    def prologue(self):
        P = self.P
        I = self.I
        for nm in ("x", "c", "ctx", "c_ctx"):
            I(nm)
        P.phase_begin()
        P.dma("sp", lambda e: e.dma_start(out=self.xres[0:NCTX, :], in_=I("ctx")[:, :]), writes=[self.u_xres])
        for j in range(8):
            P.dma("sp", lambda e, j=j: e.dma_start(out=self.xres[NCTX + j * 512:NCTX + (j + 1) * 512, :],
                                                    in_=I("x")[j * 512:(j + 1) * 512, :]), writes=[self.u_xres])
        c16 = P.psb("c16", [8, 256], F32); u_c16 = U("c16")
        P.dma("sp", lambda e: e.dma_start(out=c16[:, 0:128], in_=I("c")[:, :]), writes=[u_c16])
        P.dma("sp", lambda e: e.dma_start(out=c16[:, 128:256], in_=I("c_ctx")[:, :]), writes=[u_c16])
        ps, ups = self.nps()
        for s in range(2):
            self.tr(ps[:, s * 8:(s + 1) * 8], c16[:, s * 128:(s + 1) * 128], self.ident_f[0:8, 0:8],
                    [u_c16, self.u_const], [ups])
        for s in range(2):
            P.op("act", lambda e, s=s: e.activation(out=self.scT[:, :, s], in_=ps[:, s * 8:(s + 1) * 8], func=AF.Silu),
                 reads=[ups], writes=[self.u_scT])
        P.phase_end()

    def ada_phase(self, i):
        P = self.P
        I = self.I
        for nm in ("ada_w", "ada_b", "norm1_g", "norm2_g"):
            I(nm)
        P.phase_begin()
        wbuf = [P.psb("adaw", [128, 8, 512], F32) for _ in range(3)]
        uw = [U("adaw%d" % j) for j in range(3)]
        ab48 = P.psb("ab48", [48, 128], F32); g16 = P.psb("g16", [16, 128], F32); u_ld = U("ld")
        abT = P.psb("abT", [128, 48], F32); gT = P.psb("gT", [128, 16], F32); u_T = U("T")
        modT = P.psb("modT", [128, 48, 2], F32); u_modT = U("modT")
        modTT = P.psb("modTT", [96, 128], F32); u_modTT = U("modTT")
        P.dma("sp", lambda e: e.dma_start(out=ab48[:], in_=I("ada_b")[i]), writes=[u_ld])
        P.dma("sp", lambda e: e.dma_start(out=g16[0:8, :], in_=I("norm1_g")[i]), writes=[u_ld])
        P.dma("sp", lambda e: e.dma_start(out=g16[8:16, :], in_=I("norm2_g")[i]), writes=[u_ld])
        psA, upsA = self.nps()
        self.tr(psA[:, 0:48], ab48[:], self.ident_f[0:48, 0:48], [u_ld, self.u_const], [upsA])
        self.tr(psA[:, 64:80], g16[:], self.ident_f[0:16, 0:16], [u_ld, self.u_const], [upsA])
        P.op("dve", lambda e: e.tensor_copy(out=abT[:], in_=psA[:, 0:48]), reads=[upsA], writes=[u_T])
        P.op("dve", lambda e: e.tensor_copy(out=gT[:], in_=psA[:, 64:80]), reads=[upsA], writes=[u_T])
        psM, upsM = self.nps()
        wsrc = I("ada_w")[i].rearrange("(k p) n -> p k n", p=128)
        for piece in range(12):
            b = piece % 3
            P.dma("sp", lambda e, b=b, piece=piece: e.dma_start(out=wbuf[b][:], in_=wsrc[:, :, piece * 512:(piece + 1) * 512]),
                  writes=[uw[b]])
            for ml in range(4):
                m = piece * 4 + ml
                for k in range(8):
                    self.mm(psM[:, 2 * m:2 * m + 2], wbuf[b][:, k, ml * 128:(ml + 1) * 128], self.scT[:, k, :],
                            k == 0, k == 7, [uw[b], self.u_scT], [upsM])
        P.op("dve", lambda e: e.tensor_tensor(out=modT[:], in0=psM[:, 0:96].rearrange("p (m s) -> p m s", s=2),
                                              in1=abT[:].unsqueeze(2).to_broadcast([128, 48, 2]), op=ALU.add),
             reads=[upsM, u_T], writes=[u_modT])
        AB = self.AB
        for (slot, m0, g0) in ((0, 8, 0), (2, 32, 8)):
            P.op("dve", lambda e, slot=slot, m0=m0, g0=g0: e.scalar_tensor_tensor(
                out=AB[:, i * 4 + slot], in0=modT[:, m0:m0 + 8, :], scalar=1.0,
                in1=gT[:, g0:g0 + 8].unsqueeze(2).to_broadcast([128, 8, 2]), op0=ALU.add, op1=ALU.mult),
                reads=[u_modT, u_T], writes=[self.u_AB])
        for (slot, m0) in ((1, 0), (3, 24)):
            P.op("dve", lambda e, slot=slot, m0=m0: e.tensor_copy(out=AB[:, i * 4 + slot], in_=modT[:, m0:m0 + 8, :]),
                 reads=[u_modT], writes=[self.u_AB])
        psT, upsT = self.nps()
        self.tr(psT[0:96, 0:128], modT[:].rearrange("p m s -> p (m s)"), self.ident_f[:], [u_modT, self.u_const], [upsT])
        P.op("dve", lambda e: e.tensor_copy(out=modTT[:], in_=psT[0:96, 0:128]), reads=[upsT], writes=[u_modTT])
        P.dma("sp", lambda e: e.dma_start(out=self.modd[0, i * 12288:(i + 1) * 12288].rearrange("(r p) -> r p", p=128),
                                          in_=modTT[:]), reads=[u_modTT], writes=[self.u_modd])
        P.phase_end()

    def gate_bcast_src(self, i, which, s):
        m0 = 16 if which == 0 else 40
        base = i * 12288 + (m0 * 2 + s) * 128
        v = self.modd[0:1, base:base + 8 * 256].rearrange("o (j r) -> o j r", r=256)[:, :, 0:128]
        return v.partition_broadcast(128)[:, 0]

    def norm_phase(self, i, which, write_xs2, tiles=None):
        P = self.P
        tiles = list(range(NTT)) if tiles is None else tiles
        P.phase_begin()
        xt = [P.psb("xt", [128, D], F32) for _ in range(2)]; u_xt = [U("xt0"), U("xt1")]
        xs = [P.psb("xs", [128, D], F32) for _ in range(2)]; u_xs = [U("xs0"), U("xs1")]
        xsb = [P.psb("xsb", [128, D], BF16) for _ in range(2)]; u_xsb = [U("xsb0"), U("xsb1")]
        junk = P.psb("junk", [128, D], BF16)
        ss = P.psb("ss", [128, NTT], F32); u_ss = [U("ss%d" % t) for t in range(NTT)]
        rs = P.psb("rs", [128, NTT], F32); u_rs = [U("rs%d" % t) for t in range(NTT)]
        A = self.AB[:, i * 4 + 2 * which]
        B = self.AB[:, i * 4 + 2 * which + 1]
        for n, tt in enumerate(tiles):
            s = 1 if tt < 2 else 0
            b = n % 2
            P.dma("sp", lambda e, b=b, tt=tt: e.dma_start(out=xt[b][:], in_=self.xres[tt * 128:(tt + 1) * 128, :]),
                  reads=[self.u_xres], writes=[u_xt[b]])
            P.op("act", lambda e, b=b, tt=tt: e.activation(out=junk[:], in_=xt[b][:], func=AF.Square,
                                                           accum_out=ss[:, tt:tt + 1]),
                 reads=[u_xt[b]], writes=[u_ss[tt]])
            P.op("dve", lambda e, tt=tt: e.tensor_scalar(out=rs[:, tt:tt + 1], in0=ss[:, tt:tt + 1], scalar1=1.0 / D,
                                                         scalar2=EPS, op0=ALU.mult, op1=ALU.add),
                 reads=[u_ss[tt]], writes=[u_rs[tt]])
            P.op("act", lambda e, tt=tt: e.sqrt(out=rs[:, tt:tt + 1], in_=rs[:, tt:tt + 1]),
                 reads=[u_rs[tt]], writes=[u_rs[tt]])
            P.op("dve", lambda e, tt=tt: e.reciprocal(out=rs[:, tt:tt + 1], in_=rs[:, tt:tt + 1]),
                 reads=[u_rs[tt]], writes=[u_rs[tt]])
            P.op("dve", lambda e, b=b, tt=tt: e.tensor_scalar(out=xs[b][:], in0=xt[b][:], scalar1=rs[:, tt:tt + 1],
                                                              scalar2=None, op0=ALU.mult),
                 reads=[u_xt[b], u_rs[tt]], writes=[u_xs[b]])
            if write_xs2:
                P.op("pool", lambda e, b=b: e.tensor_copy(out=xsb[b][:], in_=xs[b][:]), reads=[u_xs[b]], writes=[u_xsb[b]])
                P.dma("pool", lambda e, b=b, tt=tt: e.dma_start(out=self.xs2[tt * 128:(tt + 1) * 128, :], in_=xsb[b][:]),
                      reads=[u_xsb[b]], writes=[self.u_xs2])
            for half in range(2):
                ps, ups = self.nps()
                for kk in range(4):
                    k = half * 4 + kk
                    self.tr(ps[:, kk * 128:(kk + 1) * 128], xs[b][:, k * 128:(k + 1) * 128], self.ident_f[:],
                            [u_xs[b], self.u_const], [ups])
                for kk in range(4):
                    k = half * 4 + kk
                    P.op("act", lambda e, k=k, kk=kk, tt=tt, s=s, ps=ps: e.activation(
                        out=self.hT[:, k, tt * 128:(tt + 1) * 128], in_=ps[:, kk * 128:(kk + 1) * 128],
                        func=AF.Identity, scale=A[:, k, s:s + 1], bias=B[:, k, s:s + 1]),
                        reads=[ups, self.u_AB], writes=[self.u_hT[tt]])
        P.phase_end()

    def moe_phase(self, i, do_ctx):
        P = self.P
        I = self.I
        for nm in ("moe_router", "moe_w1", "moe_w3", "moe_w2"):
            I(nm)
        NS = 544 if do_ctx else 512
        P.phase_begin()
        wr = P.psb("wr", [128, 8, 16], BF16); u_wr = U("wr")
        E = P.psb("E", [16, NT], F32); u_E = U("E")
        wk = P.psb("wk", [16, NT], F32); u_wkl = U("wkl"); u_wkc = U("wkc")
        mx = P.psb("mx", [16, 544], F32); u_mx = U("mx")
        ix = P.psb("ix", [16, 544], U32); u_ix = U("ix")
        ixf = P.psb("ixf", [16, 544], F32); u_ixf = U("ixf")
        rcp = P.psb("rcp", [16, 512], F32); u_rcp = U("rcp")
        P.dma("pool", lambda e: e.dma_start(out=wr[:], in_=I("moe_router")[i].rearrange("(k p) n -> p k n", p=128)),
              writes=[u_wr])
        chunks = [(c0, min(512, NT - c0)) for c0 in range(0, NT, 512)]
        for (c0, n) in chunks:
            ps, ups = self.nps()
            tts = list(range(c0 // 128, (c0 + n) // 128))
            for k in range(8):
                self.mm(ps[0:16, 0:n], wr[:, k, :], self.hT[:, k, c0:c0 + n], k == 0, k == 7,
                        [u_wr] + [self.u_hT[t] for t in tts], [ups])
            P.op("act", lambda e, ps=ps, c0=c0, n=n: e.activation(out=E[:, c0:c0 + n], in_=ps[0:16, 0:n], func=AF.Exp),
                 reads=[ups], writes=[u_E])
        for (c0, n) in chunks:
            ps, ups = self.nps()
            self.mm(ps[0:16, 0:n], self.ones_f[0:16, 0:16], E[:, c0:c0 + n], True, True, [u_E, self.u_const], [ups])
            P.op("dve", lambda e, ps=ps, n=n: e.reciprocal(out=rcp[:, 0:n], in_=ps[0:16, 0:n]),
                 reads=[ups], writes=[u_rcp])
            P.op("dve", lambda e, c0=c0, n=n: e.tensor_tensor(out=E[:, c0:c0 + n], in0=E[:, c0:c0 + n],
                                                              in1=rcp[:, 0:n], op=ALU.mult),
                 reads=[u_rcp, u_E], writes=[u_E])
        sets = [(NCTX, NLAT, 0, 64, u_wkl)]
        if do_ctx:
            sets.append((0, NCTX, 512, 4, u_wkc))
        for (t0, n, s0, iters, u_wk) in sets:
            src, us = E, u_E
            for it in range(iters):
                sl = slice(s0 + it * 8, s0 + it * 8 + 8)
                P.op("dve", lambda e, src=src, sl=sl, t0=t0, n=n: e.max(out=mx[:, sl], in_=src[:, t0:t0 + n]),
                     reads=[us], writes=[u_mx])
                P.op("dve", lambda e, src=src, sl=sl, t0=t0, n=n: e.max_index(out=ix[:, sl], in_max=mx[:, sl],
                                                                               in_values=src[:, t0:t0 + n]),
                     reads=[us, u_mx], writes=[u_ix])
                if it < iters - 1:
                    P.op("dve", lambda e, src=src, sl=sl, t0=t0, n=n: e.match_replace(
                        out=wk[:, t0:t0 + n], in_to_replace=mx[:, sl], in_values=src[:, t0:t0 + n], imm_value=0.0),
                        reads=[us, u_mx], writes=[u_wk])
                src, us = wk, u_wk
        P.op("dve", lambda e: e.tensor_copy(out=ixf[:, 0:NS], in_=ix[:, 0:NS]), reads=[u_ix], writes=[u_ixf])
        P.op("dve", lambda e: e.tensor_scalar(out=ixf[:, 0:512], in0=ixf[:, 0:512], scalar1=float(NCTX), scalar2=None,
                                              op0=ALU.add), reads=[u_ixf], writes=[u_ixf])
        u_gateT = U("gateT"); u_idxT = U("idxT")
        nch = 5 if do_ctx else 4
        for ch in range(nch):
            rows = 128 if ch < 4 else 32
            ps, ups = self.nps()
            self.tr(ps[0:rows, 0:16], mx[:, ch * 128:ch * 128 + rows], self.ident_f[0:16, 0:16], [u_mx, self.u_const], [ups])
            self.tr(ps[0:rows, 16:32], ixf[:, ch * 128:ch * 128 + rows], self.ident_f[0:16, 0:16], [u_ixf, self.u_const], [ups])
            P.op("dve", lambda e, ps=ps, ch=ch, rows=rows: e.tensor_copy(out=self.gateT[0:rows, ch, :], in_=ps[0:rows, 0:16]),
                 reads=[ups], writes=[u_gateT])
            P.op("dve", lambda e, ps=ps, ch=ch, rows=rows: e.tensor_copy(out=self.idxT[0:rows, ch, :], in_=ps[0:rows, 16:32]),
                 reads=[ups], writes=[u_idxT])
        P.phase_end()
        P.phase_begin()
        NWB = 6
        wb = [self.hT[:, :, j * D:(j + 1) * D] for j in range(4)] + [P.psb("wb", [128, 8, D], BF16)[:] for _ in range(2)]
        u_wb = [U("wb%d" % j) for j in range(NWB)]
        NXI = 5
        xin = [P.psb("xin", [128, D], BF16) for _ in range(NXI)]; u_xin = [U("xin%d" % j) for j in range(NXI)]
        xinT = [P.psb("xinT", [128, 8, 640], BF16) for _ in range(2)]; u_xinT = [U("xinT0"), U("xinT1")]
        hidT = P.psb("hidT", [128, 8, 640], BF16); u_hidT = U("hidT")
        sg = [P.psb("sg", [128, 512], F32) for _ in range(2)]; u_sg = [U("sg0"), U("sg1")]
        ysb = [P.psb("ysb", [128, D], F32) for _ in range(2)]; u_ysb = [U("ysb0"), U("ysb1")]
        G = P.psb("G", [128, 2, 8, 128], F32); u_G = U("G")
        for s in range(2 if do_ctx else 1):
            P.dma("sp", lambda e, s=s: e.dma_start(out=G[:, s], in_=self.gate_bcast_src(i, 1, s)),
                  reads=[self.u_modd], writes=[u_G])
        A2 = self.AB[:, i * 4 + 2]
        B2 = self.AB[:, i * 4 + 3]
        wn = 0
        gn = 0
        yn = 0
        segs = [(0, 512, 0)] + ([(512, 32, 1)] if do_ctx else [])
        def emit_weights(ex):
            nonlocal wn
            wl = {}
            for nm in ("moe_w1", "moe_w3", "moe_w2"):
                b = wn % NWB
                wn += 1
                P.dma("pool", lambda e, nm=nm, b=b, ex=ex: e.dma_start(
                    out=wb[b], in_=I(nm)[i, ex].rearrange("(k p) n -> p k n", p=128)), writes=[u_wb[b]])
                wl[nm] = (wb[b], u_wb[b])
            return wl

        def emit_gathers(ex):
            nonlocal gn
            gb = []
            for ch in range(nch):
                rows = 128 if ch < 4 else 32
                b = gn % NXI
                gn += 1
                P.dma("pool", lambda e, b=b, ch=ch, rows=rows, ex=ex: e.indirect_dma_start(
                    out=xin[b][0:rows, :], out_offset=None, in_=self.xs2[:, :],
                    in_offset=bass.IndirectOffsetOnAxis(ap=self.idxT[0:rows, ch, ex:ex + 1], axis=0)),
                    reads=[u_idxT, self.u_xs2], writes=[u_xin[b]])
                gb.append(b)
            return gb

        pre = {0: (emit_weights(0), emit_gathers(0))}
        for ex in range(16):
            wl, gbufs = pre.pop(ex)
            xT, u_xT = xinT[ex % 2], u_xinT[ex % 2]
            for ch in range(nch):
                rows = 128 if ch < 4 else 32
                s = 0 if ch < 4 else 1
                b = gbufs[ch]
                pb, upb = self.npsb()
                for k in range(8):
                    self.tr(pb[:, k * 128:k * 128 + rows], xin[b][0:rows, k * 128:(k + 1) * 128],
                            self.ident_b[0:rows, 0:rows], [u_xin[b], self.u_const], [upb])
                for k in range(8):
                    P.op("act", lambda e, k=k, ch=ch, rows=rows, s=s, pb=pb, xT=xT: e.activation(
                        out=xT[:, k, ch * 128:ch * 128 + rows], in_=pb[:, k * 128:k * 128 + rows],
                        func=AF.Identity, scale=A2[:, k, s:s + 1], bias=B2[:, k, s:s + 1]),
                        reads=[upb, self.u_AB], writes=[u_xT])
            if ex + 1 < 16:
                pre[ex + 1] = (emit_weights(ex + 1), emit_gathers(ex + 1))
            w1, u_w1 = wl["moe_w1"]
            w3, u_w3 = wl["moe_w3"]
            w2, u_w2 = wl["moe_w2"]
            for f in range(8):
                for (lo, n, s) in segs:
                    p1, up1 = self.nps()
                    p3, up3 = self.nps()
                    for k in range(8):
                        self.mm(p1[:, 0:n], w1[:, k, f * 128:(f + 1) * 128], xT[:, k, lo:lo + n], k == 0, k == 7,
                                [u_w1, u_xT], [up1])
                    for k in range(8):
                        self.mm(p3[:, 0:n], w3[:, k, f * 128:(f + 1) * 128], xT[:, k, lo:lo + n], k == 0, k == 7,
                                [u_w3, u_xT], [up3])
                    sb_ = (f * 2 + s) % 2
                    P.op("act", lambda e, p1=p1, n=n, sb_=sb_: e.activation(out=sg[sb_][:, 0:n], in_=p1[:, 0:n], func=AF.Silu),
                         reads=[up1], writes=[u_sg[sb_]])
                    P.op("dve", lambda e, p3=p3, n=n, sb_=sb_, f=f, lo=lo: e.tensor_tensor(
                        out=hidT[:, f, lo:lo + n], in0=sg[sb_][:, 0:n], in1=p3[:, 0:n], op=ALU.mult),
                        reads=[up3, u_sg[sb_]], writes=[u_hidT])
            for ch in range(nch):
                rows = 128 if ch < 4 else 32
                s = 0 if ch < 4 else 1
                t0, tn = (NCTX, NLAT) if ch < 4 else (0, NCTX)
                yb = yn % 2
                yn += 1
                for half in range(2):
                    py, upy = self.nps()
                    for k in range(8):
                        self.mm(py[0:rows, :], hidT[:, k, ch * 128:ch * 128 + rows], w2[:, k, half * 512:(half + 1) * 512],
                                k == 0, k == 7, [u_w2, u_hidT], [upy])
                    P.op("dve", lambda e, py=py, rows=rows, ch=ch, half=half, s=s, yb=yb, ex=ex: e.scalar_tensor_tensor(
                        out=ysb[yb][0:rows, half * 512:(half + 1) * 512], in0=py[0:rows, :],
                        scalar=self.gateT[0:rows, ch, ex:ex + 1],
                        in1=G[0:rows, s, half * 4:(half + 1) * 4, :].rearrange("p a b -> p (a b)"),
                        op0=ALU.mult, op1=ALU.mult), reads=[upy, u_gateT, u_G], writes=[u_ysb[yb]])
                P.dma("pool", lambda e, rows=rows, ch=ch, yb=yb, t0=t0, tn=tn, ex=ex: e.indirect_dma_start(
                    out=self.xres[:, :],
                    out_offset=bass.IndirectOffsetOnAxis(ap=self.idxT[0:rows, ch, ex:ex + 1], axis=0),
                    in_=ysb[yb][0:rows, :], in_offset=None, compute_op=ALU.add),
                    reads=[u_ysb[yb], u_idxT], writes=[self.u_xres])
        P.phase_end()

    def epilogue(self):
        P = self.P
        for j in range(8):
            P.dma("sp", lambda e, j=j: e.dma_start(out=self.out[j * 512:(j + 1) * 512, :],
                                                    in_=self.xres[NCTX + j * 512:NCTX + (j + 1) * 512, :]),
                  reads=[self.u_xres], is_out=True)
        if self.cfg.get("dump_ctx"):
            oc = P.dram("out_ctx", [NCTX, D], F32, kind="ExternalOutput")
            P.dma("sp", lambda e: e.dma_start(out=oc[:, :], in_=self.xres[0:NCTX, :]), reads=[self.u_xres], is_out=True)
        return P.finish()
NEG = -30000.0


class KMix:
    def mix_scratch(self):
        if hasattr(self, "QTd"):
            return
        P = self.P
        self.QTd = P.dram("QTd", [1536, NT], BF16); self.u_QT = U("QTd")
        self.KTd = P.dram("KTd", [1024, NT], BF16); self.u_KT = U("KTd")
        self.KRd = P.dram("KRd", [32, NT], BF16); self.u_KR = U("KRd")
        self.Vd = P.dram("Vd", [NT, 1024], BF16); self.u_V = U("Vd")
        self.OTd = P.dram("OTd", [1024, NT], BF16); self.u_OT = U("OTd")

    def wload(self, name, src, kch, ncols):
        P = self.P
        t = P.psb(name, [128, kch, ncols], BF16); u = U(name)
        P.dma("pool", lambda e: e.dma_start(out=t[:], in_=src.rearrange("(k p) n -> p k n", p=128)), writes=[u])
        return t, u

    def bload(self, name, src_row, n, scale=None, parts=128):
        P = self.P
        t = P.psb(name, [parts, n], F32); u = U(name)
        P.dma("sp", lambda e: e.dma_start(out=t[:], in_=src_row.partition_broadcast(parts)[:, 0]), writes=[u])
        if scale is not None:
            P.op("dve", lambda e: e.tensor_scalar(out=t[:], in0=t[:], scalar1=float(scale), scalar2=None, op0=ALU.mult),
                 reads=[u], writes=[u])
        return t, u

    def norm_scratch(self):
        P = self.P
        sc = {"sq": P.psb("nsq", [128, 1536], F32), "u_sq": U("nsq"),
              "ss": P.psb("nss", [128, 16], F32), "u_ss": U("nss"),
              "r": [P.psb("rp", [128, 512], F32) for _ in range(4)], "u_r": [U("rp%d" % j) for j in range(4)]}
        return sc

    def headnorm(self, src, us, H, n, gb, ugb, out, uo, sc):
        P = self.P
        sq = sc["sq"][:, 0:H * n].rearrange("p (h n) -> p h n", n=n)
        ss = sc["ss"][:, 0:H]
        u_sq, u_ss = sc["u_sq"], sc["u_ss"]
        P.op("dve", lambda e: e.tensor_tensor(out=sq, in0=src, in1=src, op=ALU.mult), reads=[us], writes=[u_sq])
        P.op("dve", lambda e: e.tensor_reduce(out=ss, in_=sq, axis=AX.X, op=ALU.add), reads=[u_sq], writes=[u_ss])
        P.op("dve", lambda e: e.tensor_scalar(out=ss, in0=ss, scalar1=1.0 / n, scalar2=EPS, op0=ALU.mult, op1=ALU.add),
             reads=[u_ss], writes=[u_ss])
        P.op("act", lambda e: e.sqrt(out=ss, in_=ss), reads=[u_ss], writes=[u_ss])
        P.op("dve", lambda e: e.reciprocal(out=ss, in_=ss), reads=[u_ss], writes=[u_ss])
        P.op("dve", lambda e: e.tensor_tensor(out=sq, in0=src, in1=ss.unsqueeze(2).to_broadcast([128, H, n]), op=ALU.mult),
             reads=[us, u_ss], writes=[u_sq])
        P.op("dve", lambda e: e.tensor_tensor(out=out, in0=sq, in1=gb[:, 0:n].unsqueeze(1).to_broadcast([128, H, n]),
                                              op=ALU.mult), reads=[u_sq, ugb], writes=[uo])

    def rope(self, x, ux, H, half, cs, sn, ucs, sc):
        P = self.P
        x1 = x[:, :, 0:half]
        x2 = x[:, :, half:2 * half]
        cb = cs.unsqueeze(1).to_broadcast([128, H, half])
        sb = sn.unsqueeze(1).to_broadcast([128, H, half])
        t = [sc["r"][j][:, 0:H * half].rearrange("p (h n) -> p h n", n=half) for j in range(4)]
        ut = sc["u_r"]
        for j, (a, b) in enumerate(((x1, cb), (x2, sb), (x2, cb), (x1, sb))):
            P.op("dve", lambda e, j=j, a=a, b=b: e.tensor_tensor(out=t[j], in0=a, in1=b, op=ALU.mult),
                 reads=[ux, ucs], writes=[ut[j]])
        P.op("dve", lambda e: e.tensor_tensor(out=x1, in0=t[0], in1=t[1], op=ALU.subtract), reads=[ut[0], ut[1]], writes=[ux])
        P.op("dve", lambda e: e.tensor_tensor(out=x2, in0=t[2], in1=t[3], op=ALU.add), reads=[ut[2], ut[3]], writes=[ux])

    def tpose_to_dram(self, blocks, ub, rows, dst, udst, stage, ustage):
        P = self.P
        pb, upb = self.npsb()
        nb = len(blocks)
        for j, blk in enumerate(blocks):
            self.tr(pb[0:rows, j * 128:(j + 1) * 128], blk, self.ident_b[:], [ub, self.u_const], [upb])
        P.op("act", lambda e: e.copy(out=stage[0:rows, 0:nb, :], in_=pb[0:rows, 0:nb * 128].rearrange("p (j t) -> p j t", t=128)),
             reads=[upb], writes=[ustage])
        P.dma("sp", lambda e: e.dma_start(out=dst, in_=stage[0:rows, 0:nb, :]), reads=[ustage], writes=[udst])

    def attn_setup(self):
        P = self.P
        a = {"pt": [P.psb("pt", [128, 512], BF16) for _ in range(4)], "u_pt": [U("pt%d" % j) for j in range(4)], "nblk": 0, "nsc": 0,
             "tmp": [P.psb("tmpb", [128, 512], F32) for _ in range(2)], "u_tmp": [U("tmp0"), U("tmp1")],
             "rd": P.psb("rd", [64, 512], F32), "u_rd": U("rd"), "n": 0}
        return a

    def attn_block(self, a, rhs_q, uq, nq, dk, keys, out_ap, uout, sink_fn=None, qg=1):
        P = self.P
        LA = 2
        bi = 2 + 2 * (a["nblk"] % 2)
        a["nblk"] += 1
        pso, upso = self.psf[bi][0:64, 0:nq], self.u_psf[bi]
        psd, upsd = self.psf[bi + 1][0:64, 0:nq], self.u_psf[bi + 1]
        n_k = len(keys)
        last = n_k - 1
        pend = {}
        for j in range(n_k + LA):
            if j < n_k:
                (kt, uk, v, uv, nk, bias, ubias) = keys[j]
                si = a["nsc"] % 2
                a["nsc"] += 1
                ps, ups = self.psf[si], self.u_psf[si]
                so = ps[0:nk, 0:nq] if qg == 1 else ps[0:nk, 0:nq].rearrange("p (g t) -> p g t", g=qg)
                self.mm(so, kt, rhs_q, True, True, [uk, uq], [ups])
                n = a["n"]; a["n"] += 1
                pt, upt = a["pt"][n % 4], a["u_pt"][n % 4]
                if bias is not None:
                    tmp, utmp = a["tmp"][n % 2], a["u_tmp"][n % 2]
                    P.op("dve", lambda e, ps=ps, nk=nk, tmp=tmp, bias=bias: e.tensor_tensor(
                        out=tmp[0:nk, 0:nq], in0=ps[0:nk, 0:nq], in1=bias, op=ALU.add), reads=[ups, ubias], writes=[utmp])
                    P.op("act", lambda e, nk=nk, tmp=tmp, pt=pt: e.activation(out=pt[0:nk, 0:nq], in_=tmp[0:nk, 0:nq], func=AF.Exp),
                         reads=[utmp], writes=[upt])
                else:
                    P.op("act", lambda e, ps=ps, nk=nk, pt=pt: e.activation(out=pt[0:nk, 0:nq], in_=ps[0:nk, 0:nq], func=AF.Exp),
                         reads=[ups], writes=[upt])
                pend[j] = (pt, upt)
            jj = j - LA
            if jj >= 0:
                (kt, uk, v, uv, nk, bias, ubias) = keys[jj]
                pt, upt = pend.pop(jj)
                self.mm(pso, v, pt[0:nk, 0:nq], jj == 0, jj == last, [uv, upt], [upso])
                self.mm(psd, self.ones_b[0:nk, 0:64], pt[0:nk, 0:nq], jj == 0, jj == last, [upt, self.u_const], [upsd])
        rd, urd = a["rd"], a["u_rd"]
        if sink_fn is not None:
            sink_fn(psd, upsd, rd, urd)
            P.op("dve", lambda e: e.reciprocal(out=rd[:, 0:nq], in_=rd[:, 0:nq]), reads=[urd], writes=[urd])
        else:
            P.op("dve", lambda e: e.reciprocal(out=rd[:, 0:nq], in_=psd), reads=[upsd], writes=[urd])
        if qg == 1:
            o_in, r_in = pso, rd[:, 0:nq]
        else:
            o_in = pso.rearrange("p (g t) -> p g t", g=qg)
            r_in = rd[:, 0:nq].rearrange("p (g t) -> p g t", g=qg)
        P.op("dve", lambda e: e.tensor_tensor(out=out_ap, in0=o_in, in1=r_in, op=ALU.mult),
             reads=[upso, urd], writes=[uout])

    def outproj_phase(self, i, wname, do_ctx):
        P = self.P
        wsrc = self.I(wname)
        P.phase_begin()
        wo, uwo = self.wload("wo", wsrc, 8, D)
        for k in range(8):
            P.dma("sp", lambda e, k=k: e.dma_start(out=self.hT[:, k, :], in_=self.OTd[k * 128:(k + 1) * 128, :]),
                  reads=[self.u_OT], writes=self.u_hT)
        G = P.psb("G1", [128, 2, 8, 128], F32); u_G = U("G1")
        for s in range(2):
            P.dma("sp", lambda e, s=s: e.dma_start(out=G[:, s], in_=self.gate_bcast_src(i, 0, s)),
                  reads=[self.u_modd], writes=[u_G])
        xt = [P.psb("xto", [128, D], F32) for _ in range(2)]; u_xt = [U("xto0"), U("xto1")]
        tmp = [P.psb("tmpo", [128, 512], F32) for _ in range(2)]; u_tmp = [U("tmpo0"), U("tmpo1")]
        tiles = list(range(NTT)) if do_ctx else list(range(2, NTT))
        for n, tt in enumerate(tiles):
            s = 1 if tt < 2 else 0
            b = n % 2
            P.dma("sp", lambda e, b=b, tt=tt: e.dma_start(out=xt[b][:], in_=self.xres[tt * 128:(tt + 1) * 128, :]),
                  reads=[self.u_xres], writes=[u_xt[b]])
            for half in range(2):
                ps, ups = self.nps()
                for k in range(8):
                    self.mm(ps[:, :], self.hT[:, k, tt * 128:(tt + 1) * 128], wo[:, k, half * 512:(half + 1) * 512],
                            k == 0, k == 7, [self.u_hT[tt], uwo], [ups])
                P.op("dve", lambda e, ps=ps, half=half, s=s: e.tensor_tensor(
                    out=tmp[half][:], in0=ps[:, :], in1=G[:, s, half * 4:(half + 1) * 4, :].rearrange("p a b -> p (a b)"),
                    op=ALU.mult), reads=[ups, u_G], writes=[u_tmp[half]])
                P.op("dve", lambda e, half=half, b=b: e.tensor_tensor(
                    out=xt[b][:, half * 512:(half + 1) * 512], in0=xt[b][:, half * 512:(half + 1) * 512],
                    in1=tmp[half][:], op=ALU.add), reads=[u_tmp[half], u_xt[b]], writes=[u_xt[b]])
            P.dma("pool", lambda e, b=b, tt=tt: e.dma_start(out=self.xres[tt * 128:(tt + 1) * 128, :], in_=xt[b][:]),
                  reads=[u_xt[b]], writes=[self.u_xres])
        P.phase_end()

    def mla_proj(self, i):
        P = self.P
        I = self.I
        for nm in ("mla_w_down", "mla_q_norm_g", "mla_kv_norm_g", "mla_w_uq", "mla_w_ukv", "mla_qn_g", "mla_qr_g",
                   "mla_kn_g", "mla_kr_g", "mla_cos", "mla_sin"):
            I(nm)
        scale = 96.0 ** -0.5
        P.phase_begin()
        wd, uwd = self.wload("wd", I("mla_w_down"), 8, 672)
        wuq, uwuq = self.wload("wuq", I("mla_w_uq"), 3, 1536)
        wukv, uwukv = self.wload("wukv", I("mla_w_ukv"), 2, 2048)
        gq, ugq = self.bload("gq", I("mla_q_norm_g")[0:1, :], 384)
        gkv, ugkv = self.bload("gkv", I("mla_kv_norm_g")[0:1, :], 256)
        gkr, ugkr = self.bload("gkr", I("mla_kr_g")[0:1, :], 32)
        gqn, ugqn = self.bload("gqn", I("mla_qn_g")[0:1, :], 64, scale=scale)
        gqr, ugqr = self.bload("gqr", I("mla_qr_g")[0:1, :], 32, scale=scale)
        gkn, ugkn = self.bload("gkn", I("mla_kn_g")[0:1, :], 64)
        sc = self.norm_scratch()
        cqT = P.psb("cqT", [128, 3, NT], BF16); u_cqT = [U("cqT%d" % t) for t in range(NTT)]
        ckvT = P.psb("ckvT", [128, 2, NT], BF16); u_ckvT = [U("ckvT%d" % t) for t in range(NTT)]
        df = P.psb("df", [128, 672], F32); u_df = U("df")
        dn = P.psb("dn", [128, 672], F32); u_dn = U("dn")
        db = P.psb("db", [128, 768], BF16); u_db = U("db")
        cs = [P.psb("cs", [128, 16], F32) for _ in range(2)]; sn = [P.psb("sn", [128, 16], F32) for _ in range(2)]
        u_cs = [U("cs0"), U("cs1")]
        krs = P.psb("krs", [32, 1, 128], BF16); u_krs = U("krs")
        P.op("pool", lambda e: e.memset(db[:], 0.0), writes=[u_db])
        for tt in range(NTT):
            lat = tt >= 2
            ps0, up0 = self.nps()
            ps1, up1 = self.nps()
            for k in range(8):
                self.mm(ps0[:, 0:512], self.hT[:, k, tt * 128:(tt + 1) * 128], wd[:, k, 0:512], k == 0, k == 7,
                        [self.u_hT[tt], uwd], [up0])
            for k in range(8):
                self.mm(ps1[:, 0:160], self.hT[:, k, tt * 128:(tt + 1) * 128], wd[:, k, 512:672], k == 0, k == 7,
                        [self.u_hT[tt], uwd], [up1])
            P.op("act", lambda e, ps0=ps0: e.copy(out=df[:, 0:512], in_=ps0[:, 0:512]), reads=[up0], writes=[u_df])
            P.op("act", lambda e, ps1=ps1: e.copy(out=df[:, 512:672], in_=ps1[:, 0:160]), reads=[up1], writes=[u_df])
            for (c0, n, g, ug) in ((0, 384, gq, ugq), (384, 256, gkv, ugkv), (640, 32, gkr, ugkr)):
                self.headnorm(df[:, c0:c0 + n].unsqueeze(1), u_df, 1, n, g, ug, dn[:, c0:c0 + n].unsqueeze(1), u_dn, sc)
            if lat:
                b = tt % 2
                t0 = (tt - 2) * 128
                P.dma("sp", lambda e, b=b, t0=t0: e.dma_start(out=cs[b][:], in_=I("mla_cos")[t0:t0 + 128, :]), writes=[u_cs[b]])
                P.dma("sp", lambda e, b=b, t0=t0: e.dma_start(out=sn[b][:], in_=I("mla_sin")[t0:t0 + 128, :]), writes=[u_cs[b]])
                self.rope(dn[:, 640:672].unsqueeze(1), u_dn, 1, 16, cs[b][:], sn[b][:], u_cs[b], sc)
            P.op("act", lambda e: e.copy(out=db[:, 0:672], in_=dn[:, 0:672]), reads=[u_dn], writes=[u_db])
            pb, upb = self.npsb()
            for j in range(5):
                self.tr(pb[:, j * 128:(j + 1) * 128], db[:, j * 128:(j + 1) * 128], self.ident_b[:], [u_db, self.u_const], [upb])
            self.tr(pb[:, 640:768], db[:, 640:768], self.ident_b[:], [u_db, self.u_const], [upb])
            P.op("act", lambda e, pb=pb, tt=tt: e.copy(out=cqT[:, :, tt * 128:(tt + 1) * 128],
                                                       in_=pb[:, 0:384].rearrange("p (j t) -> p j t", t=128)),
                 reads=[upb], writes=[u_cqT[tt]])
            P.op("act", lambda e, pb=pb, tt=tt: e.copy(out=ckvT[:, :, tt * 128:(tt + 1) * 128],
                                                       in_=pb[:, 384:640].rearrange("p (j t) -> p j t", t=128)),
                 reads=[upb], writes=[u_ckvT[tt]])
            P.op("act", lambda e, pb=pb: e.copy(out=krs[:, 0, :], in_=pb[0:32, 640:768]), reads=[upb], writes=[u_krs])
            P.dma("sp", lambda e, tt=tt: e.dma_start(out=self.KRd[:, tt * 128:(tt + 1) * 128], in_=krs[:, 0, :]),
                  reads=[u_krs], writes=[self.u_KR])
        qf = P.psb("qf", [128, 16, 96], F32); u_qf = U("qf")
        qn = P.psb("qn", [128, 16, 96], F32); u_qn = U("qn")
        qb = P.psb("qb", [128, 16, 96], BF16); u_qb = U("qb")
        kvf = P.psb("kvf", [128, 16, 128], F32); u_kvf = U("kvf")
        knf = P.psb("knf", [128, 16, 64], F32); u_knf = U("knf")
        knb = P.psb("knb", [128, 16, 64], BF16); u_knb = U("knb")
        vb = P.psb("vb", [128, 16, 64], BF16); u_vb = U("vb")
        stq = [P.psb("stq", [96, 8, 128], BF16) for _ in range(2)]; u_stq = [U("stq0"), U("stq1")]
        stk = P.psb("stk", [128, 8, 128], BF16); u_stk = U("stk")
        qflat = qf[:].rearrange("p h n -> p (h n)")
        kvflat = kvf[:].rearrange("p h n -> p (h n)")
        for tt in range(NTT):
            lat = tt >= 2
            for cc in range(3):
                ps, ups = self.nps()
                for k in range(3):
                    self.mm(ps[:, :], cqT[:, k, tt * 128:(tt + 1) * 128], wuq[:, k, cc * 512:(cc + 1) * 512], k == 0, k == 2,
                            [u_cqT[tt], uwuq], [ups])
                P.op("act", lambda e, ps=ps, cc=cc: e.copy(out=qflat[:, cc * 512:(cc + 1) * 512], in_=ps[:, :]),
                     reads=[ups], writes=[u_qf])
            self.headnorm(qf[:, :, 0:64], u_qf, 16, 64, gqn, ugqn, qn[:, :, 0:64], u_qn, sc)
            self.headnorm(qf[:, :, 64:96], u_qf, 16, 32, gqr, ugqr, qn[:, :, 64:96], u_qn, sc)
            if lat:
                b = tt % 2
                t0 = (tt - 2) * 128
                P.dma("sp", lambda e, b=b, t0=t0: e.dma_start(out=cs[b][:], in_=I("mla_cos")[t0:t0 + 128, :]), writes=[u_cs[b]])
                P.dma("sp", lambda e, b=b, t0=t0: e.dma_start(out=sn[b][:], in_=I("mla_sin")[t0:t0 + 128, :]), writes=[u_cs[b]])
                self.rope(qn[:, :, 64:96], u_qn, 16, 16, cs[b][:], sn[b][:], u_cs[b], sc)
            P.op("act", lambda e: e.copy(out=qb[:], in_=qn[:]), reads=[u_qn], writes=[u_qb])
            for hh in range(2):
                self.tpose_to_dram([qb[:, hh * 8 + j, :] for j in range(8)], u_qb, 96,
                                   self.QTd[hh * 768:(hh + 1) * 768, tt * 128:(tt + 1) * 128].rearrange("(j d) t -> d j t", d=96),
                                   self.u_QT, stq[hh], u_stq[hh])
            for cc in range(4):
                ps, ups = self.nps()
                for k in range(2):
                    self.mm(ps[:, :], ckvT[:, k, tt * 128:(tt + 1) * 128], wukv[:, k, cc * 512:(cc + 1) * 512], k == 0, k == 1,
                            [u_ckvT[tt], uwukv], [ups])
                P.op("act", lambda e, ps=ps, cc=cc: e.copy(out=kvflat[:, cc * 512:(cc + 1) * 512], in_=ps[:, :]),
                     reads=[ups], writes=[u_kvf])
            self.headnorm(kvf[:, :, 0:64], u_kvf, 16, 64, gkn, ugkn, knf[:], u_knf, sc)
            P.op("act", lambda e: e.copy(out=knb[:], in_=knf[:]), reads=[u_knf], writes=[u_knb])
            P.op("pool", lambda e: e.tensor_copy(out=vb[:], in_=kvf[:, :, 64:128]), reads=[u_kvf], writes=[u_vb])
            P.dma("sp", lambda e, tt=tt: e.dma_start(out=self.Vd[tt * 128:(tt + 1) * 128, :].rearrange("t (h n) -> t h n", n=64),
                                                     in_=vb[:]), reads=[u_vb], writes=[self.u_V])
            knb2 = knb[:].rearrange("p h n -> p (h n)")
            self.tpose_to_dram([knb2[:, j * 128:(j + 1) * 128] for j in range(8)], u_knb, 128,
                               self.KTd[:, tt * 128:(tt + 1) * 128].rearrange("(j p) t -> p j t", p=128),
                               self.u_KT, stk, u_stk)
        P.phase_end()

    def mla_attn(self, do_ctx):
        P = self.P
        P.phase_begin()
        a = self.attn_setup()
        QT = [P.psb("QTh", [96, NT], BF16) for _ in range(2)]; uQ = [U("QTh0"), U("QTh1")]
        KT = [P.psb("KTh", [96, NT], BF16) for _ in range(2)]; uK = [U("KTh0"), U("KTh1")]
        V = [P.psb("Vh", [128, NTT, 64], BF16) for _ in range(2)]; uV = [U("Vh0"), U("Vh1")]
        OS = [P.psb("OSh", [64, NT], BF16) for _ in range(2)]; uOS = [U("OSh0"), U("OSh1")]
        for h in range(16):
            b = h % 2
            P.dma("sp", lambda e, b=b, h=h: e.dma_start(out=QT[b][:], in_=self.QTd[h * 96:(h + 1) * 96, :]),
                  reads=[self.u_QT], writes=[uQ[b]])
            P.dma("sp", lambda e, b=b, h=h: e.dma_start(out=KT[b][0:64, :], in_=self.KTd[h * 64:(h + 1) * 64, :]),
                  reads=[self.u_KT], writes=[uK[b]])
            P.dma("sp", lambda e, b=b: e.dma_start(out=KT[b][64:96, :], in_=self.KRd[:, :]), reads=[self.u_KR], writes=[uK[b]])
            P.dma("sp", lambda e, b=b, h=h: e.dma_start(
                out=V[b][:], in_=self.Vd[:, h * 64:(h + 1) * 64].rearrange("(t p) c -> p t c", p=128)),
                reads=[self.u_V], writes=[uV[b]])
            blocks = [(NCTX + c * 512, 512, list(range(NTT))) for c in range(8)]
            if do_ctx:
                blocks.append((0, NCTX, [0, 1]))
            for (q0, nq, kts) in blocks:
                keys = [(KT[b][:, kt * 128:(kt + 1) * 128], uK[b], V[b][:, kt, :], uV[b], 128, None, None) for kt in kts]
                self.attn_block(a, QT[b][:, q0:q0 + nq], uQ[b], nq, 96, keys, OS[b][:, q0:q0 + nq], uOS[b])
            c0 = 0 if do_ctx else NCTX
            P.dma("pool", lambda e, b=b, h=h, c0=c0: e.dma_start(out=self.OTd[h * 64:(h + 1) * 64, c0:NT], in_=OS[b][:, c0:NT]),
                  reads=[uOS[b]], writes=[self.u_OT])
        P.phase_end()


    def qkv_proj(self, wname, Hk, gqname, gkname, scale, rope=None):
        P = self.P
        I = self.I
        for nm in (wname, gqname, gkname) + (tuple(rope) if rope else ()):
            I(nm)
        ncol = 1024 + 2 * Hk * 64
        P.phase_begin()
        w, uw = self.wload("wqkv", I(wname), 8, ncol)
        gq, ugq = self.bload("gq", I(gqname)[0:1, :], 64, scale=scale)
        gk, ugk = self.bload("gk", I(gkname)[0:1, :], 64)
        sc = self.norm_scratch()
        qf = P.psb("qf", [128, ncol], F32); u_qf = U("qf")
        qn = P.psb("qn", [128, 16, 64], F32); u_qn = U("qn")
        kn = P.psb("kn", [128, Hk, 64], F32); u_kn = U("kn")
        qb = P.psb("qb", [128, 1024], BF16); u_qb = U("qb")
        kb = P.psb("kb", [128, Hk * 64], BF16); u_kb = U("kb")
        vb = P.psb("vb", [128, Hk * 64], BF16); u_vb = U("vb")
        stq = P.psb("stq", [128, 8, 128], BF16); u_stq = U("stq")
        stk = P.psb("stk", [128, 8, 128], BF16); u_stk = U("stk")
        if rope:
            cs = [P.psb("cs", [128, 32], F32) for _ in range(2)]; sn = [P.psb("sn", [128, 32], F32) for _ in range(2)]
            u_cs = [U("cs0"), U("cs1")]
        nb = ncol // 512
        kc0 = 1024
        vc0 = 1024 + Hk * 64
        for tt in range(NTT):
            lat = tt >= 2
            for cc in range(nb):
                ps, ups = self.nps()
                for k in range(8):
                    self.mm(ps[:, :], self.hT[:, k, tt * 128:(tt + 1) * 128], w[:, k, cc * 512:(cc + 1) * 512], k == 0, k == 7,
                            [self.u_hT[tt], uw], [ups])
                P.op("act", lambda e, ps=ps, cc=cc: e.copy(out=qf[:, cc * 512:(cc + 1) * 512], in_=ps[:, :]),
                     reads=[ups], writes=[u_qf])
            self.headnorm(qf[:, 0:1024].rearrange("p (h n) -> p h n", n=64), u_qf, 16, 64, gq, ugq, qn[:], u_qn, sc)
            self.headnorm(qf[:, kc0:kc0 + Hk * 64].rearrange("p (h n) -> p h n", n=64), u_qf, Hk, 64, gk, ugk, kn[:], u_kn, sc)
            if rope and lat:
                b = tt % 2
                t0 = (tt - 2) * 128
                P.dma("sp", lambda e, b=b, t0=t0: e.dma_start(out=cs[b][:], in_=I(rope[0])[t0:t0 + 128, :]), writes=[u_cs[b]])
                P.dma("sp", lambda e, b=b, t0=t0: e.dma_start(out=sn[b][:], in_=I(rope[1])[t0:t0 + 128, :]), writes=[u_cs[b]])
                self.rope(qn[:], u_qn, 16, 32, cs[b][:], sn[b][:], u_cs[b], sc)
                self.rope(kn[:], u_kn, Hk, 32, cs[b][:], sn[b][:], u_cs[b], sc)
            P.op("act", lambda e: e.copy(out=qb[:], in_=qn[:].rearrange("p h n -> p (h n)")), reads=[u_qn], writes=[u_qb])
            P.op("act", lambda e: e.copy(out=kb[:], in_=kn[:].rearrange("p h n -> p (h n)")), reads=[u_kn], writes=[u_kb])
            P.op("pool", lambda e: e.tensor_copy(out=vb[:], in_=qf[:, vc0:vc0 + Hk * 64]), reads=[u_qf], writes=[u_vb])
            P.dma("sp", lambda e, tt=tt: e.dma_start(out=self.Vd[tt * 128:(tt + 1) * 128, 0:Hk * 64], in_=vb[:]),
                  reads=[u_vb], writes=[self.u_V])
            self.tpose_to_dram([qb[:, j * 128:(j + 1) * 128] for j in range(8)], u_qb, 128,
                               self.QTd[0:1024, tt * 128:(tt + 1) * 128].rearrange("(j p) t -> p j t", p=128),
                               self.u_QT, stq, u_stq)
            nkb = Hk * 64 // 128
            self.tpose_to_dram([kb[:, j * 128:(j + 1) * 128] for j in range(nkb)], u_kb, 128,
                               self.KTd[0:Hk * 64, tt * 128:(tt + 1) * 128].rearrange("(j p) t -> p j t", p=128),
                               self.u_KT, stk, u_stk)
        P.phase_end()

    def na_proj(self, i):
        self.qkv_proj("na_w_qkv", 16, "na_q_g", "na_k_g", 64.0 ** -0.5)

    def swa_proj(self, i):
        self.qkv_proj("swa_w_qkv", 4, "swa_q_g", "swa_k_g", 64.0 ** -0.5, rope=("swa_cos", "swa_sin"))

    def swa_attn(self, do_ctx):
        P = self.P
        I = self.I
        I("swa_sink")
        P.phase_begin()
        a = self.attn_setup()
        Mp = P.psb("Mp", [128, 4, 128], F32); Mn = P.psb("Mn", [128, 4, 128], F32); u_M = U("M")
        P.op("pool", lambda e: e.memset(Mp[:], 0.0), writes=[u_M])
        P.op("pool", lambda e: e.memset(Mn[:], 0.0), writes=[u_M])
        P.op("pool", lambda e: e.affine_select(out=Mp[:], in_=Mp[:], pattern=[[0, 4], [-1, 128]], compare_op=ALU.is_ge,
                                               fill=NEG, base=0, channel_multiplier=1), reads=[u_M], writes=[u_M])
        P.op("pool", lambda e: e.affine_select(out=Mn[:], in_=Mn[:], pattern=[[0, 4], [1, 128]], compare_op=ALU.is_ge,
                                               fill=NEG, base=0, channel_multiplier=-1), reads=[u_M], writes=[u_M])
        Mp2 = Mp[:].rearrange("p g t -> p (g t)")
        Mn2 = Mn[:].rearrange("p g t -> p (g t)")
        esink, u_es = self.bload("esink", I("swa_sink")[0:1, :], 16, parts=64)
        P.op("act", lambda e: e.activation(out=esink[:], in_=esink[:], func=AF.Exp), reads=[u_es], writes=[u_es])
        Q = P.psb("Qall", [64, 4, NT], BF16); uQ = U("Qall")
        KT = P.psb("KTh", [64, NT], BF16); uK = U("KTh")
        V = P.psb("Vh", [128, NTT, 64], BF16); uV = U("Vh")
        OS = P.psb("OSh", [64, 4, NT], BF16); uOS = U("OSh")
        for hk in range(4):
            P.dma("sp", lambda e, hk=hk: e.dma_start(out=Q[:], in_=self.QTd[hk * 256:(hk + 1) * 256, :].rearrange("(g d) t -> d g t", d=64)),
                  reads=[self.u_QT], writes=[uQ])
            P.dma("sp", lambda e, hk=hk: e.dma_start(out=KT[:], in_=self.KTd[hk * 64:(hk + 1) * 64, :]),
                  reads=[self.u_KT], writes=[uK])
            P.dma("sp", lambda e, hk=hk: e.dma_start(out=V[:], in_=self.Vd[:, hk * 64:(hk + 1) * 64].rearrange("(t p) c -> p t c", p=128)),
                  reads=[self.u_V], writes=[uV])

            def sink_fn(psd, upsd, rd, urd, hk=hk):
                for g in range(4):
                    P.op("dve", lambda e, g=g: e.tensor_scalar(out=rd[:, g * 128:(g + 1) * 128], in0=psd[:, g * 128:(g + 1) * 128],
                                                                scalar1=esink[:, hk * 4 + g:hk * 4 + g + 1], scalar2=None, op0=ALU.add),
                         reads=[upsd, u_es], writes=[urd])
            tiles = list(range(2, NTT)) + ([0, 1] if do_ctx else [])
            for tile in tiles:
                def key(kt, bias):
                    return (KT[:, kt * 128:(kt + 1) * 128], uK, V[:, kt, :], uV, 128, bias, u_M)
                keys = [key(0, None), key(1, None)]
                if tile >= 2:
                    if tile > 2:
                        keys.append(key(tile - 1, Mp2))
                    keys.append(key(tile, None))
                    if tile < NTT - 1:
                        keys.append(key(tile + 1, Mn2))
                self.attn_block(a, Q[:, :, tile * 128:(tile + 1) * 128], uQ, 512, 64, keys,
                                OS[:, :, tile * 128:(tile + 1) * 128], uOS, sink_fn=sink_fn, qg=4)
            c0 = 0 if do_ctx else NCTX
            P.dma("pool", lambda e, hk=hk, c0=c0: e.dma_start(
                out=self.OTd[hk * 256:(hk + 1) * 256, c0:NT].rearrange("(g d) t -> d g t", d=64), in_=OS[:, :, c0:NT]),
                reads=[uOS], writes=[self.u_OT])
        P.phase_end()

    def na_attn(self, do_ctx):
        P = self.P
        I = self.I
        I("na_bias")
        P.phase_begin()
        a = self.attn_setup()
        QT = [P.psb("QTh", [64, NT], BF16) for _ in range(2)]; uQ = [U("QTh0"), U("QTh1")]
        KT = [P.psb("KTh", [64, NT], BF16) for _ in range(2)]; uK = [U("KTh0"), U("KTh1")]
        V = [P.psb("Vh", [128, NTT, 64], BF16) for _ in range(2)]; uV = [U("Vh0"), U("Vh1")]
        Vs = [P.psb("Vsh", [128, NTT - 1, 64], BF16) for _ in range(2)]
        B = [P.psb("Bh", [128, 14, 64], F32) for _ in range(2)]; uB = [U("Bh0"), U("Bh1")]
        OS = [P.psb("OSh", [64, NT], BF16) for _ in range(2)]; uOS = [U("OSh0"), U("OSh1")]
        for h in range(16):
            b = h % 2
            P.dma("sp", lambda e, b=b, h=h: e.dma_start(out=QT[b][:], in_=self.QTd[h * 64:(h + 1) * 64, :]),
                  reads=[self.u_QT], writes=[uQ[b]])
            P.dma("sp", lambda e, b=b, h=h: e.dma_start(out=KT[b][:], in_=self.KTd[h * 64:(h + 1) * 64, :]),
                  reads=[self.u_KT], writes=[uK[b]])
            P.dma("sp", lambda e, b=b, h=h: e.dma_start(
                out=V[b][:], in_=self.Vd[:, h * 64:(h + 1) * 64].rearrange("(t p) c -> p t c", p=128)),
                reads=[self.u_V], writes=[uV[b]])
            P.dma("sp", lambda e, b=b, h=h: e.dma_start(
                out=Vs[b][:], in_=self.Vd[64:64 + (NTT - 1) * 128, h * 64:(h + 1) * 64].rearrange("(t p) c -> p t c", p=128)),
                reads=[self.u_V], writes=[uV[b]])
            P.dma("sp", lambda e, b=b, h=h: e.dma_start(out=B[b][:], in_=I("na_bias")[h]), writes=[uB[b]])
            for r in range(64):
                q0 = NCTX + r * 64
                r0 = min(max(r - 4, 0), 56)
                keys = [(KT[b][:, 0:128], uK[b], V[b][:, 0, :], uV[b], 128, None, None),
                        (KT[b][:, 128:256], uK[b], V[b][:, 1, :], uV[b], 128, None, None)]
                for j in range(4):
                    krow = r0 + 2 * j
                    tok = NCTX + krow * 64
                    vv = V[b][:, tok // 128, :] if tok % 128 == 0 else Vs[b][:, (tok - 64) // 128, :]
                    keys.append((KT[b][:, tok:tok + 128], uK[b], vv, uV[b], 128, B[b][:, krow - r + 7, :], uB[b]))
                self.attn_block(a, QT[b][:, q0:q0 + 64], uQ[b], 64, 64, keys, OS[b][:, q0:q0 + 64], uOS[b])
            if do_ctx:
                keys = [(KT[b][:, 0:128], uK[b], V[b][:, 0, :], uV[b], 128, None, None),
                        (KT[b][:, 128:256], uK[b], V[b][:, 1, :], uV[b], 128, None, None)]
                self.attn_block(a, QT[b][:, 0:NCTX], uQ[b], NCTX, 64, keys, OS[b][:, 0:NCTX], uOS[b])
            c0 = 0 if do_ctx else NCTX
            P.dma("pool", lambda e, b=b, h=h, c0=c0: e.dma_start(out=self.OTd[h * 64:(h + 1) * 64, c0:NT], in_=OS[b][:, c0:NT]),
                  reads=[uOS[b]], writes=[self.u_OT])
        P.phase_end()

    def mixer(self, i, do_ctx):
        self.mix_scratch()
        m = i % 4
        self.norm_phase(i, 0, False)
        if m == 0:
            self.na_proj(i); self.na_attn(do_ctx); self.outproj_phase(i, "na_w_o", do_ctx)
        elif m == 1:
            self.rwkv(i, do_ctx)
        elif m == 2:
            self.mla_proj(i); self.mla_attn(do_ctx); self.outproj_phase(i, "mla_w_o", do_ctx)
        else:
            self.swa_proj(i); self.swa_attn(do_ctx); self.outproj_phase(i, "swa_w_o", do_ctx)


for _n, _f in list(vars(KMix).items()):
    if callable(_f):
        setattr(K, _n, _f)
C0 = -0.6065306597126334


class KRw:
    def rw_scratch(self):
        if hasattr(self, "rwd"):
            return
        P = self.P
        self.rwd = {}
        self.u_rwd = {}
        for nm in ("R", "K", "KK", "V", "G", "LW0", "LW1", "B0", "B1", "KT0", "KT1", "Y"):
            self.rwd[nm] = P.dram("rw_" + nm, [NT, D], F32)
            self.u_rwd[nm] = U("rw_" + nm)

    def rw_xs(self, tt, S, u_S, xx, u_xx, tmp, u_tmp, xs, u_xs, streams, mixT, u_mixT):
        P = self.P
        h = self.hT
        t0 = tt * 128
        noprev = tt in (0, 2)
        nonext = tt in (1, NTT - 1)
        a = 1 if noprev else 0
        b = 127 if nonext else 128
        uh = [self.u_hT[tt]]
        P.op("dve", lambda e: e.tensor_tensor(out=S[:, :, a:b], in0=h[:, :, t0 - 1 + a:t0 - 1 + b],
                                              in1=h[:, :, t0 + 1 + a:t0 + 1 + b], op=ALU.add), reads=uh, writes=[u_S])
        if noprev:
            P.op("dve", lambda e: e.tensor_copy(out=S[:, :, 0:1], in_=h[:, :, t0 + 1:t0 + 2]), reads=uh, writes=[u_S])
        if nonext:
            P.op("dve", lambda e: e.tensor_copy(out=S[:, :, 127:128], in_=h[:, :, t0 + 126:t0 + 127]), reads=uh, writes=[u_S])
        P.op("dve", lambda e: e.scalar_tensor_tensor(out=xx[:], in0=S[:], scalar=0.5, in1=h[:, :, t0:t0 + 128],
                                                     op0=ALU.mult, op1=ALU.subtract), reads=[u_S] + uh, writes=[u_xx])
        for n, s in enumerate(streams):
            tb = n % 2
            P.op("pool", lambda e, s=s, tb=tb: e.tensor_tensor(
                out=tmp[tb][:], in0=xx[:], in1=mixT[:, s * 8:(s + 1) * 8].unsqueeze(2).to_broadcast([128, 8, 128]),
                op=ALU.mult), reads=[u_xx, u_mixT], writes=[u_tmp[tb]])
            P.op("dve", lambda e, n=n, tb=tb: e.tensor_tensor(out=xs[n][:], in0=tmp[tb][:], in1=h[:, :, t0:t0 + 128], op=ALU.add),
                 reads=[u_tmp[tb]] + uh, writes=[u_xs[n]])

    def rw_common(self):
        P = self.P
        I = self.I
        m48 = P.psb("m48", [48, 128], F32); u_m48 = U("m48")
        mixT = P.psb("mixT", [128, 48], F32); u_mixT = U("mixT")
        P.dma("sp", lambda e: e.dma_start(out=m48[:], in_=I("rw_mix")[:, :]), writes=[u_m48])
        ps, ups = self.nps()
        self.tr(ps[:, 0:48], m48[:], self.ident_f[0:48, 0:48], [u_m48, self.u_const], [ups])
        P.op("dve", lambda e: e.tensor_copy(out=mixT[:], in_=ps[:, 0:48]), reads=[ups], writes=[u_mixT])
        S = P.psb("S", [128, 8, 128], F32); xx = P.psb("xx", [128, 8, 128], F32)
        tmp = [P.psb("xtmp", [128, 8, 128], F32) for _ in range(2)]
        xs = [P.psb("xsT", [128, 8, 128], BF16) for _ in range(3)]
        return dict(mixT=mixT, u_mixT=u_mixT, S=S, u_S=U("S"), xx=xx, u_xx=U("xx"), tmp=tmp, u_tmp=[U("xt0"), U("xt1")],
                    xs=xs, u_xs=[U("xs0"), U("xs1"), U("xs2")])

    def rw_out(self, ob, u_ob, n, name, tt):
        b = n % len(ob)
        self.P.dma("pool", lambda e: e.dma_start(out=self.rwd[name][tt * 128:(tt + 1) * 128, :], in_=ob[b][:]),
                   reads=[u_ob[b]], writes=[self.u_rwd[name]])

    def rw_proj_a(self):
        P = self.P
        I = self.I
        for nm in ("rw_mix", "rw_w_r", "rw_w_k", "rw_w_v", "rw_k_k"):
            I(nm)
        P.phase_begin()
        c = self.rw_common()
        W = {}
        for nm in ("rw_w_r", "rw_w_k", "rw_w_v"):
            W[nm] = self.wload(nm, I(nm), 8, D)
        kkb, u_kkb = self.bload("kkb", I("rw_k_k")[0:1, :], D)
        ob = [P.psb("ob", [128, D], F32) for _ in range(5)]; u_ob = [U("ob%d" % j) for j in range(5)]
        ss = P.psb("ss", [128, 16], F32); u_ss = U("ss")
        sq = P.psb("sq", [128, D], F32); u_sq = U("sq")
        n = 0
        for tt in range(NTT):
            self.rw_xs(tt, c["S"], c["u_S"], c["xx"], c["u_xx"], c["tmp"], c["u_tmp"], c["xs"], c["u_xs"], (0, 2, 3),
                       c["mixT"], c["u_mixT"])
            for si, (nm, dst) in enumerate((("rw_w_r", "R"), ("rw_w_k", "K"), ("rw_w_v", "V"))):
                w, uw = W[nm]
                b = n % 5
                for half in range(2):
                    ps, ups = self.nps()
                    for k in range(8):
                        self.mm(ps[:, :], c["xs"][si][:, k, :], w[:, k, half * 512:(half + 1) * 512], k == 0, k == 7,
                                [c["u_xs"][si], uw], [ups])
                    P.op("act", lambda e, ps=ps, b=b, half=half: e.copy(out=ob[b][:, half * 512:(half + 1) * 512], in_=ps[:, :]),
                         reads=[ups], writes=[u_ob[b]])
                self.rw_out(ob, u_ob, n, dst, tt)
                kb = b
                n += 1
                if dst == "K":
                    b2 = n % 5
                    n += 1
                    kks = ob[b2]
                    P.op("dve", lambda e, kb=kb, kks=kks: e.tensor_tensor(out=kks[:], in0=ob[kb][:], in1=kkb[:], op=ALU.mult),
                         reads=[u_ob[kb], u_kkb], writes=[u_ob[b2]])
                    P.op("dve", lambda e, kks=kks: e.tensor_tensor(out=sq[:], in0=kks[:], in1=kks[:], op=ALU.mult),
                         reads=[u_ob[b2]], writes=[u_sq])
                    P.op("dve", lambda e: e.tensor_reduce(out=ss[:], in_=sq[:].rearrange("p (h n) -> p h n", n=64), axis=AX.X,
                                                          op=ALU.add), reads=[u_sq], writes=[u_ss])
                    P.op("dve", lambda e: e.tensor_scalar(out=ss[:], in0=ss[:], scalar1=1e-24, scalar2=None, op0=ALU.max),
                         reads=[u_ss], writes=[u_ss])
                    P.op("act", lambda e: e.sqrt(out=ss[:], in_=ss[:]), reads=[u_ss], writes=[u_ss])
                    P.op("dve", lambda e: e.reciprocal(out=ss[:], in_=ss[:]), reads=[u_ss], writes=[u_ss])
                    P.op("dve", lambda e, kks=kks: e.tensor_tensor(
                        out=kks[:].rearrange("p (h n) -> p h n", n=64), in0=kks[:].rearrange("p (h n) -> p h n", n=64),
                        in1=ss[:].unsqueeze(2).to_broadcast([128, 16, 64]), op=ALU.mult), reads=[u_ob[b2], u_ss], writes=[u_ob[b2]])
                    self.rw_out(ob, u_ob, b2, "KK", tt)
        P.phase_end()

    def rw_proj_b(self):
        P = self.P
        I = self.I
        for nm in ("rw_mix", "rw_g1", "rw_g2", "rw_w0", "rw_w1", "rw_w2", "rw_a0", "rw_a1", "rw_a2", "rw_k_a"):
            I(nm)
        P.phase_begin()
        c = self.rw_common()
        g1w, u_g1 = self.wload("g1w", I("rw_g1"), 8, 128)
        g2w, u_g2 = self.wload("g2w", I("rw_g2"), 1, D)
        w1 = [self.wload("w1_%d" % z, I("rw_w1")[z], 8, 64) for z in range(2)]
        a1 = [self.wload("a1_%d" % z, I("rw_a1")[z], 8, 64) for z in range(2)]
        w2 = []; a2 = []
        for z in range(2):
            for (lst, nm) in ((w2, "rw_w2"), (a2, "rw_a2")):
                t = P.psb(nm, [64, D], BF16); u = U(nm)
                P.dma("pool", lambda e, t=t, nm=nm, z=z: e.dma_start(out=t[:], in_=I(nm)[z]), writes=[u])
                lst.append((t, u))
        def hilo(nm):
            f = P.psb(nm + "f", [33, 2 * D], F32); uf = U(nm + "f")
            hl = P.psb(nm + "hl", [33, 2 * D], BF16); uhl = U(nm + "hl")
            bk = P.psb(nm + "bk", [33, 2 * D], F32); ubk = U(nm + "bk")
            P.op("pool", lambda e: e.memset(f[:], 0.0), writes=[uf])
            src = I(nm).rearrange("(o z) n -> o (z n)", o=1)
            P.dma("sp", lambda e: e.dma_start(out=f[0:1, :], in_=src), reads=[uf], writes=[uf])
            P.dma("sp", lambda e: e.dma_start(out=f[32:33, :], in_=src), reads=[uf], writes=[uf])
            P.op("dve", lambda e: e.tensor_copy(out=hl[:], in_=f[:]), reads=[uf], writes=[uhl])
            P.op("dve", lambda e: e.tensor_copy(out=bk[:], in_=hl[:]), reads=[uhl], writes=[ubk])
            P.op("dve", lambda e: e.tensor_tensor(out=bk[:], in0=f[:], in1=bk[:], op=ALU.subtract), reads=[uf, ubk], writes=[ubk])
            P.op("dve", lambda e: e.tensor_copy(out=hl[32:33, :], in_=bk[32:33, :]), reads=[ubk, uhl], writes=[uhl])
            return hl, uhl
        w0hl, u_w0 = hilo("rw_w0")
        a0hl, u_a0 = hilo("rw_a0")
        kab, u_kab = self.bload("kab", I("rw_k_a")[0:1, :], D)
        c1b = P.psb("c1b", [128, D], F32); u_c1b = U("c1b")
        P.op("dve", lambda e: e.tensor_scalar(out=c1b[:], in0=kab[:], scalar1=-1.0, scalar2=1.0, op0=ALU.mult, op1=ALU.add),
             reads=[u_kab], writes=[u_c1b])
        ob = [P.psb("ob", [128, D], F32) for _ in range(4)]; u_ob = [U("ob%d" % j) for j in range(4)]
        kin = P.psb("kin", [128, D], F32); kkin = P.psb("kkin", [128, D], F32); u_kin = U("kin"); u_kkin = U("kkin")
        az = P.psb("az", [128, D], F32); u_az = U("az")
        lt = [P.psb("lt", [128, 128], BF16) for _ in range(2)]; u_lt = [U("lt0"), U("lt1")]
        n = 0
        nl = 0
        for tt in range(NTT):
            self.rw_xs(tt, c["S"], c["u_S"], c["xx"], c["u_xx"], c["tmp"], c["u_tmp"], c["xs"], c["u_xs"], (1, 4, 5),
                       c["mixT"], c["u_mixT"])
            xw, u_xw = c["xs"][0], c["u_xs"][0]
            xa, u_xa = c["xs"][1], c["u_xs"][1]
            xg, u_xg = c["xs"][2], c["u_xs"][2]
            P.dma("sp", lambda e, tt=tt: e.dma_start(out=kin[:], in_=self.rwd["K"][tt * 128:(tt + 1) * 128, :]),
                  reads=[self.u_rwd["K"]], writes=[u_kin])
            P.dma("sp", lambda e, tt=tt: e.dma_start(out=kkin[:], in_=self.rwd["KK"][tt * 128:(tt + 1) * 128, :]),
                  reads=[self.u_rwd["KK"]], writes=[u_kkin])

            def lora(x, u_x, w1t, u_w1, rows, func, w2t, u_w2, bias, u_bias, z, out_ap, u_out, fin):
                nonlocal nl
                psI, upsI = self.nps()
                for k in range(8):
                    self.mm(psI[0:rows, 0:128], w1t[:, k, :], x[:, k, :], k == 0, k == 7, [u_w1, u_x], [upsI])
                l = nl % 2
                nl += 1
                P.op("act", lambda e: e.activation(out=lt[l][0:rows, :], in_=psI[0:rows, 0:128], func=func),
                     reads=[upsI], writes=[u_lt[l]])
                for half in range(2):
                    ps, ups = self.nps()
                    self.mm(ps[:, :], lt[l][0:rows, :], w2t[0:rows, half * 512:(half + 1) * 512], True, bias is None,
                            [u_lt[l], u_w2], [ups])
                    if bias is not None:
                        self.mm(ps[:, :], self.ones_b[0:33, 0:128], bias[:, z * D + half * 512:z * D + (half + 1) * 512],
                                False, True, [u_bias, self.u_const], [ups])
                    P.op("act", lambda e, ps=ps, half=half: e.activation(out=out_ap[:, half * 512:(half + 1) * 512], in_=ps[:, :],
                                                                         func=fin), reads=[ups], writes=[u_out])
            b = n % 4; n += 1
            lora(xg, u_xg, g1w, u_g1, 128, AF.Sigmoid, g2w[:, 0, :], u_g2, None, None, 0, ob[b], u_ob[b], AF.Copy)
            self.rw_out(ob, u_ob, b, "G", tt)
            for z in range(2):
                b = n % 4; n += 1
                lora(xw, u_xw, w1[z][0], w1[z][1], 64, AF.Tanh, w2[z][0], w2[z][1], w0hl, u_w0, z, ob[b], u_ob[b], AF.Sigmoid)
                self.rw_out(ob, u_ob, b, "LW%d" % z, tt)
                lora(xa, u_xa, a1[z][0], a1[z][1], 64, AF.Copy, a2[z][0], a2[z][1], a0hl, u_a0, z, az, u_az, AF.Sigmoid)
                b = n % 4; n += 1
                P.op("dve", lambda e, b=b: e.tensor_tensor(out=ob[b][:], in0=az[:], in1=kab[:], op=ALU.mult),
                     reads=[u_az, u_kab], writes=[u_ob[b]])
                P.op("dve", lambda e, b=b: e.tensor_tensor(out=ob[b][:], in0=ob[b][:], in1=c1b[:], op=ALU.add),
                     reads=[u_ob[b], u_c1b], writes=[u_ob[b]])
                P.op("dve", lambda e, b=b: e.tensor_tensor(out=ob[b][:], in0=ob[b][:], in1=kin[:], op=ALU.mult),
                     reads=[u_ob[b], u_kin], writes=[u_ob[b]])
                self.rw_out(ob, u_ob, b, "KT%d" % z, tt)
                b = n % 4; n += 1
                P.op("dve", lambda e, b=b: e.tensor_tensor(out=ob[b][:], in0=az[:], in1=kkin[:], op=ALU.mult),
                     reads=[u_az, u_kkin], writes=[u_ob[b]])
                self.rw_out(ob, u_ob, b, "B%d" % z, tt)
        P.phase_end()

    def rw_scan(self, z, do_ctx):
        P = self.P
        I = self.I
        for nm in ("rw_r_k", "rw_ln_g", "rw_ln_b"):
            I(nm)
        P.phase_begin()
        fwd = z == 0
        tri = P.psb("tri", [128, 128], F32); mA = P.psb("mA", [128, 4, 128], F32); mN = P.psb("mN", [128, 128], F32)
        cvec = P.psb("cvec", [128, 2], F32); u_mk = U("masks")
        cm, pat = (-1, 1) if fwd else (1, -1)
        P.op("pool", lambda e: e.memset(tri[:], C0), writes=[u_mk])
        P.op("pool", lambda e: e.memset(mA[:], 1.0), writes=[u_mk])
        P.op("pool", lambda e: e.memset(mN[:], 1.0), writes=[u_mk])
        P.op("pool", lambda e: e.memset(cvec[:], C0), writes=[u_mk])
        P.op("pool", lambda e: e.affine_select(out=tri[:], in_=tri[:], pattern=[[pat, 128]], compare_op=ALU.is_ge, fill=0.0,
                                               base=0, channel_multiplier=cm), reads=[u_mk], writes=[u_mk])
        for q in range(4):
            P.op("pool", lambda e, q=q: e.affine_select(out=mA[:, q, :], in_=mA[:, q, :], pattern=[[pat, 128]],
                                                        compare_op=ALU.is_ge, fill=0.0, base=-(q % 2), channel_multiplier=cm),
                 reads=[u_mk], writes=[u_mk])
        P.op("pool", lambda e: e.affine_select(out=mN[:], in_=mN[:], pattern=[[-pat, 128]], compare_op=ALU.is_ge, fill=0.0,
                                               base=-1, channel_multiplier=-cm), reads=[u_mk], writes=[u_mk])
        Hst = P.psb("Hst", [64, 16, 64], F32); u_H = U("Hst")
        P.op("pool", lambda e: e.memset(Hst[:], 0.0), writes=[u_H])
        names = ("R", "KK", "V", "LW%d" % z, "B%d" % z, "KT%d" % z)
        tin = {nm: P.psb("in_" + nm, [128, D], F32) for nm in names}
        u_in = {nm: U("in_" + nm) for nm in names}
        r, kk, v, sg, bb, kt = (tin[nm] for nm in names)
        u_r, u_kk, u_v, u_sg, u_bb, u_kt = (u_in[nm] for nm in names)
        E = [P.psb("E", [128, D], F32) for _ in range(3)]; u_E = [U("E0"), U("E1"), U("E2")]
        D3 = P.psb("D3", [128, D], F32); u_D3 = U("D3")
        F4 = [E[0], E[1], E[2], D3]; u_F4 = [u_E[0], u_E[1], u_E[2], u_D3]
        FT = P.psb("FT", [64, 8, 4, 128], F32); u_FT = [U("FT%d" % j) for j in range(8)]
        AA = P.psb("AA", [128, 8, 4, 128], F32); u_AA = [U("AA%d" % j) for j in range(8)]
        Nn = P.psb("Nn", [128, 8, 128], F32); u_Nn = [U("Nn0"), U("Nn1")]
        MB = [P.psb("MB", [128, 4, 128], F32) for _ in range(2)]; u_MB = [U("MB0"), U("MB1")]
        NB = [P.psb("NB", [128, 4, 128], F32) for _ in range(2)]; u_NB = [U("NB0"), U("NB1")]
        if True:
            MB2 = [P.psb("MB2", [128, 4, 128], F32) for _ in range(2)]; u_MB2 = [U("MB20"), U("MB21")]
            NB2 = [P.psb("NB2", [128, 4, 128], F32) for _ in range(2)]; u_NB2 = [U("NB20"), U("NB21")]
        else:
            MB2, u_MB2, NB2, u_NB2 = MB, u_MB, NB, u_NB
        MBg = [MB, MB2]; u_MBg = [u_MB, u_MB2]; NBg = [NB, NB2]; u_NBg = [u_NB, u_NB2]
        Pm = P.psb("Pm", [128, 8, 128], F32); u_Pm = [U("Pm0"), U("Pm1")]
        X = P.psb("X", [128, 512], F32); u_X = U("X")
        nU = P.psb("nU", [128, 512], F32); u_nU = U("nU")
        ysb = P.psb("ysb", [128, D], F32); u_y = U("ysb")
        gl = P.psb("gl", [64, 16], F32); u_gl = U("gl")
        if not fwd:
            rkb, u_rkb = self.bload("rkb", I("rw_r_k")[0:1, :], D)
            lng, u_lng = self.bload("lng", I("rw_ln_g")[0:1, :], D)
            lnb, u_lnb = self.bload("lnb", I("rw_ln_b")[0:1, :], D)
            st = P.psb("st", [128, 48], F32); u_st = U("st")
            obf = P.psb("obf", [128, D], BF16); u_obf = U("obf")
            stg = P.psb("stg", [128, 8, 128], BF16); u_stg = U("stg")
        order = list(range(NTT)) if fwd else [1, 0] + list(range(NTT - 1, 1, -1))
        cut = self.cfg.get("scan_cut", 99)
        if "scan_tiles" in self.cfg:
            order = order[:self.cfg["scan_tiles"]]
        v3 = lambda t: t[:].rearrange("p (h n) -> p h n", n=64)
        for tt in order:
            for nm in names:
                P.dma("sp", lambda e, nm=nm, tt=tt: e.dma_start(out=tin[nm][:], in_=self.rwd[nm][tt * 128:(tt + 1) * 128, :]),
                      reads=[self.u_rwd[nm]], writes=[u_in[nm]])
            if cut < -1:
                continue
            for half in range(2):
                hs = slice(half * 512, (half + 1) * 512)
                ps, ups = self.nps()
                self.mm(ps[:, :], tri[:], sg[:, hs], True, True, [u_mk, u_sg], [ups])
                P.op("dve", lambda e, ps=ps, hs=hs: e.tensor_copy(out=E[0][:, hs], in_=ps[:, :]), reads=[ups], writes=[u_E[0]])
                P.op("dve", lambda e, hs=hs: e.scalar_tensor_tensor(out=E[1][:, hs], in0=sg[:, hs], scalar=-C0, in1=E[0][:, hs],
                                                                    op0=ALU.mult, op1=ALU.add), reads=[u_E[0], u_sg], writes=[u_E[1]])
            P.op("act", lambda e: e.activation(out=E[2][:], in_=E[0][:], func=AF.Exp, scale=-1.0), reads=[u_E[0]], writes=[u_E[2]])
            P.op("act", lambda e: e.activation(out=E[0][:], in_=E[0][:], func=AF.Exp), reads=[u_E[0], u_E[2], u_E[1]], writes=[u_E[0]])
            P.op("act", lambda e: e.activation(out=E[1][:], in_=E[1][:], func=AF.Exp), reads=[u_E[1]], writes=[u_E[1]])
            if cut < 0:
                continue
            for j, (src, us, ex) in ((3, (kt, u_kt, 2)), (0, (r, u_r, 0)), (1, (kk, u_kk, 1)), (2, (bb, u_bb, 2))):
                eng = "dve" if j % 2 == 0 else "pool"
                P.op(eng, lambda e, j=j, src=src, ex=ex: e.tensor_tensor(out=F4[j][:], in0=src[:], in1=E[ex][:], op=ALU.mult),
                     reads=[us, u_E[ex]], writes=[u_F4[j]])
            if cut < 1:
                continue
            psG, upsG = self.nps()
            for h in range(16):
                self.mm(psG[0:64, 2 * h:2 * h + 2], sg[:, h * 64:(h + 1) * 64], cvec[:, 0:2], True, True, [u_sg, u_mk], [upsG])
            P.op("dve", lambda e, psG=psG: e.tensor_copy(
                out=gl[:], in_=psG[0:64, 0:32].rearrange("p (h two) -> p h two", two=2)[:, :, 0]), reads=[upsG], writes=[u_gl])
            P.op("act", lambda e: e.activation(out=gl[:], in_=gl[:], func=AF.Exp), reads=[u_gl], writes=[u_gl])
            if cut < 2:
                continue
            for half in range(2):
                for hh in range(8):
                    h = half * 8 + hh
                    ps, ups = self.nps()
                    for j in range(4):
                        self.tr(ps[0:64, j * 128:(j + 1) * 128], F4[j][:, h * 64:(h + 1) * 64], self.ident_f[:],
                                [u_F4[j], self.u_const], [ups])
                    eng = "dve"
                    if eng == "act":
                        P.op("act", lambda e, ps=ps, hh=hh: e.copy(out=FT[:, hh].rearrange("p a t -> p (a t)"), in_=ps[0:64, :]),
                             reads=[ups], writes=[u_FT[hh]])
                    else:
                        P.op("dve", lambda e, ps=ps, hh=hh: e.tensor_copy(out=FT[:, hh].rearrange("p a t -> p (a t)"), in_=ps[0:64, :]),
                             reads=[ups], writes=[u_FT[hh]])
                if cut < 3:
                    continue
                for hh in range(8):
                    ps, ups = self.nps()
                    rhs2 = FT[:, hh, 0:2, :]
                    self.mm(ps[:, 0:256].rearrange("p (a t) -> p a t", a=2), FT[:, hh, 2, :], rhs2, True, True, [u_FT[hh]], [ups])
                    self.mm(ps[:, 256:512].rearrange("p (a t) -> p a t", a=2), FT[:, hh, 3, :], rhs2, True, True, [u_FT[hh]], [ups])
                    P.op("dve", lambda e, ps=ps, hh=hh: e.tensor_tensor(out=AA[:, hh], in0=ps[:, :].rearrange("p (a t) -> p a t", a=4),
                                                                        in1=mA[:], op=ALU.mult), reads=[ups, u_mk], writes=[u_AA[hh]])
                for g in range(2):
                    ps, ups = self.nps()
                    for j in range(4):
                        hh = g * 4 + j
                        self.mm(ps[:, j * 128:(j + 1) * 128], FT[:, hh, 1, :], FT[:, hh, 2, :], True, True, [u_FT[hh]], [ups])
                    P.op("dve", lambda e, ps=ps, g=g: e.tensor_tensor(
                        out=Nn[:, g * 4:(g + 1) * 4, :], in0=ps[:, :].rearrange("p (a t) -> p a t", a=4),
                        in1=mN[:].unsqueeze(1).to_broadcast([128, 4, 128]), op=ALU.mult), reads=[ups, u_mk], writes=[u_Nn[g]])
                if cut < 4:
                    continue
                stt = {}
                for g in range(2):
                    grp = [g * 4 + j for j in range(4)]
                    P.op("dve", lambda e, g=g: e.tensor_tensor(
                        out=Pm[:, g * 4:(g + 1) * 4, :], in0=self.ident_f[:].unsqueeze(1).to_broadcast([128, 4, 128]),
                        in1=AA[:, g * 4:(g + 1) * 4, 1, :], op=ALU.subtract), reads=[u_AA[hh] for hh in grp] + [self.u_const],
                        writes=[u_Pm[g]])
                    stt[g] = ([AA[:, hh, 1, :] for hh in grp], [u_AA[hh] for hh in grp], [Nn[:, hh, :] for hh in grp], [u_Nn[g]])
                for gs in ([0, 1],):
                    for li in range(6):
                        lastl = li == 5
                        pp = li % 2
                        banks = {}
                        for g in gs:
                            Ms, uM, Ns, uN = stt[g]
                            bM = ubM = None
                            if not lastl:
                                bM, ubM = self.nps()
                                for j in range(4):
                                    self.mm(bM[:, j * 128:(j + 1) * 128], Ns[j], Ms[j], True, True, uM + uN, [ubM])
                            bN, ubN = self.nps()
                            for j in range(4):
                                self.mm(bN[:, j * 128:(j + 1) * 128], Ms[j], Ns[j], True, True, uM + uN, [ubN])
                            banks[g] = (bM, ubM, bN, ubN)
                        for g in gs:
                            bM, ubM, bN, ubN = banks[g]
                            mb, umb = MBg[g][pp], u_MBg[g][pp]
                            nb_, unb = NBg[g][pp], u_NBg[g][pp]
                            if not lastl:
                                P.op("dve", lambda e, bM=bM, mb=mb: e.tensor_copy(out=mb[:].rearrange("p a t -> p (a t)"), in_=bM[:, :]),
                                     reads=[ubM], writes=[umb])
                            P.op("dve", lambda e, bN=bN, nb_=nb_: e.tensor_copy(out=nb_[:].rearrange("p a t -> p (a t)"), in_=bN[:, :]),
                                 reads=[ubN], writes=[unb])
                            stt[g] = ([mb[:, j, :] for j in range(4)], [umb], [nb_[:, j, :] for j in range(4)], [unb])
                        for g in gs:
                            Ms, uM, Ns, uN = stt[g]
                            bP, ubP = self.nps()
                            for j in range(4):
                                self.mm(bP[:, j * 128:(j + 1) * 128], Ns[j], Pm[:, g * 4 + j, :], True, True, uN + [u_Pm[g]], [ubP])
                            P.op("dve", lambda e, bP=bP, g=g: e.tensor_tensor(
                                out=Pm[:, g * 4:(g + 1) * 4, :], in0=Pm[:, g * 4:(g + 1) * 4, :],
                                in1=bP[:, :].rearrange("p (a t) -> p a t", a=4), op=ALU.add), reads=[ubP, u_Pm[g]], writes=[u_Pm[g]])
                if cut < 5:
                    continue
                ps, ups = self.nps()
                for hh in range(8):
                    h = half * 8 + hh
                    self.mm(ps[:, hh * 64:(hh + 1) * 64], FT[:, hh, 1, :], Hst[:, h, :], True, False, [u_FT[hh], u_H], [ups])
                    self.mm(ps[:, hh * 64:(hh + 1) * 64], AA[:, hh, 3, :], v[:, h * 64:(h + 1) * 64], False, True, [u_AA[hh], u_v], [ups])
                P.op("dve", lambda e, ps=ps: e.tensor_copy(out=X[:], in_=ps[:, :]), reads=[ups], writes=[u_X])
                ps, ups = self.nps()
                for hh in range(8):
                    self.mm(ps[:, hh * 64:(hh + 1) * 64], Pm[:, hh, :], X[:, hh * 64:(hh + 1) * 64], True, True, [u_Pm[hh // 4], u_X], [ups])
                P.op("dve", lambda e, ps=ps: e.tensor_scalar(out=nU[:], in0=ps[:, :], scalar1=-1.0, scalar2=None, op0=ALU.mult),
                     reads=[ups], writes=[u_nU])
                if cut < 6:
                    continue
                ps, ups = self.nps()
                for hh in range(8):
                    h = half * 8 + hh
                    o = ps[:, hh * 64:(hh + 1) * 64]
                    self.mm(o, FT[:, hh, 0, :], Hst[:, h, :], True, False, [u_FT[hh], u_H], [ups])
                    self.mm(o, AA[:, hh, 2, :], v[:, h * 64:(h + 1) * 64], False, False, [u_AA[hh], u_v], [ups])
                    self.mm(o, AA[:, hh, 0, :], nU[:, hh * 64:(hh + 1) * 64], False, True, [u_AA[hh], u_nU], [ups])
                P.op("dve", lambda e, ps=ps, half=half: e.tensor_copy(out=ysb[:, half * 512:(half + 1) * 512], in_=ps[:, :]),
                     reads=[ups], writes=[u_y])
                if cut < 7:
                    continue
                ps, ups = self.nps()
                for hh in range(8):
                    h = half * 8 + hh
                    o = ps[0:64, hh * 64:(hh + 1) * 64]
                    hc = slice(h * 64, (h + 1) * 64)
                    self.mm(o, F4[3][:, hc], v[:, hc], True, False, [u_F4[3], u_v], [ups])
                    self.mm(o, F4[2][:, hc], nU[:, hh * 64:(hh + 1) * 64], False, False, [u_F4[2], u_nU], [ups])
                    self.mm(o, self.ident_f[0:64, 0:64], Hst[:, h, :], False, True, [u_H, self.u_const], [ups])
                P.op("dve", lambda e, ps=ps, half=half: e.tensor_tensor(
                    out=Hst[:, half * 8:(half + 1) * 8, :], in0=ps[0:64, :].rearrange("p (a t) -> p a t", a=8),
                    in1=gl[:, half * 8:(half + 1) * 8].unsqueeze(2).to_broadcast([64, 8, 64]), op=ALU.mult),
                    reads=[ups, u_gl], writes=[u_H])
            if cut < 8:
                continue
            if fwd:
                P.dma("pool", lambda e, tt=tt: e.dma_start(out=self.rwd["Y"][tt * 128:(tt + 1) * 128, :], in_=ysb[:]),
                      reads=[u_y], writes=[self.u_rwd["Y"]])
                continue
            if tt < 2 and not do_ctx:
                continue
            yf, u_yf = kk, u_kk
            P.dma("sp", lambda e, tt=tt: e.dma_start(out=yf[:], in_=self.rwd["Y"][tt * 128:(tt + 1) * 128, :]),
                  reads=[self.u_rwd["Y"]], writes=[u_yf])
            kt0, u_kt0 = sg, u_sg
            P.dma("sp", lambda e, tt=tt: e.dma_start(out=kt0[:], in_=self.rwd["KT0"][tt * 128:(tt + 1) * 128, :]),
                  reads=[self.u_rwd["KT0"]], writes=[u_kt0])
            gg, u_gg = bb, u_bb
            P.dma("sp", lambda e, tt=tt: e.dma_start(out=gg[:], in_=self.rwd["G"][tt * 128:(tt + 1) * 128, :]),
                  reads=[self.u_rwd["G"]], writes=[u_gg])
            T0, T1, T2, T3 = F4
            uT0, uT1, uT2, uT3 = u_F4
            P.op("dve", lambda e: e.tensor_tensor(out=ysb[:], in0=ysb[:], in1=yf[:], op=ALU.add), reads=[u_y, u_yf], writes=[u_y])
            P.op("dve", lambda e: e.tensor_reduce(out=st[:, 0:16], in_=v3(ysb), axis=AX.X, op=ALU.add), reads=[u_y], writes=[u_st])
            P.op("dve", lambda e: e.tensor_scalar(out=st[:, 0:16], in0=st[:, 0:16], scalar1=1.0 / 64, scalar2=None, op0=ALU.mult),
                 reads=[u_st], writes=[u_st])
            P.op("dve", lambda e: e.tensor_tensor(out=v3(T0), in0=v3(ysb), in1=st[:, 0:16].unsqueeze(2).to_broadcast([128, 16, 64]),
                                                  op=ALU.subtract), reads=[u_y, u_st], writes=[uT0])
            P.op("dve", lambda e: e.tensor_tensor(out=T1[:], in0=T0[:], in1=T0[:], op=ALU.mult), reads=[uT0], writes=[uT1])
            P.op("dve", lambda e: e.tensor_reduce(out=st[:, 16:32], in_=v3(T1), axis=AX.X, op=ALU.add), reads=[uT1], writes=[u_st])
            P.op("dve", lambda e: e.tensor_scalar(out=st[:, 16:32], in0=st[:, 16:32], scalar1=1.0 / 64, scalar2=64e-5,
                                                  op0=ALU.mult, op1=ALU.add), reads=[u_st], writes=[u_st])
            P.op("act", lambda e: e.sqrt(out=st[:, 16:32], in_=st[:, 16:32]), reads=[u_st], writes=[u_st])
            P.op("dve", lambda e: e.reciprocal(out=st[:, 16:32], in_=st[:, 16:32]), reads=[u_st], writes=[u_st])
            P.op("dve", lambda e: e.tensor_tensor(out=v3(T0), in0=v3(T0), in1=st[:, 16:32].unsqueeze(2).to_broadcast([128, 16, 64]),
                                                  op=ALU.mult), reads=[uT0, u_st], writes=[uT0])
            P.op("dve", lambda e: e.tensor_tensor(out=T0[:], in0=T0[:], in1=lng[:], op=ALU.mult), reads=[uT0, u_lng], writes=[uT0])
            P.op("dve", lambda e: e.tensor_tensor(out=T0[:], in0=T0[:], in1=lnb[:], op=ALU.add), reads=[uT0, u_lnb], writes=[uT0])
            P.op("pool", lambda e: e.tensor_tensor(out=T1[:], in0=r[:], in1=rkb[:], op=ALU.mult), reads=[u_r, u_rkb], writes=[uT1])
            P.op("pool", lambda e: e.tensor_tensor(out=T2[:], in0=kt[:], in1=kt0[:], op=ALU.add), reads=[u_kt, u_kt0], writes=[uT2])
            P.op("dve", lambda e: e.tensor_tensor(out=T2[:], in0=T2[:], in1=T1[:], op=ALU.mult), reads=[uT1, uT2], writes=[uT2])
            P.op("dve", lambda e: e.tensor_reduce(out=st[:, 32:48], in_=v3(T2), axis=AX.X, op=ALU.add), reads=[uT2], writes=[u_st])
            P.op("dve", lambda e: e.tensor_tensor(out=v3(T3), in0=v3(v), in1=st[:, 32:48].unsqueeze(2).to_broadcast([128, 16, 64]),
                                                  op=ALU.mult), reads=[u_v, u_st], writes=[uT3])
            P.op("dve", lambda e: e.tensor_tensor(out=T0[:], in0=T0[:], in1=T3[:], op=ALU.add), reads=[uT0, uT3], writes=[uT0])
            P.op("dve", lambda e: e.tensor_tensor(out=obf[:], in0=T0[:], in1=gg[:], op=ALU.mult), reads=[uT0, u_gg], writes=[u_obf])
            self.tpose_to_dram([obf[:, j * 128:(j + 1) * 128] for j in range(8)], u_obf, 128,
                               self.OTd[0:1024, tt * 128:(tt + 1) * 128].rearrange("(j p) t -> p j t", p=128),
                               self.u_OT, stg, u_stg)
        P.phase_end()

    def rwkv(self, i, do_ctx):
        stop = self.cfg.get("rw_stop", 9)
        self.rw_scratch()
        self.rw_proj_a()
        if stop >= 2:
            self.rw_proj_b()
        if stop >= 3:
            self.rw_scan(0, do_ctx)
        if stop >= 4:
            self.rw_scan(1, do_ctx)
            self.outproj_phase(i, "rw_w_o", do_ctx)
        if self.cfg.get("rw_dump"):
            P = self.P
            for nm in self.cfg["rw_dump"]:
                o = P.dram("dump_" + nm, [NT, D], F32, kind="ExternalOutput")
                for j in range(2):
                    P.dma("sp", lambda e, o=o, nm=nm, j=j: e.dma_start(out=o[j * 2176:(j + 1) * 2176, :],
                                                                      in_=self.rwd[nm][j * 2176:(j + 1) * 2176, :]),
                          reads=[self.u_rwd[nm]], is_out=True)


for _n, _f in list(vars(KRw).items()):
    if callable(_f):
        setattr(K, _n, _f)
IN_SHAPES = None


def build_program(cfg):
    k = K(cfg)
    k.prologue()
    if cfg.get("only_scan") is not None:
        k.mix_scratch()
        k.rw_scratch()
        k.rw_scan(cfg["only_scan"], True)
        return k, k.epilogue()
    for i in cfg.get("layers", [0, 1, 2, 3]):
        do_ctx = i < 3 or cfg.get("force_ctx", False)
        if not cfg.get("skip_ada"):
            k.ada_phase(i)
        if not cfg.get("skip_mixer"):
            k.mixer(i, do_ctx)
        if not cfg.get("skip_moe"):
            tiles = None if do_ctx else list(range(2, NTT))
            k.norm_phase(i, 1, True, tiles)
            k.moe_phase(i, do_ctx)
    nc = k.epilogue()
    return k, nc


def rope_tables(d_rot):
    t = np.arange(NLAT)
    row = (t // 64).astype(np.float32)
    col = (t % 64).astype(np.float32)
    d_axis = d_rot // 2
    inv = (np.float32(10000.0) ** (-np.arange(0, d_axis, 2, dtype=np.float32) / np.float32(d_axis))).astype(np.float32)
    ang = np.concatenate([row[:, None] * inv, col[:, None] * inv], axis=-1).astype(np.float32)
    return np.cos(ang).astype(np.float32), np.sin(ang).astype(np.float32)


def na_bias_table(rpb):
    rpb = np.asarray(rpb, dtype=np.float32)
    kl = np.arange(128) // 64
    kc = np.arange(128) % 64
    c = np.arange(64)
    cstart = np.clip(c - 8, 0, 48)
    ok = (kc[:, None] >= cstart[None, :]) & (kc[:, None] < cstart[None, :] + 16)
    dcol = np.clip(kc[:, None] - c[None, :] + 15, 0, 30)
    out = np.empty((16, 128, 14, 64), np.float32)
    for di in range(14):
        dr = np.clip(di - 7 + kl + 7, 0, 14)
        g = rpb[:, dr[:, None], dcol]
        out[:, :, di, :] = np.where(ok[None], g, np.float32(-30000.0))
    return out


def make_in_maps(inputs, names):
    f = np.ascontiguousarray
    shared = {}
    for nm in names:
        if nm in ("x", "c", "ctx"):
            continue
        if nm in ("mla_cos", "mla_sin", "swa_cos", "swa_sin"):
            cs, sn = rope_tables(32 if nm.startswith("mla") else 64)
            shared[nm] = f(cs if nm.endswith("cos") else sn)
            continue
        if nm == "na_bias":
            shared[nm] = f(na_bias_table(inputs["na_rpb"][0]))
            continue
        a = np.asarray(inputs[nm], dtype=np.float32)
        if nm == "c_ctx":
            a = a.reshape(8, 128)
        elif nm in ("norm1_g", "norm2_g"):
            a = a.reshape(4, 8, 128)
        elif nm == "ada_b":
            a = a.reshape(4, 48, 128)
        elif nm.startswith(("na_", "rw_", "mla_", "swa_")):
            a = a[0]
            if nm == "rw_mix":
                a = a.reshape(48, 128)
            elif nm == "rw_r_k":
                a = a.reshape(1, -1)
            if a.ndim == 1:
                a = a.reshape(1, -1)
        shared[nm] = f(a)
    maps = []
    for b in range(8):
        m = dict(shared)
        m["x"] = f(np.asarray(inputs["x"][b], dtype=np.float32))
        m["c"] = f(np.asarray(inputs["c"][b], dtype=np.float32).reshape(8, 128))
        m["ctx"] = f(np.asarray(inputs["ctx"][b], dtype=np.float32))
        maps.append(m)
    return maps


def run(inputs, cfg):
    k, nc = build_program(cfg)
    maps = make_in_maps(inputs, list(k.inp.keys()))
    res = run_bass_kernel_spmd(nc, maps, core_ids=list(range(8)))
    return res


def kernel(**inputs):
    res = run(inputs, {})
    return np.stack([np.asarray(r["out"], dtype=np.float32) for r in res.results], axis=0)
```
